# Optimizing a Trainium2 kernel written in Bass

```python
import jax, jax.numpy as jnp
from jax import lax
import numpy as np

D_MODEL = 2048
BATCH = 8
SEQ = 2048
DEPTH = 1
DEC_BATCH = 128
DEC_SEQ = 4
PAST_LEN = 2048
PAGE_SIZE = 128

N_HEADS = 8
N_KV_HEADS = 2
HEAD_DIM = 128
ATTN_W = N_HEADS * HEAD_DIM
KV_W = N_KV_HEADS * HEAD_DIM
IDX_HEADS = 16
IDX_DIM = 64
TOPK_MAX = 256
ATTN_BLOCK = 128
ATTN_SCALE = HEAD_DIM ** -0.5
IDX_SCALE = IDX_DIM ** -0.5
LRU_W = D_MODEL // 2
LRU_BLOCKS = 8
LRU_BW = LRU_W // LRU_BLOCKS
CONV_W = 4
LRU_C = 8.0
MEM_TOKENS = 256
MEM_HEADS = 4
MEM_HEAD_DIM = 256
MEM_W = MEM_HEADS * MEM_HEAD_DIM
MEM_SCALE = MEM_HEAD_DIM ** -0.5
N_BRANCH = 3
BRANCH_W = 1024
PEER_HEADS = 8
PEER_NKEYS = 128
PEER_EXPERTS = PEER_NKEYS * PEER_NKEYS
PEER_DK = 256
PEER_TOPK = 16
PEER_CHUNK = 128
EPS = 1e-6

IN_SPLITS = (ATTN_W, KV_W, KV_W, IDX_HEADS * IDX_DIM, IDX_DIM, IDX_HEADS, LRU_W, LRU_W, MEM_W)
N_IN = sum(IN_SPLITS)

kernel_name = 'dsa_rglru_peer_hybrid_step'


def rmsnorm(x, g):
    x32 = x.astype(jnp.float32)
    y = x32 * lax.rsqrt(jnp.mean(x32 * x32, axis=-1, keepdims=True) + EPS)
    return (y * g.astype(jnp.float32)).astype(x.dtype)


def in_proj(xn, w_in):
    b, t, _ = xn.shape
    proj = xn @ w_in
    offs = np.cumsum((0,) + IN_SPLITS)
    q, k, v, iq, ik, iw, xb, yb, qm = [proj[..., int(offs[i]):int(offs[i + 1])] for i in range(len(IN_SPLITS))]
    return (q.reshape(b, t, N_HEADS, HEAD_DIM), k.reshape(b, t, N_KV_HEADS, HEAD_DIM),
            v.reshape(b, t, N_KV_HEADS, HEAD_DIM), iq.reshape(b, t, IDX_HEADS, IDX_DIM), ik,
            iw * (IDX_HEADS ** -0.5), xb, yb, qm)


def gather_rows(table, idx):
    return jax.vmap(lambda t, i: t[i])(table, idx)


def dsa_attend(q, iq, iw, qpos, ik_all, gather_kv, n_sel):
    b, nq = q.shape[:2]
    n_keys = ik_all.shape[1]
    dots = jnp.einsum('bqhd,bsd->bqsh', iq, ik_all) * IDX_SCALE
    score = jnp.einsum('bqsh,bqh->bqs', jax.nn.relu(dots), iw).astype(jnp.float32)
    causal = jnp.arange(n_keys, dtype=jnp.int32)[None, :] <= qpos[:, None]
    score = jnp.where(causal[None], score, -jnp.inf)
    sel_score, sel = lax.top_k(score, n_sel)
    valid = jnp.isfinite(sel_score)
    kg, vg = gather_kv(sel)
    qg = q.reshape(b, nq, N_KV_HEADS, N_HEADS // N_KV_HEADS, HEAD_DIM)
    logits = jnp.einsum('bqngd,bqknd->bqngk', qg, kg).astype(jnp.float32) * ATTN_SCALE
    logits = jnp.where(valid[:, :, None, None, :], logits, -jnp.inf)
    p = jax.nn.softmax(logits, axis=-1).astype(vg.dtype)
    o = jnp.einsum('bqngk,bqknd->bqngd', p, vg)
    return o.reshape(b, nq, ATTN_W)


def rg_lru_branch(xb, yb, conv_state, h0, conv_w, conv_b, w_rg, b_rg, w_ig, b_ig, lru_lambda):
    b, t, w = xb.shape
    xp = jnp.concatenate([conv_state.astype(xb.dtype), xb], axis=1)
    xc = conv_b + sum(xp[:, j:j + t] * conv_w[j] for j in range(CONV_W))
    new_conv = xp[:, -(CONV_W - 1):]
    x32 = xc.astype(jnp.float32)
    xblk = x32.reshape(b, t, LRU_BLOCKS, LRU_BW)
    r = jax.nn.sigmoid(jnp.einsum('btni,nij->btnj', xblk, w_rg.astype(jnp.float32)).reshape(b, t, w) + b_rg)
    i = jax.nn.sigmoid(jnp.einsum('btni,nij->btnj', xblk, w_ig.astype(jnp.float32)).reshape(b, t, w) + b_ig)
    log_a = -LRU_C * r * jax.nn.softplus(-lru_lambda.astype(jnp.float32))
    a = jnp.exp(log_a)
    u = jnp.sqrt(-jnp.expm1(2.0 * log_a)) * (i * x32)
    u = u.at[:, 0].add(a[:, 0] * h0.astype(jnp.float32))

    def combine(lhs, rhs):
        a1, b1 = lhs
        a2, b2 = rhs
        return a1 * a2, a2 * b1 + b2

    _, h = lax.associative_scan(combine, (a, u), axis=1)
    out = h * jax.nn.gelu(yb.astype(jnp.float32))
    return out.astype(xb.dtype), new_conv, h[:, -1]


def mem_kv(mem, g_mem, w_mem_kv):
    b, m, _ = mem.shape
    kv = rmsnorm(mem, g_mem) @ w_mem_kv
    return (kv[..., :MEM_W].reshape(b, m, MEM_HEADS, MEM_HEAD_DIM),
            kv[..., MEM_W:].reshape(b, m, MEM_HEADS, MEM_HEAD_DIM))


def mem_attend(qm, mk, mv):
    b, t, _ = qm.shape
    q = qm.reshape(b, t, MEM_HEADS, MEM_HEAD_DIM)
    logits = jnp.einsum('bthd,bmhd->bhtm', q, mk).astype(jnp.float32) * MEM_SCALE
    p = jax.nn.softmax(logits, axis=-1).astype(mv.dtype)
    return jnp.einsum('bhtm,bmhd->bthd', p, mv).reshape(b, t, MEM_W)


def merge_branches(xn, o_attn, o_lru, o_mem, w_gate, w_br, w_o):
    b, t, d = xn.shape
    gates = jax.nn.sigmoid((xn @ w_gate).astype(jnp.float32)).reshape(b, t, N_BRANCH, d)
    up = jnp.einsum('btnw,nwd->btnd', jnp.stack([o_attn, o_lru, o_mem], axis=2), w_br).astype(jnp.float32)
    return jnp.sum(gates * up, axis=2).astype(xn.dtype) @ w_o


def peer_ffn(x, w_peer_q, peer_sub_keys, peer_u, peer_v):
    shp = x.shape
    xf = x.reshape(-1, shp[-1])
    n = xf.shape[0]
    xf = jnp.pad(xf, ((0, (-n) % PEER_CHUNK), (0, 0)))
    kk = PEER_TOPK * PEER_TOPK

    def chunk(xc):
        c = xc.shape[0]
        q = (xc @ w_peer_q).reshape(c, PEER_HEADS, 2, PEER_DK // 2)
        s = jnp.einsum('chpd,phkd->chpk', q, peer_sub_keys).astype(jnp.float32)
        s1, i1 = lax.top_k(s[:, :, 0], PEER_TOPK)
        s2, i2 = lax.top_k(s[:, :, 1], PEER_TOPK)
        cand = (s1[..., :, None] + s2[..., None, :]).reshape(c, PEER_HEADS, kk)
        cidx = (i1[..., :, None] * PEER_NKEYS + i2[..., None, :]).reshape(c, PEER_HEADS, kk)
        top_s, pos = lax.top_k(cand, PEER_TOPK)
        e = jnp.take_along_axis(cidx, pos, axis=-1)
        g = jax.nn.softmax(top_s, axis=-1)
        act = jax.nn.gelu(jnp.einsum('chkd,cd->chk', peer_u[e], xc).astype(jnp.float32))
        return jnp.einsum('chk,chkd->cd', (g * act).astype(xc.dtype), peer_v[e])

    out = lax.map(chunk, xf.reshape(-1, PEER_CHUNK, shp[-1]))
    return out.reshape(-1, shp[-1])[:n].reshape(shp)


def residual_tail(x, xn, o_attn, o_lru, o_mem, merge_p, g_ffn, peer_p):
    x = x + merge_branches(xn, o_attn, o_lru, o_mem, *merge_p)
    return x + peer_ffn(rmsnorm(x, g_ffn), *peer_p)


def prompt_mixer(xn, mem, lru_p, w_in, g_mem, w_mem_kv):
    b, s, _ = xn.shape
    q, k, v, iq, ik, iw, xb, yb, qm = in_proj(xn, w_in)
    nb = s // ATTN_BLOCK
    n_sel = min(TOPK_MAX, s // 4)

    def gather_kv(sel):
        return gather_rows(k, sel), gather_rows(v, sel)

    def blocks(a):
        return jnp.swapaxes(a.reshape(b, nb, ATTN_BLOCK, *a.shape[2:]), 0, 1)

    def attend_block(args):
        qb, iqb, iwb, start = args
        qpos = start + jnp.arange(ATTN_BLOCK, dtype=jnp.int32)
        return dsa_attend(qb, iqb, iwb, qpos, ik, gather_kv, n_sel)

    starts = jnp.arange(nb, dtype=jnp.int32) * ATTN_BLOCK
    o_attn = lax.map(attend_block, (blocks(q), blocks(iq), blocks(iw), starts))
    o_attn = jnp.swapaxes(o_attn, 0, 1).reshape(b, s, ATTN_W)
    conv0 = jnp.zeros((b, CONV_W - 1, LRU_W), xb.dtype)
    h0 = jnp.zeros((b, LRU_W), jnp.float32)
    o_lru, conv_new, h_new = rg_lru_branch(xb, yb, conv0, h0, *lru_p)
    mk, mv = mem_kv(mem, g_mem, w_mem_kv)
    o_mem = mem_attend(qm, mk, mv)
    return o_attn, o_lru, o_mem, (k, v, ik, conv_new, h_new.astype(xn.dtype), mk, mv)


def sample_mixer(xn, cache_k, cache_v, cache_idx_k, page_table, state_conv, state_lru, cache_mem_k, cache_mem_v, lru_p, w_in):
    bd, tn, _ = xn.shape
    q, k, v, iq, ik, iw, xb, yb, qm = in_proj(xn, w_in)
    ps = cache_k.shape[1]
    past = page_table.shape[1] * ps
    ik_past = cache_idx_k[page_table].reshape(bd, past, IDX_DIM)
    ik_all = jnp.concatenate([ik_past, ik], axis=1)
    k_pool = cache_k.reshape(-1, N_KV_HEADS, HEAD_DIM)
    v_pool = cache_v.reshape(-1, N_KV_HEADS, HEAD_DIM)

    def gather_kv(sel):
        in_past = (sel < past)[..., None, None]
        sp = jnp.minimum(sel, past - 1)
        page = jnp.take_along_axis(page_table, (sp // ps).reshape(bd, -1), axis=1).reshape(sel.shape)
        phys = page * ps + sp % ps
        sn = jnp.clip(sel - past, 0, tn - 1)
        kg = jnp.where(in_past, k_pool[phys], gather_rows(k, sn))
        vg = jnp.where(in_past, v_pool[phys], gather_rows(v, sn))
        return kg, vg

    qpos = past + jnp.arange(tn, dtype=jnp.int32)
    n_sel = min(TOPK_MAX, (past + tn) // 4)
    o_attn = dsa_attend(q, iq, iw, qpos, ik_all, gather_kv, n_sel)
    o_lru, conv_new, h_new = rg_lru_branch(xb, yb, state_conv, state_lru, *lru_p)
    o_mem = mem_attend(qm, cache_mem_k, cache_mem_v)
    return o_attn, o_lru, o_mem, (k, v, ik, conv_new, h_new.astype(state_lru.dtype))


def setup_inputs(seed: int = 0) -> dict:
    key = jax.random.key(seed)
    ks = iter(jax.random.split(key, 40))

    def nrm(shape, scale):
        return scale * jax.random.normal(next(ks), shape, jnp.float32)

    n_pages = PAST_LEN // PAGE_SIZE
    n_used = DEC_BATCH * n_pages
    n_phys = n_used + n_used // 4
    page_table = jax.random.permutation(next(ks), n_phys)[:n_used].reshape(DEC_BATCH, n_pages).astype(jnp.int32)
    a_c = jax.random.uniform(next(ks), (DEPTH, LRU_W), jnp.float32, 0.9, 0.999)
    sig = a_c ** (1.0 / LRU_C)
    lru_lambda = jnp.log(sig) - jnp.log1p(-sig)
    L = DEPTH
    d = D_MODEL
    return {
        'x_prompt': nrm((BATCH, SEQ, d), 1.0),
        'x_sample': nrm((DEC_BATCH, DEC_SEQ, d), 1.0),
        'cache_k': nrm((L, n_phys, PAGE_SIZE, N_KV_HEADS, HEAD_DIM), 1.0),
        'cache_v': nrm((L, n_phys, PAGE_SIZE, N_KV_HEADS, HEAD_DIM), 1.0),
        'cache_idx_k': nrm((L, n_phys, PAGE_SIZE, IDX_DIM), 1.0),
        'page_table': page_table,
        'state_conv': nrm((L, DEC_BATCH, CONV_W - 1, LRU_W), 1.0),
        'state_lru': nrm((L, DEC_BATCH, LRU_W), 0.5),
        'cache_mem_k': nrm((L, DEC_BATCH, MEM_TOKENS, MEM_HEADS, MEM_HEAD_DIM), 1.0),
        'cache_mem_v': nrm((L, DEC_BATCH, MEM_TOKENS, MEM_HEADS, MEM_HEAD_DIM), 1.0),
        'mem_prompt': nrm((BATCH, MEM_TOKENS, d), 1.0),
        'g_mix': 1.0 + nrm((L, d), 0.02),
        'w_in': nrm((L, d, N_IN), d ** -0.5),
        'conv_w': nrm((L, CONV_W, LRU_W), CONV_W ** -0.5),
        'conv_b': nrm((L, LRU_W), 0.01),
        'w_rg': nrm((L, LRU_BLOCKS, LRU_BW, LRU_BW), LRU_BW ** -0.5),
        'b_rg': nrm((L, LRU_W), 0.01),
        'w_ig': nrm((L, LRU_BLOCKS, LRU_BW, LRU_BW), LRU_BW ** -0.5),
        'b_ig': nrm((L, LRU_W), 0.01),
        'lru_lambda': lru_lambda,
        'g_mem': 1.0 + nrm((L, d), 0.02),
        'w_mem_kv': nrm((L, d, 2 * MEM_W), d ** -0.5),
        'w_gate': nrm((L, d, N_BRANCH * d), d ** -0.5),
        'w_br': nrm((L, N_BRANCH, BRANCH_W, d), BRANCH_W ** -0.5),
        'w_o': nrm((L, d, d), d ** -0.5),
        'g_ffn': 1.0 + nrm((L, d), 0.02),
        'w_peer_q': nrm((L, d, PEER_HEADS * PEER_DK), d ** -0.5),
        'peer_sub_keys': nrm((L, 2, PEER_HEADS, PEER_NKEYS, PEER_DK // 2), (PEER_DK // 2) ** -0.5),
        'peer_u': nrm((L, PEER_EXPERTS, d), d ** -0.5),
        'peer_v': nrm((L, PEER_EXPERTS, d), PEER_HEADS ** -0.5),
        'g_final': 1.0 + nrm((d,), 0.02),
    }


def reference(x_prompt, x_sample, cache_k, cache_v, cache_idx_k, page_table, state_conv, state_lru,
              cache_mem_k, cache_mem_v, mem_prompt, g_mix, w_in, conv_w, conv_b, w_rg, b_rg, w_ig, b_ig,
              lru_lambda, g_mem, w_mem_kv, w_gate, w_br, w_o, g_ffn, w_peer_q, peer_sub_keys, peer_u,
              peer_v, g_final):
    x_p, x_s = x_prompt, x_sample
    st_p, st_s = [], []
    for l in range(DEPTH):
        lru_p = (conv_w[l], conv_b[l], w_rg[l], b_rg[l], w_ig[l], b_ig[l], lru_lambda[l])
        merge_p = (w_gate[l], w_br[l], w_o[l])
        peer_p = (w_peer_q[l], peer_sub_keys[l], peer_u[l], peer_v[l])
        xn = rmsnorm(x_p, g_mix[l])
        oa, ol, om, sp = prompt_mixer(xn, mem_prompt, lru_p, w_in[l], g_mem[l], w_mem_kv[l])
        x_p = residual_tail(x_p, xn, oa, ol, om, merge_p, g_ffn[l], peer_p)
        xn = rmsnorm(x_s, g_mix[l])
        oa, ol, om, ss = sample_mixer(xn, cache_k[l], cache_v[l], cache_idx_k[l], page_table, state_conv[l],
                                      state_lru[l], cache_mem_k[l], cache_mem_v[l], lru_p, w_in[l])
        x_s = residual_tail(x_s, xn, oa, ol, om, merge_p, g_ffn[l], peer_p)
        st_p.append(sp)
        st_s.append(ss)
    y_prompt = rmsnorm(x_p, g_final)
    y_sample = rmsnorm(x_s, g_final)
    k_p, v_p, ik_p, conv_p, h_p, mk_p, mv_p = [jnp.stack(c) for c in zip(*st_p)]
    k_s, v_s, ik_s, conv_s, h_s = [jnp.stack(c) for c in zip(*st_s)]
    return (y_prompt, y_sample, k_p, v_p, ik_p, conv_p, h_p, mk_p, mv_p, k_s, v_s, ik_s, conv_s, h_s)
```

```python
import numpy as np
from contextlib import ExitStack
import concourse.bass as bass
import concourse.mybir as mybir
from concourse.bass_utils import run_bass_kernel_spmd

F32 = mybir.dt.float32
BF16 = mybir.dt.bfloat16
I32 = mybir.dt.int32
U32 = mybir.dt.uint32
AF = mybir.ActivationFunctionType
ALU = mybir.AluOpType
AX = mybir.AxisListType

NCORES = 8
D = 2048
SEQ = 2048
NS_SEQ = 16
DEC = 4
TS = NS_SEQ * DEC
T = SEQ + TS
NT = 17
N_IN = 5712
EPS = 1e-6
NEG = -1.0e30
O_Q, O_K, O_V, O_IQ, O_IK, O_IW, O_XB, O_YB, O_QM = 0, 1024, 1280, 1536, 2560, 2624, 2640, 3664, 4688


def trows(tt):
    return 128 if tt < 16 else 64


class _Op:
    __slots__ = ("eng", "idx", "fn", "dma", "deps", "signaled", "cum", "sem", "tgt", "prev_tgt", "k")

    def __init__(self, eng, idx, fn, dma):
        self.eng = eng
        self.idx = idx
        self.fn = fn
        self.dma = dma
        self.deps = []
        self.signaled = False
        self.cum = 0
        self.sem = None
        self.tgt = 0
        self.prev_tgt = 0
        self.k = 0


class _Res:
    __slots__ = ("lw", "rd")

    def __init__(self):
        self.lw = None
        self.rd = []


class Prog:
    ENG = ("tensor", "vector", "scalar", "gpsimd", "sync")
    NS = 12

    def __init__(self, nc):
        self.nc = nc
        self.ops = {e: [] for e in self.ENG}
        self.res = {}
        self.ndma = {e: 0 for e in self.ENG}

    def _r(self, k):
        r = self.res.get(k)
        if r is None:
            r = self.res[k] = _Res()
        return r

    def op(self, eng, fn, r=(), w=(), dma=False):
        o = _Op(eng, len(self.ops[eng]), fn, dma)
        deps = {}
        for k in r:
            rr = self._r(k)
            if rr.lw is not None:
                deps[id(rr.lw)] = (rr.lw, True)
        for k in w:
            rr = self._r(k)
            if rr.lw is not None:
                deps[id(rr.lw)] = (rr.lw, True)
            for x in rr.rd:
                if id(x) not in deps:
                    deps[id(x)] = (x, False)
        for d, hard in deps.values():
            if d is o:
                continue
            if d.dma or o.dma or d.eng != o.eng:
                o.deps.append(d)
            elif hard and o.eng != "tensor":
                o.deps.append(d)
        for k in r:
            self._r(k).rd.append(o)
        for k in w:
            rr = self._r(k)
            rr.lw = o
            rr.rd = []
        if dma:
            o.k = self.ndma[eng]
            self.ndma[eng] += 1
        self.ops[eng].append(o)
        return o

    def dma(self, eng, out, in_, r=(), w=(), **kw):
        return self.op(eng, lambda e: e.dma_start(out=out, in_=in_, **kw), r=r, w=w, dma=True)

    def barrier(self):
        last = []
        for e in self.ENG:
            for o in reversed(self.ops[e]):
                if o.fn is not None and not o.dma:
                    last.append(o)
                    break
        alld = []
        for e in self.ENG:
            seen = 0
            for o in reversed(self.ops[e]):
                if o.dma:
                    alld.append(o)
                    seen += 1
                    if seen >= self.NS:
                        break
        for e in self.ENG:
            o = _Op(e, len(self.ops[e]), None, False)
            o.deps = [d for d in last + alld if d.eng != e or d.dma or e != "tensor"]
            self.ops[e].append(o)
        self.res = {}

    def emit(self, es):
        nc = self.nc
        sem_e = {e: es.enter_context(nc.semaphore("se_" + e)) for e in self.ENG}
        sem_d = {e: [es.enter_context(nc.semaphore("sd_%s%d" % (e, i))) for i in range(self.NS)]
                 for e in self.ENG if self.ndma[e]}
        for e in self.ENG:
            for o in self.ops[e]:
                for d in o.deps:
                    d.signaled = True
        for e in self.ENG:
            c = 0
            for o in self.ops[e]:
                if o.dma:
                    o.sem = sem_d[e][o.k % self.NS]
                    o.tgt = 16 * (o.k // self.NS + 1)
                    o.prev_tgt = o.tgt - 16
                elif o.signaled:
                    c += 1
                    o.cum = c
        finals = {}
        for e in self.ENG:
            f = {}
            for o in self.ops[e]:
                if o.dma:
                    f[o.k % self.NS] = (o.sem, o.tgt)
            finals[e] = list(f.values())
        block = es.enter_context(nc.Block())

        def mk(e):
            def body(eng):
                seen = {}
                for o in self.ops[e]:
                    waits = {}
                    for d in o.deps:
                        if d.dma:
                            key, sem, v = ("d", d.eng, d.k % self.NS), d.sem, d.tgt
                        else:
                            key, sem, v = ("e", d.eng), sem_e[d.eng], d.cum
                        if seen.get(key, 0) >= v:
                            continue
                        if key not in waits or waits[key][1] < v:
                            waits[key] = (sem, v)
                    if o.dma and o.prev_tgt > 0:
                        key = ("d", e, o.k % self.NS)
                        if seen.get(key, 0) < o.prev_tgt:
                            waits[key] = (o.sem, max(o.prev_tgt, waits.get(key, (None, 0))[1]))
                    for key, (sem, v) in waits.items():
                        eng.wait_ge(sem, v)
                        seen[key] = v
                    if o.fn is None:
                        continue
                    ins = o.fn(eng)
                    if o.dma:
                        ins.then_inc(o.sem, 16)
                    elif o.signaled:
                        ins.then_inc(sem_e[e], 1)
                for sem, v in finals[e]:
                    eng.wait_ge(sem, v)
            return body

        for e in self.ENG:
            getattr(block, e)(mk(e))


IN_SPECS = [
    ("xp", [SEQ, D], F32), ("xs", [TS, D], F32),
    ("cache_k", [2560 * 128, 256], F32), ("cache_v", [2560 * 128, 256], F32),
    ("cache_ik", [2560 * 128, 64], F32), ("ptab", [NS_SEQ, 16], I32),
    ("st_conv", [NS_SEQ * 3, 1024], F32), ("st_lru", [NS_SEQ, 1024], F32),
    ("cmk", [NS_SEQ * 256, 1024], F32), ("cmv", [NS_SEQ * 256, 1024], F32),
    ("mem", [256, D], F32),
    ("g_mix", [1, D], F32), ("w_in", [D, N_IN], F32), ("conv_w", [4, 1024], F32), ("conv_b", [1, 1024], F32),
    ("w_rg", [1024, 128], F32), ("b_rg", [1, 1024], F32), ("w_ig", [1024, 128], F32), ("b_ig", [1, 1024], F32),
    ("lam", [1, 1024], F32), ("g_mem", [1, D], F32), ("w_mem_kv", [D, 2048], F32),
    ("w_gate", [D, 3 * D], F32), ("w_br", [3 * 1024, D], F32), ("w_o", [D, D], F32), ("g_ffn", [1, D], F32),
    ("w_pq", [D, 2048], F32), ("sub_keys", [2 * 8 * 128, 128], F32), ("peer_u", [32768, 1024], F32),
    ("peer_v", [32768, 1024], F32), ("g_final", [1, D], F32),
]
OUT_SPECS = [
    ("y_p", [SEQ, D]), ("y_s", [TS, D]), ("k_p", [SEQ, 256]), ("v_p", [SEQ, 256]), ("ik_p", [SEQ, 64]),
    ("conv_p", [3, 1024]), ("h_p", [1, 1024]), ("mk_p", [256, 1024]), ("mv_p", [256, 1024]),
    ("k_s", [TS, 256]), ("v_s", [TS, 256]), ("ik_s", [TS, 64]), ("conv_s", [NS_SEQ * 3, 1024]),
    ("h_s", [NS_SEQ, 1024]),
]


_INS = {n: (s, d) for n, s, d in IN_SPECS}
_OUTS = {n: s for n, s in OUT_SPECS}


class _Lazy(dict):
    def __init__(self, mk):
        super().__init__()
        self.mk = mk

    def __missing__(self, n):
        v = self[n] = self.mk(n)
        return v


DEBUG_IO = {}


class K:
    def sub(self):
        return _Sub(self)


class _Sub:
    def __init__(self, k):
        self.k = k

    def __enter__(self):
        self.prev = self.k.scope
        self.st = ExitStack()
        self.k.scope = self.st
        return self

    def __exit__(self, *a):
        self.k.p.barrier()
        self.st.close()
        self.k.scope = self.prev
        return False


def build(phases=("all",)):
    nc = bass.Bass("TRN2", target_bir_lowering=False)
    es = ExitStack()
    k = K()
    k.nc = nc
    k.es = es
    k.I = _Lazy(lambda n: nc.dram_tensor(n, _INS[n][0], _INS[n][1], kind="ExternalInput").ap())
    k.O = _Lazy(lambda n: nc.dram_tensor(n, _OUTS[n], F32, kind="ExternalOutput").ap())
    k.S = {}
    p = k.p = Prog(nc)

    def scratch(name, shape, dt):
        if name not in k.S:
            kind = DEBUG_IO.get(name)
            if kind:
                k.S[name] = nc.dram_tensor(name, shape, dt, kind=kind).ap()
            else:
                k.S[name] = nc.dram_tensor(name, shape, dt).ap()
        return k.S[name]
    k.scratch = scratch
    k.scope = es

    k.nsb = 0

    def sb(name, shape, dt):
        k.nsb += 1
        return k.scope.enter_context(nc.sbuf_tensor("%s_%d" % (name, k.nsb), shape, dt))
    k.sb = sb
    k.ps = [es.enter_context(nc.psum_tensor("ps%d" % i, [128, 512], F32)) for i in range(8)]

    k.ident_f = sb("ident_f", [128, 128], F32)
    k.ident_b = sb("ident_b", [128, 128], BF16)
    k.ones_f = sb("ones_f", [128, 128], F32)
    p.op("gpsimd", lambda e: e.memset(k.ones_f[:], 1.0), w=["ones_f"])
    p.op("gpsimd", lambda e: e.affine_select(out=k.ident_f[:], in_=k.ones_f[:], pattern=[[-1, 128]],
                                             compare_op=ALU.is_equal, fill=0.0, base=0, channel_multiplier=1),
         r=["ones_f"], w=["ident_f"])
    p.op("vector", lambda e: e.tensor_copy(out=k.ident_b[:], in_=k.ident_f[:]), r=["ident_f"], w=["ident_b"])
    p.barrier()

    def run_phase(fn, *a):
        with ExitStack() as sc:
            k.scope = sc
            fn(k, *a)
            p.barrier()
        k.scope = es

    if "proj" in phases or "all" in phases:
        run_phase(phase_proj)
    pd = dict(PHASES)
    for name in ("mem", "lru", "dsa", "merge", "peer"):
        if name in phases or "all" in phases:
            run_phase(pd[name])
    p.emit(es)
    es.close()
    nc._used_in = list(k.I.keys())
    nc._used_out = list(k.O.keys())
    return nc


def load_bcast(k, name, dram_row, n):
    t = k.sb(name, [128, n], F32)
    k.p.dma("sync", t[:], dram_row[0, :].partition_broadcast(128), w=[name])
    return t


def rms_tile(k, xt, n, g, gkey, xn_out, tag, col):
    p = k.p
    ss, rs, junk = k.ss, k.rs, k.junk
    xk = ("xt", tag)
    p.op("scalar", lambda e: e.activation(out=junk[:n, :], in_=xt[:n, :], func=AF.Square,
                                          accum_out=ss[:n, col:col + 1]), r=[xk], w=[("ss", col), "junk"])
    p.op("vector", lambda e: e.tensor_scalar(out=rs[:n, col:col + 1], in0=ss[:n, col:col + 1],
                                             scalar1=1.0 / D, scalar2=EPS, op0=ALU.mult, op1=ALU.add),
         r=[("ss", col)], w=[("rs", col)])
    p.op("scalar", lambda e: e.sqrt(out=rs[:n, col:col + 1], in_=rs[:n, col:col + 1]),
         r=[("rs", col)], w=[("rs", col)])
    p.op("vector", lambda e: e.reciprocal(out=rs[:n, col:col + 1], in_=rs[:n, col:col + 1]),
         r=[("rs", col)], w=[("rs", col)])
    p.op("vector", lambda e: e.scalar_tensor_tensor(out=xn_out[:n, :], in0=xt[:n, :], scalar=rs[:n, col:col + 1],
                                                    in1=g[:n, :], op0=ALU.mult, op1=ALU.mult),
         r=[xk, ("rs", col), gkey], w=[("xn", tag)])


def transpose_to_T(k, xn, n, tag, dstT, t0, pbase):
    p = k.p
    for half in range(2):
        bank = pbase + half
        pv = k.ps[bank][:].bitcast(BF16)
        for j in range(8):
            c = half * 8 + j
            p.op("tensor", lambda e, c=c, j=j, pv=pv: e.transpose(
                out=pv[:, j * 128:j * 128 + n], in_=xn[:n, c * 128:(c + 1) * 128], identity=k.ident_b[:n, :n]),
                r=[("xn", tag)], w=[("ps", bank)])
        dst = dstT[:, half * 8:half * 8 + 8, t0:t0 + n]
        srcv = pv.rearrange("p (c t) -> p c t", c=8)[:, :, :n]
        if half == 0:
            p.op("scalar", lambda e, dst=dst, srcv=srcv: e.copy(out=dst, in_=srcv),
                 r=[("ps", bank)], w=[("T", id(dstT), t0, half)])
        else:
            p.op("vector", lambda e, dst=dst, srcv=srcv: e.tensor_copy(out=dst, in_=srcv),
                 r=[("ps", bank)], w=[("T", id(dstT), t0, half)])


def Tkeys(dstT, t0s):
    return [("T", id(dstT), t0, h) for t0 in t0s for h in range(2)]


def norm_stats(k):
    k.ss = k.sb("ss", [128, 64], F32)
    k.rs = k.sb("rs", [128, 64], F32)
    k.junk = k.sb("junk", [128, D], BF16)


def linear_fm(k, xT, xkeys, wname, chunks, evac, tag):
    p = k.p
    stage = [k.sb("st_%s%d" % (tag, i), [128, 16, 128], F32) for i in range(2)]
    wb = [k.sb("wb_%s%d" % (tag, i), [128, 16, 128], BF16) for i in range(2)]
    W = k.I[wname]
    for ci, pieces in enumerate(chunks):
        b = ci % 2
        r0 = 0
        for (c0, ncl) in pieces:
            src = W[:, c0:c0 + ncl].rearrange("(c p) n -> p c n", p=128)
            p.dma("sync", stage[b][:, :, r0:r0 + ncl], src, w=[("st", tag, b)])
            r0 += ncl
        rows = r0
        if ci % 2 == 0:
            p.op("scalar", lambda e, b=b, rows=rows: e.copy(out=wb[b][:, :, :rows], in_=stage[b][:, :, :rows]),
                 r=[("st", tag, b)], w=[("wb", tag, b)])
        else:
            p.op("gpsimd", lambda e, b=b, rows=rows: e.tensor_copy(out=wb[b][:, :, :rows], in_=stage[b][:, :, :rows]),
                 r=[("st", tag, b)], w=[("wb", tag, b)])
        for si in range(5):
            t0 = si * 512
            tn = min(512, T - t0)
            bank = (ci * 5 + si) % 4
            for c in range(16):
                p.op("tensor", lambda e, b=b, c=c, rows=rows, t0=t0, tn=tn, bank=bank: e.matmul(
                    out=k.ps[bank][:rows, :tn], lhsT=wb[b][:, c, :rows], rhs=xT[:, c, t0:t0 + tn],
                    start=(c == 0), stop=(c == 15)), r=[("wb", tag, b)] + xkeys, w=[("ps", bank)])
            evac(ci, si, k.ps[bank], bank, rows, t0, tn)


def phase_proj(k):
    p, nc = k.p, k.nc
    norm_stats(k)
    xnT = k.sb("xnT", [128, 16, T], BF16)
    with k.sub():
        g = load_bcast(k, "g_bc", k.I["g_mix"], D)
        xt = [k.sb("xt%d" % i, [128, D], F32) for i in range(2)]
        xn = [k.sb("xnb%d" % i, [128, D], BF16) for i in range(2)]
        for tt in range(NT):
            b = tt % 2
            n = trows(tt)
            src = k.I["xp"][tt * 128:(tt + 1) * 128, :] if tt < 16 else k.I["xs"][:, :]
            p.dma("sync", xt[b][:n, :], src, w=[("xt", b)])
            rms_tile(k, xt[b], n, g, "g_bc", xn[b], b, tt)
            transpose_to_T(k, xn[b], n, b, xnT, tt * 128, b * 2)
    allx = []
    with k.sub():
        phase_proj_tok(k, xnT)
    phase_proj_fm(k, xnT, allx)


def phase_proj_tok(k, xnT):
    p, nc = k.p, k.nc

    stage = k.sb("kv_stage", [128, 16, 592], F32)
    wbk = k.sb("kv_wb", [128, 16, 592], BF16)
    p.dma("sync", stage[:, :, 0:512], k.I["w_in"][:, O_K:O_K + 512].rearrange("(c p) n -> p c n", p=128),
          w=["kvst0"])
    p.dma("sync", stage[:, :, 512:592], k.I["w_in"][:, O_IK:O_IK + 80].rearrange("(c p) n -> p c n", p=128),
          w=["kvst1"])
    p.op("scalar", lambda e: e.copy(out=wbk[:, :, 0:512], in_=stage[:, :, 0:512]), r=["kvst0"], w=["kvwb0"])
    p.op("vector", lambda e: e.tensor_copy(out=wbk[:, :, 512:592], in_=stage[:, :, 512:592]), r=["kvst1"], w=["kvwb1"])
    vtok = k.scratch("v_tok", [T, 256], BF16)
    iwtok = k.scratch("iw_tok", [T, 16], F32)
    ob = [k.sb("kv_ob%d" % i, [128, 592], F32) for i in range(2)]
    vb = [k.sb("kv_vb%d" % i, [128, 256], BF16) for i in range(2)]
    for tt in range(NT):
        n = trows(tt)
        b = tt % 2
        t0 = tt * 128
        xk = Tkeys(xnT, [t0])
        ba, bb = 4 + b * 2, 5 + b * 2
        for c in range(16):
            p.op("tensor", lambda e, c=c, n=n, t0=t0, ba=ba: e.matmul(
                out=k.ps[ba][:n, :512], lhsT=xnT[:, c, t0:t0 + n], rhs=wbk[:, c, 0:512],
                start=(c == 0), stop=(c == 15)), r=["kvwb0"], w=[("ps", ba)])
        for c in range(16):
            p.op("tensor", lambda e, c=c, n=n, t0=t0, bb=bb: e.matmul(
                out=k.ps[bb][:n, :80], lhsT=xnT[:, c, t0:t0 + n], rhs=wbk[:, c, 512:592],
                start=(c == 0), stop=(c == 15)), r=["kvwb1"], w=[("ps", bb)])
        p.op("scalar", lambda e, n=n, b=b, ba=ba: e.copy(out=ob[b][:n, 0:512], in_=k.ps[ba][:n, :512]),
             r=[("ps", ba)], w=[("ob", b, 0)])
        p.op("vector", lambda e, n=n, b=b, bb=bb: e.tensor_copy(out=ob[b][:n, 512:592], in_=k.ps[bb][:n, :80]),
             r=[("ps", bb)], w=[("ob", b, 1)])
        p.op("vector", lambda e, n=n, b=b: e.tensor_copy(out=vb[b][:n, :], in_=ob[b][:n, 256:512]),
             r=[("ob", b, 0)], w=[("vb", b)])
        p.dma("sync", vtok[t0:t0 + n, :], vb[b][:n, :], r=[("vb", b)])
        p.dma("sync", iwtok[t0:t0 + n, :], ob[b][:n, 576:592], r=[("ob", b, 1)])
        if tt < 16:
            rows = slice(t0, t0 + 128)
            p.dma("sync", k.O["k_p"][rows, :], ob[b][:, 0:256], r=[("ob", b, 0)])
            p.dma("sync", k.O["v_p"][rows, :], ob[b][:, 256:512], r=[("ob", b, 0)])
            p.dma("sync", k.O["ik_p"][rows, :], ob[b][:, 512:576], r=[("ob", b, 1)])
        else:
            p.dma("sync", k.O["k_s"][:, :], ob[b][:64, 0:256], r=[("ob", b, 0)])
            p.dma("sync", k.O["v_s"][:, :], ob[b][:64, 256:512], r=[("ob", b, 0)])
            p.dma("sync", k.O["ik_s"][:, :], ob[b][:64, 512:576], r=[("ob", b, 1)])
    for half in range(2):
        c0 = O_XB + half * 512
        p.dma("sync", stage[:, :, 0:512], k.I["w_in"][:, c0:c0 + 512].rearrange("(c p) n -> p c n", p=128),
              w=["kvst0"])
        p.op("scalar", lambda e: e.copy(out=wbk[:, :, 0:512], in_=stage[:, :, 0:512]), r=["kvst0"], w=["kvwb0"])
        for tt in (15, 16):
            n = trows(tt)
            b = tt % 2
            t0 = tt * 128
            ba = 4 + b * 2
            for c in range(16):
                p.op("tensor", lambda e, c=c, n=n, t0=t0, ba=ba: e.matmul(
                    out=k.ps[ba][:n, :512], lhsT=xnT[:, c, t0:t0 + n], rhs=wbk[:, c, 0:512],
                    start=(c == 0), stop=(c == 15)), r=["kvwb0"], w=[("ps", ba)])
            p.op("scalar", lambda e, n=n, b=b, ba=ba: e.copy(out=ob[b][:n, 0:512], in_=k.ps[ba][:n, :512]),
                 r=[("ps", ba)], w=[("ob", b, 0)])
            cs = slice(half * 512, half * 512 + 512)
            if tt == 15:
                p.dma("sync", k.O["conv_p"][:, cs], ob[b][125:128, 0:512], r=[("ob", b, 0)])
            else:
                srcv = ob[b][:64, 0:512]
                xbs = k.scratch("xb_s", [TS, 1024], F32)
                p.dma("sync", xbs[:, cs], srcv, r=[("ob", b, 0)], w=[("xbs", half)])
                p.dma("sync", k.O["conv_s"].rearrange("(b j) n -> b j n", j=3)[:, :, cs],
                      xbs.rearrange("(b t) n -> b t n", t=4)[:, 1:4, cs], r=[("xbs", half)])


def phase_proj_fm(k, xnT, allx):
    p, nc = k.p, k.nc
    projT = k.scratch("projT", [43, 128, T], BF16)
    chunks = []
    for c0 in list(range(O_Q, O_Q + 1024, 128)) + list(range(O_K, O_K + 256, 128)) + list(range(O_IQ, O_IQ + 1024, 128)):
        chunks.append([(c0, 128)])
    chunks.append([(O_IK, 64), (O_IK, 64)])
    for base in (O_XB, O_YB, O_QM):
        for c0 in range(base, base + 1024, 128):
            chunks.append([(c0, 128)])
    obf = [k.sb("pj_ob%d" % i, [128, T], BF16) for i in range(2)]

    def evac_proj(dst):
        def evac(ci, si, ps, bank, rows, t0, tn):
            b = ci % 2
            if si % 2 == 0:
                p.op("scalar", lambda e: e.copy(out=obf[b][:rows, t0:t0 + tn], in_=ps[:rows, :tn]),
                     r=[("ps", bank)], w=[("obf", b, si)])
            else:
                p.op("vector", lambda e: e.tensor_copy(out=obf[b][:rows, t0:t0 + tn], in_=ps[:rows, :tn]),
                     r=[("ps", bank)], w=[("obf", b, si)])
            if si == 4:
                p.dma("sync", dst[ci, :rows, :], obf[b][:rows, :], r=[("obf", b, s_) for s_ in range(5)])
        return evac
    linear_fm(k, xnT, allx, "w_in", chunks, evac_proj(projT), "pj")

    gT = k.scratch("gT", [48, 128, T], BF16)

    def evac_gate(ci, si, ps, bank, rows, t0, tn):
        b = ci % 2
        p.op("scalar", lambda e: e.activation(out=obf[b][:rows, t0:t0 + tn], in_=ps[:rows, :tn], func=AF.Sigmoid),
             r=[("ps", bank)], w=[("obf", b, si)])
        if si == 4:
            p.dma("sync", gT[ci, :rows, :], obf[b][:rows, :], r=[("obf", b, s_) for s_ in range(5)])
    linear_fm(k, xnT, allx, "w_gate", [[(c0, 128)] for c0 in range(0, 3 * D, 128)], evac_gate, "gt")


PHASES = []


_NC_CACHE = {}


def make_in_maps(inp):
    f = lambda a: np.ascontiguousarray(a, dtype=np.float32)
    shared = {
        "cache_k": f(inp["cache_k"]).reshape(2560 * 128, 256), "cache_v": f(inp["cache_v"]).reshape(2560 * 128, 256),
        "cache_ik": f(inp["cache_idx_k"]).reshape(2560 * 128, 64),
        "g_mix": f(inp["g_mix"]).reshape(1, D), "w_in": f(inp["w_in"]).reshape(D, N_IN),
        "conv_w": f(inp["conv_w"]).reshape(4, 1024), "conv_b": f(inp["conv_b"]).reshape(1, 1024),
        "w_rg": f(inp["w_rg"]).reshape(1024, 128), "b_rg": f(inp["b_rg"]).reshape(1, 1024),
        "w_ig": f(inp["w_ig"]).reshape(1024, 128), "b_ig": f(inp["b_ig"]).reshape(1, 1024),
        "lam": f(inp["lru_lambda"]).reshape(1, 1024), "g_mem": f(inp["g_mem"]).reshape(1, D),
        "w_mem_kv": f(inp["w_mem_kv"]).reshape(D, 2048), "w_gate": f(inp["w_gate"]).reshape(D, 3 * D),
        "w_br": f(inp["w_br"]).reshape(3 * 1024, D), "w_o": f(inp["w_o"]).reshape(D, D),
        "g_ffn": f(inp["g_ffn"]).reshape(1, D), "w_pq": f(inp["w_peer_q"]).reshape(D, 2048),
        "sub_keys": f(inp["peer_sub_keys"]).reshape(2 * 8 * 128, 128), "peer_u": f(inp["peer_u"]).reshape(32768, 1024),
        "peer_v": f(inp["peer_v"]).reshape(32768, 1024), "g_final": f(inp["g_final"]).reshape(1, D),
    }
    maps = []
    for c in range(NCORES):
        sl = slice(c * NS_SEQ, (c + 1) * NS_SEQ)
        m = dict(shared)
        m["xp"] = f(inp["x_prompt"][c])
        m["xs"] = f(inp["x_sample"][sl]).reshape(TS, D)
        m["ptab"] = np.ascontiguousarray(inp["page_table"][sl], dtype=np.int32)
        m["st_conv"] = f(inp["state_conv"][0, sl]).reshape(NS_SEQ * 3, 1024)
        m["st_lru"] = f(inp["state_lru"][0, sl]).reshape(NS_SEQ, 1024)
        m["cmk"] = f(inp["cache_mem_k"][0, sl]).reshape(NS_SEQ * 256, 1024)
        m["cmv"] = f(inp["cache_mem_v"][0, sl]).reshape(NS_SEQ * 256, 1024)
        m["mem"] = f(inp["mem_prompt"][c])
        maps.append(m)
    return maps


def assemble(res):
    g = lambda n: [np.asarray(r[n], dtype=np.float32) for r in res]
    y_p = np.stack(g("y_p"))
    y_s = np.concatenate(g("y_s")).reshape(128, DEC, D)
    k_p = np.stack(g("k_p")).reshape(1, 8, SEQ, 2, 128)
    v_p = np.stack(g("v_p")).reshape(1, 8, SEQ, 2, 128)
    ik_p = np.stack(g("ik_p")).reshape(1, 8, SEQ, 64)
    conv_p = np.stack(g("conv_p")).reshape(1, 8, 3, 1024)
    h_p = np.stack(g("h_p")).reshape(1, 8, 1024)
    mk_p = np.stack(g("mk_p")).reshape(1, 8, 256, 4, 256)
    mv_p = np.stack(g("mv_p")).reshape(1, 8, 256, 4, 256)
    k_s = np.concatenate(g("k_s")).reshape(1, 128, DEC, 2, 128)
    v_s = np.concatenate(g("v_s")).reshape(1, 128, DEC, 2, 128)
    ik_s = np.concatenate(g("ik_s")).reshape(1, 128, DEC, 64)
    conv_s = np.concatenate(g("conv_s")).reshape(1, 128, 3, 1024)
    h_s = np.concatenate(g("h_s")).reshape(1, 128, 1024)
    return (y_p, y_s, k_p, v_p, ik_p, conv_p, h_p, mk_p, mv_p, k_s, v_s, ik_s, conv_s, h_s)


def kernel(**inputs):
    if "nc" not in _NC_CACHE:
        _NC_CACHE["nc"] = build()
    nc = _NC_CACHE["nc"]
    in_maps = [{n: m[n] for n in nc._used_in} for m in make_in_maps(inputs)]
    res = run_bass_kernel_spmd(nc, in_maps, core_ids=list(range(NCORES)))
    outs = []
    for r in res.results:
        d = dict(r)
        for n, s in OUT_SPECS:
            if n not in d:
                d[n] = np.zeros(s, np.float32)
        outs.append(d)
    return assemble(outs)


def phase_mem(k):
    p, nc = k.p, k.nc
    projT = k.scratch("projT", [43, 128, T], BF16)
    oT = k.scratch("oT", [3, 8, 128, T], BF16)
    MS = 256 ** -0.5
    norm_stats(k)
    memT = k.sb("memT", [128, 16, 256], BF16)
    mkT = k.sb("mkT", [128, 8, 256], BF16)
    mvb = k.sb("mvb", [128, 2, 1024], BF16)
    qmT = k.sb("qmT", [128, 8, T], BF16)
    p.dma("sync", qmT[:], projT[35:43].rearrange("c p t -> p c t"), w=["qmT"])
    with k.sub():
        g = load_bcast(k, "gm_bc", k.I["g_mem"], D)
        xt = [k.sb("mxt%d" % i, [128, D], F32) for i in range(2)]
        xn = [k.sb("mxn%d" % i, [128, D], BF16) for i in range(2)]
        for mt in range(2):
            p.dma("sync", xt[mt][:, :], k.I["mem"][mt * 128:(mt + 1) * 128, :], w=[("xt", mt)])
            rms_tile(k, xt[mt], 128, g, "gm_bc", xn[mt], mt, mt)
            transpose_to_T(k, xn[mt], 128, mt, memT, mt * 128, mt * 2)
    with k.sub():
        stage = k.sb("mst", [128, 16, 512], F32)
        wb = k.sb("mwb", [128, 16, 512], BF16)
        ob = [k.sb("mob%d" % i, [128, 512], F32) for i in range(2)]
        for cb in range(4):
            p.dma("sync", stage[:], k.I["w_mem_kv"][:, cb * 512:(cb + 1) * 512].rearrange("(c p) n -> p c n", p=128),
                  w=["mst"])
            p.op("scalar", lambda e: e.copy(out=wb[:], in_=stage[:]), r=["mst"], w=["mwb"])
            for mt in range(2):
                bank = mt
                for c in range(16):
                    p.op("tensor", lambda e, c=c, mt=mt, bank=bank: e.matmul(
                        out=k.ps[bank][:, :], lhsT=memT[:, c, mt * 128:(mt + 1) * 128], rhs=wb[:, c, :],
                        start=(c == 0), stop=(c == 15)), r=["mwb"], w=[("ps", bank)])
                p.op("scalar", lambda e, mt=mt, bank=bank: e.copy(out=ob[mt][:, :], in_=k.ps[bank][:, :]),
                     r=[("ps", bank)], w=[("mob", mt)])
                dst = k.O["mk_p"] if cb < 2 else k.O["mv_p"]
                cs = slice((cb % 2) * 512, (cb % 2) * 512 + 512)
                p.dma("sync", dst[mt * 128:(mt + 1) * 128, cs], ob[mt][:, :], r=[("mob", mt)])
                if cb >= 2:
                    p.op("vector", lambda e, mt=mt, cs=cs: e.tensor_copy(out=mvb[:, mt, cs], in_=ob[mt][:, :]),
                         r=[("mob", mt)], w=["mvb"])
            if cb < 2:
                for j in range(4):
                    bank = 2 + j % 2
                    for c in range(16):
                        p.op("tensor", lambda e, c=c, j=j, bank=bank: e.matmul(
                            out=k.ps[bank][:, :256], lhsT=wb[:, c, j * 128:(j + 1) * 128], rhs=memT[:, c, :],
                            start=(c == 0), stop=(c == 15)), r=["mwb"], w=[("ps", bank)])
                    p.op("vector", lambda e, j=j, bank=bank, cb=cb: e.tensor_copy(
                        out=mkT[:, cb * 4 + j, :], in_=k.ps[bank][:, :256]), r=[("ps", bank)], w=["mkT"])
    with k.sub():
        mem_attend_loop(k, qmT, mkT, mvb, oT, MS, [(tt * 128, 128) for tt in range(16)], "p")
    with k.sub():
        cm = [k.sb("cmk%d" % i, [128, 2, 1024], F32) for i in range(2)]
        cv = [k.sb("cmv%d" % i, [128, 2, 1024], F32) for i in range(2)]
        mkTs = [k.sb("mkTs%d" % i, [128, 8, 256], BF16) for i in range(2)]
        mvbs = [k.sb("mvbs%d" % i, [128, 2, 1024], BF16) for i in range(2)]
        st = mem_attend_state(k, "s")
        for b in range(NS_SEQ):
            i = b % 2
            p.dma("sync", cm[i][:], k.I["cmk"][b * 256:(b + 1) * 256, :].rearrange("(m p) n -> p m n", p=128),
                  w=[("cm", i)])
            p.dma("sync", cv[i][:], k.I["cmv"][b * 256:(b + 1) * 256, :].rearrange("(m p) n -> p m n", p=128),
                  w=[("cv", i)])
            p.op("gpsimd", lambda e, i=i: e.tensor_copy(out=mvbs[i][:], in_=cv[i][:]), r=[("cv", i)], w=[("mvbs", i)])
            for c in range(8):
                bank = 6 + c % 2
                for mt in range(2):
                    p.op("tensor", lambda e, c=c, mt=mt, bank=bank, i=i: e.transpose(
                        out=k.ps[bank][:, mt * 128:(mt + 1) * 128], in_=cm[i][:, mt, c * 128:(c + 1) * 128],
                        identity=k.ident_f[:]), r=[("cm", i)], w=[("ps", bank)])
                p.op("scalar", lambda e, c=c, bank=bank, i=i: e.copy(out=mkTs[i][:, c, :], in_=k.ps[bank][:, :256]),
                     r=[("ps", bank)], w=[("mkTs", i)])
            mem_attend_tile(k, st, qmT, mkTs[i], mvbs[i], oT, MS, SEQ + 4 * b, 4, [("mkTs", i), ("mvbs", i)])


def mem_attend_state(k, tag):
    st = K()
    st.mx = k.sb("ma_mx" + tag, [128, 4], F32)
    st.rsum = k.sb("ma_rs" + tag, [128, 4], F32)
    st.P = k.sb("ma_P" + tag, [128, 4, 256], BF16)
    st.PT = k.sb("ma_PT" + tag, [128, 8, 128], BF16)
    st.oc = k.sb("ma_oc" + tag, [128, 8, 128], BF16)
    return st


def mem_attend_loop(k, qmT, mkT, mvb, oT, MS, tiles, tag):
    st = mem_attend_state(k, tag)
    for (t0, n) in tiles:
        mem_attend_tile(k, st, qmT, mkT, mvb, oT, MS, t0, n, ["mkT", "mvb"])


def mem_attend_tile(k, st, qmT, mkT, mvb, oT, MS, t0, n, kvkeys):
    p = k.p
    for h in range(4):
        bank = h // 2
        for kc in range(2):
            p.op("tensor", lambda e, h=h, kc=kc, bank=bank: e.matmul(
                out=k.ps[bank][:n, (h % 2) * 256:(h % 2) * 256 + 256], lhsT=qmT[:, 2 * h + kc, t0:t0 + n],
                rhs=mkT[:, 2 * h + kc, :], start=(kc == 0), stop=(kc == 1)),
                r=["qmT"] + kvkeys, w=[("ps", bank)])
    for bank in range(2):
        p.op("vector", lambda e, bank=bank: e.tensor_reduce(
            out=st.mx[:n, bank * 2:bank * 2 + 2], in_=k.ps[bank][:n, :].rearrange("p (h m) -> p h m", h=2),
            axis=AX.X, op=ALU.max), r=[("ps", bank)], w=[("mx", bank)])
        p.op("vector", lambda e, bank=bank: e.tensor_scalar(
            out=st.mx[:n, bank * 2:bank * 2 + 2], in0=st.mx[:n, bank * 2:bank * 2 + 2], scalar1=-MS, scalar2=None,
            op0=ALU.mult), r=[("mx", bank)], w=[("mx", bank)])
    for h in range(4):
        bank = h // 2
        p.op("scalar", lambda e, h=h, bank=bank: e.activation(
            out=st.P[:n, h, :], in_=k.ps[bank][:n, (h % 2) * 256:(h % 2) * 256 + 256], func=AF.Exp,
            bias=st.mx[:n, h:h + 1], scale=MS, accum_out=st.rsum[:n, h:h + 1]),
            r=[("ps", bank), ("mx", bank)], w=[("P", h), ("rsum", h)])
    p.op("vector", lambda e: e.reciprocal(out=st.rsum[:n, :], in_=st.rsum[:n, :]),
         r=[("rsum", h) for h in range(4)], w=["rinv"])
    for h in range(4):
        p.op("vector", lambda e, h=h: e.tensor_scalar(out=st.P[:n, h, :], in0=st.P[:n, h, :],
                                                      scalar1=st.rsum[:n, h:h + 1], scalar2=None, op0=ALU.mult),
             r=["rinv", ("P", h)], w=[("P", h)])
    pv = k.ps[2][:].bitcast(BF16)
    for h in range(4):
        for mt in range(2):
            j = h * 2 + mt
            p.op("tensor", lambda e, h=h, mt=mt, j=j: e.transpose(
                out=pv[:, j * 128:j * 128 + n], in_=st.P[:n, h, mt * 128:(mt + 1) * 128],
                identity=k.ident_b[:n, :n]), r=[("P", h)], w=[("ps", 2)])
    p.op("scalar", lambda e: e.copy(out=st.PT[:, :, :n], in_=pv.rearrange("p (j t) -> p j t", j=8)[:, :, :n]),
         r=[("ps", 2)], w=["PT"])
    for h in range(4):
        for c2 in range(2):
            j = h * 2 + c2
            bank = 3 + j // 4
            for mt in range(2):
                p.op("tensor", lambda e, h=h, c2=c2, mt=mt, j=j, bank=bank: e.matmul(
                    out=k.ps[bank][:, (j % 4) * 128:(j % 4) * 128 + n],
                    lhsT=mvb[:, mt, h * 256 + c2 * 128:h * 256 + c2 * 128 + 128], rhs=st.PT[:, h * 2 + mt, :n],
                    start=(mt == 0), stop=(mt == 1)), r=["PT"] + kvkeys, w=[("ps", bank)])
    for half in range(2):
        bank = 3 + half
        eng = "scalar" if half == 0 else "vector"
        srcv = k.ps[bank][:].rearrange("p (j t) -> p j t", j=4)[:, :, :n]
        dst = st.oc[:, half * 4:half * 4 + 4, :n]
        if half == 0:
            p.op("scalar", lambda e, dst=dst, srcv=srcv: e.copy(out=dst, in_=srcv), r=[("ps", bank)], w=[("oc", half)])
        else:
            p.op("vector", lambda e, dst=dst, srcv=srcv: e.tensor_copy(out=dst, in_=srcv), r=[("ps", bank)],
                 w=[("oc", half)])
    p.dma("sync", oT[2, :, :, t0:t0 + n].rearrange("c p t -> p c t"), st.oc[:, :, :n], r=[("oc", 0), ("oc", 1)])


PHASES.append(("mem", phase_mem))


def gelu_mul(k, y, h, out, n, tmp1, tmp2, keys_r, key_w):
    p = k.p
    p.op("vector", lambda e: e.tensor_tensor(out=tmp1, in0=y, in1=y, op=ALU.mult), r=keys_r, w=[("g1", key_w)])
    p.op("vector", lambda e: e.tensor_scalar(out=tmp1, in0=tmp1, scalar1=0.044715, scalar2=1.0, op0=ALU.mult,
                                             op1=ALU.add), r=[("g1", key_w)], w=[("g1", key_w)])
    p.op("vector", lambda e: e.tensor_tensor(out=tmp1, in0=tmp1, in1=y, op=ALU.mult), r=[("g1", key_w)] + keys_r,
         w=[("g1", key_w)])
    p.op("scalar", lambda e: e.activation(out=tmp2, in_=tmp1, func=AF.Sigmoid, scale=1.5957691216057308),
         r=[("g1", key_w)], w=[("g2", key_w)])
    p.op("vector", lambda e: e.tensor_tensor(out=tmp2, in0=tmp2, in1=y, op=ALU.mult), r=[("g2", key_w)] + keys_r,
         w=[("g2", key_w)])
    p.op("vector", lambda e: e.tensor_tensor(out=out, in0=tmp2, in1=h, op=ALU.mult), r=[("g2", key_w)] + keys_r,
         w=[key_w])


def phase_lru(k):
    p, nc = k.p, k.nc
    projT = k.scratch("projT", [43, 128, T], BF16)
    oT = k.scratch("oT", [3, 8, 128, T], BF16)
    cw = k.sb("l_cw", [128, 8, 4], F32)
    pv = k.sb("l_pv", [128, 5, 8], F32)
    sc = k.sb("l_sc", [128, 2, 8], F32)
    for j in range(4):
        p.dma("sync", cw[:, :, j], k.I["conv_w"][j:j + 1, :].rearrange("o (n q) -> q (o n)", q=128), w=["cw"],
              allow_slow_non_contiguous=True)
    for i, nm in enumerate(("conv_b", "b_rg", "b_ig", "lam")):
        p.dma("sync", pv[:, i, :], k.I[nm].rearrange("o (n q) -> q (o n)", q=128), w=[("pv", i)],
              allow_slow_non_contiguous=True)
    p.op("scalar", lambda e: e.activation(out=pv[:, 4, :], in_=pv[:, 3, :], func=AF.Exp, scale=-1.0),
         r=[("pv", 3)], w=[("pv", 4)])
    p.op("scalar", lambda e: e.activation(out=pv[:, 4, :], in_=pv[:, 4, :], func=AF.Ln, bias=1.0),
         r=[("pv", 4)], w=[("pv", 4)])
    p.op("vector", lambda e: e.tensor_scalar(out=sc[:, 0, :], in0=pv[:, 4, :], scalar1=-8.0, scalar2=None,
                                             op0=ALU.mult), r=[("pv", 4)], w=["sc0"])
    p.op("vector", lambda e: e.tensor_scalar(out=sc[:, 1, :], in0=pv[:, 4, :], scalar1=-16.0, scalar2=None,
                                             op0=ALU.mult), r=[("pv", 4)], w=["sc1"])
    wst = k.sb("l_wst", [128, 2, 8, 128], F32)
    wg = k.sb("l_wg", [128, 2, 8, 128], BF16)
    p.dma("sync", wst[:, 0], k.I["w_rg"].rearrange("(n i) j -> i n j", i=128), w=["wst0"])
    p.dma("sync", wst[:, 1], k.I["w_ig"].rearrange("(n i) j -> i n j", i=128), w=["wst1"])
    p.op("vector", lambda e: e.tensor_copy(out=wg[:], in_=wst[:]), r=["wst0", "wst1"], w=["wg"])
    scv = k.sb("l_scv", [48, 1024], F32)
    slr = k.sb("l_slr", [16, 1024], F32)
    p.dma("sync", scv[:], k.I["st_conv"][:, :], w=["scv"])
    p.dma("sync", slr[:], k.I["st_lru"][:, :], w=["slr"])
    hl_p = k.sb("l_hlp", [128, 8], F32)
    hl_s = k.sb("l_hls", [128, 8, 16], F32)
    NP = SEQ
    xbb = [k.sb("l_xbb%d" % i, [128, T], BF16) for i in range(2)]
    ybb = [k.sb("l_ybb%d" % i, [128, T], BF16) for i in range(2)]
    xpad = k.sb("l_xpad", [128, 3 + NP], F32)
    xps = k.sb("l_xps", [128, 16, 7], F32)
    xc = k.sb("l_xc", [128, T], F32)
    xcb = k.sb("l_xcb", [128, T], BF16)
    rr = k.sb("l_r", [128, T], F32)
    ii = k.sb("l_i", [128, T], F32)
    aa = k.sb("l_a", [128, T], F32)
    uu = k.sb("l_u", [128, T], F32)
    hh = k.sb("l_h", [128, T], F32)
    yf = k.sb("l_yf", [128, T], F32)
    ob = [k.sb("l_ob%d" % i, [128, T], BF16) for i in range(2)]
    h0 = k.sb("l_h0", [128, 16], F32)
    tmpa = k.sb("l_tmpa", [128, 16], F32)
    p.op("vector", lambda e: e.memset(xpad[:, 0:3], 0.0), w=["xpad0"])
    for n in range(8):
        b = n % 2
        p.dma("sync", xbb[b][:], projT[19 + n], w=[("xbb", b)])
        p.dma("sync", ybb[b][:], projT[27 + n], w=[("ybb", b)])
        p.op("vector", lambda e, b=b: e.tensor_copy(out=xpad[:, 3:3 + NP], in_=xbb[b][:, 0:NP]),
             r=[("xbb", b), "xpad0"], w=["xpad"])
        p.op("tensor", lambda e, n=n: e.transpose(out=k.ps[4][:, 0:48], in_=scv[:, n * 128:(n + 1) * 128],
                                                  identity=k.ident_f[:48, :48]), r=["scv"], w=[("ps", 4)])
        p.op("tensor", lambda e, n=n: e.transpose(out=k.ps[5][:, 0:16], in_=slr[:, n * 128:(n + 1) * 128],
                                                  identity=k.ident_f[:16, :16]), r=["slr"], w=[("ps", 5)])
        p.op("vector", lambda e: e.tensor_copy(out=xps[:, :, 0:3], in_=k.ps[4][:, 0:48].rearrange("p (b j) -> p b j", j=3)),
             r=[("ps", 4)], w=["xps0"])
        p.op("vector", lambda e, b=b: e.tensor_copy(out=xps[:, :, 3:7],
                                                    in_=xbb[b][:, NP:T].rearrange("p (b t) -> p b t", t=4)),
             r=[("xbb", b)], w=["xps1"])
        p.op("vector", lambda e: e.tensor_copy(out=h0[:], in_=k.ps[5][:, 0:16]), r=[("ps", 5)], w=["h0"])
        p.op("scalar", lambda e, n=n: e.activation(out=xc[:, 0:NP], in_=xpad[:, 3:3 + NP], func=AF.Identity,
                                                   bias=pv[:, 0, n:n + 1], scale=cw[:, n, 3:4]),
             r=["xpad", "cw", ("pv", 0)], w=["xc_p"])
        p.op("scalar", lambda e, n=n: e.activation(out=xc[:, NP:T].rearrange("p (b t) -> p b t", t=4),
                                                   in_=xps[:, :, 3:7], func=AF.Identity,
                                                   bias=pv[:, 0, n:n + 1], scale=cw[:, n, 3:4]),
             r=["xps0", "xps1", "cw", ("pv", 0)], w=["xc_s"])
        for j in range(3):
            p.op("vector", lambda e, n=n, j=j: e.scalar_tensor_tensor(
                out=xc[:, 0:NP], in0=xpad[:, j:j + NP], scalar=cw[:, n, j:j + 1], in1=xc[:, 0:NP],
                op0=ALU.mult, op1=ALU.add), r=["xpad", "xc_p"], w=["xc_p"])
            p.op("vector", lambda e, n=n, j=j: e.scalar_tensor_tensor(
                out=xc[:, NP:T].rearrange("p (b t) -> p b t", t=4), in0=xps[:, :, j:j + 4], scalar=cw[:, n, j:j + 1],
                in1=xc[:, NP:T].rearrange("p (b t) -> p b t", t=4), op0=ALU.mult, op1=ALU.add),
                r=["xps0", "xps1", "xc_s"], w=["xc_s"])
        p.op("gpsimd", lambda e: e.tensor_copy(out=xcb[:], in_=xc[:]), r=["xc_p", "xc_s"], w=["xcb"])
        for gi, dst in ((0, rr), (1, ii)):
            for si in range(5):
                t0 = si * 512
                tn = min(512, T - t0)
                bank = (gi * 5 + si) % 4
                p.op("tensor", lambda e, gi=gi, n=n, t0=t0, tn=tn, bank=bank: e.matmul(
                    out=k.ps[bank][:, :tn], lhsT=wg[:, gi, n, :], rhs=xcb[:, t0:t0 + tn], start=True, stop=True),
                    r=["wg", "xcb"], w=[("ps", bank)])
                p.op("scalar", lambda e, gi=gi, n=n, t0=t0, tn=tn, bank=bank, dst=dst: e.activation(
                    out=dst[:, t0:t0 + tn], in_=k.ps[bank][:, :tn], func=AF.Sigmoid, bias=pv[:, 1 + gi, n:n + 1]),
                    r=[("ps", bank), ("pv", 1 + gi)], w=[("gate", gi)])
        p.op("scalar", lambda e, n=n: e.activation(out=aa[:], in_=rr[:], func=AF.Exp, scale=sc[:, 0, n:n + 1]),
             r=[("gate", 0), "sc0"], w=["aa"])
        p.op("scalar", lambda e, n=n: e.activation(out=uu[:], in_=rr[:], func=AF.Exp, scale=sc[:, 1, n:n + 1]),
             r=[("gate", 0), "sc1"], w=["uu"])
        p.op("scalar", lambda e: e.activation(out=uu[:], in_=uu[:], func=AF.Sqrt, bias=1.0, scale=-1.0),
             r=["uu"], w=["uu"])
        p.op("vector", lambda e: e.tensor_tensor(out=uu[:], in0=uu[:], in1=ii[:], op=ALU.mult),
             r=["uu", ("gate", 1)], w=["uu"])
        p.op("vector", lambda e: e.tensor_tensor(out=uu[:], in0=uu[:], in1=xc[:], op=ALU.mult),
             r=["uu", "xc_p", "xc_s"], w=["uu"])
        p.op("vector", lambda e: e.tensor_tensor_scan(out=hh[:, 0:NP], data0=aa[:, 0:NP], data1=uu[:, 0:NP],
                                                      initial=0.0, op0=ALU.mult, op1=ALU.add),
             r=["aa", "uu"], w=["hh_p"])
        hs = hh[:, NP:T].rearrange("p (b t) -> p b t", t=4)
        as_ = aa[:, NP:T].rearrange("p (b t) -> p b t", t=4)
        us = uu[:, NP:T].rearrange("p (b t) -> p b t", t=4)
        for t in range(4):
            prev = h0[:, :] if t == 0 else hs[:, :, t - 1]
            p.op("vector", lambda e, t=t, prev=prev: e.tensor_tensor(out=tmpa[:], in0=as_[:, :, t], in1=prev,
                                                                     op=ALU.mult),
                 r=["aa", "h0", "hh_s"], w=["tmpa"])
            p.op("vector", lambda e, t=t: e.tensor_tensor(out=hs[:, :, t], in0=tmpa[:], in1=us[:, :, t], op=ALU.add),
                 r=["tmpa", "uu"], w=["hh_s"])
        p.op("vector", lambda e, n=n: e.tensor_copy(out=hl_p[:, n:n + 1], in_=hh[:, NP - 1:NP]), r=["hh_p"], w=["hlp"])
        p.op("vector", lambda e, n=n: e.tensor_copy(out=hl_s[:, n, :], in_=hs[:, :, 3]), r=["hh_s"], w=["hls"])
        p.op("gpsimd", lambda e, b=b: e.tensor_copy(out=yf[:], in_=ybb[b][:]), r=[("ybb", b)], w=["yf"])
        gelu_mul(k, yf[:], hh[:], ob[b][:], T, rr[:], ii[:], ["yf", "hh_p", "hh_s", ("gate", 0), ("gate", 1), "uu"],
                 ("lob", b))
        p.dma("sync", oT[1, n], ob[b][:], r=[("lob", b)])
    hrow = k.sb("l_hrow", [16, 1024], F32)
    hrp = k.sb("l_hrp", [8, 128], F32)
    p.op("tensor", lambda e: e.transpose(out=k.ps[6][:8, 0:128], in_=hl_p[:, :], identity=k.ident_f[:, :]),
         r=["hlp"], w=[("ps", 6)])
    p.op("vector", lambda e: e.tensor_copy(out=hrp[:], in_=k.ps[6][:8, 0:128]), r=[("ps", 6)], w=["hrp"])
    p.dma("sync", k.O["h_p"].rearrange("o (n q) -> (o n) q", q=128), hrp[:], r=["hrp"])
    for n in range(8):
        bank = 6 + n % 2
        p.op("tensor", lambda e, n=n, bank=bank: e.transpose(out=k.ps[bank][:16, 0:128], in_=hl_s[:, n, :],
                                                             identity=k.ident_f[:, :]), r=["hls"], w=[("ps", bank)])
        p.op("vector", lambda e, n=n, bank=bank: e.tensor_copy(out=hrow[:, n * 128:(n + 1) * 128],
                                                               in_=k.ps[bank][:16, 0:128]),
             r=[("ps", bank)], w=["hrow"])
    p.dma("sync", k.O["h_s"][:, :], hrow[:], r=["hrow"])


PHASES.append(("lru", phase_lru))


def phase_merge(k):
    p, nc = k.p, k.nc
    oT = k.scratch("oT", [3, 8, 128, T], BF16)
    gT = k.scratch("gT", [48, 128, T], BF16)
    x1 = k.scratch("x1", [T, D], F32)
    mT = k.sb("mT", [128, 16, T], F32)
    with k.sub():
        on = k.sb("mg_on", [128, 8, T], BF16)
        wst = [k.sb("mg_wst%d" % i, [128, 8, 128], F32) for i in range(2)]
        wb = [k.sb("mg_wb%d" % i, [128, 8, 128], BF16) for i in range(2)]
        gt = [k.sb("mg_gt%d" % i, [128, T], BF16) for i in range(2)]
        tmp = [k.sb("mg_tmp%d" % i, [128, 512], F32) for i in range(2)]
        it = 0
        for n in range(3):
            p.dma("sync", on[:], oT[n].rearrange("c p t -> p c t"), w=["on"])
            for cc in range(16):
                b = it % 2
                it += 1
                p.dma("sync", wst[b][:], k.I["w_br"][n * 1024:(n + 1) * 1024, cc * 128:(cc + 1) * 128]
                      .rearrange("(c q) j -> q c j", q=128), w=[("wst", b)])
                p.dma("sync", gt[b][:], gT[n * 16 + cc], w=[("gt", b)])
                p.op("scalar", lambda e, b=b: e.copy(out=wb[b][:], in_=wst[b][:]), r=[("wst", b)], w=[("wb", b)])
                for si in range(5):
                    t0 = si * 512
                    tn = min(512, T - t0)
                    bank = si % 4
                    for c in range(8):
                        p.op("tensor", lambda e, b=b, c=c, t0=t0, tn=tn, bank=bank: e.matmul(
                            out=k.ps[bank][:, :tn], lhsT=wb[b][:, c, :], rhs=on[:, c, t0:t0 + tn],
                            start=(c == 0), stop=(c == 7)), r=[("wb", b), "on"], w=[("ps", bank)])
                    if n == 0:
                        p.op("vector", lambda e, b=b, cc=cc, t0=t0, tn=tn, bank=bank: e.tensor_tensor(
                            out=mT[:, cc, t0:t0 + tn], in0=k.ps[bank][:, :tn], in1=gt[b][:, t0:t0 + tn], op=ALU.mult),
                            r=[("ps", bank), ("gt", b)], w=[("mT", cc, si)])
                    else:
                        tb = si % 2
                        p.op("vector", lambda e, b=b, tb=tb, t0=t0, tn=tn, bank=bank: e.tensor_tensor(
                            out=tmp[tb][:, :tn], in0=k.ps[bank][:, :tn], in1=gt[b][:, t0:t0 + tn], op=ALU.mult),
                            r=[("ps", bank), ("gt", b)], w=[("tmp", tb)])
                        p.op("gpsimd", lambda e, cc=cc, tb=tb, t0=t0, tn=tn: e.tensor_tensor(
                            out=mT[:, cc, t0:t0 + tn], in0=mT[:, cc, t0:t0 + tn], in1=tmp[tb][:, :tn], op=ALU.add),
                            r=[("tmp", tb), ("mT", cc, si)], w=[("mT", cc, si)])
    with k.sub():
        stage = k.sb("wo_st", [128, 16, 512], F32)
        wo = k.sb("wo_wb", [128, 16, 512], BF16)
        mb = [k.sb("wo_mb%d" % i, [128, 16, 128], BF16) for i in range(2)]
        xr = [k.sb("wo_xr%d" % i, [128, 512], F32) for i in range(2)]
        xo = [k.sb("wo_xo%d" % i, [128, 512], F32) for i in range(2)]
        for cb in range(4):
            cs = slice(cb * 512, (cb + 1) * 512)
            p.dma("sync", stage[:], k.I["w_o"][:, cs].rearrange("(c q) n -> q c n", q=128), w=["wost"])
            p.op("scalar", lambda e: e.copy(out=wo[:], in_=stage[:]), r=["wost"], w=["wo"])
            for tt in range(NT):
                n = trows(tt)
                t0 = tt * 128
                b = tt % 2
                bank = tt % 4
                src = k.I["xp"][t0:t0 + 128, cs] if tt < 16 else k.I["xs"][:, cs]
                p.dma("sync", xr[b][:n, :], src, w=[("xr", b)])
                p.op("gpsimd", lambda e, b=b, t0=t0, n=n: e.tensor_copy(out=mb[b][:, :, :n], in_=mT[:, :, t0:t0 + n]),
                     w=[("mb", b)])
                for c in range(16):
                    p.op("tensor", lambda e, b=b, c=c, n=n, bank=bank: e.matmul(
                        out=k.ps[bank][:n, :], lhsT=mb[b][:, c, :n], rhs=wo[:, c, :], start=(c == 0), stop=(c == 15)),
                        r=[("mb", b), "wo"], w=[("ps", bank)])
                p.op("vector", lambda e, b=b, n=n, bank=bank: e.tensor_tensor(
                    out=xo[b][:n, :], in0=k.ps[bank][:n, :], in1=xr[b][:n, :], op=ALU.add),
                    r=[("ps", bank), ("xr", b)], w=[("xo", b)])
                p.dma("sync", x1[t0:t0 + n, cs], xo[b][:n, :], r=[("xo", b)])


PHASES.append(("merge", phase_merge))


ATT_SCALE = 128 ** -0.5


def slices(n_k):
    out = []
    s0 = 0
    while s0 < n_k:
        w = min(512, n_k - s0)
        out.append((s0, w))
        s0 += w
    return out


def topk_thr(k, Iv, Wv, n, n_k, m8, tag, ikeys=()):
    p = k.p
    for r in range(32):
        src = Iv if r == 0 else Wv
        p.op("vector", lambda e, src=src: e.max(out=m8[:n, :], in_=src[:n, :n_k]),
             r=[("I", tag), ("W", tag)] + list(ikeys), w=[("m8", tag)])
        if r < 31:
            p.op("vector", lambda e, src=src: e.match_replace(out=Wv[:n, :n_k], in_to_replace=m8[:n, :],
                                                              in_values=src[:n, :n_k], imm_value=NEG),
                 r=[("m8", tag), ("I", tag)] + list(ikeys), w=[("W", tag)])


def attend(k, st, n, n_k, lhsT, kT, kv, vt, mbv, mbkey, out_ps, out_bank, rkeys):
    p = k.p
    sl = slices(n_k)
    for i, (s0, w) in enumerate(sl):
        bank = i % 4
        p.op("tensor", lambda e, s0=s0, w=w, bank=bank: e.matmul(out=k.ps[bank][:n, :w], lhsT=lhsT,
                                                                 rhs=kT[:, kv, s0:s0 + w], start=True, stop=True),
             r=rkeys, w=[("ps", bank)])
        p.op("vector", lambda e, s0=s0, w=w, bank=bank: e.scalar_tensor_tensor(
            out=st.L[:n, s0:s0 + w], in0=k.ps[bank][:n, :w], scalar=ATT_SCALE, in1=mbv[:n, s0:s0 + w],
            op0=ALU.mult, op1=ALU.add), r=[("ps", bank), mbkey], w=["L"])
    p.op("vector", lambda e: e.tensor_reduce(out=st.mx[:n, :], in_=st.L[:n, :n_k], axis=AX.X, op=ALU.max),
         r=["L"], w=["mx"])
    p.op("vector", lambda e: e.tensor_scalar(out=st.mx[:n, :], in0=st.mx[:n, :], scalar1=-1.0, scalar2=None,
                                             op0=ALU.mult), r=["mx"], w=["mx"])
    p.op("scalar", lambda e: e.activation(out=st.P[:n, :n_k], in_=st.L[:n, :n_k], func=AF.Exp, bias=st.mx[:n, :],
                                          accum_out=st.rs[:n, :]), r=["L", "mx"], w=["P", "rs"])
    p.op("vector", lambda e: e.reciprocal(out=st.rs[:n, :], in_=st.rs[:n, :]), r=["rs"], w=["rs"])
    p.op("vector", lambda e: e.tensor_scalar(out=st.P[:n, :n_k], in0=st.P[:n, :n_k], scalar1=st.rs[:n, :],
                                             scalar2=None, op0=ALU.mult), r=["rs", "P"], w=["P"])
    nj = (n_k + 127) // 128
    for j in range(nj):
        w = min(128, n_k - j * 128)
        bank = (4, 5, 3)[j // 8]
        pv = k.ps[bank][:].bitcast(BF16)
        p.op("tensor", lambda e, j=j, w=w, pv=pv: e.transpose(out=pv[:w, (j % 8) * 128:(j % 8) * 128 + n],
                                                              in_=st.P[:n, j * 128:j * 128 + w],
                                                              identity=k.ident_b[:n, :n]),
             r=["P"], w=[("ps", bank)])
    for half in range((nj + 7) // 8):
        bank = (4, 5, 3)[half]
        pv = k.ps[bank][:].bitcast(BF16)
        cnt = min(8, nj - half * 8)
        rws = min(128, n_k - (half * 8 + cnt - 1) * 128) if cnt == 1 else 128
        srcv = pv.rearrange("p (j t) -> p j t", j=8)[:rws, :cnt, :n]
        dst = st.PT[:rws, half * 8:half * 8 + cnt, :n]
        if half == 0:
            p.op("scalar", lambda e, dst=dst, srcv=srcv: e.copy(out=dst, in_=srcv), r=[("ps", bank)], w=[("PT", half)])
        else:
            p.op("vector", lambda e, dst=dst, srcv=srcv: e.tensor_copy(out=dst, in_=srcv), r=[("ps", bank)],
                 w=[("PT", half)])
    for j in range(nj):
        w = min(128, n_k - j * 128)
        p.op("tensor", lambda e, j=j, w=w: e.matmul(out=out_ps, lhsT=vt[:w, j, kv * 128:(kv + 1) * 128],
                                                    rhs=st.PT[:w, j, :n], start=(j == 0), stop=(j == nj - 1)),
             r=[("PT", 0), ("PT", 1), ("PT", 2)] + rkeys, w=[("ps", out_bank)])


def phase_dsa(k):
    p, nc = k.p, k.nc
    projT = k.scratch("projT", [43, 128, T], BF16)
    oT = k.scratch("oT", [3, 8, 128, T], BF16)
    vtok = k.scratch("v_tok", [T, 256], BF16)
    iwtok = k.scratch("iw_tok", [T, 16], F32)
    NK = SEQ + DEC
    qT = k.sb("d_qT", [128, 8, T], BF16)
    iqT = k.sb("d_iqT", [128, 8, T], BF16)
    kT = k.sb("d_kT", [128, 2, T], BF16)
    ikT = k.sb("d_ikT", [128, T], BF16)
    p.dma("sync", qT[:], projT[0:8].rearrange("c p t -> p c t"), w=["qT"])
    p.dma("sync", iqT[:], projT[10:18].rearrange("c p t -> p c t"), w=["iqT"])
    p.dma("sync", kT[:], projT[8:10].rearrange("c p t -> p c t"), w=["kT"])
    p.dma("sync", ikT[:], projT[18], w=["ikT"])
    st = K()
    st.L = k.sb("d_L", [128, NK], F32)
    st.P = k.sb("d_P", [128, NK], BF16)
    st.PT = k.sb("d_PT", [128, 17, 128], BF16)
    st.mx = k.sb("d_mx", [128, 1], F32)
    st.rs = k.sb("d_rs", [128, 1], F32)
    Ib = k.sb("d_I", [128, NK], F32)
    Wb = k.sb("d_W", [128, NK], F32)
    mb = k.sb("d_mb", [128, NK], F32)
    rl = [k.sb("d_rl%d" % i, [128, 512], F32) for i in range(2)]
    m8 = k.sb("d_m8", [128, 8], F32)
    oc = [k.sb("d_oc%d" % i, [128, 8, 128], BF16) for i in range(2)]

    def indexer(n, n_k, tcol0, ikv, iw_fn, Iv, rk):
        it = 0
        for hi in range(16):
            c, half = hi // 2, hi % 2
            prt = slice(half * 64, half * 64 + 64)
            for i, (s0, w) in enumerate(slices(n_k)):
                bank = (hi % 2) * 4 + i % 4
                b = it % 2
                it += 1
                p.op("tensor", lambda e, c=c, prt=prt, s0=s0, w=w, bank=bank: e.matmul(
                    out=k.ps[bank][:n, :w], lhsT=iqT[prt, c, tcol0:tcol0 + n], rhs=ikv[prt, s0:s0 + w],
                    start=True, stop=True), r=["iqT"] + rk, w=[("ps", bank)])
                p.op("scalar", lambda e, w=w, bank=bank, b=b: e.activation(out=rl[b][:n, :w], in_=k.ps[bank][:n, :w],
                                                                            func=AF.Relu),
                     r=[("ps", bank)], w=[("rl", b)])
                if hi == 0:
                    p.op("vector", lambda e, s0=s0, w=w, b=b, hi=hi: e.tensor_scalar(
                        out=Iv[:n, s0:s0 + w], in0=rl[b][:n, :w], scalar1=iw_fn(hi), scalar2=None, op0=ALU.mult),
                        r=[("rl", b), "iw"], w=[("Isl", i)])
                else:
                    p.op("vector", lambda e, s0=s0, w=w, b=b, hi=hi: e.scalar_tensor_tensor(
                        out=Iv[:n, s0:s0 + w], in0=rl[b][:n, :w], scalar=iw_fn(hi), in1=Iv[:n, s0:s0 + w],
                        op0=ALU.mult, op1=ALU.add), r=[("rl", b), "iw", ("Isl", i)], w=[("Isl", i)])
        return [("Isl", i) for i in range(len(slices(n_k)))]

    with k.sub():
        vt = k.sb("d_vt", [128, 16, 256], BF16)
        iw = k.sb("d_iw", [128, 16, 16], F32)
        p.dma("sync", vt[:], vtok[0:SEQ, :].rearrange("(j q) n -> q j n", q=128), w=["vt"])
        p.dma("sync", iw[:], iwtok[0:SEQ, :].rearrange("(j q) n -> q j n", q=128), w=["iw"])
        for qi in range(16):
            n_k = 128 * (qi + 1)
            t0 = qi * 128
            ikeys = indexer(128, n_k, t0, ikT, lambda hi, qi=qi: iw[:, qi, hi:hi + 1], Ib, ["ikT"])
            p.op("gpsimd", lambda e, t0=t0: e.affine_select(out=Ib[:, t0:t0 + 128], in_=Ib[:, t0:t0 + 128],
                                                            pattern=[[-1, 128]], compare_op=ALU.is_ge, fill=NEG,
                                                            base=0, channel_multiplier=1),
                 r=ikeys, w=[("I", "p")] + ikeys)
            if qi >= 2:
                topk_thr(k, Ib, Wb, 128, n_k, m8, "p", ikeys)
                p.op("vector", lambda e, n_k=n_k: e.tensor_scalar(out=mb[:, :n_k], in0=Ib[:, :n_k], scalar1=m8[:, 7:8],
                                                                  scalar2=NEG, op0=ALU.is_lt, op1=ALU.mult),
                     r=[("I", "p"), ("m8", "p")] + ikeys, w=["mb"])
            else:
                p.op("vector", lambda e, n_k=n_k: e.tensor_scalar(out=mb[:, :n_k], in0=Ib[:, :n_k], scalar1=-1.0e29,
                                                                  scalar2=NEG, op0=ALU.is_lt, op1=ALU.mult),
                     r=[("I", "p")] + ikeys, w=["mb"])
            ob = oc[qi % 2]
            for h in range(8):
                bank = 6 + h // 4
                attend(k, st, 128, n_k, qT[:, h, t0:t0 + 128], kT, h // 4, vt, mb, "mb",
                       k.ps[bank][:, (h % 4) * 128:(h % 4) * 128 + 128], bank, ["qT", "kT", "vt"])
                if h % 4 == 3:
                    g = h // 4
                    srcv = k.ps[bank][:].rearrange("p (j t) -> p j t", j=4)
                    if g == 0:
                        p.op("scalar", lambda e, ob=ob, srcv=srcv: e.copy(out=ob[:, 0:4, :], in_=srcv),
                             r=[("ps", bank)], w=[("oc", qi % 2, 0)])
                    else:
                        p.op("vector", lambda e, ob=ob, srcv=srcv: e.tensor_copy(out=ob[:, 4:8, :], in_=srcv),
                             r=[("ps", bank)], w=[("oc", qi % 2, 1)])
            p.dma("sync", oT[0, :, :, t0:t0 + 128].rearrange("c q t -> q c t"), ob[:],
                  r=[("oc", qi % 2, 0), ("oc", qi % 2, 1)])

    with k.sub():
        ptb = k.sb("s_ptb", [128, 256], I32)
        ptf = k.sb("s_ptf", [128, 256], F32)
        idx = k.sb("s_idx", [128, 256], U32)
        iop = k.sb("s_iop", [128, 1], F32)
        p.dma("sync", ptb[:], k.I["ptab"].rearrange("b g -> (b g)").partition_broadcast(128), w=["ptb"])
        p.op("gpsimd", lambda e: e.iota(iop[:], pattern=[[0, 1]], base=0, channel_multiplier=1,
                                        allow_small_or_imprecise_dtypes=True), w=["iop"])
        p.op("vector", lambda e: e.tensor_copy(out=ptf[:], in_=ptb[:]), r=["ptb"], w=["ptf"])
        p.op("vector", lambda e: e.tensor_scalar(out=ptf[:], in0=ptf[:], scalar1=128.0, scalar2=iop[:, 0:1],
                                                 op0=ALU.mult, op1=ALU.add), r=["ptf", "iop"], w=["ptf"])
        p.op("vector", lambda e: e.tensor_copy(out=idx[:], in_=ptf[:]), r=["ptf"], w=["idx"])
        iws = k.sb("s_iw", [4, 16, 16], F32)
        p.dma("sync", iws[:], iwtok[SEQ:T, :].rearrange("(b t) h -> t b h", t=4), w=["iw"])
        Iall = k.sb("s_Iall", [64, NK], F32)
        Wall = Wb
        mball = mb
        sub1 = k.sub()
        sub1.__enter__()
        pg = [k.sb("s_pg%d" % i, [128, 128], F32) for i in range(4)]
        ikTb = [k.sb("s_ikTb%d" % i, [128, NK], BF16) for i in range(2)]
        for b in range(NS_SEQ):
            ib = ikTb[b % 2]
            for g in range(16):
                col = b * 16 + g
                t = pg[g % 4]
                for hf in range(2):
                    p.op("gpsimd", lambda e, t=t, hf=hf, col=col: e.indirect_dma_start(
                        out=t[:, hf * 64:(hf + 1) * 64], out_offset=None, in_=k.I["cache_ik"][:, :],
                        in_offset=bass.IndirectOffsetOnAxis(ap=idx[:, col:col + 1], axis=0)),
                        r=["idx"], w=[("pg", g % 4, hf)], dma=True)
                bank = 4 + (g // 4) % 2
                p.op("tensor", lambda e, t=t, g=g, bank=bank: e.transpose(
                    out=k.ps[bank][:, (g % 4) * 128:(g % 4) * 128 + 128], in_=t[:, :], identity=k.ident_f[:, :]),
                    r=[("pg", g % 4, 0), ("pg", g % 4, 1)], w=[("ps", bank)])
                if g % 4 == 3:
                    g0 = g - 3
                    p.op("scalar", lambda e, ib=ib, g0=g0, bank=bank: e.copy(out=ib[:, g0 * 128:g0 * 128 + 512],
                                                                            in_=k.ps[bank][:, :]),
                         r=[("ps", bank)], w=[("ikTb", b % 2)])
            p.op("vector", lambda e, ib=ib, b=b: e.tensor_copy(out=ib[:, SEQ:NK], in_=ikT[:, SEQ + 4 * b:SEQ + 4 * b + 4]),
                 r=["ikT"], w=[("ikTb", b % 2)])
            ikeys = indexer(4, NK, SEQ + 4 * b, ib, lambda hi, b=b: iws[:, b, hi:hi + 1], Ib, [("ikTb", b % 2)])
            p.op("gpsimd", lambda e: e.affine_select(out=Ib[:4, SEQ:NK], in_=Ib[:4, SEQ:NK], pattern=[[-1, 4]],
                                                     compare_op=ALU.is_ge, fill=NEG, base=0, channel_multiplier=1),
                 r=ikeys, w=[("I", "s")] + ikeys)
            p.dma("sync", Iall[4 * b:4 * b + 4, :], Ib[:4, :], r=[("I", "s")] + ikeys, w=[("I", "all")])
        sub1.__exit__(None, None, None)
        topk_thr(k, Iall, Wall, 64, NK, m8, "all")
        p.op("vector", lambda e: e.tensor_scalar(out=mball[:64, :], in0=Iall[:, :], scalar1=m8[:64, 7:8], scalar2=NEG,
                                                 op0=ALU.is_lt, op1=ALU.mult), r=[("I", "all"), ("m8", "all")],
             w=["mball"])
        kTb = [k.sb("s_kTb%d" % i, [128, 2, NK], BF16) for i in range(2)]
        vtb = [k.sb("s_vtb%d" % i, [128, 17, 256], BF16) for i in range(2)]
        kpg = [k.sb("s_kpg%d" % i, [128, 256], F32) for i in range(4)]
        vpg = [k.sb("s_vpg%d" % i, [128, 256], F32) for i in range(4)]
        mb16 = [k.sb("s_mb16%d" % i, [16, NK], F32) for i in range(2)]
        ocs = [k.sb("s_ocs%d" % i, [128, 8, 4], BF16) for i in range(2)]
        qs16 = [k.sb("s_qs16%d" % i, [128, 32], BF16) for i in range(2)]
        for b in range(NS_SEQ):
            kb, vb = kTb[b % 2], vtb[b % 2]
            for g in range(16):
                col = b * 16 + g
                tk, tv = kpg[g % 4], vpg[g % 4]
                p.op("gpsimd", lambda e, tk=tk, col=col: e.indirect_dma_start(
                    out=tk[:, :], out_offset=None, in_=k.I["cache_k"][:, :],
                    in_offset=bass.IndirectOffsetOnAxis(ap=idx[:, col:col + 1], axis=0)),
                    r=["idx"], w=[("kpg", g % 4)], dma=True)
                p.op("gpsimd", lambda e, tv=tv, col=col: e.indirect_dma_start(
                    out=tv[:, :], out_offset=None, in_=k.I["cache_v"][:, :],
                    in_offset=bass.IndirectOffsetOnAxis(ap=idx[:, col:col + 1], axis=0)),
                    r=["idx"], w=[("vpg", g % 4)], dma=True)
                p.op("vector", lambda e, vb=vb, g=g, tv=tv: e.tensor_copy(out=vb[:, g, :], in_=tv[:, :]),
                     r=[("vpg", g % 4)], w=[("vtb", b % 2)])
                bank = 4 + g % 2
                for kv in range(2):
                    p.op("tensor", lambda e, tk=tk, kv=kv, bank=bank: e.transpose(
                        out=k.ps[bank][:, kv * 128:(kv + 1) * 128], in_=tk[:, kv * 128:(kv + 1) * 128],
                        identity=k.ident_f[:, :]), r=[("kpg", g % 4)], w=[("ps", bank)])
                p.op("scalar", lambda e, kb=kb, g=g, bank=bank: e.copy(
                    out=kb[:, :, g * 128:(g + 1) * 128], in_=k.ps[bank][:, 0:256].rearrange("p (v s) -> p v s", v=2)),
                    r=[("ps", bank)], w=[("kTb", b % 2)])
            tc0 = SEQ + 4 * b
            p.op("vector", lambda e, kb=kb, tc0=tc0: e.tensor_copy(out=kb[:, :, SEQ:NK], in_=kT[:, :, tc0:tc0 + 4]),
                 r=["kT"], w=[("kTb", b % 2)])
            p.dma("sync", vb[:4, 16, :], vtok[tc0:tc0 + 4, :], w=[("vtb", b % 2)])
            m16 = mb16[b % 2]
            for i in range(4):
                p.dma("sync", m16[4 * i:4 * i + 4, :], mball[4 * b:4 * b + 4, :], r=["mball"], w=[("mb16", b % 2)])
            ob = ocs[b % 2]
            qs = qs16[b % 2]
            p.op("vector", lambda e, qs=qs, tc0=tc0: e.tensor_copy(
                out=qs[:, :].rearrange("p (h t) -> p h t", t=4), in_=qT[:, :, tc0:tc0 + 4]),
                r=["qT"], w=[("qs16", b % 2)])
            for kv in range(2):
                bank = 6 + kv
                attend(k, st, 16, NK, qs[:, kv * 16:kv * 16 + 16], kb, kv, vb, m16, ("mb16", b % 2),
                       k.ps[bank][:, 0:16], bank, [("qs16", b % 2), ("kTb", b % 2), ("vtb", b % 2)])
                p.op("vector", lambda e, ob=ob, kv=kv, bank=bank: e.tensor_copy(
                    out=ob[:, kv * 4:kv * 4 + 4, :], in_=k.ps[bank][:, 0:16].rearrange("p (h t) -> p h t", h=4)),
                    r=[("ps", bank)], w=[("ocs", b % 2, kv)])
            p.dma("sync", oT[0, :, :, tc0:tc0 + 4].rearrange("c q t -> q c t"), ob[:],
                  r=[("ocs", b % 2, 0), ("ocs", b % 2, 1)])


PHASES.append(("dsa", phase_dsa))


def bc(ap, shape):
    return ap.to_broadcast(shape)


def phase_peer(k):
    p, nc = k.p, k.nc
    x1 = k.scratch("x1", [T, D], F32)
    xn2f = k.scratch("xn2f", [T, D], F32)
    norm_stats(k)
    qpT = k.sb("pe_qpT", [128, 16, T], BF16)
    skT = k.sb("pe_skT", [128, 16, 128], BF16)
    with k.sub():
        xn2T = k.sb("pe_xn2T", [128, 16, T], BF16)
        with k.sub():
            g = load_bcast(k, "gf_bc", k.I["g_ffn"], D)
            xt = [k.sb("pxt%d" % i, [128, D], F32) for i in range(2)]
            xn = [k.sb("pxn%d" % i, [128, D], BF16) for i in range(2)]
            xf = [k.sb("pxf%d" % i, [128, D], F32) for i in range(2)]
            for tt in range(NT):
                b = tt % 2
                n = trows(tt)
                t0 = tt * 128
                p.dma("sync", xt[b][:n, :], x1[t0:t0 + n, :], w=[("xt", b)])
                rms_tile(k, xt[b], n, g, "gf_bc", xn[b], b, tt)
                p.op("gpsimd", lambda e, b=b, n=n: e.tensor_copy(out=xf[b][:n, :], in_=xn[b][:n, :]),
                     r=[("xn", b)], w=[("xf", b)])
                p.dma("sync", xn2f[t0:t0 + n, :], xf[b][:n, :], r=[("xf", b)])
                transpose_to_T(k, xn[b], n, b, xn2T, t0, b * 2)
            skt = [k.sb("pskt%d" % i, [128, 128], F32) for i in range(2)]
            for j in range(16):
                h, pp = j // 2, j % 2
                r0 = (pp * 8 + h) * 128
                p.dma("sync", skt[j % 2][:], k.I["sub_keys"][r0:r0 + 128, :], w=[("skt", j % 2)])
                bank = 4 + j % 2
                p.op("tensor", lambda e, j=j, bank=bank: e.transpose(out=k.ps[bank][:, 0:128], in_=skt[j % 2][:],
                                                                     identity=k.ident_f[:]),
                     r=[("skt", j % 2)], w=[("ps", bank)])
                p.op("vector", lambda e, j=j, bank=bank: e.tensor_copy(out=skT[:, j, :], in_=k.ps[bank][:, 0:128]),
                     r=[("ps", bank)], w=["skT"])

        def evac_q(ci, si, ps, bank, rows, t0, tn):
            if si % 2 == 0:
                p.op("scalar", lambda e: e.copy(out=qpT[:, ci, t0:t0 + tn], in_=ps[:, :tn]), r=[("ps", bank)],
                     w=[("qpT", ci, si)])
            else:
                p.op("vector", lambda e: e.tensor_copy(out=qpT[:, ci, t0:t0 + tn], in_=ps[:, :tn]), r=[("ps", bank)],
                     w=[("qpT", ci, si)])
        linear_fm(k, xn2T, [], "w_pq", [[(c0, 128)] for c0 in range(0, D, 128)], evac_q, "pq")

    with k.sub():
        sc = k.sb("pe_sc", [128, 16, 128], F32)
        scw = k.sb("pe_scw", [128, 16, 128], F32)
        vv = k.sb("pe_v", [128, 16, 16], F32)
        ix = k.sb("pe_ix", [128, 16, 16], U32)
        ixf = k.sb("pe_ixf", [128, 16, 16], F32)
        cand = k.sb("pe_cand", [128, 8, 256], F32)
        candw = k.sb("pe_candw", [128, 8, 256], F32)
        tv = k.sb("pe_tv", [128, 8, 16], F32)
        pos = k.sb("pe_pos", [128, 8, 16], U32)
        pa = k.sb("pe_pa", [128, 8, 16], U32)
        pb = k.sb("pe_pb", [128, 8, 16], U32)
        paf = k.sb("pe_paf", [128, 8, 16], F32)
        pbf = k.sb("pe_pbf", [128, 8, 16], F32)
        eq = k.sb("pe_eq", [128, 8, 16, 16], F32)
        sel = k.sb("pe_sel", [128, 2, 8, 16], F32)
        ef = k.sb("pe_ef", [128, 128], F32)
        eidx = k.sb("pe_eidx", [128, 2, 128], U32)
        gw = k.sb("pe_gw", [128, 8, 16], F32)
        gs = k.sb("pe_gs", [128, 8], F32)
        act = k.sb("pe_act", [128, 128], F32)
        ga = k.sb("pe_ga", [128, 128], F32)
        t1 = k.sb("pe_t1", [128, 128], F32)
        t2 = k.sb("pe_t2", [128, 128], F32)
        io16 = k.sb("pe_io16", [128, 16], F32)
        ug = [k.sb("pe_ug%d" % i, [128, D], F32) for i in range(2)]
        vg = [k.sb("pe_vg%d" % i, [128, D], F32) for i in range(2)]
        xq = k.sb("pe_xq", [128, D], F32)
        x1t = k.sb("pe_x1t", [128, D], F32)
        acc = k.sb("pe_acc", [128, D], F32)
        junkf = k.sb("pe_junkf", [128, D], F32)
        yo = k.sb("pe_yo", [128, D], F32)
        gfin = load_bcast(k, "gfin_bc", k.I["g_final"], D)
        p.op("gpsimd", lambda e: e.iota(io16[:], pattern=[[1, 16]], base=0, channel_multiplier=0,
                                        allow_small_or_imprecise_dtypes=True), w=["io16"])
        S4 = [128, 8, 16, 16]
        def tile_body(tt, n, t0):
            p.dma("sync", xq[:n, :], xn2f[t0:t0 + n, :], w=["xq"])
            p.dma("sync", x1t[:n, :], x1[t0:t0 + n, :], w=[("xt", "f")])
            for j in range(16):
                bank = j // 4
                p.op("tensor", lambda e, j=j, bank=bank: e.matmul(
                    out=k.ps[bank][:n, (j % 4) * 128:(j % 4) * 128 + 128], lhsT=qpT[:, j, t0:t0 + n], rhs=skT[:, j, :],
                    start=True, stop=True), r=["skT"], w=[("ps", bank)])
            for bank in range(4):
                dst = sc[:n, bank * 4:bank * 4 + 4, :]
                srcv = k.ps[bank][:n, :].rearrange("p (j q) -> p j q", j=4)
                if bank % 2 == 0:
                    p.op("scalar", lambda e, dst=dst, srcv=srcv: e.copy(out=dst, in_=srcv), r=[("ps", bank)],
                         w=[("sc", bank)])
                else:
                    p.op("vector", lambda e, dst=dst, srcv=srcv: e.tensor_copy(out=dst, in_=srcv), r=[("ps", bank)],
                         w=[("sc", bank)])
            for j in range(16):
                kj = [("sc", j // 4)]
                p.op("vector", lambda e, j=j: e.max(out=vv[:n, j, 0:8], in_=sc[:n, j, :]), r=kj, w=[("vv", j)])
                p.op("vector", lambda e, j=j: e.max_index(out=ix[:n, j, 0:8], in_max=vv[:n, j, 0:8], in_values=sc[:n, j, :]),
                     r=kj + [("vv", j)], w=[("ix", j)])
                p.op("vector", lambda e, j=j: e.match_replace(out=scw[:n, j, :], in_to_replace=vv[:n, j, 0:8],
                                                              in_values=sc[:n, j, :], imm_value=NEG),
                     r=kj + [("vv", j)], w=[("scw", j)])
                p.op("vector", lambda e, j=j: e.max(out=vv[:n, j, 8:16], in_=scw[:n, j, :]), r=[("scw", j)],
                     w=[("vv", j)])
                p.op("vector", lambda e, j=j: e.max_index(out=ix[:n, j, 8:16], in_max=vv[:n, j, 8:16],
                                                          in_values=scw[:n, j, :]),
                     r=[("scw", j), ("vv", j)], w=[("ix", j)])
            allv = [("vv", j) for j in range(16)]
            alli = [("ix", j) for j in range(16)]
            p.op("vector", lambda e: e.tensor_copy(out=ixf[:n], in_=ix[:n]), r=alli, w=["ixf"])
            v4 = vv[:n].rearrange("p (h two) a -> p h two a", two=2)
            i4 = ixf[:n].rearrange("p (h two) a -> p h two a", two=2)
            S4n = [n, 8, 16, 16]
            p.op("vector", lambda e: e.tensor_tensor(
                out=cand[:n].rearrange("p h (a b) -> p h a b", a=16), in0=bc(v4[:, :, 0, :].unsqueeze(3), S4n),
                in1=bc(v4[:, :, 1, :].unsqueeze(2), S4n), op=ALU.add), r=allv, w=["cand"])
            for h in range(8):
                p.op("vector", lambda e, h=h: e.max(out=tv[:n, h, 0:8], in_=cand[:n, h, :]), r=["cand"], w=[("tv", h)])
                p.op("vector", lambda e, h=h: e.max_index(out=pos[:n, h, 0:8], in_max=tv[:n, h, 0:8],
                                                          in_values=cand[:n, h, :]), r=["cand", ("tv", h)],
                     w=[("pos", h)])
                p.op("vector", lambda e, h=h: e.match_replace(out=candw[:n, h, :], in_to_replace=tv[:n, h, 0:8],
                                                              in_values=cand[:n, h, :], imm_value=NEG),
                     r=["cand", ("tv", h)], w=[("candw", h)])
                p.op("vector", lambda e, h=h: e.max(out=tv[:n, h, 8:16], in_=candw[:n, h, :]), r=[("candw", h)],
                     w=[("tv", h)])
                p.op("vector", lambda e, h=h: e.max_index(out=pos[:n, h, 8:16], in_max=tv[:n, h, 8:16],
                                                          in_values=candw[:n, h, :]), r=[("candw", h), ("tv", h)],
                     w=[("pos", h)])
            allt = [("tv", h) for h in range(8)]
            allp = [("pos", h) for h in range(8)]
            p.op("vector", lambda e: e.tensor_tensor(out=gw[:n], in0=tv[:n], in1=bc(tv[:n, :, 0:1], [n, 8, 16]),
                                                     op=ALU.subtract), r=allt, w=["gw"])
            p.op("scalar", lambda e: e.activation(out=gw[:n], in_=gw[:n], func=AF.Exp), r=["gw"], w=["gw"])
            p.op("vector", lambda e: e.tensor_reduce(out=gs[:n, :], in_=gw[:n], axis=AX.X, op=ALU.add), r=["gw"],
                 w=["gs"])
            p.op("vector", lambda e: e.reciprocal(out=gs[:n, :], in_=gs[:n, :]), r=["gs"], w=["gs"])
            p.op("vector", lambda e: e.tensor_tensor(out=gw[:n], in0=gw[:n], in1=bc(gs[:n, :].unsqueeze(2), [n, 8, 16]),
                                                     op=ALU.mult), r=["gs", "gw"], w=["gw"])
            p.op("vector", lambda e: e.tensor_single_scalar(out=pa[:n], in_=pos[:n], scalar=4,
                                                            op=ALU.logical_shift_right), r=allp, w=["pa"])
            p.op("vector", lambda e: e.tensor_single_scalar(out=pb[:n], in_=pos[:n], scalar=15, op=ALU.bitwise_and),
                 r=allp, w=["pb"])
            p.op("vector", lambda e: e.tensor_copy(out=paf[:n], in_=pa[:n]), r=["pa"], w=["paf"])
            p.op("vector", lambda e: e.tensor_copy(out=pbf[:n], in_=pb[:n]), r=["pb"], w=["pbf"])
            for w_, pf in ((0, paf), (1, pbf)):
                p.op("vector", lambda e, pf=pf: e.tensor_tensor(
                    out=eq[:n], in0=bc(pf[:n].unsqueeze(3), S4n), in1=bc(io16[:n, :].unsqueeze(1).unsqueeze(1), S4n),
                    op=ALU.is_equal), r=["paf", "pbf", "io16", "sel"], w=["eq"])
                p.op("vector", lambda e, w_=w_: e.tensor_tensor(
                    out=eq[:n], in0=eq[:n], in1=bc(i4[:, :, w_, :].unsqueeze(2), S4n), op=ALU.mult),
                    r=["eq", "ixf"], w=["eq"])
                p.op("vector", lambda e, w_=w_: e.tensor_reduce(out=sel[:n, w_], in_=eq[:n], axis=AX.X, op=ALU.add),
                     r=["eq"], w=["sel"])
            p.op("vector", lambda e: e.scalar_tensor_tensor(
                out=ef[:n, :], in0=sel[:n, 0].rearrange("p h k -> p (h k)"), scalar=128.0,
                in1=sel[:n, 1].rearrange("p h k -> p (h k)"), op0=ALU.mult, op1=ALU.add), r=["sel"], w=["ef"])
            p.op("vector", lambda e: e.tensor_scalar(out=ef[:n, :], in0=ef[:n, :], scalar1=2.0, scalar2=None,
                                                     op0=ALU.mult), r=["ef"], w=["ef"])
            p.op("vector", lambda e: e.tensor_copy(out=eidx[:n, 0, :], in_=ef[:n, :]), r=["ef"], w=["eidx0"])
            p.op("vector", lambda e: e.tensor_scalar(out=ef[:n, :], in0=ef[:n, :], scalar1=1.0, scalar2=None,
                                                     op0=ALU.add), r=["ef", "eidx0"], w=["ef"])
            p.op("vector", lambda e: e.tensor_copy(out=eidx[:n, 1, :], in_=ef[:n, :]), r=["ef"], w=["eidx"])
            p.op("vector", lambda e: e.memset(act[:], 0.0), r=["ga"], w=[("act", s_) for s_ in range(128)])
            for s_ in range(128):
                b = s_ % 2
                for hf in range(2):
                    p.op("gpsimd", lambda e, s_=s_, b=b, hf=hf: e.indirect_dma_start(
                        out=ug[b][:n, hf * 1024:(hf + 1) * 1024], out_offset=None, in_=k.I["peer_u"][:, :],
                        in_offset=bass.IndirectOffsetOnAxis(ap=eidx[:n, hf, s_:s_ + 1], axis=0)),
                        r=["eidx", "eidx0"], w=[("ug", b, hf)], dma=True)
                p.op("vector", lambda e, s_=s_, b=b: e.scalar_tensor_tensor(
                    out=junkf[:n, :], in0=ug[b][:n, :], scalar=1.0, in1=xq[:n, :], op0=ALU.mult, op1=ALU.mult,
                    accum_out=act[:n, s_:s_ + 1]), r=[("ug", b, 0), ("ug", b, 1), "xq"], w=[("act", s_), "junkf"])
            gelu_mul(k, act[:n, :], gw[:n].rearrange("p h k -> p (h k)"), ga[:n, :], n, t1[:n, :], t2[:n, :],
                     [("act", s_) for s_ in range(128)] + ["gw"], "ga")
            for s_ in range(128):
                b = s_ % 2
                for hf in range(2):
                    p.op("gpsimd", lambda e, s_=s_, b=b, hf=hf: e.indirect_dma_start(
                        out=vg[b][:n, hf * 1024:(hf + 1) * 1024], out_offset=None, in_=k.I["peer_v"][:, :],
                        in_offset=bass.IndirectOffsetOnAxis(ap=eidx[:n, hf, s_:s_ + 1], axis=0)),
                        r=["eidx", "eidx0"], w=[("vg", b, hf)], dma=True)
                if s_ == 0:
                    p.op("vector", lambda e, b=b: e.scalar_tensor_tensor(
                        out=acc[:n, :], in0=vg[b][:n, :], scalar=ga[:n, 0:1], in1=x1t[:n, :], op0=ALU.mult,
                        op1=ALU.add), r=[("vg", b, 0), ("vg", b, 1), "ga", ("xt", "f")], w=["acc"])
                else:
                    p.op("vector", lambda e, s_=s_, b=b: e.scalar_tensor_tensor(
                        out=acc[:n, :], in0=vg[b][:n, :], scalar=ga[:n, s_:s_ + 1], in1=acc[:n, :], op0=ALU.mult,
                        op1=ALU.add), r=[("vg", b, 0), ("vg", b, 1), "ga", "acc"], w=["acc"])
            if tt == 0 and "dbg" in DEBUG_IO:
                dbg = k.scratch("dbg", [6, 128, 128], F32)
                p.dma("sync", dbg[0], ef[:, :], r=["ef", "eidx"])
                p.dma("sync", dbg[1], gw[:].rearrange("p h k -> p (h k)"), r=["gw", "ga"])
                p.dma("sync", dbg[2], act[:, :], r=["ga"])
                p.dma("sync", dbg[3], ga[:, :], r=["ga"])
                p.dma("sync", dbg[4], tv[:].rearrange("p h k -> p (h k)"), r=allt + ["gw"])
                p.dma("sync", dbg[5], paf[:].rearrange("p h k -> p (h k)"), r=["paf", "eq"])
            p.op("vector", lambda e: e.tensor_copy(out=x1t[:n, :], in_=acc[:n, :]), r=["acc"], w=[("xt", "f")])
            rms_tile(k, x1t, n, gfin, "gfin_bc", yo, "f", 32 + tt)
            dst = k.O["y_p"][t0:t0 + 128, :] if tt < 16 else k.O["y_s"][:, :]
            p.dma("sync", dst, yo[:n, :], r=[("xn", "f")])

        for tt in range(NT):
            tile_body(tt, trows(tt), tt * 128)


PHASES.append(("peer", phase_peer))
```

```python
import numpy as np
from contextlib import ExitStack
import concourse.bass as bass
import concourse.mybir as mybir
from concourse.bass_utils import run_bass_kernel_spmd

F32 = mybir.dt.float32
BF16 = mybir.dt.bfloat16
I32 = mybir.dt.int32
U32 = mybir.dt.uint32
AF = mybir.ActivationFunctionType
ALU = mybir.AluOpType
AX = mybir.AxisListType

NCORES = 8
D = 2048
SEQ = 2048
NS_SEQ = 16
DEC = 4
TS = NS_SEQ * DEC
T = SEQ + TS
NT = 17
N_IN = 5712
EPS = 1e-6
NEG = -1.0e30
O_Q, O_K, O_V, O_IQ, O_IK, O_IW, O_XB, O_YB, O_QM = 0, 1024, 1280, 1536, 2560, 2624, 2640, 3664, 4688


def trows(tt):
    return 128 if tt < 16 else 64


class _Op:
    __slots__ = ("eng", "idx", "fn", "dma", "deps", "signaled", "cum", "sem", "tgt", "prev_tgt", "k")

    def __init__(self, eng, idx, fn, dma):
        self.eng = eng
        self.idx = idx
        self.fn = fn
        self.dma = dma
        self.deps = []
        self.signaled = False
        self.cum = 0
        self.sem = None
        self.tgt = 0
        self.prev_tgt = 0
        self.k = 0


class _Res:
    __slots__ = ("lw", "rd")

    def __init__(self):
        self.lw = None
        self.rd = []


class Prog:
    ENG = ("tensor", "vector", "scalar", "gpsimd", "sync")
    NS = 12

    def __init__(self, nc):
        self.nc = nc
        self.ops = {e: [] for e in self.ENG}
        self.res = {}
        self.ndma = {e: 0 for e in self.ENG}

    def _r(self, k):
        r = self.res.get(k)
        if r is None:
            r = self.res[k] = _Res()
        return r

    def op(self, eng, fn, r=(), w=(), dma=False):
        o = _Op(eng, len(self.ops[eng]), fn, dma)
        deps = {}
        for k in r:
            rr = self._r(k)
            if rr.lw is not None:
                deps[id(rr.lw)] = (rr.lw, True)
        for k in w:
            rr = self._r(k)
            if rr.lw is not None:
                deps[id(rr.lw)] = (rr.lw, True)
            for x in rr.rd:
                if id(x) not in deps:
                    deps[id(x)] = (x, False)
        for d, hard in deps.values():
            if d is o:
                continue
            if d.dma or o.dma or d.eng != o.eng:
                o.deps.append(d)
            elif hard and o.eng != "tensor":
                o.deps.append(d)
        for k in r:
            self._r(k).rd.append(o)
        for k in w:
            rr = self._r(k)
            rr.lw = o
            rr.rd = []
        if dma:
            o.k = self.ndma[eng]
            self.ndma[eng] += 1
        self.ops[eng].append(o)
        return o

    def dma(self, eng, out, in_, r=(), w=(), **kw):
        return self.op(eng, lambda e: e.dma_start(out=out, in_=in_, **kw), r=r, w=w, dma=True)

    def barrier(self):
        last = []
        for e in self.ENG:
            for o in reversed(self.ops[e]):
                if o.fn is not None and not o.dma:
                    last.append(o)
                    break
        alld = []
        for e in self.ENG:
            seen = 0
            for o in reversed(self.ops[e]):
                if o.dma:
                    alld.append(o)
                    seen += 1
                    if seen >= self.NS:
                        break
        for e in self.ENG:
            o = _Op(e, len(self.ops[e]), None, False)
            o.deps = [d for d in last + alld if d.eng != e or d.dma or e != "tensor"]
            self.ops[e].append(o)
        self.res = {}

    def emit(self, es):
        nc = self.nc
        sem_e = {e: es.enter_context(nc.semaphore("se_" + e)) for e in self.ENG}
        sem_d = {e: [es.enter_context(nc.semaphore("sd_%s%d" % (e, i))) for i in range(self.NS)]
                 for e in self.ENG if self.ndma[e]}
        for e in self.ENG:
            for o in self.ops[e]:
                for d in o.deps:
                    d.signaled = True
        for e in self.ENG:
            c = 0
            for o in self.ops[e]:
                if o.dma:
                    o.sem = sem_d[e][o.k % self.NS]
                    o.tgt = 16 * (o.k // self.NS + 1)
                    o.prev_tgt = o.tgt - 16
                elif o.signaled:
                    c += 1
                    o.cum = c
        finals = {}
        for e in self.ENG:
            f = {}
            for o in self.ops[e]:
                if o.dma:
                    f[o.k % self.NS] = (o.sem, o.tgt)
            finals[e] = list(f.values())
        block = es.enter_context(nc.Block())

        def mk(e):
            def body(eng):
                seen = {}
                for o in self.ops[e]:
                    waits = {}
                    for d in o.deps:
                        if d.dma:
                            key, sem, v = ("d", d.eng, d.k % self.NS), d.sem, d.tgt
                        else:
                            key, sem, v = ("e", d.eng), sem_e[d.eng], d.cum
                        if seen.get(key, 0) >= v:
                            continue
                        if key not in waits or waits[key][1] < v:
                            waits[key] = (sem, v)
                    if o.dma and o.prev_tgt > 0:
                        key = ("d", e, o.k % self.NS)
                        if seen.get(key, 0) < o.prev_tgt:
                            waits[key] = (o.sem, max(o.prev_tgt, waits.get(key, (None, 0))[1]))
                    for key, (sem, v) in waits.items():
                        eng.wait_ge(sem, v)
                        seen[key] = v
                    if o.fn is None:
                        continue
                    ins = o.fn(eng)
                    if o.dma:
                        ins.then_inc(o.sem, 16)
                    elif o.signaled:
                        ins.then_inc(sem_e[e], 1)
                for sem, v in finals[e]:
                    eng.wait_ge(sem, v)
            return body

        for e in self.ENG:
            getattr(block, e)(mk(e))


IN_SPECS = [
    ("xp", [SEQ, D], F32), ("xs", [TS, D], F32),
    ("cache_k", [2560 * 128, 256], F32), ("cache_v", [2560 * 128, 256], F32),
    ("cache_ik", [2560 * 128, 64], F32), ("ptab", [NS_SEQ, 16], I32),
    ("st_conv", [NS_SEQ * 3, 1024], F32), ("st_lru", [NS_SEQ, 1024], F32),
    ("cmk", [NS_SEQ * 256, 1024], F32), ("cmv", [NS_SEQ * 256, 1024], F32),
    ("mem", [256, D], F32),
    ("g_mix", [1, D], F32), ("w_in", [D, N_IN], F32), ("conv_w", [4, 1024], F32), ("conv_b", [1, 1024], F32),
    ("w_rg", [1024, 128], F32), ("b_rg", [1, 1024], F32), ("w_ig", [1024, 128], F32), ("b_ig", [1, 1024], F32),
    ("lam", [1, 1024], F32), ("g_mem", [1, D], F32), ("w_mem_kv", [D, 2048], F32),
    ("w_gate", [D, 3 * D], F32), ("w_br", [3 * 1024, D], F32), ("w_o", [D, D], F32), ("g_ffn", [1, D], F32),
    ("w_pq", [D, 2048], F32), ("sub_keys", [2 * 8 * 128, 128], F32), ("peer_u", [16384, D], F32),
    ("peer_v", [16384, D], F32), ("g_final", [1, D], F32),
]
OUT_SPECS = [
    ("y_p", [SEQ, D]), ("y_s", [TS, D]), ("k_p", [SEQ, 256]), ("v_p", [SEQ, 256]), ("ik_p", [SEQ, 64]),
    ("conv_p", [3, 1024]), ("h_p", [1, 1024]), ("mk_p", [256, 1024]), ("mv_p", [256, 1024]),
    ("k_s", [TS, 256]), ("v_s", [TS, 256]), ("ik_s", [TS, 64]), ("conv_s", [NS_SEQ * 3, 1024]),
    ("h_s", [NS_SEQ, 1024]),
]


_INS = {n: (s, d) for n, s, d in IN_SPECS}
_OUTS = {n: s for n, s in OUT_SPECS}


class _Lazy(dict):
    def __init__(self, mk):
        super().__init__()
        self.mk = mk

    def __missing__(self, n):
        v = self[n] = self.mk(n)
        return v


DEBUG_IO = {}


class K:
    def sub(self):
        return _Sub(self)


class _Sub:
    def __init__(self, k):
        self.k = k

    def __enter__(self):
        self.prev = self.k.scope
        self.st = ExitStack()
        self.k.scope = self.st
        return self

    def __exit__(self, *a):
        self.k.p.barrier()
        self.st.close()
        self.k.scope = self.prev
        return False


def build(phases=("all",)):
    nc = bass.Bass("TRN2", target_bir_lowering=False)
    es = ExitStack()
    k = K()
    k.nc = nc
    k.es = es
    k.I = _Lazy(lambda n: nc.dram_tensor(n, _INS[n][0], _INS[n][1], kind="ExternalInput").ap())
    k.O = _Lazy(lambda n: nc.dram_tensor(n, _OUTS[n], F32, kind="ExternalOutput").ap())
    k.S = {}
    p = k.p = Prog(nc)

    def scratch(name, shape, dt):
        if name not in k.S:
            kind = DEBUG_IO.get(name)
            if kind:
                k.S[name] = nc.dram_tensor(name, shape, dt, kind=kind).ap()
            else:
                k.S[name] = nc.dram_tensor(name, shape, dt).ap()
        return k.S[name]
    k.scratch = scratch
    k.scope = es

    k.nsb = 0

    def sb(name, shape, dt):
        k.nsb += 1
        return k.scope.enter_context(nc.sbuf_tensor("%s_%d" % (name, k.nsb), shape, dt))
    k.sb = sb
    k.ps = [es.enter_context(nc.psum_tensor("ps%d" % i, [128, 512], F32)) for i in range(8)]

    k.ident_f = sb("ident_f", [128, 128], F32)
    k.ident_b = sb("ident_b", [128, 128], BF16)
    k.ones_f = sb("ones_f", [128, 128], F32)
    p.op("gpsimd", lambda e: e.memset(k.ones_f[:], 1.0), w=["ones_f"])
    p.op("gpsimd", lambda e: e.affine_select(out=k.ident_f[:], in_=k.ones_f[:], pattern=[[-1, 128]],
                                             compare_op=ALU.is_equal, fill=0.0, base=0, channel_multiplier=1),
         r=["ones_f"], w=["ident_f"])
    p.op("vector", lambda e: e.tensor_copy(out=k.ident_b[:], in_=k.ident_f[:]), r=["ident_f"], w=["ident_b"])
    p.barrier()

    def run_phase(fn, *a):
        with ExitStack() as sc:
            k.scope = sc
            fn(k, *a)
            p.barrier()
        k.scope = es

    if "proj" in phases or "all" in phases:
        run_phase(phase_proj)
    pd = dict(PHASES)
    for name in ("mem", "lru", "dsa", "merge", "peer"):
        if name in phases or "all" in phases:
            run_phase(pd[name])
    p.emit(es)
    es.close()
    nc._used_in = list(k.I.keys())
    nc._used_out = list(k.O.keys())
    return nc


def load_bcast(k, name, dram_row, n):
    t = k.sb(name, [128, n], F32)
    k.p.dma("sync", t[:], dram_row[0, :].partition_broadcast(128), w=[name])
    return t


def rms_tile(k, xt, n, g, gkey, xn_out, tag, col):
    p = k.p
    ss, rs, junk = k.ss, k.rs, k.junk
    xk = ("xt", tag)
    p.op("scalar", lambda e: e.activation(out=junk[:n, :], in_=xt[:n, :], func=AF.Square,
                                          accum_out=ss[:n, col:col + 1]), r=[xk], w=[("ss", col), "junk"])
    p.op("vector", lambda e: e.tensor_scalar(out=rs[:n, col:col + 1], in0=ss[:n, col:col + 1],
                                             scalar1=1.0 / D, scalar2=EPS, op0=ALU.mult, op1=ALU.add),
         r=[("ss", col)], w=[("rs", col)])
    p.op("scalar", lambda e: e.sqrt(out=rs[:n, col:col + 1], in_=rs[:n, col:col + 1]),
         r=[("rs", col)], w=[("rs", col)])
    p.op("vector", lambda e: e.reciprocal(out=rs[:n, col:col + 1], in_=rs[:n, col:col + 1]),
         r=[("rs", col)], w=[("rs", col)])
    p.op("vector", lambda e: e.scalar_tensor_tensor(out=xn_out[:n, :], in0=xt[:n, :], scalar=rs[:n, col:col + 1],
                                                    in1=g[:n, :], op0=ALU.mult, op1=ALU.mult),
         r=[xk, ("rs", col), gkey], w=[("xn", tag)])


def transpose_to_T(k, xn, n, tag, dstT, t0, pbase):
    p = k.p
    for half in range(2):
        bank = pbase + half
        pv = k.ps[bank][:].bitcast(BF16)
        for j in range(8):
            c = half * 8 + j
            p.op("tensor", lambda e, c=c, j=j, pv=pv: e.transpose(
                out=pv[:, j * 128:j * 128 + n], in_=xn[:n, c * 128:(c + 1) * 128], identity=k.ident_b[:n, :n]),
                r=[("xn", tag)], w=[("ps", bank)])
        dst = dstT[:, half * 8:half * 8 + 8, t0:t0 + n]
        srcv = pv.rearrange("p (c t) -> p c t", c=8)[:, :, :n]
        if half == 0:
            p.op("scalar", lambda e, dst=dst, srcv=srcv: e.copy(out=dst, in_=srcv),
                 r=[("ps", bank)], w=[("T", id(dstT), t0, half)])
        else:
            p.op("vector", lambda e, dst=dst, srcv=srcv: e.tensor_copy(out=dst, in_=srcv),
                 r=[("ps", bank)], w=[("T", id(dstT), t0, half)])


def Tkeys(dstT, t0s):
    return [("T", id(dstT), t0, h) for t0 in t0s for h in range(2)]


def norm_stats(k):
    k.ss = k.sb("ss", [128, 64], F32)
    k.rs = k.sb("rs", [128, 64], F32)
    k.junk = k.sb("junk", [128, D], BF16)


def linear_fm(k, xT, xkeys, wname, chunks, evac, tag):
    p = k.p
    stage = [k.sb("st_%s%d" % (tag, i), [128, 16, 128], F32) for i in range(2)]
    wb = [k.sb("wb_%s%d" % (tag, i), [128, 16, 128], BF16) for i in range(2)]
    W = k.I[wname]
    for ci, pieces in enumerate(chunks):
        b = ci % 2
        r0 = 0
        for (c0, ncl) in pieces:
            src = W[:, c0:c0 + ncl].rearrange("(c p) n -> p c n", p=128)
            p.dma("sync", stage[b][:, :, r0:r0 + ncl], src, w=[("st", tag, b)])
            r0 += ncl
        rows = r0
        if ci % 2 == 0:
            p.op("scalar", lambda e, b=b, rows=rows: e.copy(out=wb[b][:, :, :rows], in_=stage[b][:, :, :rows]),
                 r=[("st", tag, b)], w=[("wb", tag, b)])
        else:
            p.op("gpsimd", lambda e, b=b, rows=rows: e.tensor_copy(out=wb[b][:, :, :rows], in_=stage[b][:, :, :rows]),
                 r=[("st", tag, b)], w=[("wb", tag, b)])
        for si in range(5):
            t0 = si * 512
            tn = min(512, T - t0)
            bank = (ci * 5 + si) % 4
            for c in range(16):
                p.op("tensor", lambda e, b=b, c=c, rows=rows, t0=t0, tn=tn, bank=bank: e.matmul(
                    out=k.ps[bank][:rows, :tn], lhsT=wb[b][:, c, :rows], rhs=xT[:, c, t0:t0 + tn],
                    start=(c == 0), stop=(c == 15)), r=[("wb", tag, b)] + xkeys, w=[("ps", bank)])
            evac(ci, si, k.ps[bank], bank, rows, t0, tn)


def phase_proj(k):
    p, nc = k.p, k.nc
    norm_stats(k)
    xnT = k.sb("xnT", [128, 16, T], BF16)
    with k.sub():
        g = load_bcast(k, "g_bc", k.I["g_mix"], D)
        xt = [k.sb("xt%d" % i, [128, D], F32) for i in range(2)]
        xn = [k.sb("xnb%d" % i, [128, D], BF16) for i in range(2)]
        for tt in range(NT):
            b = tt % 2
            n = trows(tt)
            src = k.I["xp"][tt * 128:(tt + 1) * 128, :] if tt < 16 else k.I["xs"][:, :]
            p.dma("sync", xt[b][:n, :], src, w=[("xt", b)])
            rms_tile(k, xt[b], n, g, "g_bc", xn[b], b, tt)
            transpose_to_T(k, xn[b], n, b, xnT, tt * 128, b * 2)
    allx = []
    with k.sub():
        phase_proj_tok(k, xnT)
    phase_proj_fm(k, xnT, allx)


def phase_proj_tok(k, xnT):
    p, nc = k.p, k.nc

    stage = k.sb("kv_stage", [128, 16, 592], F32)
    wbk = k.sb("kv_wb", [128, 16, 592], BF16)
    p.dma("sync", stage[:, :, 0:512], k.I["w_in"][:, O_K:O_K + 512].rearrange("(c p) n -> p c n", p=128),
          w=["kvst0"])
    p.dma("sync", stage[:, :, 512:592], k.I["w_in"][:, O_IK:O_IK + 80].rearrange("(c p) n -> p c n", p=128),
          w=["kvst1"])
    p.op("scalar", lambda e: e.copy(out=wbk[:, :, 0:512], in_=stage[:, :, 0:512]), r=["kvst0"], w=["kvwb0"])
    p.op("vector", lambda e: e.tensor_copy(out=wbk[:, :, 512:592], in_=stage[:, :, 512:592]), r=["kvst1"], w=["kvwb1"])
    vtok = k.scratch("v_tok", [T, 256], BF16)
    iwtok = k.scratch("iw_tok", [T, 16], F32)
    ob = [k.sb("kv_ob%d" % i, [128, 592], F32) for i in range(2)]
    vb = [k.sb("kv_vb%d" % i, [128, 256], BF16) for i in range(2)]
    for tt in range(NT):
        n = trows(tt)
        b = tt % 2
        t0 = tt * 128
        xk = Tkeys(xnT, [t0])
        ba, bb = 4 + b * 2, 5 + b * 2
        for c in range(16):
            p.op("tensor", lambda e, c=c, n=n, t0=t0, ba=ba: e.matmul(
                out=k.ps[ba][:n, :512], lhsT=xnT[:, c, t0:t0 + n], rhs=wbk[:, c, 0:512],
                start=(c == 0), stop=(c == 15)), r=["kvwb0"], w=[("ps", ba)])
        for c in range(16):
            p.op("tensor", lambda e, c=c, n=n, t0=t0, bb=bb: e.matmul(
                out=k.ps[bb][:n, :80], lhsT=xnT[:, c, t0:t0 + n], rhs=wbk[:, c, 512:592],
                start=(c == 0), stop=(c == 15)), r=["kvwb1"], w=[("ps", bb)])
        p.op("scalar", lambda e, n=n, b=b, ba=ba: e.copy(out=ob[b][:n, 0:512], in_=k.ps[ba][:n, :512]),
             r=[("ps", ba)], w=[("ob", b, 0)])
        p.op("vector", lambda e, n=n, b=b, bb=bb: e.tensor_copy(out=ob[b][:n, 512:592], in_=k.ps[bb][:n, :80]),
             r=[("ps", bb)], w=[("ob", b, 1)])
        p.op("vector", lambda e, n=n, b=b: e.tensor_copy(out=vb[b][:n, :], in_=ob[b][:n, 256:512]),
             r=[("ob", b, 0)], w=[("vb", b)])
        p.dma("sync", vtok[t0:t0 + n, :], vb[b][:n, :], r=[("vb", b)])
        p.dma("sync", iwtok[t0:t0 + n, :], ob[b][:n, 576:592], r=[("ob", b, 1)])
        if tt < 16:
            rows = slice(t0, t0 + 128)
            p.dma("sync", k.O["k_p"][rows, :], ob[b][:, 0:256], r=[("ob", b, 0)])
            p.dma("sync", k.O["v_p"][rows, :], ob[b][:, 256:512], r=[("ob", b, 0)])
            p.dma("sync", k.O["ik_p"][rows, :], ob[b][:, 512:576], r=[("ob", b, 1)])
        else:
            p.dma("sync", k.O["k_s"][:, :], ob[b][:64, 0:256], r=[("ob", b, 0)])
            p.dma("sync", k.O["v_s"][:, :], ob[b][:64, 256:512], r=[("ob", b, 0)])
            p.dma("sync", k.O["ik_s"][:, :], ob[b][:64, 512:576], r=[("ob", b, 1)])
    for half in range(2):
        c0 = O_XB + half * 512
        p.dma("sync", stage[:, :, 0:512], k.I["w_in"][:, c0:c0 + 512].rearrange("(c p) n -> p c n", p=128),
              w=["kvst0"])
        p.op("scalar", lambda e: e.copy(out=wbk[:, :, 0:512], in_=stage[:, :, 0:512]), r=["kvst0"], w=["kvwb0"])
        for tt in (15, 16):
            n = trows(tt)
            b = tt % 2
            t0 = tt * 128
            ba = 4 + b * 2
            for c in range(16):
                p.op("tensor", lambda e, c=c, n=n, t0=t0, ba=ba: e.matmul(
                    out=k.ps[ba][:n, :512], lhsT=xnT[:, c, t0:t0 + n], rhs=wbk[:, c, 0:512],
                    start=(c == 0), stop=(c == 15)), r=["kvwb0"], w=[("ps", ba)])
            p.op("scalar", lambda e, n=n, b=b, ba=ba: e.copy(out=ob[b][:n, 0:512], in_=k.ps[ba][:n, :512]),
                 r=[("ps", ba)], w=[("ob", b, 0)])
            cs = slice(half * 512, half * 512 + 512)
            if tt == 15:
                p.dma("sync", k.O["conv_p"][:, cs], ob[b][125:128, 0:512], r=[("ob", b, 0)])
            else:
                srcv = ob[b][:64, 0:512]
                xbs = k.scratch("xb_s", [TS, 1024], F32)
                p.dma("sync", xbs[:, cs], srcv, r=[("ob", b, 0)], w=[("xbs", half)])
                p.dma("sync", k.O["conv_s"].rearrange("(b j) n -> b j n", j=3)[:, :, cs],
                      xbs.rearrange("(b t) n -> b t n", t=4)[:, 1:4, cs], r=[("xbs", half)])


def phase_proj_fm(k, xnT, allx):
    p, nc = k.p, k.nc
    projT = k.scratch("projT", [43, 128, T], BF16)
    chunks = []
    for c0 in list(range(O_Q, O_Q + 1024, 128)) + list(range(O_K, O_K + 256, 128)) + list(range(O_IQ, O_IQ + 1024, 128)):
        chunks.append([(c0, 128)])
    chunks.append([(O_IK, 64), (O_IK, 64)])
    for base in (O_XB, O_YB, O_QM):
        for c0 in range(base, base + 1024, 128):
            chunks.append([(c0, 128)])
    obf = [k.sb("pj_ob%d" % i, [128, T], BF16) for i in range(2)]

    def evac_proj(dst):
        def evac(ci, si, ps, bank, rows, t0, tn):
            b = ci % 2
            if si % 2 == 0:
                p.op("scalar", lambda e: e.copy(out=obf[b][:rows, t0:t0 + tn], in_=ps[:rows, :tn]),
                     r=[("ps", bank)], w=[("obf", b, si)])
            else:
                p.op("vector", lambda e: e.tensor_copy(out=obf[b][:rows, t0:t0 + tn], in_=ps[:rows, :tn]),
                     r=[("ps", bank)], w=[("obf", b, si)])
            if si == 4:
                p.dma("sync", dst[ci, :rows, :], obf[b][:rows, :], r=[("obf", b, s_) for s_ in range(5)])
        return evac
    linear_fm(k, xnT, allx, "w_in", chunks, evac_proj(projT), "pj")

    gT = k.scratch("gT", [48, 128, T], BF16)

    def evac_gate(ci, si, ps, bank, rows, t0, tn):
        b = ci % 2
        p.op("scalar", lambda e: e.activation(out=obf[b][:rows, t0:t0 + tn], in_=ps[:rows, :tn], func=AF.Sigmoid),
             r=[("ps", bank)], w=[("obf", b, si)])
        if si == 4:
            p.dma("sync", gT[ci, :rows, :], obf[b][:rows, :], r=[("obf", b, s_) for s_ in range(5)])
    linear_fm(k, xnT, allx, "w_gate", [[(c0, 128)] for c0 in range(0, 3 * D, 128)], evac_gate, "gt")


PHASES = []


_NC_CACHE = {}


def make_in_maps(inp):
    f = lambda a: np.ascontiguousarray(a, dtype=np.float32)
    shared = {
        "cache_k": f(inp["cache_k"]).reshape(2560 * 128, 256), "cache_v": f(inp["cache_v"]).reshape(2560 * 128, 256),
        "cache_ik": f(inp["cache_idx_k"]).reshape(2560 * 128, 64),
        "g_mix": f(inp["g_mix"]).reshape(1, D), "w_in": f(inp["w_in"]).reshape(D, N_IN),
        "conv_w": f(inp["conv_w"]).reshape(4, 1024), "conv_b": f(inp["conv_b"]).reshape(1, 1024),
        "w_rg": f(inp["w_rg"]).reshape(1024, 128), "b_rg": f(inp["b_rg"]).reshape(1, 1024),
        "w_ig": f(inp["w_ig"]).reshape(1024, 128), "b_ig": f(inp["b_ig"]).reshape(1, 1024),
        "lam": f(inp["lru_lambda"]).reshape(1, 1024), "g_mem": f(inp["g_mem"]).reshape(1, D),
        "w_mem_kv": f(inp["w_mem_kv"]).reshape(D, 2048), "w_gate": f(inp["w_gate"]).reshape(D, 3 * D),
        "w_br": f(inp["w_br"]).reshape(3 * 1024, D), "w_o": f(inp["w_o"]).reshape(D, D),
        "g_ffn": f(inp["g_ffn"]).reshape(1, D), "w_pq": f(inp["w_peer_q"]).reshape(D, 2048),
        "sub_keys": f(inp["peer_sub_keys"]).reshape(2 * 8 * 128, 128), "peer_u": f(inp["peer_u"]).reshape(16384, D),
        "peer_v": f(inp["peer_v"]).reshape(16384, D), "g_final": f(inp["g_final"]).reshape(1, D),
    }
    maps = []
    for c in range(NCORES):
        sl = slice(c * NS_SEQ, (c + 1) * NS_SEQ)
        m = dict(shared)
        m["xp"] = f(inp["x_prompt"][c])
        m["xs"] = f(inp["x_sample"][sl]).reshape(TS, D)
        m["ptab"] = np.ascontiguousarray(inp["page_table"][sl], dtype=np.int32)
        m["st_conv"] = f(inp["state_conv"][0, sl]).reshape(NS_SEQ * 3, 1024)
        m["st_lru"] = f(inp["state_lru"][0, sl]).reshape(NS_SEQ, 1024)
        m["cmk"] = f(inp["cache_mem_k"][0, sl]).reshape(NS_SEQ * 256, 1024)
        m["cmv"] = f(inp["cache_mem_v"][0, sl]).reshape(NS_SEQ * 256, 1024)
        m["mem"] = f(inp["mem_prompt"][c])
        maps.append(m)
    return maps


def assemble(res):
    g = lambda n: [np.asarray(r[n], dtype=np.float32) for r in res]
    y_p = np.stack(g("y_p"))
    y_s = np.concatenate(g("y_s")).reshape(128, DEC, D)
    k_p = np.stack(g("k_p")).reshape(1, 8, SEQ, 2, 128)
    v_p = np.stack(g("v_p")).reshape(1, 8, SEQ, 2, 128)
    ik_p = np.stack(g("ik_p")).reshape(1, 8, SEQ, 64)
    conv_p = np.stack(g("conv_p")).reshape(1, 8, 3, 1024)
    h_p = np.stack(g("h_p")).reshape(1, 8, 1024)
    mk_p = np.stack(g("mk_p")).reshape(1, 8, 256, 4, 256)
    mv_p = np.stack(g("mv_p")).reshape(1, 8, 256, 4, 256)
    k_s = np.concatenate(g("k_s")).reshape(1, 128, DEC, 2, 128)
    v_s = np.concatenate(g("v_s")).reshape(1, 128, DEC, 2, 128)
    ik_s = np.concatenate(g("ik_s")).reshape(1, 128, DEC, 64)
    conv_s = np.concatenate(g("conv_s")).reshape(1, 128, 3, 1024)
    h_s = np.concatenate(g("h_s")).reshape(1, 128, 1024)
    return (y_p, y_s, k_p, v_p, ik_p, conv_p, h_p, mk_p, mv_p, k_s, v_s, ik_s, conv_s, h_s)


def kernel(**inputs):
    if "nc" not in _NC_CACHE:
        _NC_CACHE["nc"] = build()
    nc = _NC_CACHE["nc"]
    in_maps = [{n: m[n] for n in nc._used_in} for m in make_in_maps(inputs)]
    res = run_bass_kernel_spmd(nc, in_maps, core_ids=list(range(NCORES)))
    outs = []
    for r in res.results:
        d = dict(r)
        for n, s in OUT_SPECS:
            if n not in d:
                d[n] = np.zeros(s, np.float32)
        outs.append(d)
    return assemble(outs)


def phase_mem(k):
    p, nc = k.p, k.nc
    projT = k.scratch("projT", [43, 128, T], BF16)
    oT = k.scratch("oT", [3, 8, 128, T], BF16)
    MS = 256 ** -0.5
    norm_stats(k)
    memT = k.sb("memT", [128, 16, 256], BF16)
    mkT = k.sb("mkT", [128, 8, 256], BF16)
    mvb = k.sb("mvb", [128, 2, 1024], BF16)
    qmT = k.sb("qmT", [128, 8, T], BF16)
    p.dma("sync", qmT[:], projT[35:43].rearrange("c p t -> p c t"), w=["qmT"])
    with k.sub():
        g = load_bcast(k, "gm_bc", k.I["g_mem"], D)
        xt = [k.sb("mxt%d" % i, [128, D], F32) for i in range(2)]
        xn = [k.sb("mxn%d" % i, [128, D], BF16) for i in range(2)]
        for mt in range(2):
            p.dma("sync", xt[mt][:, :], k.I["mem"][mt * 128:(mt + 1) * 128, :], w=[("xt", mt)])
            rms_tile(k, xt[mt], 128, g, "gm_bc", xn[mt], mt, mt)
            transpose_to_T(k, xn[mt], 128, mt, memT, mt * 128, mt * 2)
    with k.sub():
        stage = k.sb("mst", [128, 16, 512], F32)
        wb = k.sb("mwb", [128, 16, 512], BF16)
        ob = [k.sb("mob%d" % i, [128, 512], F32) for i in range(2)]
        for cb in range(4):
            p.dma("sync", stage[:], k.I["w_mem_kv"][:, cb * 512:(cb + 1) * 512].rearrange("(c p) n -> p c n", p=128),
                  w=["mst"])
            p.op("scalar", lambda e: e.copy(out=wb[:], in_=stage[:]), r=["mst"], w=["mwb"])
            for mt in range(2):
                bank = mt
                for c in range(16):
                    p.op("tensor", lambda e, c=c, mt=mt, bank=bank: e.matmul(
                        out=k.ps[bank][:, :], lhsT=memT[:, c, mt * 128:(mt + 1) * 128], rhs=wb[:, c, :],
                        start=(c == 0), stop=(c == 15)), r=["mwb"], w=[("ps", bank)])
                p.op("scalar", lambda e, mt=mt, bank=bank: e.copy(out=ob[mt][:, :], in_=k.ps[bank][:, :]),
                     r=[("ps", bank)], w=[("mob", mt)])
                dst = k.O["mk_p"] if cb < 2 else k.O["mv_p"]
                cs = slice((cb % 2) * 512, (cb % 2) * 512 + 512)
                p.dma("sync", dst[mt * 128:(mt + 1) * 128, cs], ob[mt][:, :], r=[("mob", mt)])
                if cb >= 2:
                    p.op("vector", lambda e, mt=mt, cs=cs: e.tensor_copy(out=mvb[:, mt, cs], in_=ob[mt][:, :]),
                         r=[("mob", mt)], w=["mvb"])
            if cb < 2:
                for j in range(4):
                    bank = 2 + j % 2
                    for c in range(16):
                        p.op("tensor", lambda e, c=c, j=j, bank=bank: e.matmul(
                            out=k.ps[bank][:, :256], lhsT=wb[:, c, j * 128:(j + 1) * 128], rhs=memT[:, c, :],
                            start=(c == 0), stop=(c == 15)), r=["mwb"], w=[("ps", bank)])
                    p.op("vector", lambda e, j=j, bank=bank, cb=cb: e.tensor_copy(
                        out=mkT[:, cb * 4 + j, :], in_=k.ps[bank][:, :256]), r=[("ps", bank)], w=["mkT"])
    with k.sub():
        mem_attend_loop(k, qmT, mkT, mvb, oT, MS, [(tt * 128, 128) for tt in range(16)], "p")
    with k.sub():
        cm = [k.sb("cmk%d" % i, [128, 2, 1024], F32) for i in range(2)]
        cv = [k.sb("cmv%d" % i, [128, 2, 1024], F32) for i in range(2)]
        mkTs = [k.sb("mkTs%d" % i, [128, 8, 256], BF16) for i in range(2)]
        mvbs = [k.sb("mvbs%d" % i, [128, 2, 1024], BF16) for i in range(2)]
        st = mem_attend_state(k, "s")
        for b in range(NS_SEQ):
            i = b % 2
            p.dma("sync", cm[i][:], k.I["cmk"][b * 256:(b + 1) * 256, :].rearrange("(m p) n -> p m n", p=128),
                  w=[("cm", i)])
            p.dma("sync", cv[i][:], k.I["cmv"][b * 256:(b + 1) * 256, :].rearrange("(m p) n -> p m n", p=128),
                  w=[("cv", i)])
            p.op("gpsimd", lambda e, i=i: e.tensor_copy(out=mvbs[i][:], in_=cv[i][:]), r=[("cv", i)], w=[("mvbs", i)])
            for c in range(8):
                bank = 6 + c % 2
                for mt in range(2):
                    p.op("tensor", lambda e, c=c, mt=mt, bank=bank, i=i: e.transpose(
                        out=k.ps[bank][:, mt * 128:(mt + 1) * 128], in_=cm[i][:, mt, c * 128:(c + 1) * 128],
                        identity=k.ident_f[:]), r=[("cm", i)], w=[("ps", bank)])
                p.op("scalar", lambda e, c=c, bank=bank, i=i: e.copy(out=mkTs[i][:, c, :], in_=k.ps[bank][:, :256]),
                     r=[("ps", bank)], w=[("mkTs", i)])
            mem_attend_tile(k, st, qmT, mkTs[i], mvbs[i], oT, MS, SEQ + 4 * b, 4, [("mkTs", i), ("mvbs", i)])


def mem_attend_state(k, tag):
    st = K()
    st.mx = k.sb("ma_mx" + tag, [128, 4], F32)
    st.rsum = k.sb("ma_rs" + tag, [128, 4], F32)
    st.P = k.sb("ma_P" + tag, [128, 4, 256], BF16)
    st.PT = k.sb("ma_PT" + tag, [128, 8, 128], BF16)
    st.oc = k.sb("ma_oc" + tag, [128, 8, 128], BF16)
    return st


def mem_attend_loop(k, qmT, mkT, mvb, oT, MS, tiles, tag):
    st = mem_attend_state(k, tag)
    for (t0, n) in tiles:
        mem_attend_tile(k, st, qmT, mkT, mvb, oT, MS, t0, n, ["mkT", "mvb"])


def mem_attend_tile(k, st, qmT, mkT, mvb, oT, MS, t0, n, kvkeys):
    p = k.p
    for h in range(4):
        bank = h // 2
        for kc in range(2):
            p.op("tensor", lambda e, h=h, kc=kc, bank=bank: e.matmul(
                out=k.ps[bank][:n, (h % 2) * 256:(h % 2) * 256 + 256], lhsT=qmT[:, 2 * h + kc, t0:t0 + n],
                rhs=mkT[:, 2 * h + kc, :], start=(kc == 0), stop=(kc == 1)),
                r=["qmT"] + kvkeys, w=[("ps", bank)])
    for bank in range(2):
        p.op("vector", lambda e, bank=bank: e.tensor_reduce(
            out=st.mx[:n, bank * 2:bank * 2 + 2], in_=k.ps[bank][:n, :].rearrange("p (h m) -> p h m", h=2),
            axis=AX.X, op=ALU.max), r=[("ps", bank)], w=[("mx", bank)])
        p.op("vector", lambda e, bank=bank: e.tensor_scalar(
            out=st.mx[:n, bank * 2:bank * 2 + 2], in0=st.mx[:n, bank * 2:bank * 2 + 2], scalar1=-MS, scalar2=None,
            op0=ALU.mult), r=[("mx", bank)], w=[("mx", bank)])
    for h in range(4):
        bank = h // 2
        p.op("scalar", lambda e, h=h, bank=bank: e.activation(
            out=st.P[:n, h, :], in_=k.ps[bank][:n, (h % 2) * 256:(h % 2) * 256 + 256], func=AF.Exp,
            bias=st.mx[:n, h:h + 1], scale=MS, accum_out=st.rsum[:n, h:h + 1]),
            r=[("ps", bank), ("mx", bank)], w=[("P", h), ("rsum", h)])
    p.op("vector", lambda e: e.reciprocal(out=st.rsum[:n, :], in_=st.rsum[:n, :]),
         r=[("rsum", h) for h in range(4)], w=["rinv"])
    for h in range(4):
        p.op("vector", lambda e, h=h: e.tensor_scalar(out=st.P[:n, h, :], in0=st.P[:n, h, :],
                                                      scalar1=st.rsum[:n, h:h + 1], scalar2=None, op0=ALU.mult),
             r=["rinv", ("P", h)], w=[("P", h)])
    pv = k.ps[2][:].bitcast(BF16)
    for h in range(4):
        for mt in range(2):
            j = h * 2 + mt
            p.op("tensor", lambda e, h=h, mt=mt, j=j: e.transpose(
                out=pv[:, j * 128:j * 128 + n], in_=st.P[:n, h, mt * 128:(mt + 1) * 128],
                identity=k.ident_b[:n, :n]), r=[("P", h)], w=[("ps", 2)])
    p.op("scalar", lambda e: e.copy(out=st.PT[:, :, :n], in_=pv.rearrange("p (j t) -> p j t", j=8)[:, :, :n]),
         r=[("ps", 2)], w=["PT"])
    for h in range(4):
        for c2 in range(2):
            j = h * 2 + c2
            bank = 3 + j // 4
            for mt in range(2):
                p.op("tensor", lambda e, h=h, c2=c2, mt=mt, j=j, bank=bank: e.matmul(
                    out=k.ps[bank][:, (j % 4) * 128:(j % 4) * 128 + n],
                    lhsT=mvb[:, mt, h * 256 + c2 * 128:h * 256 + c2 * 128 + 128], rhs=st.PT[:, h * 2 + mt, :n],
                    start=(mt == 0), stop=(mt == 1)), r=["PT"] + kvkeys, w=[("ps", bank)])
    for half in range(2):
        bank = 3 + half
        eng = "scalar" if half == 0 else "vector"
        srcv = k.ps[bank][:].rearrange("p (j t) -> p j t", j=4)[:, :, :n]
        dst = st.oc[:, half * 4:half * 4 + 4, :n]
        if half == 0:
            p.op("scalar", lambda e, dst=dst, srcv=srcv: e.copy(out=dst, in_=srcv), r=[("ps", bank)], w=[("oc", half)])
        else:
            p.op("vector", lambda e, dst=dst, srcv=srcv: e.tensor_copy(out=dst, in_=srcv), r=[("ps", bank)],
                 w=[("oc", half)])
    p.dma("sync", oT[2, :, :, t0:t0 + n].rearrange("c p t -> p c t"), st.oc[:, :, :n], r=[("oc", 0), ("oc", 1)])


PHASES.append(("mem", phase_mem))


def gelu_mul(k, y, h, out, n, tmp1, tmp2, keys_r, key_w):
    p = k.p
    p.op("vector", lambda e: e.tensor_tensor(out=tmp1, in0=y, in1=y, op=ALU.mult), r=keys_r, w=[("g1", key_w)])
    p.op("vector", lambda e: e.tensor_scalar(out=tmp1, in0=tmp1, scalar1=0.044715, scalar2=1.0, op0=ALU.mult,
                                             op1=ALU.add), r=[("g1", key_w)], w=[("g1", key_w)])
    p.op("vector", lambda e: e.tensor_tensor(out=tmp1, in0=tmp1, in1=y, op=ALU.mult), r=[("g1", key_w)] + keys_r,
         w=[("g1", key_w)])
    p.op("scalar", lambda e: e.activation(out=tmp2, in_=tmp1, func=AF.Sigmoid, scale=1.5957691216057308),
         r=[("g1", key_w)], w=[("g2", key_w)])
    p.op("vector", lambda e: e.tensor_tensor(out=tmp2, in0=tmp2, in1=y, op=ALU.mult), r=[("g2", key_w)] + keys_r,
         w=[("g2", key_w)])
    p.op("vector", lambda e: e.tensor_tensor(out=out, in0=tmp2, in1=h, op=ALU.mult), r=[("g2", key_w)] + keys_r,
         w=[key_w])


def phase_lru(k):
    p, nc = k.p, k.nc
    projT = k.scratch("projT", [43, 128, T], BF16)
    oT = k.scratch("oT", [3, 8, 128, T], BF16)
    cw = k.sb("l_cw", [128, 8, 4], F32)
    pv = k.sb("l_pv", [128, 5, 8], F32)
    sc = k.sb("l_sc", [128, 2, 8], F32)
    for j in range(4):
        p.dma("sync", cw[:, :, j], k.I["conv_w"][j:j + 1, :].rearrange("o (n q) -> q (o n)", q=128), w=["cw"],
              allow_slow_non_contiguous=True)
    for i, nm in enumerate(("conv_b", "b_rg", "b_ig", "lam")):
        p.dma("sync", pv[:, i, :], k.I[nm].rearrange("o (n q) -> q (o n)", q=128), w=[("pv", i)],
              allow_slow_non_contiguous=True)
    p.op("scalar", lambda e: e.activation(out=pv[:, 4, :], in_=pv[:, 3, :], func=AF.Exp, scale=-1.0),
         r=[("pv", 3)], w=[("pv", 4)])
    p.op("scalar", lambda e: e.activation(out=pv[:, 4, :], in_=pv[:, 4, :], func=AF.Ln, bias=1.0),
         r=[("pv", 4)], w=[("pv", 4)])
    p.op("vector", lambda e: e.tensor_scalar(out=sc[:, 0, :], in0=pv[:, 4, :], scalar1=-8.0, scalar2=None,
                                             op0=ALU.mult), r=[("pv", 4)], w=["sc0"])
    p.op("vector", lambda e: e.tensor_scalar(out=sc[:, 1, :], in0=pv[:, 4, :], scalar1=-16.0, scalar2=None,
                                             op0=ALU.mult), r=[("pv", 4)], w=["sc1"])
    wst = k.sb("l_wst", [128, 2, 8, 128], F32)
    wg = k.sb("l_wg", [128, 2, 8, 128], BF16)
    p.dma("sync", wst[:, 0], k.I["w_rg"].rearrange("(n i) j -> i n j", i=128), w=["wst0"])
    p.dma("sync", wst[:, 1], k.I["w_ig"].rearrange("(n i) j -> i n j", i=128), w=["wst1"])
    p.op("vector", lambda e: e.tensor_copy(out=wg[:], in_=wst[:]), r=["wst0", "wst1"], w=["wg"])
    scv = k.sb("l_scv", [48, 1024], F32)
    slr = k.sb("l_slr", [16, 1024], F32)
    p.dma("sync", scv[:], k.I["st_conv"][:, :], w=["scv"])
    p.dma("sync", slr[:], k.I["st_lru"][:, :], w=["slr"])
    hl_p = k.sb("l_hlp", [128, 8], F32)
    hl_s = k.sb("l_hls", [128, 8, 16], F32)
    NP = SEQ
    xbb = [k.sb("l_xbb%d" % i, [128, T], BF16) for i in range(2)]
    ybb = [k.sb("l_ybb%d" % i, [128, T], BF16) for i in range(2)]
    xpad = k.sb("l_xpad", [128, 3 + NP], F32)
    xps = k.sb("l_xps", [128, 16, 7], F32)
    xc = k.sb("l_xc", [128, T], F32)
    xcb = k.sb("l_xcb", [128, T], BF16)
    rr = k.sb("l_r", [128, T], F32)
    ii = k.sb("l_i", [128, T], F32)
    aa = k.sb("l_a", [128, T], F32)
    uu = k.sb("l_u", [128, T], F32)
    hh = k.sb("l_h", [128, T], F32)
    yf = k.sb("l_yf", [128, T], F32)
    ob = [k.sb("l_ob%d" % i, [128, T], BF16) for i in range(2)]
    h0 = k.sb("l_h0", [128, 16], F32)
    tmpa = k.sb("l_tmpa", [128, 16], F32)
    p.op("vector", lambda e: e.memset(xpad[:, 0:3], 0.0), w=["xpad0"])
    for n in range(8):
        b = n % 2
        p.dma("sync", xbb[b][:], projT[19 + n], w=[("xbb", b)])
        p.dma("sync", ybb[b][:], projT[27 + n], w=[("ybb", b)])
        p.op("vector", lambda e, b=b: e.tensor_copy(out=xpad[:, 3:3 + NP], in_=xbb[b][:, 0:NP]),
             r=[("xbb", b), "xpad0"], w=["xpad"])
        p.op("tensor", lambda e, n=n: e.transpose(out=k.ps[4][:, 0:48], in_=scv[:, n * 128:(n + 1) * 128],
                                                  identity=k.ident_f[:48, :48]), r=["scv"], w=[("ps", 4)])
        p.op("tensor", lambda e, n=n: e.transpose(out=k.ps[5][:, 0:16], in_=slr[:, n * 128:(n + 1) * 128],
                                                  identity=k.ident_f[:16, :16]), r=["slr"], w=[("ps", 5)])
        p.op("vector", lambda e: e.tensor_copy(out=xps[:, :, 0:3], in_=k.ps[4][:, 0:48].rearrange("p (b j) -> p b j", j=3)),
             r=[("ps", 4)], w=["xps0"])
        p.op("vector", lambda e, b=b: e.tensor_copy(out=xps[:, :, 3:7],
                                                    in_=xbb[b][:, NP:T].rearrange("p (b t) -> p b t", t=4)),
             r=[("xbb", b)], w=["xps1"])
        p.op("vector", lambda e: e.tensor_copy(out=h0[:], in_=k.ps[5][:, 0:16]), r=[("ps", 5)], w=["h0"])
        p.op("scalar", lambda e, n=n: e.activation(out=xc[:, 0:NP], in_=xpad[:, 3:3 + NP], func=AF.Identity,
                                                   bias=pv[:, 0, n:n + 1], scale=cw[:, n, 3:4]),
             r=["xpad", "cw", ("pv", 0)], w=["xc_p"])
        p.op("scalar", lambda e, n=n: e.activation(out=xc[:, NP:T].rearrange("p (b t) -> p b t", t=4),
                                                   in_=xps[:, :, 3:7], func=AF.Identity,
                                                   bias=pv[:, 0, n:n + 1], scale=cw[:, n, 3:4]),
             r=["xps0", "xps1", "cw", ("pv", 0)], w=["xc_s"])
        for j in range(3):
            p.op("vector", lambda e, n=n, j=j: e.scalar_tensor_tensor(
                out=xc[:, 0:NP], in0=xpad[:, j:j + NP], scalar=cw[:, n, j:j + 1], in1=xc[:, 0:NP],
                op0=ALU.mult, op1=ALU.add), r=["xpad", "xc_p"], w=["xc_p"])
            p.op("vector", lambda e, n=n, j=j: e.scalar_tensor_tensor(
                out=xc[:, NP:T].rearrange("p (b t) -> p b t", t=4), in0=xps[:, :, j:j + 4], scalar=cw[:, n, j:j + 1],
                in1=xc[:, NP:T].rearrange("p (b t) -> p b t", t=4), op0=ALU.mult, op1=ALU.add),
                r=["xps0", "xps1", "xc_s"], w=["xc_s"])
        p.op("gpsimd", lambda e: e.tensor_copy(out=xcb[:], in_=xc[:]), r=["xc_p", "xc_s"], w=["xcb"])
        for gi, dst in ((0, rr), (1, ii)):
            for si in range(5):
                t0 = si * 512
                tn = min(512, T - t0)
                bank = (gi * 5 + si) % 4
                p.op("tensor", lambda e, gi=gi, n=n, t0=t0, tn=tn, bank=bank: e.matmul(
                    out=k.ps[bank][:, :tn], lhsT=wg[:, gi, n, :], rhs=xcb[:, t0:t0 + tn], start=True, stop=True),
                    r=["wg", "xcb"], w=[("ps", bank)])
                p.op("scalar", lambda e, gi=gi, n=n, t0=t0, tn=tn, bank=bank, dst=dst: e.activation(
                    out=dst[:, t0:t0 + tn], in_=k.ps[bank][:, :tn], func=AF.Sigmoid, bias=pv[:, 1 + gi, n:n + 1]),
                    r=[("ps", bank), ("pv", 1 + gi)], w=[("gate", gi)])
        p.op("scalar", lambda e, n=n: e.activation(out=aa[:], in_=rr[:], func=AF.Exp, scale=sc[:, 0, n:n + 1]),
             r=[("gate", 0), "sc0"], w=["aa"])
        p.op("scalar", lambda e, n=n: e.activation(out=uu[:], in_=rr[:], func=AF.Exp, scale=sc[:, 1, n:n + 1]),
             r=[("gate", 0), "sc1"], w=["uu"])
        p.op("scalar", lambda e: e.activation(out=uu[:], in_=uu[:], func=AF.Sqrt, bias=1.0, scale=-1.0),
             r=["uu"], w=["uu"])
        p.op("vector", lambda e: e.tensor_tensor(out=uu[:], in0=uu[:], in1=ii[:], op=ALU.mult),
             r=["uu", ("gate", 1)], w=["uu"])
        p.op("vector", lambda e: e.tensor_tensor(out=uu[:], in0=uu[:], in1=xc[:], op=ALU.mult),
             r=["uu", "xc_p", "xc_s"], w=["uu"])
        p.op("vector", lambda e: e.tensor_tensor_scan(out=hh[:, 0:NP], data0=aa[:, 0:NP], data1=uu[:, 0:NP],
                                                      initial=0.0, op0=ALU.mult, op1=ALU.add),
             r=["aa", "uu"], w=["hh_p"])
        hs = hh[:, NP:T].rearrange("p (b t) -> p b t", t=4)
        as_ = aa[:, NP:T].rearrange("p (b t) -> p b t", t=4)
        us = uu[:, NP:T].rearrange("p (b t) -> p b t", t=4)
        for t in range(4):
            prev = h0[:, :] if t == 0 else hs[:, :, t - 1]
            p.op("vector", lambda e, t=t, prev=prev: e.tensor_tensor(out=tmpa[:], in0=as_[:, :, t], in1=prev,
                                                                     op=ALU.mult),
                 r=["aa", "h0", "hh_s"], w=["tmpa"])
            p.op("vector", lambda e, t=t: e.tensor_tensor(out=hs[:, :, t], in0=tmpa[:], in1=us[:, :, t], op=ALU.add),
                 r=["tmpa", "uu"], w=["hh_s"])
        p.op("vector", lambda e, n=n: e.tensor_copy(out=hl_p[:, n:n + 1], in_=hh[:, NP - 1:NP]), r=["hh_p"], w=["hlp"])
        p.op("vector", lambda e, n=n: e.tensor_copy(out=hl_s[:, n, :], in_=hs[:, :, 3]), r=["hh_s"], w=["hls"])
        p.op("gpsimd", lambda e, b=b: e.tensor_copy(out=yf[:], in_=ybb[b][:]), r=[("ybb", b)], w=["yf"])
        gelu_mul(k, yf[:], hh[:], ob[b][:], T, rr[:], ii[:], ["yf", "hh_p", "hh_s", ("gate", 0), ("gate", 1), "uu"],
                 ("lob", b))
        p.dma("sync", oT[1, n], ob[b][:], r=[("lob", b)])
    hrow = k.sb("l_hrow", [16, 1024], F32)
    hrp = k.sb("l_hrp", [8, 128], F32)
    p.op("tensor", lambda e: e.transpose(out=k.ps[6][:8, 0:128], in_=hl_p[:, :], identity=k.ident_f[:, :]),
         r=["hlp"], w=[("ps", 6)])
    p.op("vector", lambda e: e.tensor_copy(out=hrp[:], in_=k.ps[6][:8, 0:128]), r=[("ps", 6)], w=["hrp"])
    p.dma("sync", k.O["h_p"].rearrange("o (n q) -> (o n) q", q=128), hrp[:], r=["hrp"])
    for n in range(8):
        bank = 6 + n % 2
        p.op("tensor", lambda e, n=n, bank=bank: e.transpose(out=k.ps[bank][:16, 0:128], in_=hl_s[:, n, :],
                                                             identity=k.ident_f[:, :]), r=["hls"], w=[("ps", bank)])
        p.op("vector", lambda e, n=n, bank=bank: e.tensor_copy(out=hrow[:, n * 128:(n + 1) * 128],
                                                               in_=k.ps[bank][:16, 0:128]),
             r=[("ps", bank)], w=["hrow"])
    p.dma("sync", k.O["h_s"][:, :], hrow[:], r=["hrow"])


PHASES.append(("lru", phase_lru))


def phase_merge(k):
    p, nc = k.p, k.nc
    oT = k.scratch("oT", [3, 8, 128, T], BF16)
    gT = k.scratch("gT", [48, 128, T], BF16)
    x1 = k.scratch("x1", [T, D], F32)
    mT = k.sb("mT", [128, 16, T], F32)
    with k.sub():
        on = k.sb("mg_on", [128, 8, T], BF16)
        wst = [k.sb("mg_wst%d" % i, [128, 8, 128], F32) for i in range(2)]
        wb = [k.sb("mg_wb%d" % i, [128, 8, 128], BF16) for i in range(2)]
        gt = [k.sb("mg_gt%d" % i, [128, T], BF16) for i in range(2)]
        tmp = [k.sb("mg_tmp%d" % i, [128, 512], F32) for i in range(2)]
        it = 0
        for n in range(3):
            p.dma("sync", on[:], oT[n].rearrange("c p t -> p c t"), w=["on"])
            for cc in range(16):
                b = it % 2
                it += 1
                p.dma("sync", wst[b][:], k.I["w_br"][n * 1024:(n + 1) * 1024, cc * 128:(cc + 1) * 128]
                      .rearrange("(c q) j -> q c j", q=128), w=[("wst", b)])
                p.dma("sync", gt[b][:], gT[n * 16 + cc], w=[("gt", b)])
                p.op("scalar", lambda e, b=b: e.copy(out=wb[b][:], in_=wst[b][:]), r=[("wst", b)], w=[("wb", b)])
                for si in range(5):
                    t0 = si * 512
                    tn = min(512, T - t0)
                    bank = si % 4
                    for c in range(8):
                        p.op("tensor", lambda e, b=b, c=c, t0=t0, tn=tn, bank=bank: e.matmul(
                            out=k.ps[bank][:, :tn], lhsT=wb[b][:, c, :], rhs=on[:, c, t0:t0 + tn],
                            start=(c == 0), stop=(c == 7)), r=[("wb", b), "on"], w=[("ps", bank)])
                    if n == 0:
                        p.op("vector", lambda e, b=b, cc=cc, t0=t0, tn=tn, bank=bank: e.tensor_tensor(
                            out=mT[:, cc, t0:t0 + tn], in0=k.ps[bank][:, :tn], in1=gt[b][:, t0:t0 + tn], op=ALU.mult),
                            r=[("ps", bank), ("gt", b)], w=[("mT", cc, si)])
                    else:
                        tb = si % 2
                        p.op("vector", lambda e, b=b, tb=tb, t0=t0, tn=tn, bank=bank: e.tensor_tensor(
                            out=tmp[tb][:, :tn], in0=k.ps[bank][:, :tn], in1=gt[b][:, t0:t0 + tn], op=ALU.mult),
                            r=[("ps", bank), ("gt", b)], w=[("tmp", tb)])
                        p.op("gpsimd", lambda e, cc=cc, tb=tb, t0=t0, tn=tn: e.tensor_tensor(
                            out=mT[:, cc, t0:t0 + tn], in0=mT[:, cc, t0:t0 + tn], in1=tmp[tb][:, :tn], op=ALU.add),
                            r=[("tmp", tb), ("mT", cc, si)], w=[("mT", cc, si)])
    with k.sub():
        stage = k.sb("wo_st", [128, 16, 512], F32)
        wo = k.sb("wo_wb", [128, 16, 512], BF16)
        mb = [k.sb("wo_mb%d" % i, [128, 16, 128], BF16) for i in range(2)]
        xr = [k.sb("wo_xr%d" % i, [128, 512], F32) for i in range(2)]
        xo = [k.sb("wo_xo%d" % i, [128, 512], F32) for i in range(2)]
        for cb in range(4):
            cs = slice(cb * 512, (cb + 1) * 512)
            p.dma("sync", stage[:], k.I["w_o"][:, cs].rearrange("(c q) n -> q c n", q=128), w=["wost"])
            p.op("scalar", lambda e: e.copy(out=wo[:], in_=stage[:]), r=["wost"], w=["wo"])
            for tt in range(NT):
                n = trows(tt)
                t0 = tt * 128
                b = tt % 2
                bank = tt % 4
                src = k.I["xp"][t0:t0 + 128, cs] if tt < 16 else k.I["xs"][:, cs]
                p.dma("sync", xr[b][:n, :], src, w=[("xr", b)])
                p.op("gpsimd", lambda e, b=b, t0=t0, n=n: e.tensor_copy(out=mb[b][:, :, :n], in_=mT[:, :, t0:t0 + n]),
                     w=[("mb", b)])
                for c in range(16):
                    p.op("tensor", lambda e, b=b, c=c, n=n, bank=bank: e.matmul(
                        out=k.ps[bank][:n, :], lhsT=mb[b][:, c, :n], rhs=wo[:, c, :], start=(c == 0), stop=(c == 15)),
                        r=[("mb", b), "wo"], w=[("ps", bank)])
                p.op("vector", lambda e, b=b, n=n, bank=bank: e.tensor_tensor(
                    out=xo[b][:n, :], in0=k.ps[bank][:n, :], in1=xr[b][:n, :], op=ALU.add),
                    r=[("ps", bank), ("xr", b)], w=[("xo", b)])
                p.dma("sync", x1[t0:t0 + n, cs], xo[b][:n, :], r=[("xo", b)])


PHASES.append(("merge", phase_merge))


ATT_SCALE = 128 ** -0.5


def slices(n_k):
    out = []
    s0 = 0
    while s0 < n_k:
        w = min(512, n_k - s0)
        out.append((s0, w))
        s0 += w
    return out


def topk_thr(k, Iv, Wv, n, n_k, m8, tag, ikeys=()):
    p = k.p
    for r in range(32):
        src = Iv if r == 0 else Wv
        p.op("vector", lambda e, src=src: e.max(out=m8[:n, :], in_=src[:n, :n_k]),
             r=[("I", tag), ("W", tag)] + list(ikeys), w=[("m8", tag)])
        if r < 31:
            p.op("vector", lambda e, src=src: e.match_replace(out=Wv[:n, :n_k], in_to_replace=m8[:n, :],
                                                              in_values=src[:n, :n_k], imm_value=NEG),
                 r=[("m8", tag), ("I", tag)] + list(ikeys), w=[("W", tag)])


def attend(k, st, n, n_k, lhsT, kT, kv, vt, mbv, mbkey, out_ps, out_bank, rkeys):
    p = k.p
    sl = slices(n_k)
    for i, (s0, w) in enumerate(sl):
        bank = i % 4
        p.op("tensor", lambda e, s0=s0, w=w, bank=bank: e.matmul(out=k.ps[bank][:n, :w], lhsT=lhsT,
                                                                 rhs=kT[:, kv, s0:s0 + w], start=True, stop=True),
             r=rkeys, w=[("ps", bank)])
        p.op("vector", lambda e, s0=s0, w=w, bank=bank: e.scalar_tensor_tensor(
            out=st.L[:n, s0:s0 + w], in0=k.ps[bank][:n, :w], scalar=ATT_SCALE, in1=mbv[:n, s0:s0 + w],
            op0=ALU.mult, op1=ALU.add), r=[("ps", bank), mbkey], w=["L"])
    p.op("vector", lambda e: e.tensor_reduce(out=st.mx[:n, :], in_=st.L[:n, :n_k], axis=AX.X, op=ALU.max),
         r=["L"], w=["mx"])
    p.op("vector", lambda e: e.tensor_scalar(out=st.mx[:n, :], in0=st.mx[:n, :], scalar1=-1.0, scalar2=None,
                                             op0=ALU.mult), r=["mx"], w=["mx"])
    p.op("scalar", lambda e: e.activation(out=st.P[:n, :n_k], in_=st.L[:n, :n_k], func=AF.Exp, bias=st.mx[:n, :],
                                          accum_out=st.rs[:n, :]), r=["L", "mx"], w=["P", "rs"])
    p.op("vector", lambda e: e.reciprocal(out=st.rs[:n, :], in_=st.rs[:n, :]), r=["rs"], w=["rs"])
    p.op("vector", lambda e: e.tensor_scalar(out=st.P[:n, :n_k], in0=st.P[:n, :n_k], scalar1=st.rs[:n, :],
                                             scalar2=None, op0=ALU.mult), r=["rs", "P"], w=["P"])
    nj = (n_k + 127) // 128
    for j in range(nj):
        w = min(128, n_k - j * 128)
        bank = (4, 5, 3)[j // 8]
        pv = k.ps[bank][:].bitcast(BF16)
        p.op("tensor", lambda e, j=j, w=w, pv=pv: e.transpose(out=pv[:w, (j % 8) * 128:(j % 8) * 128 + n],
                                                              in_=st.P[:n, j * 128:j * 128 + w],
                                                              identity=k.ident_b[:n, :n]),
             r=["P"], w=[("ps", bank)])
    for half in range((nj + 7) // 8):
        bank = (4, 5, 3)[half]
        pv = k.ps[bank][:].bitcast(BF16)
        cnt = min(8, nj - half * 8)
        rws = min(128, n_k - (half * 8 + cnt - 1) * 128) if cnt == 1 else 128
        srcv = pv.rearrange("p (j t) -> p j t", j=8)[:rws, :cnt, :n]
        dst = st.PT[:rws, half * 8:half * 8 + cnt, :n]
        if half == 0:
            p.op("scalar", lambda e, dst=dst, srcv=srcv: e.copy(out=dst, in_=srcv), r=[("ps", bank)], w=[("PT", half)])
        else:
            p.op("vector", lambda e, dst=dst, srcv=srcv: e.tensor_copy(out=dst, in_=srcv), r=[("ps", bank)],
                 w=[("PT", half)])
    for j in range(nj):
        w = min(128, n_k - j * 128)
        p.op("tensor", lambda e, j=j, w=w: e.matmul(out=out_ps, lhsT=vt[:w, j, kv * 128:(kv + 1) * 128],
                                                    rhs=st.PT[:w, j, :n], start=(j == 0), stop=(j == nj - 1)),
             r=[("PT", 0), ("PT", 1), ("PT", 2)] + rkeys, w=[("ps", out_bank)])


def phase_dsa(k):
    p, nc = k.p, k.nc
    projT = k.scratch("projT", [43, 128, T], BF16)
    oT = k.scratch("oT", [3, 8, 128, T], BF16)
    vtok = k.scratch("v_tok", [T, 256], BF16)
    iwtok = k.scratch("iw_tok", [T, 16], F32)
    NK = SEQ + DEC
    qT = k.sb("d_qT", [128, 8, T], BF16)
    iqT = k.sb("d_iqT", [128, 8, T], BF16)
    kT = k.sb("d_kT", [128, 2, T], BF16)
    ikT = k.sb("d_ikT", [128, T], BF16)
    p.dma("sync", qT[:], projT[0:8].rearrange("c p t -> p c t"), w=["qT"])
    p.dma("sync", iqT[:], projT[10:18].rearrange("c p t -> p c t"), w=["iqT"])
    p.dma("sync", kT[:], projT[8:10].rearrange("c p t -> p c t"), w=["kT"])
    p.dma("sync", ikT[:], projT[18], w=["ikT"])
    st = K()
    st.L = k.sb("d_L", [128, NK], F32)
    st.P = k.sb("d_P", [128, NK], BF16)
    st.PT = k.sb("d_PT", [128, 17, 128], BF16)
    st.mx = k.sb("d_mx", [128, 1], F32)
    st.rs = k.sb("d_rs", [128, 1], F32)
    Ib = k.sb("d_I", [128, NK], F32)
    Wb = k.sb("d_W", [128, NK], F32)
    mb = k.sb("d_mb", [128, NK], F32)
    rl = [k.sb("d_rl%d" % i, [128, 512], F32) for i in range(2)]
    m8 = k.sb("d_m8", [128, 8], F32)
    oc = [k.sb("d_oc%d" % i, [128, 8, 128], BF16) for i in range(2)]

    def indexer(n, n_k, tcol0, ikv, iw_fn, Iv, rk):
        it = 0
        for hi in range(16):
            c, half = hi // 2, hi % 2
            prt = slice(half * 64, half * 64 + 64)
            for i, (s0, w) in enumerate(slices(n_k)):
                bank = (hi % 2) * 4 + i % 4
                b = it % 2
                it += 1
                p.op("tensor", lambda e, c=c, prt=prt, s0=s0, w=w, bank=bank: e.matmul(
                    out=k.ps[bank][:n, :w], lhsT=iqT[prt, c, tcol0:tcol0 + n], rhs=ikv[prt, s0:s0 + w],
                    start=True, stop=True), r=["iqT"] + rk, w=[("ps", bank)])
                p.op("scalar", lambda e, w=w, bank=bank, b=b: e.activation(out=rl[b][:n, :w], in_=k.ps[bank][:n, :w],
                                                                            func=AF.Relu),
                     r=[("ps", bank)], w=[("rl", b)])
                if hi == 0:
                    p.op("vector", lambda e, s0=s0, w=w, b=b, hi=hi: e.tensor_scalar(
                        out=Iv[:n, s0:s0 + w], in0=rl[b][:n, :w], scalar1=iw_fn(hi), scalar2=None, op0=ALU.mult),
                        r=[("rl", b), "iw"], w=[("Isl", i)])
                else:
                    p.op("vector", lambda e, s0=s0, w=w, b=b, hi=hi: e.scalar_tensor_tensor(
                        out=Iv[:n, s0:s0 + w], in0=rl[b][:n, :w], scalar=iw_fn(hi), in1=Iv[:n, s0:s0 + w],
                        op0=ALU.mult, op1=ALU.add), r=[("rl", b), "iw", ("Isl", i)], w=[("Isl", i)])
        return [("Isl", i) for i in range(len(slices(n_k)))]

    with k.sub():
        vt = k.sb("d_vt", [128, 16, 256], BF16)
        iw = k.sb("d_iw", [128, 16, 16], F32)
        p.dma("sync", vt[:], vtok[0:SEQ, :].rearrange("(j q) n -> q j n", q=128), w=["vt"])
        p.dma("sync", iw[:], iwtok[0:SEQ, :].rearrange("(j q) n -> q j n", q=128), w=["iw"])
        for qi in range(16):
            n_k = 128 * (qi + 1)
            t0 = qi * 128
            ikeys = indexer(128, n_k, t0, ikT, lambda hi, qi=qi: iw[:, qi, hi:hi + 1], Ib, ["ikT"])
            p.op("gpsimd", lambda e, t0=t0: e.affine_select(out=Ib[:, t0:t0 + 128], in_=Ib[:, t0:t0 + 128],
                                                            pattern=[[-1, 128]], compare_op=ALU.is_ge, fill=NEG,
                                                            base=0, channel_multiplier=1),
                 r=ikeys, w=[("I", "p")] + ikeys)
            if qi >= 2:
                topk_thr(k, Ib, Wb, 128, n_k, m8, "p", ikeys)
                p.op("vector", lambda e, n_k=n_k: e.tensor_scalar(out=mb[:, :n_k], in0=Ib[:, :n_k], scalar1=m8[:, 7:8],
                                                                  scalar2=NEG, op0=ALU.is_lt, op1=ALU.mult),
                     r=[("I", "p"), ("m8", "p")] + ikeys, w=["mb"])
            else:
                p.op("vector", lambda e, n_k=n_k: e.tensor_scalar(out=mb[:, :n_k], in0=Ib[:, :n_k], scalar1=-1.0e29,
                                                                  scalar2=NEG, op0=ALU.is_lt, op1=ALU.mult),
                     r=[("I", "p")] + ikeys, w=["mb"])
            ob = oc[qi % 2]
            for h in range(8):
                bank = 6 + h // 4
                attend(k, st, 128, n_k, qT[:, h, t0:t0 + 128], kT, h // 4, vt, mb, "mb",
                       k.ps[bank][:, (h % 4) * 128:(h % 4) * 128 + 128], bank, ["qT", "kT", "vt"])
                if h % 4 == 3:
                    g = h // 4
                    srcv = k.ps[bank][:].rearrange("p (j t) -> p j t", j=4)
                    if g == 0:
                        p.op("scalar", lambda e, ob=ob, srcv=srcv: e.copy(out=ob[:, 0:4, :], in_=srcv),
                             r=[("ps", bank)], w=[("oc", qi % 2, 0)])
                    else:
                        p.op("vector", lambda e, ob=ob, srcv=srcv: e.tensor_copy(out=ob[:, 4:8, :], in_=srcv),
                             r=[("ps", bank)], w=[("oc", qi % 2, 1)])
            p.dma("sync", oT[0, :, :, t0:t0 + 128].rearrange("c q t -> q c t"), ob[:],
                  r=[("oc", qi % 2, 0), ("oc", qi % 2, 1)])

    with k.sub():
        ptb = k.sb("s_ptb", [128, 256], I32)
        ptf = k.sb("s_ptf", [128, 256], F32)
        idx = k.sb("s_idx", [128, 256], U32)
        iop = k.sb("s_iop", [128, 1], F32)
        p.dma("sync", ptb[:], k.I["ptab"].rearrange("b g -> (b g)").partition_broadcast(128), w=["ptb"])
        p.op("gpsimd", lambda e: e.iota(iop[:], pattern=[[0, 1]], base=0, channel_multiplier=1,
                                        allow_small_or_imprecise_dtypes=True), w=["iop"])
        p.op("vector", lambda e: e.tensor_copy(out=ptf[:], in_=ptb[:]), r=["ptb"], w=["ptf"])
        p.op("vector", lambda e: e.tensor_scalar(out=ptf[:], in0=ptf[:], scalar1=128.0, scalar2=iop[:, 0:1],
                                                 op0=ALU.mult, op1=ALU.add), r=["ptf", "iop"], w=["ptf"])
        p.op("vector", lambda e: e.tensor_copy(out=idx[:], in_=ptf[:]), r=["ptf"], w=["idx"])
        iws = k.sb("s_iw", [4, 16, 16], F32)
        p.dma("sync", iws[:], iwtok[SEQ:T, :].rearrange("(b t) h -> t b h", t=4), w=["iw"])
        Iall = k.sb("s_Iall", [64, NK], F32)
        Wall = Wb
        mball = mb
        sub1 = k.sub()
        sub1.__enter__()
        pg = [k.sb("s_pg%d" % i, [128, 128], F32) for i in range(4)]
        ikTb = [k.sb("s_ikTb%d" % i, [128, NK], BF16) for i in range(2)]
        for b in range(NS_SEQ):
            ib = ikTb[b % 2]
            for g in range(16):
                col = b * 16 + g
                t = pg[g % 4]
                for hf in range(2):
                    p.op("gpsimd", lambda e, t=t, hf=hf, col=col: e.indirect_dma_start(
                        out=t[:, hf * 64:(hf + 1) * 64], out_offset=None, in_=k.I["cache_ik"][:, :],
                        in_offset=bass.IndirectOffsetOnAxis(ap=idx[:, col:col + 1], axis=0)),
                        r=["idx"], w=[("pg", g % 4, hf)], dma=True)
                bank = 4 + (g // 4) % 2
                p.op("tensor", lambda e, t=t, g=g, bank=bank: e.transpose(
                    out=k.ps[bank][:, (g % 4) * 128:(g % 4) * 128 + 128], in_=t[:, :], identity=k.ident_f[:, :]),
                    r=[("pg", g % 4, 0), ("pg", g % 4, 1)], w=[("ps", bank)])
                if g % 4 == 3:
                    g0 = g - 3
                    p.op("scalar", lambda e, ib=ib, g0=g0, bank=bank: e.copy(out=ib[:, g0 * 128:g0 * 128 + 512],
                                                                            in_=k.ps[bank][:, :]),
                         r=[("ps", bank)], w=[("ikTb", b % 2)])
            p.op("vector", lambda e, ib=ib, b=b: e.tensor_copy(out=ib[:, SEQ:NK], in_=ikT[:, SEQ + 4 * b:SEQ + 4 * b + 4]),
                 r=["ikT"], w=[("ikTb", b % 2)])
            ikeys = indexer(4, NK, SEQ + 4 * b, ib, lambda hi, b=b: iws[:, b, hi:hi + 1], Ib, [("ikTb", b % 2)])
            p.op("gpsimd", lambda e: e.affine_select(out=Ib[:4, SEQ:NK], in_=Ib[:4, SEQ:NK], pattern=[[-1, 4]],
                                                     compare_op=ALU.is_ge, fill=NEG, base=0, channel_multiplier=1),
                 r=ikeys, w=[("I", "s")] + ikeys)
            p.dma("sync", Iall[4 * b:4 * b + 4, :], Ib[:4, :], r=[("I", "s")] + ikeys, w=[("I", "all")])
        sub1.__exit__(None, None, None)
        topk_thr(k, Iall, Wall, 64, NK, m8, "all")
        p.op("vector", lambda e: e.tensor_scalar(out=mball[:64, :], in0=Iall[:, :], scalar1=m8[:64, 7:8], scalar2=NEG,
                                                 op0=ALU.is_lt, op1=ALU.mult), r=[("I", "all"), ("m8", "all")],
             w=["mball"])
        kTb = [k.sb("s_kTb%d" % i, [128, 2, NK], BF16) for i in range(2)]
        vtb = [k.sb("s_vtb%d" % i, [128, 17, 256], BF16) for i in range(2)]
        kpg = [k.sb("s_kpg%d" % i, [128, 256], F32) for i in range(4)]
        vpg = [k.sb("s_vpg%d" % i, [128, 256], F32) for i in range(4)]
        mb16 = [k.sb("s_mb16%d" % i, [16, NK], F32) for i in range(2)]
        ocs = [k.sb("s_ocs%d" % i, [128, 8, 4], BF16) for i in range(2)]
        qs16 = [k.sb("s_qs16%d" % i, [128, 32], BF16) for i in range(2)]
        for b in range(NS_SEQ):
            kb, vb = kTb[b % 2], vtb[b % 2]
            for g in range(16):
                col = b * 16 + g
                tk, tv = kpg[g % 4], vpg[g % 4]
                p.op("gpsimd", lambda e, tk=tk, col=col: e.indirect_dma_start(
                    out=tk[:, :], out_offset=None, in_=k.I["cache_k"][:, :],
                    in_offset=bass.IndirectOffsetOnAxis(ap=idx[:, col:col + 1], axis=0)),
                    r=["idx"], w=[("kpg", g % 4)], dma=True)
                p.op("gpsimd", lambda e, tv=tv, col=col: e.indirect_dma_start(
                    out=tv[:, :], out_offset=None, in_=k.I["cache_v"][:, :],
                    in_offset=bass.IndirectOffsetOnAxis(ap=idx[:, col:col + 1], axis=0)),
                    r=["idx"], w=[("vpg", g % 4)], dma=True)
                p.op("vector", lambda e, vb=vb, g=g, tv=tv: e.tensor_copy(out=vb[:, g, :], in_=tv[:, :]),
                     r=[("vpg", g % 4)], w=[("vtb", b % 2)])
                bank = 4 + g % 2
                for kv in range(2):
                    p.op("tensor", lambda e, tk=tk, kv=kv, bank=bank: e.transpose(
                        out=k.ps[bank][:, kv * 128:(kv + 1) * 128], in_=tk[:, kv * 128:(kv + 1) * 128],
                        identity=k.ident_f[:, :]), r=[("kpg", g % 4)], w=[("ps", bank)])
                p.op("scalar", lambda e, kb=kb, g=g, bank=bank: e.copy(
                    out=kb[:, :, g * 128:(g + 1) * 128], in_=k.ps[bank][:, 0:256].rearrange("p (v s) -> p v s", v=2)),
                    r=[("ps", bank)], w=[("kTb", b % 2)])
            tc0 = SEQ + 4 * b
            p.op("vector", lambda e, kb=kb, tc0=tc0: e.tensor_copy(out=kb[:, :, SEQ:NK], in_=kT[:, :, tc0:tc0 + 4]),
                 r=["kT"], w=[("kTb", b % 2)])
            p.dma("sync", vb[:4, 16, :], vtok[tc0:tc0 + 4, :], w=[("vtb", b % 2)])
            m16 = mb16[b % 2]
            for i in range(4):
                p.dma("sync", m16[4 * i:4 * i + 4, :], mball[4 * b:4 * b + 4, :], r=["mball"], w=[("mb16", b % 2)])
            ob = ocs[b % 2]
            qs = qs16[b % 2]
            p.op("vector", lambda e, qs=qs, tc0=tc0: e.tensor_copy(
                out=qs[:, :].rearrange("p (h t) -> p h t", t=4), in_=qT[:, :, tc0:tc0 + 4]),
                r=["qT"], w=[("qs16", b % 2)])
            for kv in range(2):
                bank = 6 + kv
                attend(k, st, 16, NK, qs[:, kv * 16:kv * 16 + 16], kb, kv, vb, m16, ("mb16", b % 2),
                       k.ps[bank][:, 0:16], bank, [("qs16", b % 2), ("kTb", b % 2), ("vtb", b % 2)])
                p.op("vector", lambda e, ob=ob, kv=kv, bank=bank: e.tensor_copy(
                    out=ob[:, kv * 4:kv * 4 + 4, :], in_=k.ps[bank][:, 0:16].rearrange("p (h t) -> p h t", h=4)),
                    r=[("ps", bank)], w=[("ocs", b % 2, kv)])
            p.dma("sync", oT[0, :, :, tc0:tc0 + 4].rearrange("c q t -> q c t"), ob[:],
                  r=[("ocs", b % 2, 0), ("ocs", b % 2, 1)])


PHASES.append(("dsa", phase_dsa))


def bc(ap, shape):
    return ap.to_broadcast(shape)


def phase_peer(k):
    p, nc = k.p, k.nc
    x1 = k.scratch("x1", [T, D], F32)
    xn2f = k.scratch("xn2f", [T, D], F32)
    norm_stats(k)
    qpT = k.sb("pe_qpT", [128, 16, T], BF16)
    skT = k.sb("pe_skT", [128, 16, 128], BF16)
    with k.sub():
        xn2T = k.sb("pe_xn2T", [128, 16, T], BF16)
        with k.sub():
            g = load_bcast(k, "gf_bc", k.I["g_ffn"], D)
            xt = [k.sb("pxt%d" % i, [128, D], F32) for i in range(2)]
            xn = [k.sb("pxn%d" % i, [128, D], BF16) for i in range(2)]
            xf = [k.sb("pxf%d" % i, [128, D], F32) for i in range(2)]
            for tt in range(NT):
                b = tt % 2
                n = trows(tt)
                t0 = tt * 128
                p.dma("sync", xt[b][:n, :], x1[t0:t0 + n, :], w=[("xt", b)])
                rms_tile(k, xt[b], n, g, "gf_bc", xn[b], b, tt)
                p.op("gpsimd", lambda e, b=b, n=n: e.tensor_copy(out=xf[b][:n, :], in_=xn[b][:n, :]),
                     r=[("xn", b)], w=[("xf", b)])
                p.dma("sync", xn2f[t0:t0 + n, :], xf[b][:n, :], r=[("xf", b)])
                transpose_to_T(k, xn[b], n, b, xn2T, t0, b * 2)
            skt = [k.sb("pskt%d" % i, [128, 128], F32) for i in range(2)]
            for j in range(16):
                h, pp = j // 2, j % 2
                r0 = (pp * 8 + h) * 128
                p.dma("sync", skt[j % 2][:], k.I["sub_keys"][r0:r0 + 128, :], w=[("skt", j % 2)])
                bank = 4 + j % 2
                p.op("tensor", lambda e, j=j, bank=bank: e.transpose(out=k.ps[bank][:, 0:128], in_=skt[j % 2][:],
                                                                     identity=k.ident_f[:]),
                     r=[("skt", j % 2)], w=[("ps", bank)])
                p.op("vector", lambda e, j=j, bank=bank: e.tensor_copy(out=skT[:, j, :], in_=k.ps[bank][:, 0:128]),
                     r=[("ps", bank)], w=["skT"])

        def evac_q(ci, si, ps, bank, rows, t0, tn):
            if si % 2 == 0:
                p.op("scalar", lambda e: e.copy(out=qpT[:, ci, t0:t0 + tn], in_=ps[:, :tn]), r=[("ps", bank)],
                     w=[("qpT", ci, si)])
            else:
                p.op("vector", lambda e: e.tensor_copy(out=qpT[:, ci, t0:t0 + tn], in_=ps[:, :tn]), r=[("ps", bank)],
                     w=[("qpT", ci, si)])
        linear_fm(k, xn2T, [], "w_pq", [[(c0, 128)] for c0 in range(0, D, 128)], evac_q, "pq")

    with k.sub():
        sc = k.sb("pe_sc", [128, 16, 128], F32)
        scw = k.sb("pe_scw", [128, 16, 128], F32)
        vv = k.sb("pe_v", [128, 16, 16], F32)
        ix = k.sb("pe_ix", [128, 16, 16], U32)
        ixf = k.sb("pe_ixf", [128, 16, 16], F32)
        cand = k.sb("pe_cand", [128, 8, 256], F32)
        candw = k.sb("pe_candw", [128, 8, 256], F32)
        tv = k.sb("pe_tv", [128, 8, 16], F32)
        pos = k.sb("pe_pos", [128, 8, 16], U32)
        pa = k.sb("pe_pa", [128, 8, 16], U32)
        pb = k.sb("pe_pb", [128, 8, 16], U32)
        paf = k.sb("pe_paf", [128, 8, 16], F32)
        pbf = k.sb("pe_pbf", [128, 8, 16], F32)
        eq = k.sb("pe_eq", [128, 8, 16, 16], F32)
        sel = k.sb("pe_sel", [128, 2, 8, 16], F32)
        ef = k.sb("pe_ef", [128, 128], F32)
        eidx = k.sb("pe_eidx", [128, 128], U32)
        gw = k.sb("pe_gw", [128, 8, 16], F32)
        gs = k.sb("pe_gs", [128, 8], F32)
        act = k.sb("pe_act", [128, 128], F32)
        ga = k.sb("pe_ga", [128, 128], F32)
        t1 = k.sb("pe_t1", [128, 128], F32)
        t2 = k.sb("pe_t2", [128, 128], F32)
        io16 = k.sb("pe_io16", [128, 16], F32)
        gb = [k.sb("pe_gb%d" % i, [128, D], F32) for i in range(4)]
        xq = k.sb("pe_xq", [128, D], F32)
        x1t = k.sb("pe_x1t", [128, D], F32)
        acc = k.sb("pe_acc", [128, D], F32)
        junkf = k.sb("pe_junkf", [128, D], F32)
        yo = k.sb("pe_yo", [128, D], F32)
        gfin = load_bcast(k, "gfin_bc", k.I["g_final"], D)
        p.op("gpsimd", lambda e: e.iota(io16[:], pattern=[[1, 16]], base=0, channel_multiplier=0,
                                        allow_small_or_imprecise_dtypes=True), w=["io16"])
        S4 = [128, 8, 16, 16]
        def tile_body(tt, n, t0):
            p.dma("sync", xq[:n, :], xn2f[t0:t0 + n, :], w=["xq"])
            p.dma("sync", x1t[:n, :], x1[t0:t0 + n, :], w=[("xt", "f")])
            for j in range(16):
                bank = j // 4
                p.op("tensor", lambda e, j=j, bank=bank: e.matmul(
                    out=k.ps[bank][:n, (j % 4) * 128:(j % 4) * 128 + 128], lhsT=qpT[:, j, t0:t0 + n], rhs=skT[:, j, :],
                    start=True, stop=True), r=["skT"], w=[("ps", bank)])
            for bank in range(4):
                dst = sc[:n, bank * 4:bank * 4 + 4, :]
                srcv = k.ps[bank][:n, :].rearrange("p (j q) -> p j q", j=4)
                if bank % 2 == 0:
                    p.op("scalar", lambda e, dst=dst, srcv=srcv: e.copy(out=dst, in_=srcv), r=[("ps", bank)],
                         w=[("sc", bank)])
                else:
                    p.op("vector", lambda e, dst=dst, srcv=srcv: e.tensor_copy(out=dst, in_=srcv), r=[("ps", bank)],
                         w=[("sc", bank)])
            for j in range(16):
                kj = [("sc", j // 4)]
                p.op("vector", lambda e, j=j: e.max(out=vv[:n, j, 0:8], in_=sc[:n, j, :]), r=kj, w=[("vv", j)])
                p.op("vector", lambda e, j=j: e.max_index(out=ix[:n, j, 0:8], in_max=vv[:n, j, 0:8], in_values=sc[:n, j, :]),
                     r=kj + [("vv", j)], w=[("ix", j)])
                p.op("vector", lambda e, j=j: e.match_replace(out=scw[:n, j, :], in_to_replace=vv[:n, j, 0:8],
                                                              in_values=sc[:n, j, :], imm_value=NEG),
                     r=kj + [("vv", j)], w=[("scw", j)])
                p.op("vector", lambda e, j=j: e.max(out=vv[:n, j, 8:16], in_=scw[:n, j, :]), r=[("scw", j)],
                     w=[("vv", j)])
                p.op("vector", lambda e, j=j: e.max_index(out=ix[:n, j, 8:16], in_max=vv[:n, j, 8:16],
                                                          in_values=scw[:n, j, :]),
                     r=[("scw", j), ("vv", j)], w=[("ix", j)])
            allv = [("vv", j) for j in range(16)]
            alli = [("ix", j) for j in range(16)]
            p.op("vector", lambda e: e.tensor_copy(out=ixf[:n], in_=ix[:n]), r=alli, w=["ixf"])
            v4 = vv[:n].rearrange("p (h two) a -> p h two a", two=2)
            i4 = ixf[:n].rearrange("p (h two) a -> p h two a", two=2)
            S4n = [n, 8, 16, 16]
            p.op("vector", lambda e: e.tensor_tensor(
                out=cand[:n].rearrange("p h (a b) -> p h a b", a=16), in0=bc(v4[:, :, 0, :].unsqueeze(3), S4n),
                in1=bc(v4[:, :, 1, :].unsqueeze(2), S4n), op=ALU.add), r=allv, w=["cand"])
            for h in range(8):
                p.op("vector", lambda e, h=h: e.max(out=tv[:n, h, 0:8], in_=cand[:n, h, :]), r=["cand"], w=[("tv", h)])
                p.op("vector", lambda e, h=h: e.max_index(out=pos[:n, h, 0:8], in_max=tv[:n, h, 0:8],
                                                          in_values=cand[:n, h, :]), r=["cand", ("tv", h)],
                     w=[("pos", h)])
                p.op("vector", lambda e, h=h: e.match_replace(out=candw[:n, h, :], in_to_replace=tv[:n, h, 0:8],
                                                              in_values=cand[:n, h, :], imm_value=NEG),
                     r=["cand", ("tv", h)], w=[("candw", h)])
                p.op("vector", lambda e, h=h: e.max(out=tv[:n, h, 8:16], in_=candw[:n, h, :]), r=[("candw", h)],
                     w=[("tv", h)])
                p.op("vector", lambda e, h=h: e.max_index(out=pos[:n, h, 8:16], in_max=tv[:n, h, 8:16],
                                                          in_values=candw[:n, h, :]), r=[("candw", h), ("tv", h)],
                     w=[("pos", h)])
            allt = [("tv", h) for h in range(8)]
            allp = [("pos", h) for h in range(8)]
            p.op("vector", lambda e: e.tensor_tensor(out=gw[:n], in0=tv[:n], in1=bc(tv[:n, :, 0:1], [n, 8, 16]),
                                                     op=ALU.subtract), r=allt, w=["gw"])
            p.op("scalar", lambda e: e.activation(out=gw[:n], in_=gw[:n], func=AF.Exp), r=["gw"], w=["gw"])
            p.op("vector", lambda e: e.tensor_reduce(out=gs[:n, :], in_=gw[:n], axis=AX.X, op=ALU.add), r=["gw"],
                 w=["gs"])
            p.op("vector", lambda e: e.reciprocal(out=gs[:n, :], in_=gs[:n, :]), r=["gs"], w=["gs"])
            p.op("vector", lambda e: e.tensor_tensor(out=gw[:n], in0=gw[:n], in1=bc(gs[:n, :].unsqueeze(2), [n, 8, 16]),
                                                     op=ALU.mult), r=["gs", "gw"], w=["gw"])
            p.op("vector", lambda e: e.tensor_single_scalar(out=pa[:n], in_=pos[:n], scalar=4,
                                                            op=ALU.logical_shift_right), r=allp, w=["pa"])
            p.op("vector", lambda e: e.tensor_single_scalar(out=pb[:n], in_=pos[:n], scalar=15, op=ALU.bitwise_and),
                 r=allp, w=["pb"])
            p.op("vector", lambda e: e.tensor_copy(out=paf[:n], in_=pa[:n]), r=["pa"], w=["paf"])
            p.op("vector", lambda e: e.tensor_copy(out=pbf[:n], in_=pb[:n]), r=["pb"], w=["pbf"])
            for w_, pf in ((0, paf), (1, pbf)):
                p.op("vector", lambda e, pf=pf: e.tensor_tensor(
                    out=eq[:n], in0=bc(pf[:n].unsqueeze(3), S4n), in1=bc(io16[:n, :].unsqueeze(1).unsqueeze(1), S4n),
                    op=ALU.is_equal), r=["paf", "pbf", "io16", "sel"], w=["eq"])
                p.op("vector", lambda e, w_=w_: e.tensor_tensor(
                    out=eq[:n], in0=eq[:n], in1=bc(i4[:, :, w_, :].unsqueeze(2), S4n), op=ALU.mult),
                    r=["eq", "ixf"], w=["eq"])
                p.op("vector", lambda e, w_=w_: e.tensor_reduce(out=sel[:n, w_], in_=eq[:n], axis=AX.X, op=ALU.add),
                     r=["eq"], w=["sel"])
            p.op("vector", lambda e: e.scalar_tensor_tensor(
                out=ef[:n, :], in0=sel[:n, 0].rearrange("p h k -> p (h k)"), scalar=128.0,
                in1=sel[:n, 1].rearrange("p h k -> p (h k)"), op0=ALU.mult, op1=ALU.add), r=["sel"], w=["ef"])
            p.op("vector", lambda e: e.tensor_copy(out=eidx[:n, :], in_=ef[:n, :]), r=["ef"], w=["eidx"])
            p.op("vector", lambda e: e.memset(act[:], 0.0), r=["ga"], w=[("act", s_) for s_ in range(128)])
            for s_ in range(128):
                b = s_ % 4
                p.op("gpsimd", lambda e, s_=s_, b=b: e.indirect_dma_start(
                    out=gb[b][:n, :], out_offset=None, in_=k.I["peer_u"][:, :],
                    in_offset=bass.IndirectOffsetOnAxis(ap=eidx[:n, s_:s_ + 1], axis=0)),
                    r=["eidx"], w=[("gb", b)], dma=True)
                p.op("vector", lambda e, s_=s_, b=b: e.scalar_tensor_tensor(
                    out=junkf[:n, :], in0=gb[b][:n, :], scalar=1.0, in1=xq[:n, :], op0=ALU.mult, op1=ALU.mult,
                    accum_out=act[:n, s_:s_ + 1]), r=[("gb", b), "xq"], w=[("act", s_), "junkf"])
            gelu_mul(k, act[:n, :], gw[:n].rearrange("p h k -> p (h k)"), ga[:n, :], n, t1[:n, :], t2[:n, :],
                     [("act", s_) for s_ in range(128)] + ["gw"], "ga")
            for s_ in range(128):
                b = s_ % 4
                p.op("gpsimd", lambda e, s_=s_, b=b: e.indirect_dma_start(
                    out=gb[b][:n, :], out_offset=None, in_=k.I["peer_v"][:, :],
                    in_offset=bass.IndirectOffsetOnAxis(ap=eidx[:n, s_:s_ + 1], axis=0)),
                    r=["eidx"], w=[("gb", b)], dma=True)
                if s_ == 0:
                    p.op("vector", lambda e, b=b: e.scalar_tensor_tensor(
                        out=acc[:n, :], in0=gb[b][:n, :], scalar=ga[:n, 0:1], in1=x1t[:n, :], op0=ALU.mult,
                        op1=ALU.add), r=[("gb", b), "ga", ("xt", "f")], w=["acc"])
                else:
                    p.op("vector", lambda e, s_=s_, b=b: e.scalar_tensor_tensor(
                        out=acc[:n, :], in0=gb[b][:n, :], scalar=ga[:n, s_:s_ + 1], in1=acc[:n, :], op0=ALU.mult,
                        op1=ALU.add), r=[("gb", b), "ga", "acc"], w=["acc"])
            if tt == 0 and "dbg" in DEBUG_IO:
                dbg = k.scratch("dbg", [6, 128, 128], F32)
                p.dma("sync", dbg[0], ef[:, :], r=["ef", "eidx"])
                p.dma("sync", dbg[1], gw[:].rearrange("p h k -> p (h k)"), r=["gw", "ga"])
                p.dma("sync", dbg[2], act[:, :], r=["ga"])
                p.dma("sync", dbg[3], ga[:, :], r=["ga"])
                p.dma("sync", dbg[4], tv[:].rearrange("p h k -> p (h k)"), r=allt + ["gw"])
                p.dma("sync", dbg[5], paf[:].rearrange("p h k -> p (h k)"), r=["paf", "eq"])
            p.op("vector", lambda e: e.tensor_copy(out=x1t[:n, :], in_=acc[:n, :]), r=["acc"], w=[("xt", "f")])
            rms_tile(k, x1t, n, gfin, "gfin_bc", yo, "f", 32 + tt)
            dst = k.O["y_p"][t0:t0 + 128, :] if tt < 16 else k.O["y_s"][:, :]
            p.dma("sync", dst, yo[:n, :], r=[("xn", "f")])

        for tt in range(NT):
            tile_body(tt, trows(tt), tt * 128)


PHASES.append(("peer", phase_peer))
```

```python
import numpy as np
from contextlib import ExitStack
import concourse.bass as bass
import concourse.mybir as mybir
from concourse.bass_utils import run_bass_kernel_spmd

F32 = mybir.dt.float32
BF16 = mybir.dt.bfloat16
I32 = mybir.dt.int32
U32 = mybir.dt.uint32
AF = mybir.ActivationFunctionType
ALU = mybir.AluOpType
AX = mybir.AxisListType

NCORES = 8
D = 2048
SEQ = 2048
NS_SEQ = 16
DEC = 4
TS = NS_SEQ * DEC
T = SEQ + TS
NT = 17
N_IN = 5712
EPS = 1e-6
NEG = -1.0e30
O_Q, O_K, O_V, O_IQ, O_IK, O_IW, O_XB, O_YB, O_QM = 0, 1024, 1280, 1536, 2560, 2624, 2640, 3664, 4688


def trows(tt):
    return 128 if tt < 16 else 64


class _Op:
    __slots__ = ("eng", "idx", "fn", "dma", "deps", "signaled", "cum", "sem", "tgt", "prev_tgt", "k")

    def __init__(self, eng, idx, fn, dma):
        self.eng = eng
        self.idx = idx
        self.fn = fn
        self.dma = dma
        self.deps = []
        self.signaled = False
        self.cum = 0
        self.sem = None
        self.tgt = 0
        self.prev_tgt = 0
        self.k = 0


class _Res:
    __slots__ = ("lw", "rd")

    def __init__(self):
        self.lw = None
        self.rd = []


class Prog:
    ENG = ("tensor", "vector", "scalar", "gpsimd", "sync")
    NS = 12

    def __init__(self, nc):
        self.nc = nc
        self.ops = {e: [] for e in self.ENG}
        self.res = {}
        self.ndma = {e: 0 for e in self.ENG}

    def _r(self, k):
        r = self.res.get(k)
        if r is None:
            r = self.res[k] = _Res()
        return r

    def op(self, eng, fn, r=(), w=(), dma=False):
        o = _Op(eng, len(self.ops[eng]), fn, dma)
        deps = {}
        for k in r:
            rr = self._r(k)
            if rr.lw is not None:
                deps[id(rr.lw)] = (rr.lw, True)
        for k in w:
            rr = self._r(k)
            if rr.lw is not None:
                deps[id(rr.lw)] = (rr.lw, True)
            for x in rr.rd:
                if id(x) not in deps:
                    deps[id(x)] = (x, False)
        for d, hard in deps.values():
            if d is o:
                continue
            if d.dma or o.dma or d.eng != o.eng:
                o.deps.append(d)
            elif hard and o.eng != "tensor":
                o.deps.append(d)
        for k in r:
            self._r(k).rd.append(o)
        for k in w:
            rr = self._r(k)
            rr.lw = o
            rr.rd = []
        if dma:
            o.k = self.ndma[eng]
            self.ndma[eng] += 1
        self.ops[eng].append(o)
        return o

    def dma(self, eng, out, in_, r=(), w=(), **kw):
        return self.op(eng, lambda e: e.dma_start(out=out, in_=in_, **kw), r=r, w=w, dma=True)

    def barrier(self):
        last = []
        for e in self.ENG:
            for o in reversed(self.ops[e]):
                if o.fn is not None and not o.dma:
                    last.append(o)
                    break
        alld = []
        for e in self.ENG:
            seen = 0
            for o in reversed(self.ops[e]):
                if o.dma:
                    alld.append(o)
                    seen += 1
                    if seen >= self.NS:
                        break
        for e in self.ENG:
            o = _Op(e, len(self.ops[e]), None, False)
            o.deps = [d for d in last + alld if d.eng != e or d.dma or e != "tensor"]
            self.ops[e].append(o)
        self.res = {}

    def emit(self, es):
        nc = self.nc
        sem_e = {e: es.enter_context(nc.semaphore("se_" + e)) for e in self.ENG}
        sem_d = {e: [es.enter_context(nc.semaphore("sd_%s%d" % (e, i))) for i in range(self.NS)]
                 for e in self.ENG if self.ndma[e]}
        for e in self.ENG:
            for o in self.ops[e]:
                for d in o.deps:
                    d.signaled = True
        for e in self.ENG:
            c = 0
            for o in self.ops[e]:
                if o.dma:
                    o.sem = sem_d[e][o.k % self.NS]
                    o.tgt = 16 * (o.k // self.NS + 1)
                    o.prev_tgt = o.tgt - 16
                elif o.signaled:
                    c += 1
                    o.cum = c
        finals = {}
        for e in self.ENG:
            f = {}
            for o in self.ops[e]:
                if o.dma:
                    f[o.k % self.NS] = (o.sem, o.tgt)
            finals[e] = list(f.values())
        block = es.enter_context(nc.Block())

        def mk(e):
            def body(eng):
                seen = {}
                for o in self.ops[e]:
                    waits = {}
                    for d in o.deps:
                        if d.dma:
                            key, sem, v = ("d", d.eng, d.k % self.NS), d.sem, d.tgt
                        else:
                            key, sem, v = ("e", d.eng), sem_e[d.eng], d.cum
                        if seen.get(key, 0) >= v:
                            continue
                        if key not in waits or waits[key][1] < v:
                            waits[key] = (sem, v)
                    if o.dma and o.prev_tgt > 0:
                        key = ("d", e, o.k % self.NS)
                        if seen.get(key, 0) < o.prev_tgt:
                            waits[key] = (o.sem, max(o.prev_tgt, waits.get(key, (None, 0))[1]))
                    for key, (sem, v) in waits.items():
                        eng.wait_ge(sem, v)
                        seen[key] = v
                    if o.fn is None:
                        continue
                    ins = o.fn(eng)
                    if o.dma:
                        ins.then_inc(o.sem, 16)
                    elif o.signaled:
                        ins.then_inc(sem_e[e], 1)
                for sem, v in finals[e]:
                    eng.wait_ge(sem, v)
            return body

        for e in self.ENG:
            getattr(block, e)(mk(e))


IN_SPECS = [
    ("xp", [SEQ, D], F32), ("xs", [TS, D], F32),
    ("cache_k", [2560 * 128, 256], F32), ("cache_v", [2560 * 128, 256], F32),
    ("cache_ik", [2560 * 128, 64], F32), ("ptab", [NS_SEQ, 16], I32),
    ("st_conv", [NS_SEQ * 3, 1024], F32), ("st_lru", [NS_SEQ, 1024], F32),
    ("cmk", [NS_SEQ * 256, 1024], F32), ("cmv", [NS_SEQ * 256, 1024], F32),
    ("mem", [256, D], F32),
    ("g_mix", [1, D], F32), ("w_in", [D, N_IN], F32), ("conv_w", [4, 1024], F32), ("conv_b", [1, 1024], F32),
    ("w_rg", [1024, 128], F32), ("b_rg", [1, 1024], F32), ("w_ig", [1024, 128], F32), ("b_ig", [1, 1024], F32),
    ("lam", [1, 1024], F32), ("g_mem", [1, D], F32), ("w_mem_kv", [D, 2048], F32),
    ("w_gate", [D, 3 * D], F32), ("w_br", [3 * 1024, D], F32), ("w_o", [D, D], F32), ("g_ffn", [1, D], F32),
    ("w_pq", [D, 2048], F32), ("sub_keys", [2 * 8 * 128, 128], F32), ("peer_u", [16384, D], F32),
    ("peer_v", [16384, D], F32), ("g_final", [1, D], F32),
]
OUT_SPECS = [
    ("y_p", [SEQ, D]), ("y_s", [TS, D]), ("k_p", [SEQ, 256]), ("v_p", [SEQ, 256]), ("ik_p", [SEQ, 64]),
    ("conv_p", [3, 1024]), ("h_p", [1, 1024]), ("mk_p", [256, 1024]), ("mv_p", [256, 1024]),
    ("k_s", [TS, 256]), ("v_s", [TS, 256]), ("ik_s", [TS, 64]), ("conv_s", [NS_SEQ * 3, 1024]),
    ("h_s", [NS_SEQ, 1024]),
]


_INS = {n: (s, d) for n, s, d in IN_SPECS}
_OUTS = {n: s for n, s in OUT_SPECS}


class _Lazy(dict):
    def __init__(self, mk):
        super().__init__()
        self.mk = mk

    def __missing__(self, n):
        v = self[n] = self.mk(n)
        return v


DEBUG_IO = {}


class K:
    def sub(self):
        return _Sub(self)


class _Sub:
    def __init__(self, k):
        self.k = k

    def __enter__(self):
        self.prev = self.k.scope
        self.st = ExitStack()
        self.k.scope = self.st
        return self

    def __exit__(self, *a):
        self.k.p.barrier()
        self.st.close()
        self.k.scope = self.prev
        return False


def build(phases=("all",)):
    nc = bass.Bass("TRN2", target_bir_lowering=False)
    es = ExitStack()
    k = K()
    k.nc = nc
    k.es = es
    k.I = _Lazy(lambda n: nc.dram_tensor(n, _INS[n][0], _INS[n][1], kind="ExternalInput").ap())
    k.O = _Lazy(lambda n: nc.dram_tensor(n, _OUTS[n], F32, kind="ExternalOutput").ap())
    k.S = {}
    p = k.p = Prog(nc)

    def scratch(name, shape, dt):
        if name not in k.S:
            kind = DEBUG_IO.get(name)
            if kind:
                k.S[name] = nc.dram_tensor(name, shape, dt, kind=kind).ap()
            else:
                k.S[name] = nc.dram_tensor(name, shape, dt).ap()
        return k.S[name]
    k.scratch = scratch
    k.scope = es

    k.nsb = 0

    def sb(name, shape, dt):
        k.nsb += 1
        return k.scope.enter_context(nc.sbuf_tensor("%s_%d" % (name, k.nsb), shape, dt))
    k.sb = sb
    k.ps = [es.enter_context(nc.psum_tensor("ps%d" % i, [128, 512], F32)) for i in range(8)]

    k.ident_f = sb("ident_f", [128, 128], F32)
    k.ident_b = sb("ident_b", [128, 128], BF16)
    k.ones_f = sb("ones_f", [128, 128], F32)
    p.op("gpsimd", lambda e: e.memset(k.ones_f[:], 1.0), w=["ones_f"])
    p.op("gpsimd", lambda e: e.affine_select(out=k.ident_f[:], in_=k.ones_f[:], pattern=[[-1, 128]],
                                             compare_op=ALU.is_equal, fill=0.0, base=0, channel_multiplier=1),
         r=["ones_f"], w=["ident_f"])
    p.op("vector", lambda e: e.tensor_copy(out=k.ident_b[:], in_=k.ident_f[:]), r=["ident_f"], w=["ident_b"])
    p.barrier()

    def run_phase(fn, *a):
        with ExitStack() as sc:
            k.scope = sc
            fn(k, *a)
            p.barrier()
        k.scope = es

    if "proj" in phases or "all" in phases:
        run_phase(phase_proj)
    pd = dict(PHASES)
    for name in ("mem", "lru", "dsa", "merge", "peer"):
        if name in phases or "all" in phases:
            run_phase(pd[name])
    p.emit(es)
    es.close()
    nc._used_in = list(k.I.keys())
    nc._used_out = list(k.O.keys())
    return nc


def load_bcast(k, name, dram_row, n):
    t = k.sb(name, [128, n], F32)
    k.p.dma("sync", t[:], dram_row[0, :].partition_broadcast(128), w=[name])
    return t


def rms_tile(k, xt, n, g, gkey, xn_out, tag, col):
    p = k.p
    ss, rs, junk = k.ss, k.rs, k.junk
    xk = ("xt", tag)
    p.op("scalar", lambda e: e.activation(out=junk[:n, :], in_=xt[:n, :], func=AF.Square,
                                          accum_out=ss[:n, col:col + 1]), r=[xk], w=[("ss", col), "junk"])
    p.op("vector", lambda e: e.tensor_scalar(out=rs[:n, col:col + 1], in0=ss[:n, col:col + 1],
                                             scalar1=1.0 / D, scalar2=EPS, op0=ALU.mult, op1=ALU.add),
         r=[("ss", col)], w=[("rs", col)])
    p.op("scalar", lambda e: e.sqrt(out=rs[:n, col:col + 1], in_=rs[:n, col:col + 1]),
         r=[("rs", col)], w=[("rs", col)])
    p.op("vector", lambda e: e.reciprocal(out=rs[:n, col:col + 1], in_=rs[:n, col:col + 1]),
         r=[("rs", col)], w=[("rs", col)])
    p.op("vector", lambda e: e.scalar_tensor_tensor(out=xn_out[:n, :], in0=xt[:n, :], scalar=rs[:n, col:col + 1],
                                                    in1=g[:n, :], op0=ALU.mult, op1=ALU.mult),
         r=[xk, ("rs", col), gkey], w=[("xn", tag)])


def transpose_to_T(k, xn, n, tag, dstT, t0, pbase):
    p = k.p
    for half in range(2):
        bank = pbase + half
        pv = k.ps[bank][:].bitcast(BF16)
        for j in range(8):
            c = half * 8 + j
            p.op("tensor", lambda e, c=c, j=j, pv=pv: e.transpose(
                out=pv[:, j * 128:j * 128 + n], in_=xn[:n, c * 128:(c + 1) * 128], identity=k.ident_b[:n, :n]),
                r=[("xn", tag)], w=[("ps", bank)])
        dst = dstT[:, half * 8:half * 8 + 8, t0:t0 + n]
        srcv = pv.rearrange("p (c t) -> p c t", c=8)[:, :, :n]
        if half == 0:
            p.op("scalar", lambda e, dst=dst, srcv=srcv: e.copy(out=dst, in_=srcv),
                 r=[("ps", bank)], w=[("T", id(dstT), t0, half)])
        else:
            p.op("vector", lambda e, dst=dst, srcv=srcv: e.tensor_copy(out=dst, in_=srcv),
                 r=[("ps", bank)], w=[("T", id(dstT), t0, half)])


def Tkeys(dstT, t0s):
    return [("T", id(dstT), t0, h) for t0 in t0s for h in range(2)]


def norm_stats(k):
    k.ss = k.sb("ss", [128, 64], F32)
    k.rs = k.sb("rs", [128, 64], F32)
    k.junk = k.sb("junk", [128, D], BF16)


def linear_fm(k, xT, xkeys, wname, chunks, evac, tag):
    p = k.p
    stage = [k.sb("st_%s%d" % (tag, i), [128, 16, 128], F32) for i in range(2)]
    wb = [k.sb("wb_%s%d" % (tag, i), [128, 16, 128], BF16) for i in range(2)]
    W = k.I[wname]
    for ci, pieces in enumerate(chunks):
        b = ci % 2
        r0 = 0
        for (c0, ncl) in pieces:
            src = W[:, c0:c0 + ncl].rearrange("(c p) n -> p c n", p=128)
            p.dma("sync", stage[b][:, :, r0:r0 + ncl], src, w=[("st", tag, b)])
            r0 += ncl
        rows = r0
        if ci % 2 == 0:
            p.op("scalar", lambda e, b=b, rows=rows: e.copy(out=wb[b][:, :, :rows], in_=stage[b][:, :, :rows]),
                 r=[("st", tag, b)], w=[("wb", tag, b)])
        else:
            p.op("gpsimd", lambda e, b=b, rows=rows: e.tensor_copy(out=wb[b][:, :, :rows], in_=stage[b][:, :, :rows]),
                 r=[("st", tag, b)], w=[("wb", tag, b)])
        for si in range(5):
            t0 = si * 512
            tn = min(512, T - t0)
            bank = (ci * 5 + si) % 4
            for c in range(16):
                p.op("tensor", lambda e, b=b, c=c, rows=rows, t0=t0, tn=tn, bank=bank: e.matmul(
                    out=k.ps[bank][:rows, :tn], lhsT=wb[b][:, c, :rows], rhs=xT[:, c, t0:t0 + tn],
                    start=(c == 0), stop=(c == 15)), r=[("wb", tag, b)] + xkeys, w=[("ps", bank)])
            evac(ci, si, k.ps[bank], bank, rows, t0, tn)


def phase_proj(k):
    p, nc = k.p, k.nc
    norm_stats(k)
    xnT = k.sb("xnT", [128, 16, T], BF16)
    with k.sub():
        g = load_bcast(k, "g_bc", k.I["g_mix"], D)
        xt = [k.sb("xt%d" % i, [128, D], F32) for i in range(2)]
        xn = [k.sb("xnb%d" % i, [128, D], BF16) for i in range(2)]
        for tt in range(NT):
            b = tt % 2
            n = trows(tt)
            src = k.I["xp"][tt * 128:(tt + 1) * 128, :] if tt < 16 else k.I["xs"][:, :]
            p.dma("sync", xt[b][:n, :], src, w=[("xt", b)])
            rms_tile(k, xt[b], n, g, "g_bc", xn[b], b, tt)
            transpose_to_T(k, xn[b], n, b, xnT, tt * 128, b * 2)
    allx = []
    with k.sub():
        phase_proj_tok(k, xnT)
    phase_proj_fm(k, xnT, allx)


def phase_proj_tok(k, xnT):
    p, nc = k.p, k.nc

    stage = k.sb("kv_stage", [128, 16, 592], F32)
    wbk = k.sb("kv_wb", [128, 16, 592], BF16)
    p.dma("sync", stage[:, :, 0:512], k.I["w_in"][:, O_K:O_K + 512].rearrange("(c p) n -> p c n", p=128),
          w=["kvst0"])
    p.dma("sync", stage[:, :, 512:592], k.I["w_in"][:, O_IK:O_IK + 80].rearrange("(c p) n -> p c n", p=128),
          w=["kvst1"])
    p.op("scalar", lambda e: e.copy(out=wbk[:, :, 0:512], in_=stage[:, :, 0:512]), r=["kvst0"], w=["kvwb0"])
    p.op("vector", lambda e: e.tensor_copy(out=wbk[:, :, 512:592], in_=stage[:, :, 512:592]), r=["kvst1"], w=["kvwb1"])
    vtok = k.scratch("v_tok", [T, 256], BF16)
    iwtok = k.scratch("iw_tok", [T, 16], F32)
    ob = [k.sb("kv_ob%d" % i, [128, 592], F32) for i in range(2)]
    vb = [k.sb("kv_vb%d" % i, [128, 256], BF16) for i in range(2)]
    for tt in range(NT):
        n = trows(tt)
        b = tt % 2
        t0 = tt * 128
        xk = Tkeys(xnT, [t0])
        ba, bb = 4 + b * 2, 5 + b * 2
        for c in range(16):
            p.op("tensor", lambda e, c=c, n=n, t0=t0, ba=ba: e.matmul(
                out=k.ps[ba][:n, :512], lhsT=xnT[:, c, t0:t0 + n], rhs=wbk[:, c, 0:512],
                start=(c == 0), stop=(c == 15)), r=["kvwb0"], w=[("ps", ba)])
        for c in range(16):
            p.op("tensor", lambda e, c=c, n=n, t0=t0, bb=bb: e.matmul(
                out=k.ps[bb][:n, :80], lhsT=xnT[:, c, t0:t0 + n], rhs=wbk[:, c, 512:592],
                start=(c == 0), stop=(c == 15)), r=["kvwb1"], w=[("ps", bb)])
        p.op("scalar", lambda e, n=n, b=b, ba=ba: e.copy(out=ob[b][:n, 0:512], in_=k.ps[ba][:n, :512]),
             r=[("ps", ba)], w=[("ob", b, 0)])
        p.op("vector", lambda e, n=n, b=b, bb=bb: e.tensor_copy(out=ob[b][:n, 512:592], in_=k.ps[bb][:n, :80]),
             r=[("ps", bb)], w=[("ob", b, 1)])
        p.op("vector", lambda e, n=n, b=b: e.tensor_copy(out=vb[b][:n, :], in_=ob[b][:n, 256:512]),
             r=[("ob", b, 0)], w=[("vb", b)])
        p.dma("sync", vtok[t0:t0 + n, :], vb[b][:n, :], r=[("vb", b)])
        p.dma("sync", iwtok[t0:t0 + n, :], ob[b][:n, 576:592], r=[("ob", b, 1)])
        if tt < 16:
            rows = slice(t0, t0 + 128)
            p.dma("sync", k.O["k_p"][rows, :], ob[b][:, 0:256], r=[("ob", b, 0)])
            p.dma("sync", k.O["v_p"][rows, :], ob[b][:, 256:512], r=[("ob", b, 0)])
            p.dma("sync", k.O["ik_p"][rows, :], ob[b][:, 512:576], r=[("ob", b, 1)])
        else:
            p.dma("sync", k.O["k_s"][:, :], ob[b][:64, 0:256], r=[("ob", b, 0)])
            p.dma("sync", k.O["v_s"][:, :], ob[b][:64, 256:512], r=[("ob", b, 0)])
            p.dma("sync", k.O["ik_s"][:, :], ob[b][:64, 512:576], r=[("ob", b, 1)])
    for half in range(2):
        c0 = O_XB + half * 512
        p.dma("sync", stage[:, :, 0:512], k.I["w_in"][:, c0:c0 + 512].rearrange("(c p) n -> p c n", p=128),
              w=["kvst0"])
        p.op("scalar", lambda e: e.copy(out=wbk[:, :, 0:512], in_=stage[:, :, 0:512]), r=["kvst0"], w=["kvwb0"])
        for tt in (15, 16):
            n = trows(tt)
            b = tt % 2
            t0 = tt * 128
            ba = 4 + b * 2
            for c in range(16):
                p.op("tensor", lambda e, c=c, n=n, t0=t0, ba=ba: e.matmul(
                    out=k.ps[ba][:n, :512], lhsT=xnT[:, c, t0:t0 + n], rhs=wbk[:, c, 0:512],
                    start=(c == 0), stop=(c == 15)), r=["kvwb0"], w=[("ps", ba)])
            p.op("scalar", lambda e, n=n, b=b, ba=ba: e.copy(out=ob[b][:n, 0:512], in_=k.ps[ba][:n, :512]),
                 r=[("ps", ba)], w=[("ob", b, 0)])
            cs = slice(half * 512, half * 512 + 512)
            if tt == 15:
                p.dma("sync", k.O["conv_p"][:, cs], ob[b][125:128, 0:512], r=[("ob", b, 0)])
            else:
                srcv = ob[b][:64, 0:512]
                xbs = k.scratch("xb_s", [TS, 1024], F32)
                p.dma("sync", xbs[:, cs], srcv, r=[("ob", b, 0)], w=[("xbs", half)])
                p.dma("sync", k.O["conv_s"].rearrange("(b j) n -> b j n", j=3)[:, :, cs],
                      xbs.rearrange("(b t) n -> b t n", t=4)[:, 1:4, cs], r=[("xbs", half)])


def phase_proj_fm(k, xnT, allx):
    p, nc = k.p, k.nc
    projT = k.scratch("projT", [43, 128, T], BF16)
    chunks = []
    for c0 in list(range(O_Q, O_Q + 1024, 128)) + list(range(O_K, O_K + 256, 128)) + list(range(O_IQ, O_IQ + 1024, 128)):
        chunks.append([(c0, 128)])
    chunks.append([(O_IK, 64), (O_IK, 64)])
    for base in (O_XB, O_YB, O_QM):
        for c0 in range(base, base + 1024, 128):
            chunks.append([(c0, 128)])
    obf = [k.sb("pj_ob%d" % i, [128, T], BF16) for i in range(2)]

    def evac_proj(dst):
        def evac(ci, si, ps, bank, rows, t0, tn):
            b = ci % 2
            if si % 2 == 0:
                p.op("scalar", lambda e: e.copy(out=obf[b][:rows, t0:t0 + tn], in_=ps[:rows, :tn]),
                     r=[("ps", bank)], w=[("obf", b, si)])
            else:
                p.op("vector", lambda e: e.tensor_copy(out=obf[b][:rows, t0:t0 + tn], in_=ps[:rows, :tn]),
                     r=[("ps", bank)], w=[("obf", b, si)])
            if si == 4:
                p.dma("sync", dst[ci, :rows, :], obf[b][:rows, :], r=[("obf", b, s_) for s_ in range(5)])
        return evac
    linear_fm(k, xnT, allx, "w_in", chunks, evac_proj(projT), "pj")

    gT = k.scratch("gT", [48, 128, T], BF16)

    def evac_gate(ci, si, ps, bank, rows, t0, tn):
        b = ci % 2
        p.op("scalar", lambda e: e.activation(out=obf[b][:rows, t0:t0 + tn], in_=ps[:rows, :tn], func=AF.Sigmoid),
             r=[("ps", bank)], w=[("obf", b, si)])
        if si == 4:
            p.dma("sync", gT[ci, :rows, :], obf[b][:rows, :], r=[("obf", b, s_) for s_ in range(5)])
    linear_fm(k, xnT, allx, "w_gate", [[(c0, 128)] for c0 in range(0, 3 * D, 128)], evac_gate, "gt")


PHASES = []


_NC_CACHE = {}


def make_in_maps(inp):
    f = lambda a: np.ascontiguousarray(a, dtype=np.float32)
    shared = {
        "cache_k": f(inp["cache_k"]).reshape(2560 * 128, 256), "cache_v": f(inp["cache_v"]).reshape(2560 * 128, 256),
        "cache_ik": f(inp["cache_idx_k"]).reshape(2560 * 128, 64),
        "g_mix": f(inp["g_mix"]).reshape(1, D), "w_in": f(inp["w_in"]).reshape(D, N_IN),
        "conv_w": f(inp["conv_w"]).reshape(4, 1024), "conv_b": f(inp["conv_b"]).reshape(1, 1024),
        "w_rg": f(inp["w_rg"]).reshape(1024, 128), "b_rg": f(inp["b_rg"]).reshape(1, 1024),
        "w_ig": f(inp["w_ig"]).reshape(1024, 128), "b_ig": f(inp["b_ig"]).reshape(1, 1024),
        "lam": f(inp["lru_lambda"]).reshape(1, 1024), "g_mem": f(inp["g_mem"]).reshape(1, D),
        "w_mem_kv": f(inp["w_mem_kv"]).reshape(D, 2048), "w_gate": f(inp["w_gate"]).reshape(D, 3 * D),
        "w_br": f(inp["w_br"]).reshape(3 * 1024, D), "w_o": f(inp["w_o"]).reshape(D, D),
        "g_ffn": f(inp["g_ffn"]).reshape(1, D), "w_pq": f(inp["w_peer_q"]).reshape(D, 2048),
        "sub_keys": f(inp["peer_sub_keys"]).reshape(2 * 8 * 128, 128), "peer_u": f(inp["peer_u"]).reshape(16384, D),
        "peer_v": f(inp["peer_v"]).reshape(16384, D), "g_final": f(inp["g_final"]).reshape(1, D),
    }
    maps = []
    for c in range(NCORES):
        sl = slice(c * NS_SEQ, (c + 1) * NS_SEQ)
        m = dict(shared)
        m["xp"] = f(inp["x_prompt"][c])
        m["xs"] = f(inp["x_sample"][sl]).reshape(TS, D)
        m["ptab"] = np.ascontiguousarray(inp["page_table"][sl], dtype=np.int32)
        m["st_conv"] = f(inp["state_conv"][0, sl]).reshape(NS_SEQ * 3, 1024)
        m["st_lru"] = f(inp["state_lru"][0, sl]).reshape(NS_SEQ, 1024)
        m["cmk"] = f(inp["cache_mem_k"][0, sl]).reshape(NS_SEQ * 256, 1024)
        m["cmv"] = f(inp["cache_mem_v"][0, sl]).reshape(NS_SEQ * 256, 1024)
        m["mem"] = f(inp["mem_prompt"][c])
        maps.append(m)
    return maps


def assemble(res):
    g = lambda n: [np.asarray(r[n], dtype=np.float32) for r in res]
    y_p = np.stack(g("y_p"))
    y_s = np.concatenate(g("y_s")).reshape(128, DEC, D)
    k_p = np.stack(g("k_p")).reshape(1, 8, SEQ, 2, 128)
    v_p = np.stack(g("v_p")).reshape(1, 8, SEQ, 2, 128)
    ik_p = np.stack(g("ik_p")).reshape(1, 8, SEQ, 64)
    conv_p = np.stack(g("conv_p")).reshape(1, 8, 3, 1024)
    h_p = np.stack(g("h_p")).reshape(1, 8, 1024)
    mk_p = np.stack(g("mk_p")).reshape(1, 8, 256, 4, 256)
    mv_p = np.stack(g("mv_p")).reshape(1, 8, 256, 4, 256)
    k_s = np.concatenate(g("k_s")).reshape(1, 128, DEC, 2, 128)
    v_s = np.concatenate(g("v_s")).reshape(1, 128, DEC, 2, 128)
    ik_s = np.concatenate(g("ik_s")).reshape(1, 128, DEC, 64)
    conv_s = np.concatenate(g("conv_s")).reshape(1, 128, 3, 1024)
    h_s = np.concatenate(g("h_s")).reshape(1, 128, 1024)
    return (y_p, y_s, k_p, v_p, ik_p, conv_p, h_p, mk_p, mv_p, k_s, v_s, ik_s, conv_s, h_s)


def kernel(**inputs):
    if "nc" not in _NC_CACHE:
        _NC_CACHE["nc"] = build()
    nc = _NC_CACHE["nc"]
    in_maps = [{n: m[n] for n in nc._used_in} for m in make_in_maps(inputs)]
    res = run_bass_kernel_spmd(nc, in_maps, core_ids=list(range(NCORES)))
    outs = []
    for r in res.results:
        d = dict(r)
        for n, s in OUT_SPECS:
            if n not in d:
                d[n] = np.zeros(s, np.float32)
        outs.append(d)
    return assemble(outs)


def phase_mem(k):
    p, nc = k.p, k.nc
    projT = k.scratch("projT", [43, 128, T], BF16)
    oT = k.scratch("oT", [3, 8, 128, T], BF16)
    MS = 256 ** -0.5
    norm_stats(k)
    memT = k.sb("memT", [128, 16, 256], BF16)
    mkT = k.sb("mkT", [128, 8, 256], BF16)
    mvb = k.sb("mvb", [128, 2, 1024], BF16)
    qmT = k.sb("qmT", [128, 8, T], BF16)
    p.dma("sync", qmT[:], projT[35:43].rearrange("c p t -> p c t"), w=["qmT"])
    with k.sub():
        g = load_bcast(k, "gm_bc", k.I["g_mem"], D)
        xt = [k.sb("mxt%d" % i, [128, D], F32) for i in range(2)]
        xn = [k.sb("mxn%d" % i, [128, D], BF16) for i in range(2)]
        for mt in range(2):
            p.dma("sync", xt[mt][:, :], k.I["mem"][mt * 128:(mt + 1) * 128, :], w=[("xt", mt)])
            rms_tile(k, xt[mt], 128, g, "gm_bc", xn[mt], mt, mt)
            transpose_to_T(k, xn[mt], 128, mt, memT, mt * 128, mt * 2)
    with k.sub():
        stage = k.sb("mst", [128, 16, 512], F32)
        wb = k.sb("mwb", [128, 16, 512], BF16)
        ob = [k.sb("mob%d" % i, [128, 512], F32) for i in range(2)]
        for cb in range(4):
            p.dma("sync", stage[:], k.I["w_mem_kv"][:, cb * 512:(cb + 1) * 512].rearrange("(c p) n -> p c n", p=128),
                  w=["mst"])
            p.op("scalar", lambda e: e.copy(out=wb[:], in_=stage[:]), r=["mst"], w=["mwb"])
            for mt in range(2):
                bank = mt
                for c in range(16):
                    p.op("tensor", lambda e, c=c, mt=mt, bank=bank: e.matmul(
                        out=k.ps[bank][:, :], lhsT=memT[:, c, mt * 128:(mt + 1) * 128], rhs=wb[:, c, :],
                        start=(c == 0), stop=(c == 15)), r=["mwb"], w=[("ps", bank)])
                p.op("scalar", lambda e, mt=mt, bank=bank: e.copy(out=ob[mt][:, :], in_=k.ps[bank][:, :]),
                     r=[("ps", bank)], w=[("mob", mt)])
                dst = k.O["mk_p"] if cb < 2 else k.O["mv_p"]
                cs = slice((cb % 2) * 512, (cb % 2) * 512 + 512)
                p.dma("sync", dst[mt * 128:(mt + 1) * 128, cs], ob[mt][:, :], r=[("mob", mt)])
                if cb >= 2:
                    p.op("vector", lambda e, mt=mt, cs=cs: e.tensor_copy(out=mvb[:, mt, cs], in_=ob[mt][:, :]),
                         r=[("mob", mt)], w=["mvb"])
            if cb < 2:
                for j in range(4):
                    bank = 2 + j % 2
                    for c in range(16):
                        p.op("tensor", lambda e, c=c, j=j, bank=bank: e.matmul(
                            out=k.ps[bank][:, :256], lhsT=wb[:, c, j * 128:(j + 1) * 128], rhs=memT[:, c, :],
                            start=(c == 0), stop=(c == 15)), r=["mwb"], w=[("ps", bank)])
                    p.op("vector", lambda e, j=j, bank=bank, cb=cb: e.tensor_copy(
                        out=mkT[:, cb * 4 + j, :], in_=k.ps[bank][:, :256]), r=[("ps", bank)], w=["mkT"])
    with k.sub():
        mem_attend_loop(k, qmT, mkT, mvb, oT, MS, [(tt * 128, 128) for tt in range(16)], "p")
    with k.sub():
        cm = [k.sb("cmk%d" % i, [128, 2, 1024], F32) for i in range(2)]
        cv = [k.sb("cmv%d" % i, [128, 2, 1024], F32) for i in range(2)]
        mkTs = [k.sb("mkTs%d" % i, [128, 8, 256], BF16) for i in range(2)]
        mvbs = [k.sb("mvbs%d" % i, [128, 2, 1024], BF16) for i in range(2)]
        st = mem_attend_state(k, "s")
        for b in range(NS_SEQ):
            i = b % 2
            p.dma("sync", cm[i][:], k.I["cmk"][b * 256:(b + 1) * 256, :].rearrange("(m p) n -> p m n", p=128),
                  w=[("cm", i)])
            p.dma("sync", cv[i][:], k.I["cmv"][b * 256:(b + 1) * 256, :].rearrange("(m p) n -> p m n", p=128),
                  w=[("cv", i)])
            p.op("gpsimd", lambda e, i=i: e.tensor_copy(out=mvbs[i][:], in_=cv[i][:]), r=[("cv", i)], w=[("mvbs", i)])
            for c in range(8):
                bank = 6 + c % 2
                for mt in range(2):
                    p.op("tensor", lambda e, c=c, mt=mt, bank=bank, i=i: e.transpose(
                        out=k.ps[bank][:, mt * 128:(mt + 1) * 128], in_=cm[i][:, mt, c * 128:(c + 1) * 128],
                        identity=k.ident_f[:]), r=[("cm", i)], w=[("ps", bank)])
                p.op("scalar", lambda e, c=c, bank=bank, i=i: e.copy(out=mkTs[i][:, c, :], in_=k.ps[bank][:, :256]),
                     r=[("ps", bank)], w=[("mkTs", i)])
            mem_attend_tile(k, st, qmT, mkTs[i], mvbs[i], oT, MS, SEQ + 4 * b, 4, [("mkTs", i), ("mvbs", i)])


def mem_attend_state(k, tag):
    st = K()
    st.mx = k.sb("ma_mx" + tag, [128, 4], F32)
    st.rsum = k.sb("ma_rs" + tag, [128, 4], F32)
    st.P = k.sb("ma_P" + tag, [128, 4, 256], BF16)
    st.PT = k.sb("ma_PT" + tag, [128, 8, 128], BF16)
    st.oc = k.sb("ma_oc" + tag, [128, 8, 128], BF16)
    return st


def mem_attend_loop(k, qmT, mkT, mvb, oT, MS, tiles, tag):
    st = mem_attend_state(k, tag)
    for (t0, n) in tiles:
        mem_attend_tile(k, st, qmT, mkT, mvb, oT, MS, t0, n, ["mkT", "mvb"])


def mem_attend_tile(k, st, qmT, mkT, mvb, oT, MS, t0, n, kvkeys):
    p = k.p
    for h in range(4):
        bank = h // 2
        for kc in range(2):
            p.op("tensor", lambda e, h=h, kc=kc, bank=bank: e.matmul(
                out=k.ps[bank][:n, (h % 2) * 256:(h % 2) * 256 + 256], lhsT=qmT[:, 2 * h + kc, t0:t0 + n],
                rhs=mkT[:, 2 * h + kc, :], start=(kc == 0), stop=(kc == 1)),
                r=["qmT"] + kvkeys, w=[("ps", bank)])
    for bank in range(2):
        p.op("vector", lambda e, bank=bank: e.tensor_reduce(
            out=st.mx[:n, bank * 2:bank * 2 + 2], in_=k.ps[bank][:n, :].rearrange("p (h m) -> p h m", h=2),
            axis=AX.X, op=ALU.max), r=[("ps", bank)], w=[("mx", bank)])
        p.op("vector", lambda e, bank=bank: e.tensor_scalar(
            out=st.mx[:n, bank * 2:bank * 2 + 2], in0=st.mx[:n, bank * 2:bank * 2 + 2], scalar1=-MS, scalar2=None,
            op0=ALU.mult), r=[("mx", bank)], w=[("mx", bank)])
    for h in range(4):
        bank = h // 2
        p.op("scalar", lambda e, h=h, bank=bank: e.activation(
            out=st.P[:n, h, :], in_=k.ps[bank][:n, (h % 2) * 256:(h % 2) * 256 + 256], func=AF.Exp,
            bias=st.mx[:n, h:h + 1], scale=MS, accum_out=st.rsum[:n, h:h + 1]),
            r=[("ps", bank), ("mx", bank)], w=[("P", h), ("rsum", h)])
    p.op("vector", lambda e: e.reciprocal(out=st.rsum[:n, :], in_=st.rsum[:n, :]),
         r=[("rsum", h) for h in range(4)], w=["rinv"])
    for h in range(4):
        p.op("vector", lambda e, h=h: e.tensor_scalar(out=st.P[:n, h, :], in0=st.P[:n, h, :],
                                                      scalar1=st.rsum[:n, h:h + 1], scalar2=None, op0=ALU.mult),
             r=["rinv", ("P", h)], w=[("P", h)])
    pv = k.ps[2][:].bitcast(BF16)
    for h in range(4):
        for mt in range(2):
            j = h * 2 + mt
            p.op("tensor", lambda e, h=h, mt=mt, j=j: e.transpose(
                out=pv[:, j * 128:j * 128 + n], in_=st.P[:n, h, mt * 128:(mt + 1) * 128],
                identity=k.ident_b[:n, :n]), r=[("P", h)], w=[("ps", 2)])
    p.op("scalar", lambda e: e.copy(out=st.PT[:, :, :n], in_=pv.rearrange("p (j t) -> p j t", j=8)[:, :, :n]),
         r=[("ps", 2)], w=["PT"])
    for h in range(4):
        for c2 in range(2):
            j = h * 2 + c2
            bank = 3 + j // 4
            for mt in range(2):
                p.op("tensor", lambda e, h=h, c2=c2, mt=mt, j=j, bank=bank: e.matmul(
                    out=k.ps[bank][:, (j % 4) * 128:(j % 4) * 128 + n],
                    lhsT=mvb[:, mt, h * 256 + c2 * 128:h * 256 + c2 * 128 + 128], rhs=st.PT[:, h * 2 + mt, :n],
                    start=(mt == 0), stop=(mt == 1)), r=["PT"] + kvkeys, w=[("ps", bank)])
    for half in range(2):
        bank = 3 + half
        eng = "scalar" if half == 0 else "vector"
        srcv = k.ps[bank][:].rearrange("p (j t) -> p j t", j=4)[:, :, :n]
        dst = st.oc[:, half * 4:half * 4 + 4, :n]
        if half == 0:
            p.op("scalar", lambda e, dst=dst, srcv=srcv: e.copy(out=dst, in_=srcv), r=[("ps", bank)], w=[("oc", half)])
        else:
            p.op("vector", lambda e, dst=dst, srcv=srcv: e.tensor_copy(out=dst, in_=srcv), r=[("ps", bank)],
                 w=[("oc", half)])
    p.dma("sync", oT[2, :, :, t0:t0 + n].rearrange("c p t -> p c t"), st.oc[:, :, :n], r=[("oc", 0), ("oc", 1)])


PHASES.append(("mem", phase_mem))


def gelu_mul(k, y, h, out, n, tmp1, tmp2, keys_r, key_w):
    p = k.p
    p.op("vector", lambda e: e.tensor_tensor(out=tmp1, in0=y, in1=y, op=ALU.mult), r=keys_r, w=[("g1", key_w)])
    p.op("vector", lambda e: e.tensor_scalar(out=tmp1, in0=tmp1, scalar1=0.044715, scalar2=1.0, op0=ALU.mult,
                                             op1=ALU.add), r=[("g1", key_w)], w=[("g1", key_w)])
    p.op("vector", lambda e: e.tensor_tensor(out=tmp1, in0=tmp1, in1=y, op=ALU.mult), r=[("g1", key_w)] + keys_r,
         w=[("g1", key_w)])
    p.op("scalar", lambda e: e.activation(out=tmp2, in_=tmp1, func=AF.Sigmoid, scale=1.5957691216057308),
         r=[("g1", key_w)], w=[("g2", key_w)])
    p.op("vector", lambda e: e.tensor_tensor(out=tmp2, in0=tmp2, in1=y, op=ALU.mult), r=[("g2", key_w)] + keys_r,
         w=[("g2", key_w)])
    p.op("vector", lambda e: e.tensor_tensor(out=out, in0=tmp2, in1=h, op=ALU.mult), r=[("g2", key_w)] + keys_r,
         w=[key_w])


def phase_lru(k):
    p, nc = k.p, k.nc
    projT = k.scratch("projT", [43, 128, T], BF16)
    oT = k.scratch("oT", [3, 8, 128, T], BF16)
    cw = k.sb("l_cw", [128, 8, 4], F32)
    pv = k.sb("l_pv", [128, 5, 8], F32)
    sc = k.sb("l_sc", [128, 2, 8], F32)
    for j in range(4):
        p.dma("sync", cw[:, :, j], k.I["conv_w"][j:j + 1, :].rearrange("o (n q) -> q (o n)", q=128), w=["cw"],
              allow_slow_non_contiguous=True)
    for i, nm in enumerate(("conv_b", "b_rg", "b_ig", "lam")):
        p.dma("sync", pv[:, i, :], k.I[nm].rearrange("o (n q) -> q (o n)", q=128), w=[("pv", i)],
              allow_slow_non_contiguous=True)
    p.op("scalar", lambda e: e.activation(out=pv[:, 4, :], in_=pv[:, 3, :], func=AF.Exp, scale=-1.0),
         r=[("pv", 3)], w=[("pv", 4)])
    p.op("scalar", lambda e: e.activation(out=pv[:, 4, :], in_=pv[:, 4, :], func=AF.Ln, bias=1.0),
         r=[("pv", 4)], w=[("pv", 4)])
    p.op("vector", lambda e: e.tensor_scalar(out=sc[:, 0, :], in0=pv[:, 4, :], scalar1=-8.0, scalar2=None,
                                             op0=ALU.mult), r=[("pv", 4)], w=["sc0"])
    p.op("vector", lambda e: e.tensor_scalar(out=sc[:, 1, :], in0=pv[:, 4, :], scalar1=-16.0, scalar2=None,
                                             op0=ALU.mult), r=[("pv", 4)], w=["sc1"])
    wst = k.sb("l_wst", [128, 2, 8, 128], F32)
    wg = k.sb("l_wg", [128, 2, 8, 128], BF16)
    p.dma("sync", wst[:, 0], k.I["w_rg"].rearrange("(n i) j -> i n j", i=128), w=["wst0"])
    p.dma("sync", wst[:, 1], k.I["w_ig"].rearrange("(n i) j -> i n j", i=128), w=["wst1"])
    p.op("vector", lambda e: e.tensor_copy(out=wg[:], in_=wst[:]), r=["wst0", "wst1"], w=["wg"])
    scv = k.sb("l_scv", [48, 1024], F32)
    slr = k.sb("l_slr", [16, 1024], F32)
    p.dma("sync", scv[:], k.I["st_conv"][:, :], w=["scv"])
    p.dma("sync", slr[:], k.I["st_lru"][:, :], w=["slr"])
    hl_p = k.sb("l_hlp", [128, 8], F32)
    hl_s = k.sb("l_hls", [128, 8, 16], F32)
    NP = SEQ
    xbb = [k.sb("l_xbb%d" % i, [128, T], BF16) for i in range(2)]
    ybb = [k.sb("l_ybb%d" % i, [128, T], BF16) for i in range(2)]
    xpad = k.sb("l_xpad", [128, 3 + NP], F32)
    xps = k.sb("l_xps", [128, 16, 7], F32)
    xc = k.sb("l_xc", [128, T], F32)
    xcb = k.sb("l_xcb", [128, T], BF16)
    rr = k.sb("l_r", [128, T], F32)
    ii = k.sb("l_i", [128, T], F32)
    aa = k.sb("l_a", [128, T], F32)
    uu = k.sb("l_u", [128, T], F32)
    hh = k.sb("l_h", [128, T], F32)
    yf = k.sb("l_yf", [128, T], F32)
    ob = [k.sb("l_ob%d" % i, [128, T], BF16) for i in range(2)]
    h0 = k.sb("l_h0", [128, 16], F32)
    tmpa = k.sb("l_tmpa", [128, 16], F32)
    p.op("vector", lambda e: e.memset(xpad[:, 0:3], 0.0), w=["xpad0"])
    for n in range(8):
        b = n % 2
        p.dma("sync", xbb[b][:], projT[19 + n], w=[("xbb", b)])
        p.dma("sync", ybb[b][:], projT[27 + n], w=[("ybb", b)])
        p.op("vector", lambda e, b=b: e.tensor_copy(out=xpad[:, 3:3 + NP], in_=xbb[b][:, 0:NP]),
             r=[("xbb", b), "xpad0"], w=["xpad"])
        p.op("tensor", lambda e, n=n: e.transpose(out=k.ps[4][:, 0:48], in_=scv[:, n * 128:(n + 1) * 128],
                                                  identity=k.ident_f[:48, :48]), r=["scv"], w=[("ps", 4)])
        p.op("tensor", lambda e, n=n: e.transpose(out=k.ps[5][:, 0:16], in_=slr[:, n * 128:(n + 1) * 128],
                                                  identity=k.ident_f[:16, :16]), r=["slr"], w=[("ps", 5)])
        p.op("vector", lambda e: e.tensor_copy(out=xps[:, :, 0:3], in_=k.ps[4][:, 0:48].rearrange("p (b j) -> p b j", j=3)),
             r=[("ps", 4)], w=["xps0"])
        p.op("vector", lambda e, b=b: e.tensor_copy(out=xps[:, :, 3:7],
                                                    in_=xbb[b][:, NP:T].rearrange("p (b t) -> p b t", t=4)),
             r=[("xbb", b)], w=["xps1"])
        p.op("vector", lambda e: e.tensor_copy(out=h0[:], in_=k.ps[5][:, 0:16]), r=[("ps", 5)], w=["h0"])
        p.op("scalar", lambda e, n=n: e.activation(out=xc[:, 0:NP], in_=xpad[:, 3:3 + NP], func=AF.Identity,
                                                   bias=pv[:, 0, n:n + 1], scale=cw[:, n, 3:4]),
             r=["xpad", "cw", ("pv", 0)], w=["xc_p"])
        p.op("scalar", lambda e, n=n: e.activation(out=xc[:, NP:T].rearrange("p (b t) -> p b t", t=4),
                                                   in_=xps[:, :, 3:7], func=AF.Identity,
                                                   bias=pv[:, 0, n:n + 1], scale=cw[:, n, 3:4]),
             r=["xps0", "xps1", "cw", ("pv", 0)], w=["xc_s"])
        for j in range(3):
            p.op("vector", lambda e, n=n, j=j: e.scalar_tensor_tensor(
                out=xc[:, 0:NP], in0=xpad[:, j:j + NP], scalar=cw[:, n, j:j + 1], in1=xc[:, 0:NP],
                op0=ALU.mult, op1=ALU.add), r=["xpad", "xc_p"], w=["xc_p"])
            p.op("vector", lambda e, n=n, j=j: e.scalar_tensor_tensor(
                out=xc[:, NP:T].rearrange("p (b t) -> p b t", t=4), in0=xps[:, :, j:j + 4], scalar=cw[:, n, j:j + 1],
                in1=xc[:, NP:T].rearrange("p (b t) -> p b t", t=4), op0=ALU.mult, op1=ALU.add),
                r=["xps0", "xps1", "xc_s"], w=["xc_s"])
        p.op("gpsimd", lambda e: e.tensor_copy(out=xcb[:], in_=xc[:]), r=["xc_p", "xc_s"], w=["xcb"])
        for gi, dst in ((0, rr), (1, ii)):
            for si in range(5):
                t0 = si * 512
                tn = min(512, T - t0)
                bank = (gi * 5 + si) % 4
                p.op("tensor", lambda e, gi=gi, n=n, t0=t0, tn=tn, bank=bank: e.matmul(
                    out=k.ps[bank][:, :tn], lhsT=wg[:, gi, n, :], rhs=xcb[:, t0:t0 + tn], start=True, stop=True),
                    r=["wg", "xcb"], w=[("ps", bank)])
                p.op("scalar", lambda e, gi=gi, n=n, t0=t0, tn=tn, bank=bank, dst=dst: e.activation(
                    out=dst[:, t0:t0 + tn], in_=k.ps[bank][:, :tn], func=AF.Sigmoid, bias=pv[:, 1 + gi, n:n + 1]),
                    r=[("ps", bank), ("pv", 1 + gi)], w=[("gate", gi)])
        p.op("scalar", lambda e, n=n: e.activation(out=aa[:], in_=rr[:], func=AF.Exp, scale=sc[:, 0, n:n + 1]),
             r=[("gate", 0), "sc0"], w=["aa"])
        p.op("scalar", lambda e, n=n: e.activation(out=uu[:], in_=rr[:], func=AF.Exp, scale=sc[:, 1, n:n + 1]),
             r=[("gate", 0), "sc1"], w=["uu"])
        p.op("scalar", lambda e: e.activation(out=uu[:], in_=uu[:], func=AF.Sqrt, bias=1.0, scale=-1.0),
             r=["uu"], w=["uu"])
        p.op("vector", lambda e: e.tensor_tensor(out=uu[:], in0=uu[:], in1=ii[:], op=ALU.mult),
             r=["uu", ("gate", 1)], w=["uu"])
        p.op("vector", lambda e: e.tensor_tensor(out=uu[:], in0=uu[:], in1=xc[:], op=ALU.mult),
             r=["uu", "xc_p", "xc_s"], w=["uu"])
        p.op("vector", lambda e: e.tensor_tensor_scan(out=hh[:, 0:NP], data0=aa[:, 0:NP], data1=uu[:, 0:NP],
                                                      initial=0.0, op0=ALU.mult, op1=ALU.add),
             r=["aa", "uu"], w=["hh_p"])
        hs = hh[:, NP:T].rearrange("p (b t) -> p b t", t=4)
        as_ = aa[:, NP:T].rearrange("p (b t) -> p b t", t=4)
        us = uu[:, NP:T].rearrange("p (b t) -> p b t", t=4)
        for t in range(4):
            prev = h0[:, :] if t == 0 else hs[:, :, t - 1]
            p.op("vector", lambda e, t=t, prev=prev: e.tensor_tensor(out=tmpa[:], in0=as_[:, :, t], in1=prev,
                                                                     op=ALU.mult),
                 r=["aa", "h0", "hh_s"], w=["tmpa"])
            p.op("vector", lambda e, t=t: e.tensor_tensor(out=hs[:, :, t], in0=tmpa[:], in1=us[:, :, t], op=ALU.add),
                 r=["tmpa", "uu"], w=["hh_s"])
        p.op("vector", lambda e, n=n: e.tensor_copy(out=hl_p[:, n:n + 1], in_=hh[:, NP - 1:NP]), r=["hh_p"], w=["hlp"])
        p.op("vector", lambda e, n=n: e.tensor_copy(out=hl_s[:, n, :], in_=hs[:, :, 3]), r=["hh_s"], w=["hls"])
        p.op("gpsimd", lambda e, b=b: e.tensor_copy(out=yf[:], in_=ybb[b][:]), r=[("ybb", b)], w=["yf"])
        gelu_mul(k, yf[:], hh[:], ob[b][:], T, rr[:], ii[:], ["yf", "hh_p", "hh_s", ("gate", 0), ("gate", 1), "uu"],
                 ("lob", b))
        p.dma("sync", oT[1, n], ob[b][:], r=[("lob", b)])
    hrow = k.sb("l_hrow", [16, 1024], F32)
    hrp = k.sb("l_hrp", [8, 128], F32)
    p.op("tensor", lambda e: e.transpose(out=k.ps[6][:8, 0:128], in_=hl_p[:, :], identity=k.ident_f[:, :]),
         r=["hlp"], w=[("ps", 6)])
    p.op("vector", lambda e: e.tensor_copy(out=hrp[:], in_=k.ps[6][:8, 0:128]), r=[("ps", 6)], w=["hrp"])
    p.dma("sync", k.O["h_p"].rearrange("o (n q) -> (o n) q", q=128), hrp[:], r=["hrp"])
    for n in range(8):
        bank = 6 + n % 2
        p.op("tensor", lambda e, n=n, bank=bank: e.transpose(out=k.ps[bank][:16, 0:128], in_=hl_s[:, n, :],
                                                             identity=k.ident_f[:, :]), r=["hls"], w=[("ps", bank)])
        p.op("vector", lambda e, n=n, bank=bank: e.tensor_copy(out=hrow[:, n * 128:(n + 1) * 128],
                                                               in_=k.ps[bank][:16, 0:128]),
             r=[("ps", bank)], w=["hrow"])
    p.dma("sync", k.O["h_s"][:, :], hrow[:], r=["hrow"])


PHASES.append(("lru", phase_lru))


def phase_merge(k):
    p, nc = k.p, k.nc
    oT = k.scratch("oT", [3, 8, 128, T], BF16)
    gT = k.scratch("gT", [48, 128, T], BF16)
    x1 = k.scratch("x1", [T, D], F32)
    mT = k.sb("mT", [128, 16, T], F32)
    with k.sub():
        on = k.sb("mg_on", [128, 8, T], BF16)
        wst = [k.sb("mg_wst%d" % i, [128, 8, 128], F32) for i in range(2)]
        wb = [k.sb("mg_wb%d" % i, [128, 8, 128], BF16) for i in range(2)]
        gt = [k.sb("mg_gt%d" % i, [128, T], BF16) for i in range(2)]
        tmp = [k.sb("mg_tmp%d" % i, [128, 512], F32) for i in range(2)]
        it = 0
        for n in range(3):
            p.dma("sync", on[:], oT[n].rearrange("c p t -> p c t"), w=["on"])
            for cc in range(16):
                b = it % 2
                it += 1
                p.dma("sync", wst[b][:], k.I["w_br"][n * 1024:(n + 1) * 1024, cc * 128:(cc + 1) * 128]
                      .rearrange("(c q) j -> q c j", q=128), w=[("wst", b)])
                p.dma("sync", gt[b][:], gT[n * 16 + cc], w=[("gt", b)])
                p.op("scalar", lambda e, b=b: e.copy(out=wb[b][:], in_=wst[b][:]), r=[("wst", b)], w=[("wb", b)])
                for si in range(5):
                    t0 = si * 512
                    tn = min(512, T - t0)
                    bank = si % 4
                    for c in range(8):
                        p.op("tensor", lambda e, b=b, c=c, t0=t0, tn=tn, bank=bank: e.matmul(
                            out=k.ps[bank][:, :tn], lhsT=wb[b][:, c, :], rhs=on[:, c, t0:t0 + tn],
                            start=(c == 0), stop=(c == 7)), r=[("wb", b), "on"], w=[("ps", bank)])
                    if n == 0:
                        p.op("vector", lambda e, b=b, cc=cc, t0=t0, tn=tn, bank=bank: e.tensor_tensor(
                            out=mT[:, cc, t0:t0 + tn], in0=k.ps[bank][:, :tn], in1=gt[b][:, t0:t0 + tn], op=ALU.mult),
                            r=[("ps", bank), ("gt", b)], w=[("mT", cc, si)])
                    else:
                        tb = si % 2
                        p.op("vector", lambda e, b=b, tb=tb, t0=t0, tn=tn, bank=bank: e.tensor_tensor(
                            out=tmp[tb][:, :tn], in0=k.ps[bank][:, :tn], in1=gt[b][:, t0:t0 + tn], op=ALU.mult),
                            r=[("ps", bank), ("gt", b)], w=[("tmp", tb)])
                        p.op("gpsimd", lambda e, cc=cc, tb=tb, t0=t0, tn=tn: e.tensor_tensor(
                            out=mT[:, cc, t0:t0 + tn], in0=mT[:, cc, t0:t0 + tn], in1=tmp[tb][:, :tn], op=ALU.add),
                            r=[("tmp", tb), ("mT", cc, si)], w=[("mT", cc, si)])
    with k.sub():
        stage = k.sb("wo_st", [128, 16, 512], F32)
        wo = k.sb("wo_wb", [128, 16, 512], BF16)
        mb = [k.sb("wo_mb%d" % i, [128, 16, 128], BF16) for i in range(2)]
        xr = [k.sb("wo_xr%d" % i, [128, 512], F32) for i in range(2)]
        xo = [k.sb("wo_xo%d" % i, [128, 512], F32) for i in range(2)]
        for cb in range(4):
            cs = slice(cb * 512, (cb + 1) * 512)
            p.dma("sync", stage[:], k.I["w_o"][:, cs].rearrange("(c q) n -> q c n", q=128), w=["wost"])
            p.op("scalar", lambda e: e.copy(out=wo[:], in_=stage[:]), r=["wost"], w=["wo"])
            for tt in range(NT):
                n = trows(tt)
                t0 = tt * 128
                b = tt % 2
                bank = tt % 4
                src = k.I["xp"][t0:t0 + 128, cs] if tt < 16 else k.I["xs"][:, cs]
                p.dma("sync", xr[b][:n, :], src, w=[("xr", b)])
                p.op("gpsimd", lambda e, b=b, t0=t0, n=n: e.tensor_copy(out=mb[b][:, :, :n], in_=mT[:, :, t0:t0 + n]),
                     w=[("mb", b)])
                for c in range(16):
                    p.op("tensor", lambda e, b=b, c=c, n=n, bank=bank: e.matmul(
                        out=k.ps[bank][:n, :], lhsT=mb[b][:, c, :n], rhs=wo[:, c, :], start=(c == 0), stop=(c == 15)),
                        r=[("mb", b), "wo"], w=[("ps", bank)])
                p.op("vector", lambda e, b=b, n=n, bank=bank: e.tensor_tensor(
                    out=xo[b][:n, :], in0=k.ps[bank][:n, :], in1=xr[b][:n, :], op=ALU.add),
                    r=[("ps", bank), ("xr", b)], w=[("xo", b)])
                p.dma("sync", x1[t0:t0 + n, cs], xo[b][:n, :], r=[("xo", b)])


PHASES.append(("merge", phase_merge))


ATT_SCALE = 128 ** -0.5


def slices(n_k):
    out = []
    s0 = 0
    while s0 < n_k:
        w = min(512, n_k - s0)
        out.append((s0, w))
        s0 += w
    return out


def topk_thr(k, Iv, Wv, n, n_k, m8, tag, ikeys=()):
    p = k.p
    for r in range(32):
        src = Iv if r == 0 else Wv
        p.op("vector", lambda e, src=src: e.max(out=m8[:n, :], in_=src[:n, :n_k]),
             r=[("I", tag), ("W", tag)] + list(ikeys), w=[("m8", tag)])
        if r < 31:
            p.op("vector", lambda e, src=src: e.match_replace(out=Wv[:n, :n_k], in_to_replace=m8[:n, :],
                                                              in_values=src[:n, :n_k], imm_value=NEG),
                 r=[("m8", tag), ("I", tag)] + list(ikeys), w=[("W", tag)])


def attend(k, st, n, n_k, lhsT, kT, kv, vt, mbv, mbkey, out_ps, out_bank, rkeys):
    p = k.p
    sl = slices(n_k)
    for i, (s0, w) in enumerate(sl):
        bank = i % 4
        p.op("tensor", lambda e, s0=s0, w=w, bank=bank: e.matmul(out=k.ps[bank][:n, :w], lhsT=lhsT,
                                                                 rhs=kT[:, kv, s0:s0 + w], start=True, stop=True),
             r=rkeys, w=[("ps", bank)])
        p.op("vector", lambda e, s0=s0, w=w, bank=bank: e.scalar_tensor_tensor(
            out=st.L[:n, s0:s0 + w], in0=k.ps[bank][:n, :w], scalar=ATT_SCALE, in1=mbv[:n, s0:s0 + w],
            op0=ALU.mult, op1=ALU.add), r=[("ps", bank), mbkey], w=[("L", st.tag)])
    p.op("vector", lambda e: e.tensor_reduce(out=st.mx[:n, :], in_=st.L[:n, :n_k], axis=AX.X, op=ALU.max),
         r=[("L", st.tag)], w=[("mx", st.tag)])
    p.op("vector", lambda e: e.tensor_scalar(out=st.mx[:n, :], in0=st.mx[:n, :], scalar1=-1.0, scalar2=None,
                                             op0=ALU.mult), r=[("mx", st.tag)], w=[("mx", st.tag)])
    p.op("scalar", lambda e: e.activation(out=st.P[:n, :n_k], in_=st.L[:n, :n_k], func=AF.Exp, bias=st.mx[:n, :],
                                          accum_out=st.rs[:n, :]), r=[("L", st.tag), ("mx", st.tag)], w=[("P", st.tag), ("rs", st.tag)])
    p.op("vector", lambda e: e.reciprocal(out=st.rs[:n, :], in_=st.rs[:n, :]), r=[("rs", st.tag)], w=[("rs", st.tag)])
    p.op("vector", lambda e: e.tensor_scalar(out=st.P[:n, :n_k], in0=st.P[:n, :n_k], scalar1=st.rs[:n, :],
                                             scalar2=None, op0=ALU.mult), r=[("rs", st.tag), ("P", st.tag)], w=[("P", st.tag)])
    nj = (n_k + 127) // 128
    for j in range(nj):
        w = min(128, n_k - j * 128)
        bank = (4, 5, 3)[j // 8]
        pv = k.ps[bank][:].bitcast(BF16)
        p.op("tensor", lambda e, j=j, w=w, pv=pv: e.transpose(out=pv[:w, (j % 8) * 128:(j % 8) * 128 + n],
                                                              in_=st.P[:n, j * 128:j * 128 + w],
                                                              identity=k.ident_b[:n, :n]),
             r=[("P", st.tag)], w=[("ps", bank)])
    for half in range((nj + 7) // 8):
        bank = (4, 5, 3)[half]
        pv = k.ps[bank][:].bitcast(BF16)
        cnt = min(8, nj - half * 8)
        rws = min(128, n_k - (half * 8 + cnt - 1) * 128) if cnt == 1 else 128
        srcv = pv.rearrange("p (j t) -> p j t", j=8)[:rws, :cnt, :n]
        dst = st.PT[:rws, half * 8:half * 8 + cnt, :n]
        if half == 0:
            p.op("scalar", lambda e, dst=dst, srcv=srcv: e.copy(out=dst, in_=srcv), r=[("ps", bank)], w=[("PT", half, st.tag)])
        else:
            p.op("vector", lambda e, dst=dst, srcv=srcv: e.tensor_copy(out=dst, in_=srcv), r=[("ps", bank)],
                 w=[("PT", half, st.tag)])
    for j in range(nj):
        w = min(128, n_k - j * 128)
        p.op("tensor", lambda e, j=j, w=w: e.matmul(out=out_ps, lhsT=vt[:w, j, kv * 128:(kv + 1) * 128],
                                                    rhs=st.PT[:w, j, :n], start=(j == 0), stop=(j == nj - 1)),
             r=[("PT", 0, st.tag), ("PT", 1, st.tag), ("PT", 2, st.tag)] + rkeys, w=[("ps", out_bank)])


def phase_dsa(k):
    p, nc = k.p, k.nc
    projT = k.scratch("projT", [43, 128, T], BF16)
    oT = k.scratch("oT", [3, 8, 128, T], BF16)
    vtok = k.scratch("v_tok", [T, 256], BF16)
    iwtok = k.scratch("iw_tok", [T, 16], F32)
    NK = SEQ + DEC
    qT = k.sb("d_qT", [128, 8, T], BF16)
    iqT = k.sb("d_iqT", [128, 8, T], BF16)
    kT = k.sb("d_kT", [128, 2, T], BF16)
    ikT = k.sb("d_ikT", [128, T], BF16)
    p.dma("sync", qT[:], projT[0:8].rearrange("c p t -> p c t"), w=["qT"])
    p.dma("sync", iqT[:], projT[10:18].rearrange("c p t -> p c t"), w=["iqT"])
    p.dma("sync", kT[:], projT[8:10].rearrange("c p t -> p c t"), w=["kT"])
    p.dma("sync", ikT[:], projT[18], w=["ikT"])
    st = K()
    st.tag = 0
    st.L = k.sb("d_L", [128, NK], F32)
    st.P = k.sb("d_P", [128, NK], BF16)
    st.PT = k.sb("d_PT", [128, 17, 128], BF16)
    st.mx = k.sb("d_mx", [128, 1], F32)
    st.rs = k.sb("d_rs", [128, 1], F32)
    Ib = k.sb("d_I", [128, NK], F32)
    Wb = k.sb("d_W", [128, NK], F32)
    mb = k.sb("d_mb", [128, NK], F32)
    rl = [k.sb("d_rl%d" % i, [128, 512], F32) for i in range(4)]
    m8 = k.sb("d_m8", [128, 8], F32)
    oc = [k.sb("d_oc%d" % i, [128, 8, 128], BF16) for i in range(2)]

    def indexer(n, n_k, tcol0, ikv, iw_fn, Iv, rk):
        it = 0
        for hi in range(16):
            c, half = hi // 2, hi % 2
            prt = slice(half * 64, half * 64 + 64)
            for i, (s0, w) in enumerate(slices(n_k)):
                bank = (hi % 2) * 4 + i % 4
                b = it % 4
                it += 1
                p.op("tensor", lambda e, c=c, prt=prt, s0=s0, w=w, bank=bank: e.matmul(
                    out=k.ps[bank][:n, :w], lhsT=iqT[prt, c, tcol0:tcol0 + n], rhs=ikv[prt, s0:s0 + w],
                    start=True, stop=True), r=["iqT"] + rk, w=[("ps", bank)])
                p.op("scalar", lambda e, w=w, bank=bank, b=b: e.activation(out=rl[b][:n, :w], in_=k.ps[bank][:n, :w],
                                                                            func=AF.Relu),
                     r=[("ps", bank)], w=[("rl", b)])
                if hi == 0:
                    p.op("vector", lambda e, s0=s0, w=w, b=b, hi=hi: e.tensor_scalar(
                        out=Iv[:n, s0:s0 + w], in0=rl[b][:n, :w], scalar1=iw_fn(hi), scalar2=None, op0=ALU.mult),
                        r=[("rl", b), "iw"], w=[("Isl", i)])
                else:
                    p.op("vector", lambda e, s0=s0, w=w, b=b, hi=hi: e.scalar_tensor_tensor(
                        out=Iv[:n, s0:s0 + w], in0=rl[b][:n, :w], scalar=iw_fn(hi), in1=Iv[:n, s0:s0 + w],
                        op0=ALU.mult, op1=ALU.add), r=[("rl", b), "iw", ("Isl", i)], w=[("Isl", i)])
        return [("Isl", i) for i in range(len(slices(n_k)))]

    with k.sub():
        vt = k.sb("d_vt", [128, 16, 256], BF16)
        iw = k.sb("d_iw", [128, 16, 16], F32)
        p.dma("sync", vt[:], vtok[0:SEQ, :].rearrange("(j q) n -> q j n", q=128), w=["vt"])
        p.dma("sync", iw[:], iwtok[0:SEQ, :].rearrange("(j q) n -> q j n", q=128), w=["iw"])
        st2 = K()
        st2.tag = 1
        st2.L = k.sb("d_L2", [128, SEQ], F32)
        st2.P = k.sb("d_P2", [128, SEQ], BF16)
        st2.PT = k.sb("d_PT2", [128, 16, 128], BF16)
        st2.mx = k.sb("d_mx2", [128, 1], F32)
        st2.rs = k.sb("d_rs2", [128, 1], F32)
        sts = (st, st2)
        for qi in range(16):
            n_k = 128 * (qi + 1)
            t0 = qi * 128
            ikeys = indexer(128, n_k, t0, ikT, lambda hi, qi=qi: iw[:, qi, hi:hi + 1], Ib, ["ikT"])
            p.op("gpsimd", lambda e, t0=t0: e.affine_select(out=Ib[:, t0:t0 + 128], in_=Ib[:, t0:t0 + 128],
                                                            pattern=[[-1, 128]], compare_op=ALU.is_ge, fill=NEG,
                                                            base=0, channel_multiplier=1),
                 r=ikeys, w=[("I", "p")] + ikeys)
            if qi >= 2:
                topk_thr(k, Ib, Wb, 128, n_k, m8, "p", ikeys)
                p.op("vector", lambda e, n_k=n_k: e.tensor_scalar(out=mb[:, :n_k], in0=Ib[:, :n_k], scalar1=m8[:, 7:8],
                                                                  scalar2=NEG, op0=ALU.is_lt, op1=ALU.mult),
                     r=[("I", "p"), ("m8", "p")] + ikeys, w=["mb"])
            else:
                p.op("vector", lambda e, n_k=n_k: e.tensor_scalar(out=mb[:, :n_k], in0=Ib[:, :n_k], scalar1=-1.0e29,
                                                                  scalar2=NEG, op0=ALU.is_lt, op1=ALU.mult),
                     r=[("I", "p")] + ikeys, w=["mb"])
            ob = oc[qi % 2]
            for h in range(8):
                bank = 6 + h // 4
                attend(k, sts[h % 2], 128, n_k, qT[:, h, t0:t0 + 128], kT, h // 4, vt, mb, "mb",
                       k.ps[bank][:, (h % 4) * 128:(h % 4) * 128 + 128], bank, ["qT", "kT", "vt"])
                if h % 4 == 3:
                    g = h // 4
                    srcv = k.ps[bank][:].rearrange("p (j t) -> p j t", j=4)
                    if g == 0:
                        p.op("scalar", lambda e, ob=ob, srcv=srcv: e.copy(out=ob[:, 0:4, :], in_=srcv),
                             r=[("ps", bank)], w=[("oc", qi % 2, 0)])
                    else:
                        p.op("vector", lambda e, ob=ob, srcv=srcv: e.tensor_copy(out=ob[:, 4:8, :], in_=srcv),
                             r=[("ps", bank)], w=[("oc", qi % 2, 1)])
            p.dma("sync", oT[0, :, :, t0:t0 + 128].rearrange("c q t -> q c t"), ob[:],
                  r=[("oc", qi % 2, 0), ("oc", qi % 2, 1)])

    with k.sub():
        ptb = k.sb("s_ptb", [128, 256], I32)
        ptf = k.sb("s_ptf", [128, 256], F32)
        idx = k.sb("s_idx", [128, 256], U32)
        iop = k.sb("s_iop", [128, 1], F32)
        p.dma("sync", ptb[:], k.I["ptab"].rearrange("b g -> (b g)").partition_broadcast(128), w=["ptb"])
        p.op("gpsimd", lambda e: e.iota(iop[:], pattern=[[0, 1]], base=0, channel_multiplier=1,
                                        allow_small_or_imprecise_dtypes=True), w=["iop"])
        p.op("vector", lambda e: e.tensor_copy(out=ptf[:], in_=ptb[:]), r=["ptb"], w=["ptf"])
        p.op("vector", lambda e: e.tensor_scalar(out=ptf[:], in0=ptf[:], scalar1=128.0, scalar2=iop[:, 0:1],
                                                 op0=ALU.mult, op1=ALU.add), r=["ptf", "iop"], w=["ptf"])
        p.op("vector", lambda e: e.tensor_copy(out=idx[:], in_=ptf[:]), r=["ptf"], w=["idx"])
        iws = k.sb("s_iw", [4, 16, 16], F32)
        p.dma("sync", iws[:], iwtok[SEQ:T, :].rearrange("(b t) h -> t b h", t=4), w=["iw"])
        Iall = k.sb("s_Iall", [64, NK], F32)
        Wall = Wb
        mball = mb
        sub1 = k.sub()
        sub1.__enter__()
        pg = [k.sb("s_pg%d" % i, [128, 128], F32) for i in range(4)]
        ikTb = [k.sb("s_ikTb%d" % i, [128, NK], BF16) for i in range(2)]
        for b in range(NS_SEQ):
            ib = ikTb[b % 2]
            for g in range(16):
                col = b * 16 + g
                t = pg[g % 4]
                for hf in range(2):
                    p.op("gpsimd", lambda e, t=t, hf=hf, col=col: e.indirect_dma_start(
                        out=t[:, hf * 64:(hf + 1) * 64], out_offset=None, in_=k.I["cache_ik"][:, :],
                        in_offset=bass.IndirectOffsetOnAxis(ap=idx[:, col:col + 1], axis=0)),
                        r=["idx"], w=[("pg", g % 4, hf)], dma=True)
                bank = 4 + (g // 4) % 2
                p.op("tensor", lambda e, t=t, g=g, bank=bank: e.transpose(
                    out=k.ps[bank][:, (g % 4) * 128:(g % 4) * 128 + 128], in_=t[:, :], identity=k.ident_f[:, :]),
                    r=[("pg", g % 4, 0), ("pg", g % 4, 1)], w=[("ps", bank)])
                if g % 4 == 3:
                    g0 = g - 3
                    p.op("scalar", lambda e, ib=ib, g0=g0, bank=bank: e.copy(out=ib[:, g0 * 128:g0 * 128 + 512],
                                                                            in_=k.ps[bank][:, :]),
                         r=[("ps", bank)], w=[("ikTb", b % 2)])
            p.op("vector", lambda e, ib=ib, b=b: e.tensor_copy(out=ib[:, SEQ:NK], in_=ikT[:, SEQ + 4 * b:SEQ + 4 * b + 4]),
                 r=["ikT"], w=[("ikTb", b % 2)])
            ikeys = indexer(4, NK, SEQ + 4 * b, ib, lambda hi, b=b: iws[:, b, hi:hi + 1], Ib, [("ikTb", b % 2)])
            p.op("gpsimd", lambda e: e.affine_select(out=Ib[:4, SEQ:NK], in_=Ib[:4, SEQ:NK], pattern=[[-1, 4]],
                                                     compare_op=ALU.is_ge, fill=NEG, base=0, channel_multiplier=1),
                 r=ikeys, w=[("I", "s")] + ikeys)
            p.dma("sync", Iall[4 * b:4 * b + 4, :], Ib[:4, :], r=[("I", "s")] + ikeys, w=[("I", "all")])
        sub1.__exit__(None, None, None)
        topk_thr(k, Iall, Wall, 64, NK, m8, "all")
        p.op("vector", lambda e: e.tensor_scalar(out=mball[:64, :], in0=Iall[:, :], scalar1=m8[:64, 7:8], scalar2=NEG,
                                                 op0=ALU.is_lt, op1=ALU.mult), r=[("I", "all"), ("m8", "all")],
             w=["mball"])
        kTb = [k.sb("s_kTb%d" % i, [128, 2, NK], BF16) for i in range(2)]
        vtb = [k.sb("s_vtb%d" % i, [128, 17, 256], BF16) for i in range(2)]
        kpg = [k.sb("s_kpg%d" % i, [128, 256], F32) for i in range(4)]
        vpg = [k.sb("s_vpg%d" % i, [128, 256], F32) for i in range(4)]
        mb16 = [k.sb("s_mb16%d" % i, [16, NK], F32) for i in range(2)]
        ocs = [k.sb("s_ocs%d" % i, [128, 8, 4], BF16) for i in range(2)]
        qs16 = [k.sb("s_qs16%d" % i, [128, 32], BF16) for i in range(2)]
        for b in range(NS_SEQ):
            kb, vb = kTb[b % 2], vtb[b % 2]
            for g in range(16):
                col = b * 16 + g
                tk, tv = kpg[g % 4], vpg[g % 4]
                p.op("gpsimd", lambda e, tk=tk, col=col: e.indirect_dma_start(
                    out=tk[:, :], out_offset=None, in_=k.I["cache_k"][:, :],
                    in_offset=bass.IndirectOffsetOnAxis(ap=idx[:, col:col + 1], axis=0)),
                    r=["idx"], w=[("kpg", g % 4)], dma=True)
                p.op("gpsimd", lambda e, tv=tv, col=col: e.indirect_dma_start(
                    out=tv[:, :], out_offset=None, in_=k.I["cache_v"][:, :],
                    in_offset=bass.IndirectOffsetOnAxis(ap=idx[:, col:col + 1], axis=0)),
                    r=["idx"], w=[("vpg", g % 4)], dma=True)
                p.op("vector", lambda e, vb=vb, g=g, tv=tv: e.tensor_copy(out=vb[:, g, :], in_=tv[:, :]),
                     r=[("vpg", g % 4)], w=[("vtb", b % 2)])
                bank = 4 + g % 2
                for kv in range(2):
                    p.op("tensor", lambda e, tk=tk, kv=kv, bank=bank: e.transpose(
                        out=k.ps[bank][:, kv * 128:(kv + 1) * 128], in_=tk[:, kv * 128:(kv + 1) * 128],
                        identity=k.ident_f[:, :]), r=[("kpg", g % 4)], w=[("ps", bank)])
                p.op("scalar", lambda e, kb=kb, g=g, bank=bank: e.copy(
                    out=kb[:, :, g * 128:(g + 1) * 128], in_=k.ps[bank][:, 0:256].rearrange("p (v s) -> p v s", v=2)),
                    r=[("ps", bank)], w=[("kTb", b % 2)])
            tc0 = SEQ + 4 * b
            p.op("vector", lambda e, kb=kb, tc0=tc0: e.tensor_copy(out=kb[:, :, SEQ:NK], in_=kT[:, :, tc0:tc0 + 4]),
                 r=["kT"], w=[("kTb", b % 2)])
            p.dma("sync", vb[:4, 16, :], vtok[tc0:tc0 + 4, :], w=[("vtb", b % 2)])
            m16 = mb16[b % 2]
            for i in range(4):
                p.dma("sync", m16[4 * i:4 * i + 4, :], mball[4 * b:4 * b + 4, :], r=["mball"], w=[("mb16", b % 2)])
            ob = ocs[b % 2]
            qs = qs16[b % 2]
            p.op("vector", lambda e, qs=qs, tc0=tc0: e.tensor_copy(
                out=qs[:, :].rearrange("p (h t) -> p h t", t=4), in_=qT[:, :, tc0:tc0 + 4]),
                r=["qT"], w=[("qs16", b % 2)])
            for kv in range(2):
                bank = 6 + kv
                attend(k, st, 16, NK, qs[:, kv * 16:kv * 16 + 16], kb, kv, vb, m16, ("mb16", b % 2),
                       k.ps[bank][:, 0:16], bank, [("qs16", b % 2), ("kTb", b % 2), ("vtb", b % 2)])
                p.op("vector", lambda e, ob=ob, kv=kv, bank=bank: e.tensor_copy(
                    out=ob[:, kv * 4:kv * 4 + 4, :], in_=k.ps[bank][:, 0:16].rearrange("p (h t) -> p h t", h=4)),
                    r=[("ps", bank)], w=[("ocs", b % 2, kv)])
            p.dma("sync", oT[0, :, :, tc0:tc0 + 4].rearrange("c q t -> q c t"), ob[:],
                  r=[("ocs", b % 2, 0), ("ocs", b % 2, 1)])


PHASES.append(("dsa", phase_dsa))


def bc(ap, shape):
    return ap.to_broadcast(shape)


def phase_peer(k):
    p, nc = k.p, k.nc
    x1 = k.scratch("x1", [T, D], F32)
    xn2f = k.scratch("xn2f", [T, D], F32)
    norm_stats(k)
    qpT = k.sb("pe_qpT", [128, 16, T], BF16)
    skT = k.sb("pe_skT", [128, 16, 128], BF16)
    with k.sub():
        xn2T = k.sb("pe_xn2T", [128, 16, T], BF16)
        with k.sub():
            g = load_bcast(k, "gf_bc", k.I["g_ffn"], D)
            xt = [k.sb("pxt%d" % i, [128, D], F32) for i in range(2)]
            xn = [k.sb("pxn%d" % i, [128, D], BF16) for i in range(2)]
            xf = [k.sb("pxf%d" % i, [128, D], F32) for i in range(2)]
            for tt in range(NT):
                b = tt % 2
                n = trows(tt)
                t0 = tt * 128
                p.dma("sync", xt[b][:n, :], x1[t0:t0 + n, :], w=[("xt", b)])
                rms_tile(k, xt[b], n, g, "gf_bc", xn[b], b, tt)
                p.op("gpsimd", lambda e, b=b, n=n: e.tensor_copy(out=xf[b][:n, :], in_=xn[b][:n, :]),
                     r=[("xn", b)], w=[("xf", b)])
                p.dma("sync", xn2f[t0:t0 + n, :], xf[b][:n, :], r=[("xf", b)])
                transpose_to_T(k, xn[b], n, b, xn2T, t0, b * 2)
            skt = [k.sb("pskt%d" % i, [128, 128], F32) for i in range(2)]
            for j in range(16):
                h, pp = j // 2, j % 2
                r0 = (pp * 8 + h) * 128
                p.dma("sync", skt[j % 2][:], k.I["sub_keys"][r0:r0 + 128, :], w=[("skt", j % 2)])
                bank = 4 + j % 2
                p.op("tensor", lambda e, j=j, bank=bank: e.transpose(out=k.ps[bank][:, 0:128], in_=skt[j % 2][:],
                                                                     identity=k.ident_f[:]),
                     r=[("skt", j % 2)], w=[("ps", bank)])
                p.op("vector", lambda e, j=j, bank=bank: e.tensor_copy(out=skT[:, j, :], in_=k.ps[bank][:, 0:128]),
                     r=[("ps", bank)], w=["skT"])

        def evac_q(ci, si, ps, bank, rows, t0, tn):
            if si % 2 == 0:
                p.op("scalar", lambda e: e.copy(out=qpT[:, ci, t0:t0 + tn], in_=ps[:, :tn]), r=[("ps", bank)],
                     w=[("qpT", ci, si)])
            else:
                p.op("vector", lambda e: e.tensor_copy(out=qpT[:, ci, t0:t0 + tn], in_=ps[:, :tn]), r=[("ps", bank)],
                     w=[("qpT", ci, si)])
        linear_fm(k, xn2T, [], "w_pq", [[(c0, 128)] for c0 in range(0, D, 128)], evac_q, "pq")

    with k.sub():
        sc = k.sb("pe_sc", [128, 16, 128], F32)
        scw = k.sb("pe_scw", [128, 16, 128], F32)
        vv = k.sb("pe_v", [128, 16, 16], F32)
        ix = k.sb("pe_ix", [128, 16, 16], U32)
        ixf = k.sb("pe_ixf", [128, 16, 16], F32)
        cand = k.sb("pe_cand", [128, 8, 256], F32)
        candw = k.sb("pe_candw", [128, 8, 256], F32)
        tv = k.sb("pe_tv", [128, 8, 16], F32)
        pos = k.sb("pe_pos", [128, 8, 16], U32)
        pa = k.sb("pe_pa", [128, 8, 16], U32)
        pb = k.sb("pe_pb", [128, 8, 16], U32)
        paf = k.sb("pe_paf", [128, 8, 16], F32)
        pbf = k.sb("pe_pbf", [128, 8, 16], F32)
        eq = k.sb("pe_eq", [128, 8, 16, 16], F32)
        sel = k.sb("pe_sel", [128, 2, 8, 16], F32)
        ef = k.sb("pe_ef", [128, 128], F32)
        eidx = k.sb("pe_eidx", [128, 128], U32)
        gw = k.sb("pe_gw", [128, 8, 16], F32)
        gs = k.sb("pe_gs", [128, 8], F32)
        act = k.sb("pe_act", [128, 128], F32)
        ga = k.sb("pe_ga", [128, 128], F32)
        t1 = k.sb("pe_t1", [128, 128], F32)
        t2 = k.sb("pe_t2", [128, 128], F32)
        io16 = k.sb("pe_io16", [128, 16], F32)
        gb = [k.sb("pe_gb%d" % i, [128, D], F32) for i in range(4)]
        xq = k.sb("pe_xq", [128, D], F32)
        x1t = k.sb("pe_x1t", [128, D], F32)
        acc = k.sb("pe_acc", [128, D], F32)
        junkf = k.sb("pe_junkf", [128, D], F32)
        yo = k.sb("pe_yo", [128, D], F32)
        gfin = load_bcast(k, "gfin_bc", k.I["g_final"], D)
        p.op("gpsimd", lambda e: e.iota(io16[:], pattern=[[1, 16]], base=0, channel_multiplier=0,
                                        allow_small_or_imprecise_dtypes=True), w=["io16"])
        S4 = [128, 8, 16, 16]
        def tile_body(tt, n, t0):
            p.dma("sync", xq[:n, :], xn2f[t0:t0 + n, :], w=["xq"])
            p.dma("sync", x1t[:n, :], x1[t0:t0 + n, :], w=[("xt", "f")])
            for j in range(16):
                bank = j // 4
                p.op("tensor", lambda e, j=j, bank=bank: e.matmul(
                    out=k.ps[bank][:n, (j % 4) * 128:(j % 4) * 128 + 128], lhsT=qpT[:, j, t0:t0 + n], rhs=skT[:, j, :],
                    start=True, stop=True), r=["skT"], w=[("ps", bank)])
            for bank in range(4):
                dst = sc[:n, bank * 4:bank * 4 + 4, :]
                srcv = k.ps[bank][:n, :].rearrange("p (j q) -> p j q", j=4)
                if bank % 2 == 0:
                    p.op("scalar", lambda e, dst=dst, srcv=srcv: e.copy(out=dst, in_=srcv), r=[("ps", bank)],
                         w=[("sc", bank)])
                else:
                    p.op("vector", lambda e, dst=dst, srcv=srcv: e.tensor_copy(out=dst, in_=srcv), r=[("ps", bank)],
                         w=[("sc", bank)])
            for j in range(16):
                kj = [("sc", j // 4)]
                p.op("vector", lambda e, j=j: e.max(out=vv[:n, j, 0:8], in_=sc[:n, j, :]), r=kj, w=[("vv", j)])
                p.op("vector", lambda e, j=j: e.max_index(out=ix[:n, j, 0:8], in_max=vv[:n, j, 0:8], in_values=sc[:n, j, :]),
                     r=kj + [("vv", j)], w=[("ix", j)])
                p.op("vector", lambda e, j=j: e.match_replace(out=scw[:n, j, :], in_to_replace=vv[:n, j, 0:8],
                                                              in_values=sc[:n, j, :], imm_value=NEG),
                     r=kj + [("vv", j)], w=[("scw", j)])
                p.op("vector", lambda e, j=j: e.max(out=vv[:n, j, 8:16], in_=scw[:n, j, :]), r=[("scw", j)],
                     w=[("vv", j)])
                p.op("vector", lambda e, j=j: e.max_index(out=ix[:n, j, 8:16], in_max=vv[:n, j, 8:16],
                                                          in_values=scw[:n, j, :]),
                     r=[("scw", j), ("vv", j)], w=[("ix", j)])
            allv = [("vv", j) for j in range(16)]
            alli = [("ix", j) for j in range(16)]
            p.op("vector", lambda e: e.tensor_copy(out=ixf[:n], in_=ix[:n]), r=alli, w=["ixf"])
            v4 = vv[:n].rearrange("p (h two) a -> p h two a", two=2)
            i4 = ixf[:n].rearrange("p (h two) a -> p h two a", two=2)
            S4n = [n, 8, 16, 16]
            p.op("vector", lambda e: e.tensor_tensor(
                out=cand[:n].rearrange("p h (a b) -> p h a b", a=16), in0=bc(v4[:, :, 0, :].unsqueeze(3), S4n),
                in1=bc(v4[:, :, 1, :].unsqueeze(2), S4n), op=ALU.add), r=allv, w=["cand"])
            for h in range(8):
                p.op("vector", lambda e, h=h: e.max(out=tv[:n, h, 0:8], in_=cand[:n, h, :]), r=["cand"], w=[("tv", h)])
                p.op("vector", lambda e, h=h: e.max_index(out=pos[:n, h, 0:8], in_max=tv[:n, h, 0:8],
                                                          in_values=cand[:n, h, :]), r=["cand", ("tv", h)],
                     w=[("pos", h)])
                p.op("vector", lambda e, h=h: e.match_replace(out=candw[:n, h, :], in_to_replace=tv[:n, h, 0:8],
                                                              in_values=cand[:n, h, :], imm_value=NEG),
                     r=["cand", ("tv", h)], w=[("candw", h)])
                p.op("vector", lambda e, h=h: e.max(out=tv[:n, h, 8:16], in_=candw[:n, h, :]), r=[("candw", h)],
                     w=[("tv", h)])
                p.op("vector", lambda e, h=h: e.max_index(out=pos[:n, h, 8:16], in_max=tv[:n, h, 8:16],
                                                          in_values=candw[:n, h, :]), r=[("candw", h), ("tv", h)],
                     w=[("pos", h)])
            allt = [("tv", h) for h in range(8)]
            allp = [("pos", h) for h in range(8)]
            p.op("vector", lambda e: e.tensor_tensor(out=gw[:n], in0=tv[:n], in1=bc(tv[:n, :, 0:1], [n, 8, 16]),
                                                     op=ALU.subtract), r=allt, w=["gw"])
            p.op("scalar", lambda e: e.activation(out=gw[:n], in_=gw[:n], func=AF.Exp), r=["gw"], w=["gw"])
            p.op("vector", lambda e: e.tensor_reduce(out=gs[:n, :], in_=gw[:n], axis=AX.X, op=ALU.add), r=["gw"],
                 w=["gs"])
            p.op("vector", lambda e: e.reciprocal(out=gs[:n, :], in_=gs[:n, :]), r=["gs"], w=["gs"])
            p.op("vector", lambda e: e.tensor_tensor(out=gw[:n], in0=gw[:n], in1=bc(gs[:n, :].unsqueeze(2), [n, 8, 16]),
                                                     op=ALU.mult), r=["gs", "gw"], w=["gw"])
            p.op("vector", lambda e: e.tensor_single_scalar(out=pa[:n], in_=pos[:n], scalar=4,
                                                            op=ALU.logical_shift_right), r=allp, w=["pa"])
            p.op("vector", lambda e: e.tensor_single_scalar(out=pb[:n], in_=pos[:n], scalar=15, op=ALU.bitwise_and),
                 r=allp, w=["pb"])
            p.op("vector", lambda e: e.tensor_copy(out=paf[:n], in_=pa[:n]), r=["pa"], w=["paf"])
            p.op("vector", lambda e: e.tensor_copy(out=pbf[:n], in_=pb[:n]), r=["pb"], w=["pbf"])
            for w_, pf in ((0, paf), (1, pbf)):
                p.op("vector", lambda e, pf=pf: e.tensor_tensor(
                    out=eq[:n], in0=bc(pf[:n].unsqueeze(3), S4n), in1=bc(io16[:n, :].unsqueeze(1).unsqueeze(1), S4n),
                    op=ALU.is_equal), r=["paf", "pbf", "io16", "sel"], w=["eq"])
                p.op("vector", lambda e, w_=w_: e.tensor_tensor(
                    out=eq[:n], in0=eq[:n], in1=bc(i4[:, :, w_, :].unsqueeze(2), S4n), op=ALU.mult),
                    r=["eq", "ixf"], w=["eq"])
                p.op("vector", lambda e, w_=w_: e.tensor_reduce(out=sel[:n, w_], in_=eq[:n], axis=AX.X, op=ALU.add),
                     r=["eq"], w=["sel"])
            p.op("vector", lambda e: e.scalar_tensor_tensor(
                out=ef[:n, :], in0=sel[:n, 0].rearrange("p h k -> p (h k)"), scalar=128.0,
                in1=sel[:n, 1].rearrange("p h k -> p (h k)"), op0=ALU.mult, op1=ALU.add), r=["sel"], w=["ef"])
            p.op("vector", lambda e: e.tensor_copy(out=eidx[:n, :], in_=ef[:n, :]), r=["ef"], w=["eidx"])
            p.op("vector", lambda e: e.memset(act[:], 0.0), r=["ga"], w=[("act", s_) for s_ in range(128)])
            for s_ in range(128):
                b = s_ % 4
                p.op("gpsimd", lambda e, s_=s_, b=b: e.indirect_dma_start(
                    out=gb[b][:n, :], out_offset=None, in_=k.I["peer_u"][:, :],
                    in_offset=bass.IndirectOffsetOnAxis(ap=eidx[:n, s_:s_ + 1], axis=0)),
                    r=["eidx"], w=[("gb", b)], dma=True)
                p.op("vector", lambda e, s_=s_, b=b: e.scalar_tensor_tensor(
                    out=junkf[:n, :], in0=gb[b][:n, :], scalar=1.0, in1=xq[:n, :], op0=ALU.mult, op1=ALU.mult,
                    accum_out=act[:n, s_:s_ + 1]), r=[("gb", b), "xq"], w=[("act", s_), "junkf"])
            gelu_mul(k, act[:n, :], gw[:n].rearrange("p h k -> p (h k)"), ga[:n, :], n, t1[:n, :], t2[:n, :],
                     [("act", s_) for s_ in range(128)] + ["gw"], "ga")
            for s_ in range(128):
                b = s_ % 4
                p.op("gpsimd", lambda e, s_=s_, b=b: e.indirect_dma_start(
                    out=gb[b][:n, :], out_offset=None, in_=k.I["peer_v"][:, :],
                    in_offset=bass.IndirectOffsetOnAxis(ap=eidx[:n, s_:s_ + 1], axis=0)),
                    r=["eidx"], w=[("gb", b)], dma=True)
                if s_ == 0:
                    p.op("vector", lambda e, b=b: e.scalar_tensor_tensor(
                        out=acc[:n, :], in0=gb[b][:n, :], scalar=ga[:n, 0:1], in1=x1t[:n, :], op0=ALU.mult,
                        op1=ALU.add), r=[("gb", b), "ga", ("xt", "f")], w=["acc"])
                else:
                    p.op("vector", lambda e, s_=s_, b=b: e.scalar_tensor_tensor(
                        out=acc[:n, :], in0=gb[b][:n, :], scalar=ga[:n, s_:s_ + 1], in1=acc[:n, :], op0=ALU.mult,
                        op1=ALU.add), r=[("gb", b), "ga", "acc"], w=["acc"])
            if tt == 0 and "dbg" in DEBUG_IO:
                dbg = k.scratch("dbg", [6, 128, 128], F32)
                p.dma("sync", dbg[0], ef[:, :], r=["ef", "eidx"])
                p.dma("sync", dbg[1], gw[:].rearrange("p h k -> p (h k)"), r=["gw", "ga"])
                p.dma("sync", dbg[2], act[:, :], r=["ga"])
                p.dma("sync", dbg[3], ga[:, :], r=["ga"])
                p.dma("sync", dbg[4], tv[:].rearrange("p h k -> p (h k)"), r=allt + ["gw"])
                p.dma("sync", dbg[5], paf[:].rearrange("p h k -> p (h k)"), r=["paf", "eq"])
            p.op("vector", lambda e: e.tensor_copy(out=x1t[:n, :], in_=acc[:n, :]), r=["acc"], w=[("xt", "f")])
            rms_tile(k, x1t, n, gfin, "gfin_bc", yo, "f", 32 + tt)
            dst = k.O["y_p"][t0:t0 + 128, :] if tt < 16 else k.O["y_s"][:, :]
            p.dma("sync", dst, yo[:n, :], r=[("xn", "f")])

        for tt in range(NT):
            tile_body(tt, trows(tt), tt * 128)


PHASES.append(("peer", phase_peer))
```

```python
import numpy as np
from contextlib import ExitStack
import concourse.bass as bass
import concourse.mybir as mybir
from concourse.bass_utils import run_bass_kernel_spmd

F32 = mybir.dt.float32
BF16 = mybir.dt.bfloat16
I32 = mybir.dt.int32
U32 = mybir.dt.uint32
AF = mybir.ActivationFunctionType
ALU = mybir.AluOpType
AX = mybir.AxisListType

NCORES = 8
D = 2048
SEQ = 2048
NS_SEQ = 16
DEC = 4
TS = NS_SEQ * DEC
T = SEQ + TS
NT = 17
N_IN = 5712
EPS = 1e-6
NEG = -1.0e30
O_Q, O_K, O_V, O_IQ, O_IK, O_IW, O_XB, O_YB, O_QM = 0, 1024, 1280, 1536, 2560, 2624, 2640, 3664, 4688


def trows(tt):
    return 128 if tt < 16 else 64


class _Op:
    __slots__ = ("eng", "idx", "fn", "dma", "deps", "signaled", "cum", "sem", "tgt", "prev_tgt", "k")

    def __init__(self, eng, idx, fn, dma):
        self.eng = eng
        self.idx = idx
        self.fn = fn
        self.dma = dma
        self.deps = []
        self.signaled = False
        self.cum = 0
        self.sem = None
        self.tgt = 0
        self.prev_tgt = 0
        self.k = 0


class _Res:
    __slots__ = ("lw", "rd")

    def __init__(self):
        self.lw = None
        self.rd = []


class Prog:
    ENG = ("tensor", "vector", "scalar", "gpsimd", "sync")
    NS = 12

    def __init__(self, nc):
        self.nc = nc
        self.ops = {e: [] for e in self.ENG}
        self.res = {}
        self.ndma = {e: 0 for e in self.ENG}

    def _r(self, k):
        r = self.res.get(k)
        if r is None:
            r = self.res[k] = _Res()
        return r

    def op(self, eng, fn, r=(), w=(), dma=False):
        o = _Op(eng, len(self.ops[eng]), fn, dma)
        deps = {}
        for k in r:
            rr = self._r(k)
            if rr.lw is not None:
                deps[id(rr.lw)] = (rr.lw, True)
        for k in w:
            rr = self._r(k)
            if rr.lw is not None:
                deps[id(rr.lw)] = (rr.lw, True)
            for x in rr.rd:
                if id(x) not in deps:
                    deps[id(x)] = (x, False)
        for d, hard in deps.values():
            if d is o:
                continue
            if d.dma or o.dma or d.eng != o.eng:
                o.deps.append(d)
            elif hard and o.eng != "tensor":
                o.deps.append(d)
        for k in r:
            self._r(k).rd.append(o)
        for k in w:
            rr = self._r(k)
            rr.lw = o
            rr.rd = []
        if dma:
            o.k = self.ndma[eng]
            self.ndma[eng] += 1
        self.ops[eng].append(o)
        return o

    def dma(self, eng, out, in_, r=(), w=(), **kw):
        return self.op(eng, lambda e: e.dma_start(out=out, in_=in_, **kw), r=r, w=w, dma=True)

    def barrier(self):
        last = []
        for e in self.ENG:
            for o in reversed(self.ops[e]):
                if o.fn is not None and not o.dma:
                    last.append(o)
                    break
        alld = []
        for e in self.ENG:
            seen = 0
            for o in reversed(self.ops[e]):
                if o.dma:
                    alld.append(o)
                    seen += 1
                    if seen >= self.NS:
                        break
        for e in self.ENG:
            o = _Op(e, len(self.ops[e]), None, False)
            o.deps = [d for d in last + alld if d.eng != e or d.dma or e != "tensor"]
            self.ops[e].append(o)
        self.res = {}

    def emit(self, es):
        nc = self.nc
        sem_e = {e: es.enter_context(nc.semaphore("se_" + e)) for e in self.ENG}
        sem_d = {e: [es.enter_context(nc.semaphore("sd_%s%d" % (e, i))) for i in range(self.NS)]
                 for e in self.ENG if self.ndma[e]}
        for e in self.ENG:
            for o in self.ops[e]:
                for d in o.deps:
                    d.signaled = True
        for e in self.ENG:
            c = 0
            for o in self.ops[e]:
                if o.dma:
                    o.sem = sem_d[e][o.k % self.NS]
                    o.tgt = 16 * (o.k // self.NS + 1)
                    o.prev_tgt = o.tgt - 16
                elif o.signaled:
                    c += 1
                    o.cum = c
        finals = {}
        for e in self.ENG:
            f = {}
            for o in self.ops[e]:
                if o.dma:
                    f[o.k % self.NS] = (o.sem, o.tgt)
            finals[e] = list(f.values())
        block = es.enter_context(nc.Block())

        def mk(e):
            def body(eng):
                seen = {}
                for o in self.ops[e]:
                    waits = {}
                    for d in o.deps:
                        if d.dma:
                            key, sem, v = ("d", d.eng, d.k % self.NS), d.sem, d.tgt
                        else:
                            key, sem, v = ("e", d.eng), sem_e[d.eng], d.cum
                        if seen.get(key, 0) >= v:
                            continue
                        if key not in waits or waits[key][1] < v:
                            waits[key] = (sem, v)
                    if o.dma and o.prev_tgt > 0:
                        key = ("d", e, o.k % self.NS)
                        if seen.get(key, 0) < o.prev_tgt:
                            waits[key] = (o.sem, max(o.prev_tgt, waits.get(key, (None, 0))[1]))
                    for key, (sem, v) in waits.items():
                        eng.wait_ge(sem, v)
                        seen[key] = v
                    if o.fn is None:
                        continue
                    ins = o.fn(eng)
                    if o.dma:
                        ins.then_inc(o.sem, 16)
                    elif o.signaled:
                        ins.then_inc(sem_e[e], 1)
                for sem, v in finals[e]:
                    eng.wait_ge(sem, v)
            return body

        for e in self.ENG:
            getattr(block, e)(mk(e))


IN_SPECS = [
    ("xp", [SEQ, D], F32), ("xs", [TS, D], F32),
    ("cache_k", [2560 * 128, 256], F32), ("cache_v", [2560 * 128, 256], F32),
    ("cache_ik", [2560 * 128, 64], F32), ("ptab", [NS_SEQ, 16], I32),
    ("st_conv", [NS_SEQ * 3, 1024], F32), ("st_lru", [NS_SEQ, 1024], F32),
    ("cmk", [NS_SEQ * 256, 1024], F32), ("cmv", [NS_SEQ * 256, 1024], F32),
    ("mem", [256, D], F32),
    ("g_mix", [1, D], F32), ("w_in", [D, N_IN], F32), ("conv_w", [4, 1024], F32), ("conv_b", [1, 1024], F32),
    ("w_rg", [1024, 128], F32), ("b_rg", [1, 1024], F32), ("w_ig", [1024, 128], F32), ("b_ig", [1, 1024], F32),
    ("lam", [1, 1024], F32), ("g_mem", [1, D], F32), ("w_mem_kv", [D, 2048], F32),
    ("w_gate", [D, 3 * D], F32), ("w_br", [3 * 1024, D], F32), ("w_o", [D, D], F32), ("g_ffn", [1, D], F32),
    ("w_pq", [D, 2048], F32), ("sub_keys", [2 * 8 * 128, 128], F32), ("peer_u", [16384, D], F32),
    ("peer_v", [16384, D], F32), ("g_final", [1, D], F32),
]
OUT_SPECS = [
    ("y_p", [SEQ, D]), ("y_s", [TS, D]), ("k_p", [SEQ, 256]), ("v_p", [SEQ, 256]), ("ik_p", [SEQ, 64]),
    ("conv_p", [3, 1024]), ("h_p", [1, 1024]), ("mk_p", [256, 1024]), ("mv_p", [256, 1024]),
    ("k_s", [TS, 256]), ("v_s", [TS, 256]), ("ik_s", [TS, 64]), ("conv_s", [NS_SEQ * 3, 1024]),
    ("h_s", [NS_SEQ, 1024]),
]


_INS = {n: (s, d) for n, s, d in IN_SPECS}
_OUTS = {n: s for n, s in OUT_SPECS}


class _Lazy(dict):
    def __init__(self, mk):
        super().__init__()
        self.mk = mk

    def __missing__(self, n):
        v = self[n] = self.mk(n)
        return v


DEBUG_IO = {}


class K:
    def sub(self):
        return _Sub(self)


class _Sub:
    def __init__(self, k):
        self.k = k

    def __enter__(self):
        self.prev = self.k.scope
        self.st = ExitStack()
        self.k.scope = self.st
        return self

    def __exit__(self, *a):
        self.k.p.barrier()
        self.st.close()
        self.k.scope = self.prev
        return False


def build(phases=("all",)):
    nc = bass.Bass("TRN2", target_bir_lowering=False)
    es = ExitStack()
    k = K()
    k.nc = nc
    k.es = es
    k.I = _Lazy(lambda n: nc.dram_tensor(n, _INS[n][0], _INS[n][1], kind="ExternalInput").ap())
    k.O = _Lazy(lambda n: nc.dram_tensor(n, _OUTS[n], F32, kind="ExternalOutput").ap())
    k.S = {}
    p = k.p = Prog(nc)

    def scratch(name, shape, dt):
        if name not in k.S:
            kind = DEBUG_IO.get(name)
            if kind:
                k.S[name] = nc.dram_tensor(name, shape, dt, kind=kind).ap()
            else:
                k.S[name] = nc.dram_tensor(name, shape, dt).ap()
        return k.S[name]
    k.scratch = scratch
    k.scope = es

    k.nsb = 0

    def sb(name, shape, dt):
        k.nsb += 1
        return k.scope.enter_context(nc.sbuf_tensor("%s_%d" % (name, k.nsb), shape, dt))
    k.sb = sb
    k.ps = [es.enter_context(nc.psum_tensor("ps%d" % i, [128, 512], F32)) for i in range(8)]

    k.ident_f = sb("ident_f", [128, 128], F32)
    k.ident_b = sb("ident_b", [128, 128], BF16)
    k.ones_f = sb("ones_f", [128, 128], F32)
    p.op("gpsimd", lambda e: e.memset(k.ones_f[:], 1.0), w=["ones_f"])
    p.op("gpsimd", lambda e: e.affine_select(out=k.ident_f[:], in_=k.ones_f[:], pattern=[[-1, 128]],
                                             compare_op=ALU.is_equal, fill=0.0, base=0, channel_multiplier=1),
         r=["ones_f"], w=["ident_f"])
    p.op("vector", lambda e: e.tensor_copy(out=k.ident_b[:], in_=k.ident_f[:]), r=["ident_f"], w=["ident_b"])
    p.barrier()

    def run_phase(fn, *a):
        with ExitStack() as sc:
            k.scope = sc
            fn(k, *a)
            p.barrier()
        k.scope = es

    if "proj" in phases or "all" in phases:
        run_phase(phase_proj)
    pd = dict(PHASES)
    for name in ("cast", "mem", "lru", "dsa", "merge", "peer"):
        if name in phases or "all" in phases:
            run_phase(pd[name])
    p.emit(es)
    es.close()
    nc._used_in = list(k.I.keys())
    nc._used_out = list(k.O.keys())
    return nc


def load_bcast(k, name, dram_row, n):
    t = k.sb(name, [128, n], F32)
    k.p.dma("sync", t[:], dram_row[0, :].partition_broadcast(128), w=[name])
    return t


def rms_tile(k, xt, n, g, gkey, xn_out, tag, col):
    p = k.p
    ss, rs, junk = k.ss, k.rs, k.junk
    xk = ("xt", tag)
    p.op("scalar", lambda e: e.activation(out=junk[:n, :], in_=xt[:n, :], func=AF.Square,
                                          accum_out=ss[:n, col:col + 1]), r=[xk], w=[("ss", col), "junk"])
    p.op("vector", lambda e: e.tensor_scalar(out=rs[:n, col:col + 1], in0=ss[:n, col:col + 1],
                                             scalar1=1.0 / D, scalar2=EPS, op0=ALU.mult, op1=ALU.add),
         r=[("ss", col)], w=[("rs", col)])
    p.op("scalar", lambda e: e.sqrt(out=rs[:n, col:col + 1], in_=rs[:n, col:col + 1]),
         r=[("rs", col)], w=[("rs", col)])
    p.op("vector", lambda e: e.reciprocal(out=rs[:n, col:col + 1], in_=rs[:n, col:col + 1]),
         r=[("rs", col)], w=[("rs", col)])
    p.op("vector", lambda e: e.scalar_tensor_tensor(out=xn_out[:n, :], in0=xt[:n, :], scalar=rs[:n, col:col + 1],
                                                    in1=g[:n, :], op0=ALU.mult, op1=ALU.mult),
         r=[xk, ("rs", col), gkey], w=[("xn", tag)])


def transpose_to_T(k, xn, n, tag, dstT, t0, pbase):
    p = k.p
    for half in range(2):
        bank = pbase + half
        pv = k.ps[bank][:].bitcast(BF16)
        for j in range(8):
            c = half * 8 + j
            p.op("tensor", lambda e, c=c, j=j, pv=pv: e.transpose(
                out=pv[:, j * 128:j * 128 + n], in_=xn[:n, c * 128:(c + 1) * 128], identity=k.ident_b[:n, :n]),
                r=[("xn", tag)], w=[("ps", bank)])
        dst = dstT[:, half * 8:half * 8 + 8, t0:t0 + n]
        srcv = pv.rearrange("p (c t) -> p c t", c=8)[:, :, :n]
        if half == 0:
            p.op("scalar", lambda e, dst=dst, srcv=srcv: e.copy(out=dst, in_=srcv),
                 r=[("ps", bank)], w=[("T", id(dstT), t0, half)])
        else:
            p.op("vector", lambda e, dst=dst, srcv=srcv: e.tensor_copy(out=dst, in_=srcv),
                 r=[("ps", bank)], w=[("T", id(dstT), t0, half)])


def Tkeys(dstT, t0s):
    return [("T", id(dstT), t0, h) for t0 in t0s for h in range(2)]


def norm_stats(k):
    k.ss = k.sb("ss", [128, 64], F32)
    k.rs = k.sb("rs", [128, 64], F32)
    k.junk = k.sb("junk", [128, D], BF16)


def linear_fm(k, xT, xkeys, wname, chunks, evac, tag):
    p = k.p
    stage = [k.sb("st_%s%d" % (tag, i), [128, 16, 128], F32) for i in range(2)]
    wb = [k.sb("wb_%s%d" % (tag, i), [128, 16, 128], BF16) for i in range(2)]
    W = k.I[wname]
    for ci, pieces in enumerate(chunks):
        b = ci % 2
        r0 = 0
        for (c0, ncl) in pieces:
            src = W[:, c0:c0 + ncl].rearrange("(c p) n -> p c n", p=128)
            p.dma("sync", stage[b][:, :, r0:r0 + ncl], src, w=[("st", tag, b)])
            r0 += ncl
        rows = r0
        if ci % 2 == 0:
            p.op("scalar", lambda e, b=b, rows=rows: e.copy(out=wb[b][:, :, :rows], in_=stage[b][:, :, :rows]),
                 r=[("st", tag, b)], w=[("wb", tag, b)])
        else:
            p.op("gpsimd", lambda e, b=b, rows=rows: e.tensor_copy(out=wb[b][:, :, :rows], in_=stage[b][:, :, :rows]),
                 r=[("st", tag, b)], w=[("wb", tag, b)])
        for si in range(5):
            t0 = si * 512
            tn = min(512, T - t0)
            bank = (ci * 5 + si) % 4
            for c in range(16):
                p.op("tensor", lambda e, b=b, c=c, rows=rows, t0=t0, tn=tn, bank=bank: e.matmul(
                    out=k.ps[bank][:rows, :tn], lhsT=wb[b][:, c, :rows], rhs=xT[:, c, t0:t0 + tn],
                    start=(c == 0), stop=(c == 15)), r=[("wb", tag, b)] + xkeys, w=[("ps", bank)])
            evac(ci, si, k.ps[bank], bank, rows, t0, tn)


def phase_proj(k):
    p, nc = k.p, k.nc
    norm_stats(k)
    xnT = k.sb("xnT", [128, 16, T], BF16)
    with k.sub():
        g = load_bcast(k, "g_bc", k.I["g_mix"], D)
        xt = [k.sb("xt%d" % i, [128, D], F32) for i in range(2)]
        xn = [k.sb("xnb%d" % i, [128, D], BF16) for i in range(2)]
        for tt in range(NT):
            b = tt % 2
            n = trows(tt)
            src = k.I["xp"][tt * 128:(tt + 1) * 128, :] if tt < 16 else k.I["xs"][:, :]
            p.dma("sync", xt[b][:n, :], src, w=[("xt", b)])
            rms_tile(k, xt[b], n, g, "g_bc", xn[b], b, tt)
            transpose_to_T(k, xn[b], n, b, xnT, tt * 128, b * 2)
    allx = []
    with k.sub():
        phase_proj_tok(k, xnT)
    phase_proj_fm(k, xnT, allx)


def phase_proj_tok(k, xnT):
    p, nc = k.p, k.nc

    stage = k.sb("kv_stage", [128, 16, 592], F32)
    wbk = k.sb("kv_wb", [128, 16, 592], BF16)
    p.dma("sync", stage[:, :, 0:512], k.I["w_in"][:, O_K:O_K + 512].rearrange("(c p) n -> p c n", p=128),
          w=["kvst0"])
    p.dma("sync", stage[:, :, 512:592], k.I["w_in"][:, O_IK:O_IK + 80].rearrange("(c p) n -> p c n", p=128),
          w=["kvst1"])
    p.op("scalar", lambda e: e.copy(out=wbk[:, :, 0:512], in_=stage[:, :, 0:512]), r=["kvst0"], w=["kvwb0"])
    p.op("vector", lambda e: e.tensor_copy(out=wbk[:, :, 512:592], in_=stage[:, :, 512:592]), r=["kvst1"], w=["kvwb1"])
    vtok = k.scratch("v_tok", [T, 256], BF16)
    iwtok = k.scratch("iw_tok", [T, 16], F32)
    ob = [k.sb("kv_ob%d" % i, [128, 592], F32) for i in range(2)]
    vb = [k.sb("kv_vb%d" % i, [128, 256], BF16) for i in range(2)]
    for tt in range(NT):
        n = trows(tt)
        b = tt % 2
        t0 = tt * 128
        xk = Tkeys(xnT, [t0])
        ba, bb = 4 + b * 2, 5 + b * 2
        for c in range(16):
            p.op("tensor", lambda e, c=c, n=n, t0=t0, ba=ba: e.matmul(
                out=k.ps[ba][:n, :512], lhsT=xnT[:, c, t0:t0 + n], rhs=wbk[:, c, 0:512],
                start=(c == 0), stop=(c == 15)), r=["kvwb0"], w=[("ps", ba)])
        for c in range(16):
            p.op("tensor", lambda e, c=c, n=n, t0=t0, bb=bb: e.matmul(
                out=k.ps[bb][:n, :80], lhsT=xnT[:, c, t0:t0 + n], rhs=wbk[:, c, 512:592],
                start=(c == 0), stop=(c == 15)), r=["kvwb1"], w=[("ps", bb)])
        p.op("scalar", lambda e, n=n, b=b, ba=ba: e.copy(out=ob[b][:n, 0:512], in_=k.ps[ba][:n, :512]),
             r=[("ps", ba)], w=[("ob", b, 0)])
        p.op("vector", lambda e, n=n, b=b, bb=bb: e.tensor_copy(out=ob[b][:n, 512:592], in_=k.ps[bb][:n, :80]),
             r=[("ps", bb)], w=[("ob", b, 1)])
        p.op("vector", lambda e, n=n, b=b: e.tensor_copy(out=vb[b][:n, :], in_=ob[b][:n, 256:512]),
             r=[("ob", b, 0)], w=[("vb", b)])
        p.dma("sync", vtok[t0:t0 + n, :], vb[b][:n, :], r=[("vb", b)])
        p.dma("sync", iwtok[t0:t0 + n, :], ob[b][:n, 576:592], r=[("ob", b, 1)])
        if tt < 16:
            rows = slice(t0, t0 + 128)
            p.dma("sync", k.O["k_p"][rows, :], ob[b][:, 0:256], r=[("ob", b, 0)])
            p.dma("sync", k.O["v_p"][rows, :], ob[b][:, 256:512], r=[("ob", b, 0)])
            p.dma("sync", k.O["ik_p"][rows, :], ob[b][:, 512:576], r=[("ob", b, 1)])
        else:
            p.dma("sync", k.O["k_s"][:, :], ob[b][:64, 0:256], r=[("ob", b, 0)])
            p.dma("sync", k.O["v_s"][:, :], ob[b][:64, 256:512], r=[("ob", b, 0)])
            p.dma("sync", k.O["ik_s"][:, :], ob[b][:64, 512:576], r=[("ob", b, 1)])
    for half in range(2):
        c0 = O_XB + half * 512
        p.dma("sync", stage[:, :, 0:512], k.I["w_in"][:, c0:c0 + 512].rearrange("(c p) n -> p c n", p=128),
              w=["kvst0"])
        p.op("scalar", lambda e: e.copy(out=wbk[:, :, 0:512], in_=stage[:, :, 0:512]), r=["kvst0"], w=["kvwb0"])
        for tt in (15, 16):
            n = trows(tt)
            b = tt % 2
            t0 = tt * 128
            ba = 4 + b * 2
            for c in range(16):
                p.op("tensor", lambda e, c=c, n=n, t0=t0, ba=ba: e.matmul(
                    out=k.ps[ba][:n, :512], lhsT=xnT[:, c, t0:t0 + n], rhs=wbk[:, c, 0:512],
                    start=(c == 0), stop=(c == 15)), r=["kvwb0"], w=[("ps", ba)])
            p.op("scalar", lambda e, n=n, b=b, ba=ba: e.copy(out=ob[b][:n, 0:512], in_=k.ps[ba][:n, :512]),
                 r=[("ps", ba)], w=[("ob", b, 0)])
            cs = slice(half * 512, half * 512 + 512)
            if tt == 15:
                p.dma("sync", k.O["conv_p"][:, cs], ob[b][125:128, 0:512], r=[("ob", b, 0)])
            else:
                srcv = ob[b][:64, 0:512]
                xbs = k.scratch("xb_s", [TS, 1024], F32)
                p.dma("sync", xbs[:, cs], srcv, r=[("ob", b, 0)], w=[("xbs", half)])
                p.dma("sync", k.O["conv_s"].rearrange("(b j) n -> b j n", j=3)[:, :, cs],
                      xbs.rearrange("(b t) n -> b t n", t=4)[:, 1:4, cs], r=[("xbs", half)])


def phase_proj_fm(k, xnT, allx):
    p, nc = k.p, k.nc
    projT = k.scratch("projT", [43, 128, T], BF16)
    chunks = []
    for c0 in list(range(O_Q, O_Q + 1024, 128)) + list(range(O_K, O_K + 256, 128)) + list(range(O_IQ, O_IQ + 1024, 128)):
        chunks.append([(c0, 128)])
    chunks.append([(O_IK, 64), (O_IK, 64)])
    for base in (O_XB, O_YB, O_QM):
        for c0 in range(base, base + 1024, 128):
            chunks.append([(c0, 128)])
    obf = [k.sb("pj_ob%d" % i, [128, T], BF16) for i in range(2)]

    def evac_proj(dst):
        def evac(ci, si, ps, bank, rows, t0, tn):
            b = ci % 2
            if si % 2 == 0:
                p.op("scalar", lambda e: e.copy(out=obf[b][:rows, t0:t0 + tn], in_=ps[:rows, :tn]),
                     r=[("ps", bank)], w=[("obf", b, si)])
            else:
                p.op("vector", lambda e: e.tensor_copy(out=obf[b][:rows, t0:t0 + tn], in_=ps[:rows, :tn]),
                     r=[("ps", bank)], w=[("obf", b, si)])
            if si == 4:
                p.dma("sync", dst[ci, :rows, :], obf[b][:rows, :], r=[("obf", b, s_) for s_ in range(5)])
        return evac
    linear_fm(k, xnT, allx, "w_in", chunks, evac_proj(projT), "pj")

    gT = k.scratch("gT", [48, 128, T], BF16)

    def evac_gate(ci, si, ps, bank, rows, t0, tn):
        b = ci % 2
        p.op("scalar", lambda e: e.activation(out=obf[b][:rows, t0:t0 + tn], in_=ps[:rows, :tn], func=AF.Sigmoid),
             r=[("ps", bank)], w=[("obf", b, si)])
        if si == 4:
            p.dma("sync", gT[ci, :rows, :], obf[b][:rows, :], r=[("obf", b, s_) for s_ in range(5)])
    linear_fm(k, xnT, allx, "w_gate", [[(c0, 128)] for c0 in range(0, 3 * D, 128)], evac_gate, "gt")


PHASES = []


_NC_CACHE = {}


def make_in_maps(inp):
    f = lambda a: np.ascontiguousarray(a, dtype=np.float32)
    shared = {
        "cache_k": f(inp["cache_k"]).reshape(2560 * 128, 256), "cache_v": f(inp["cache_v"]).reshape(2560 * 128, 256),
        "cache_ik": f(inp["cache_idx_k"]).reshape(2560 * 128, 64),
        "g_mix": f(inp["g_mix"]).reshape(1, D), "w_in": f(inp["w_in"]).reshape(D, N_IN),
        "conv_w": f(inp["conv_w"]).reshape(4, 1024), "conv_b": f(inp["conv_b"]).reshape(1, 1024),
        "w_rg": f(inp["w_rg"]).reshape(1024, 128), "b_rg": f(inp["b_rg"]).reshape(1, 1024),
        "w_ig": f(inp["w_ig"]).reshape(1024, 128), "b_ig": f(inp["b_ig"]).reshape(1, 1024),
        "lam": f(inp["lru_lambda"]).reshape(1, 1024), "g_mem": f(inp["g_mem"]).reshape(1, D),
        "w_mem_kv": f(inp["w_mem_kv"]).reshape(D, 2048), "w_gate": f(inp["w_gate"]).reshape(D, 3 * D),
        "w_br": f(inp["w_br"]).reshape(3 * 1024, D), "w_o": f(inp["w_o"]).reshape(D, D),
        "g_ffn": f(inp["g_ffn"]).reshape(1, D), "w_pq": f(inp["w_peer_q"]).reshape(D, 2048),
        "sub_keys": f(inp["peer_sub_keys"]).reshape(2 * 8 * 128, 128), "peer_u": f(inp["peer_u"]).reshape(16384, D),
        "peer_v": f(inp["peer_v"]).reshape(16384, D), "g_final": f(inp["g_final"]).reshape(1, D),
    }
    maps = []
    for c in range(NCORES):
        sl = slice(c * NS_SEQ, (c + 1) * NS_SEQ)
        m = dict(shared)
        m["xp"] = f(inp["x_prompt"][c])
        m["xs"] = f(inp["x_sample"][sl]).reshape(TS, D)
        m["ptab"] = np.ascontiguousarray(inp["page_table"][sl], dtype=np.int32)
        m["st_conv"] = f(inp["state_conv"][0, sl]).reshape(NS_SEQ * 3, 1024)
        m["st_lru"] = f(inp["state_lru"][0, sl]).reshape(NS_SEQ, 1024)
        m["cmk"] = f(inp["cache_mem_k"][0, sl]).reshape(NS_SEQ * 256, 1024)
        m["cmv"] = f(inp["cache_mem_v"][0, sl]).reshape(NS_SEQ * 256, 1024)
        m["mem"] = f(inp["mem_prompt"][c])
        maps.append(m)
    return maps


def assemble(res):
    g = lambda n: [np.asarray(r[n], dtype=np.float32) for r in res]
    y_p = np.stack(g("y_p"))
    y_s = np.concatenate(g("y_s")).reshape(128, DEC, D)
    k_p = np.stack(g("k_p")).reshape(1, 8, SEQ, 2, 128)
    v_p = np.stack(g("v_p")).reshape(1, 8, SEQ, 2, 128)
    ik_p = np.stack(g("ik_p")).reshape(1, 8, SEQ, 64)
    conv_p = np.stack(g("conv_p")).reshape(1, 8, 3, 1024)
    h_p = np.stack(g("h_p")).reshape(1, 8, 1024)
    mk_p = np.stack(g("mk_p")).reshape(1, 8, 256, 4, 256)
    mv_p = np.stack(g("mv_p")).reshape(1, 8, 256, 4, 256)
    k_s = np.concatenate(g("k_s")).reshape(1, 128, DEC, 2, 128)
    v_s = np.concatenate(g("v_s")).reshape(1, 128, DEC, 2, 128)
    ik_s = np.concatenate(g("ik_s")).reshape(1, 128, DEC, 64)
    conv_s = np.concatenate(g("conv_s")).reshape(1, 128, 3, 1024)
    h_s = np.concatenate(g("h_s")).reshape(1, 128, 1024)
    return (y_p, y_s, k_p, v_p, ik_p, conv_p, h_p, mk_p, mv_p, k_s, v_s, ik_s, conv_s, h_s)


def kernel(**inputs):
    if "nc" not in _NC_CACHE:
        _NC_CACHE["nc"] = build()
    nc = _NC_CACHE["nc"]
    in_maps = [{n: m[n] for n in nc._used_in} for m in make_in_maps(inputs)]
    res = run_bass_kernel_spmd(nc, in_maps, core_ids=list(range(NCORES)))
    outs = []
    for r in res.results:
        d = dict(r)
        for n, s in OUT_SPECS:
            if n not in d:
                d[n] = np.zeros(s, np.float32)
        outs.append(d)
    return assemble(outs)


def phase_mem(k):
    p, nc = k.p, k.nc
    projT = k.scratch("projT", [43, 128, T], BF16)
    oT = k.scratch("oT", [3, 8, 128, T], BF16)
    MS = 256 ** -0.5
    norm_stats(k)
    memT = k.sb("memT", [128, 16, 256], BF16)
    mkT = k.sb("mkT", [128, 8, 256], BF16)
    mvb = k.sb("mvb", [128, 2, 1024], BF16)
    qmT = k.sb("qmT", [128, 8, T], BF16)
    p.dma("sync", qmT[:], projT[35:43].rearrange("c p t -> p c t"), w=["qmT"])
    with k.sub():
        g = load_bcast(k, "gm_bc", k.I["g_mem"], D)
        xt = [k.sb("mxt%d" % i, [128, D], F32) for i in range(2)]
        xn = [k.sb("mxn%d" % i, [128, D], BF16) for i in range(2)]
        for mt in range(2):
            p.dma("sync", xt[mt][:, :], k.I["mem"][mt * 128:(mt + 1) * 128, :], w=[("xt", mt)])
            rms_tile(k, xt[mt], 128, g, "gm_bc", xn[mt], mt, mt)
            transpose_to_T(k, xn[mt], 128, mt, memT, mt * 128, mt * 2)
    with k.sub():
        stage = k.sb("mst", [128, 16, 512], F32)
        wb = k.sb("mwb", [128, 16, 512], BF16)
        ob = [k.sb("mob%d" % i, [128, 512], F32) for i in range(2)]
        for cb in range(4):
            p.dma("sync", stage[:], k.I["w_mem_kv"][:, cb * 512:(cb + 1) * 512].rearrange("(c p) n -> p c n", p=128),
                  w=["mst"])
            p.op("scalar", lambda e: e.copy(out=wb[:], in_=stage[:]), r=["mst"], w=["mwb"])
            for mt in range(2):
                bank = mt
                for c in range(16):
                    p.op("tensor", lambda e, c=c, mt=mt, bank=bank: e.matmul(
                        out=k.ps[bank][:, :], lhsT=memT[:, c, mt * 128:(mt + 1) * 128], rhs=wb[:, c, :],
                        start=(c == 0), stop=(c == 15)), r=["mwb"], w=[("ps", bank)])
                p.op("scalar", lambda e, mt=mt, bank=bank: e.copy(out=ob[mt][:, :], in_=k.ps[bank][:, :]),
                     r=[("ps", bank)], w=[("mob", mt)])
                dst = k.O["mk_p"] if cb < 2 else k.O["mv_p"]
                cs = slice((cb % 2) * 512, (cb % 2) * 512 + 512)
                p.dma("sync", dst[mt * 128:(mt + 1) * 128, cs], ob[mt][:, :], r=[("mob", mt)])
                if cb >= 2:
                    p.op("vector", lambda e, mt=mt, cs=cs: e.tensor_copy(out=mvb[:, mt, cs], in_=ob[mt][:, :]),
                         r=[("mob", mt)], w=["mvb"])
            if cb < 2:
                for j in range(4):
                    bank = 2 + j % 2
                    for c in range(16):
                        p.op("tensor", lambda e, c=c, j=j, bank=bank: e.matmul(
                            out=k.ps[bank][:, :256], lhsT=wb[:, c, j * 128:(j + 1) * 128], rhs=memT[:, c, :],
                            start=(c == 0), stop=(c == 15)), r=["mwb"], w=[("ps", bank)])
                    p.op("vector", lambda e, j=j, bank=bank, cb=cb: e.tensor_copy(
                        out=mkT[:, cb * 4 + j, :], in_=k.ps[bank][:, :256]), r=[("ps", bank)], w=["mkT"])
    with k.sub():
        mem_attend_loop(k, qmT, mkT, mvb, oT, MS, [(tt * 128, 128) for tt in range(16)], "p")
    with k.sub():
        cm = [k.sb("cmk%d" % i, [128, 2, 1024], F32) for i in range(2)]
        cv = [k.sb("cmv%d" % i, [128, 2, 1024], F32) for i in range(2)]
        mkTs = [k.sb("mkTs%d" % i, [128, 8, 256], BF16) for i in range(2)]
        mvbs = [k.sb("mvbs%d" % i, [128, 2, 1024], BF16) for i in range(2)]
        st = mem_attend_state(k, "s")
        for b in range(NS_SEQ):
            i = b % 2
            p.dma("sync", cm[i][:], k.I["cmk"][b * 256:(b + 1) * 256, :].rearrange("(m p) n -> p m n", p=128),
                  w=[("cm", i)])
            p.dma("sync", cv[i][:], k.I["cmv"][b * 256:(b + 1) * 256, :].rearrange("(m p) n -> p m n", p=128),
                  w=[("cv", i)])
            p.op("gpsimd", lambda e, i=i: e.tensor_copy(out=mvbs[i][:], in_=cv[i][:]), r=[("cv", i)], w=[("mvbs", i)])
            for c in range(8):
                bank = 6 + c % 2
                for mt in range(2):
                    p.op("tensor", lambda e, c=c, mt=mt, bank=bank, i=i: e.transpose(
                        out=k.ps[bank][:, mt * 128:(mt + 1) * 128], in_=cm[i][:, mt, c * 128:(c + 1) * 128],
                        identity=k.ident_f[:]), r=[("cm", i)], w=[("ps", bank)])
                p.op("scalar", lambda e, c=c, bank=bank, i=i: e.copy(out=mkTs[i][:, c, :], in_=k.ps[bank][:, :256]),
                     r=[("ps", bank)], w=[("mkTs", i)])
            mem_attend_tile(k, st, qmT, mkTs[i], mvbs[i], oT, MS, SEQ + 4 * b, 4, [("mkTs", i), ("mvbs", i)])


def mem_attend_state(k, tag):
    st = K()
    st.mx = k.sb("ma_mx" + tag, [128, 4], F32)
    st.rsum = k.sb("ma_rs" + tag, [128, 4], F32)
    st.P = k.sb("ma_P" + tag, [128, 4, 256], BF16)
    st.PT = k.sb("ma_PT" + tag, [128, 8, 128], BF16)
    st.oc = k.sb("ma_oc" + tag, [128, 8, 128], BF16)
    return st


def mem_attend_loop(k, qmT, mkT, mvb, oT, MS, tiles, tag):
    st = mem_attend_state(k, tag)
    for (t0, n) in tiles:
        mem_attend_tile(k, st, qmT, mkT, mvb, oT, MS, t0, n, ["mkT", "mvb"])


def mem_attend_tile(k, st, qmT, mkT, mvb, oT, MS, t0, n, kvkeys):
    p = k.p
    for h in range(4):
        bank = h // 2
        for kc in range(2):
            p.op("tensor", lambda e, h=h, kc=kc, bank=bank: e.matmul(
                out=k.ps[bank][:n, (h % 2) * 256:(h % 2) * 256 + 256], lhsT=qmT[:, 2 * h + kc, t0:t0 + n],
                rhs=mkT[:, 2 * h + kc, :], start=(kc == 0), stop=(kc == 1)),
                r=["qmT"] + kvkeys, w=[("ps", bank)])
    for bank in range(2):
        p.op("vector", lambda e, bank=bank: e.tensor_reduce(
            out=st.mx[:n, bank * 2:bank * 2 + 2], in_=k.ps[bank][:n, :].rearrange("p (h m) -> p h m", h=2),
            axis=AX.X, op=ALU.max), r=[("ps", bank)], w=[("mx", bank)])
        p.op("vector", lambda e, bank=bank: e.tensor_scalar(
            out=st.mx[:n, bank * 2:bank * 2 + 2], in0=st.mx[:n, bank * 2:bank * 2 + 2], scalar1=-MS, scalar2=None,
            op0=ALU.mult), r=[("mx", bank)], w=[("mx", bank)])
    for h in range(4):
        bank = h // 2
        p.op("scalar", lambda e, h=h, bank=bank: e.activation(
            out=st.P[:n, h, :], in_=k.ps[bank][:n, (h % 2) * 256:(h % 2) * 256 + 256], func=AF.Exp,
            bias=st.mx[:n, h:h + 1], scale=MS, accum_out=st.rsum[:n, h:h + 1]),
            r=[("ps", bank), ("mx", bank)], w=[("P", h), ("rsum", h)])
    p.op("vector", lambda e: e.reciprocal(out=st.rsum[:n, :], in_=st.rsum[:n, :]),
         r=[("rsum", h) for h in range(4)], w=["rinv"])
    for h in range(4):
        p.op("vector", lambda e, h=h: e.tensor_scalar(out=st.P[:n, h, :], in0=st.P[:n, h, :],
                                                      scalar1=st.rsum[:n, h:h + 1], scalar2=None, op0=ALU.mult),
             r=["rinv", ("P", h)], w=[("P", h)])
    pv = k.ps[2][:].bitcast(BF16)
    for h in range(4):
        for mt in range(2):
            j = h * 2 + mt
            p.op("tensor", lambda e, h=h, mt=mt, j=j: e.transpose(
                out=pv[:, j * 128:j * 128 + n], in_=st.P[:n, h, mt * 128:(mt + 1) * 128],
                identity=k.ident_b[:n, :n]), r=[("P", h)], w=[("ps", 2)])
    p.op("scalar", lambda e: e.copy(out=st.PT[:, :, :n], in_=pv.rearrange("p (j t) -> p j t", j=8)[:, :, :n]),
         r=[("ps", 2)], w=["PT"])
    for h in range(4):
        for c2 in range(2):
            j = h * 2 + c2
            bank = 3 + j // 4
            for mt in range(2):
                p.op("tensor", lambda e, h=h, c2=c2, mt=mt, j=j, bank=bank: e.matmul(
                    out=k.ps[bank][:, (j % 4) * 128:(j % 4) * 128 + n],
                    lhsT=mvb[:, mt, h * 256 + c2 * 128:h * 256 + c2 * 128 + 128], rhs=st.PT[:, h * 2 + mt, :n],
                    start=(mt == 0), stop=(mt == 1)), r=["PT"] + kvkeys, w=[("ps", bank)])
    for half in range(2):
        bank = 3 + half
        eng = "scalar" if half == 0 else "vector"
        srcv = k.ps[bank][:].rearrange("p (j t) -> p j t", j=4)[:, :, :n]
        dst = st.oc[:, half * 4:half * 4 + 4, :n]
        if half == 0:
            p.op("scalar", lambda e, dst=dst, srcv=srcv: e.copy(out=dst, in_=srcv), r=[("ps", bank)], w=[("oc", half)])
        else:
            p.op("vector", lambda e, dst=dst, srcv=srcv: e.tensor_copy(out=dst, in_=srcv), r=[("ps", bank)],
                 w=[("oc", half)])
    p.dma("sync", oT[2, :, :, t0:t0 + n].rearrange("c p t -> p c t"), st.oc[:, :, :n], r=[("oc", 0), ("oc", 1)])


PHASES.append(("mem", phase_mem))


def gelu_mul(k, y, h, out, n, tmp1, tmp2, keys_r, key_w):
    p = k.p
    p.op("vector", lambda e: e.tensor_tensor(out=tmp1, in0=y, in1=y, op=ALU.mult), r=keys_r, w=[("g1", key_w)])
    p.op("vector", lambda e: e.tensor_scalar(out=tmp1, in0=tmp1, scalar1=0.044715, scalar2=1.0, op0=ALU.mult,
                                             op1=ALU.add), r=[("g1", key_w)], w=[("g1", key_w)])
    p.op("vector", lambda e: e.tensor_tensor(out=tmp1, in0=tmp1, in1=y, op=ALU.mult), r=[("g1", key_w)] + keys_r,
         w=[("g1", key_w)])
    p.op("scalar", lambda e: e.activation(out=tmp2, in_=tmp1, func=AF.Sigmoid, scale=1.5957691216057308),
         r=[("g1", key_w)], w=[("g2", key_w)])
    p.op("vector", lambda e: e.tensor_tensor(out=tmp2, in0=tmp2, in1=y, op=ALU.mult), r=[("g2", key_w)] + keys_r,
         w=[("g2", key_w)])
    p.op("vector", lambda e: e.tensor_tensor(out=out, in0=tmp2, in1=h, op=ALU.mult), r=[("g2", key_w)] + keys_r,
         w=[key_w])


def phase_lru(k):
    p, nc = k.p, k.nc
    projT = k.scratch("projT", [43, 128, T], BF16)
    oT = k.scratch("oT", [3, 8, 128, T], BF16)
    cw = k.sb("l_cw", [128, 8, 4], F32)
    pv = k.sb("l_pv", [128, 5, 8], F32)
    sc = k.sb("l_sc", [128, 2, 8], F32)
    for j in range(4):
        p.dma("sync", cw[:, :, j], k.I["conv_w"][j:j + 1, :].rearrange("o (n q) -> q (o n)", q=128), w=["cw"],
              allow_slow_non_contiguous=True)
    for i, nm in enumerate(("conv_b", "b_rg", "b_ig", "lam")):
        p.dma("sync", pv[:, i, :], k.I[nm].rearrange("o (n q) -> q (o n)", q=128), w=[("pv", i)],
              allow_slow_non_contiguous=True)
    p.op("scalar", lambda e: e.activation(out=pv[:, 4, :], in_=pv[:, 3, :], func=AF.Exp, scale=-1.0),
         r=[("pv", 3)], w=[("pv", 4)])
    p.op("scalar", lambda e: e.activation(out=pv[:, 4, :], in_=pv[:, 4, :], func=AF.Ln, bias=1.0),
         r=[("pv", 4)], w=[("pv", 4)])
    p.op("vector", lambda e: e.tensor_scalar(out=sc[:, 0, :], in0=pv[:, 4, :], scalar1=-8.0, scalar2=None,
                                             op0=ALU.mult), r=[("pv", 4)], w=["sc0"])
    p.op("vector", lambda e: e.tensor_scalar(out=sc[:, 1, :], in0=pv[:, 4, :], scalar1=-16.0, scalar2=None,
                                             op0=ALU.mult), r=[("pv", 4)], w=["sc1"])
    wst = k.sb("l_wst", [128, 2, 8, 128], F32)
    wg = k.sb("l_wg", [128, 2, 8, 128], BF16)
    p.dma("sync", wst[:, 0], k.I["w_rg"].rearrange("(n i) j -> i n j", i=128), w=["wst0"])
    p.dma("sync", wst[:, 1], k.I["w_ig"].rearrange("(n i) j -> i n j", i=128), w=["wst1"])
    p.op("vector", lambda e: e.tensor_copy(out=wg[:], in_=wst[:]), r=["wst0", "wst1"], w=["wg"])
    scv = k.sb("l_scv", [48, 1024], F32)
    slr = k.sb("l_slr", [16, 1024], F32)
    p.dma("sync", scv[:], k.I["st_conv"][:, :], w=["scv"])
    p.dma("sync", slr[:], k.I["st_lru"][:, :], w=["slr"])
    hl_p = k.sb("l_hlp", [128, 8], F32)
    hl_s = k.sb("l_hls", [128, 8, 16], F32)
    NP = SEQ
    xbb = [k.sb("l_xbb%d" % i, [128, T], BF16) for i in range(2)]
    ybb = [k.sb("l_ybb%d" % i, [128, T], BF16) for i in range(2)]
    xpad = k.sb("l_xpad", [128, 3 + NP], F32)
    xps = k.sb("l_xps", [128, 16, 7], F32)
    xc = k.sb("l_xc", [128, T], F32)
    xcb = k.sb("l_xcb", [128, T], BF16)
    rr = k.sb("l_r", [128, T], F32)
    ii = k.sb("l_i", [128, T], F32)
    aa = k.sb("l_a", [128, T], F32)
    uu = k.sb("l_u", [128, T], F32)
    hh = k.sb("l_h", [128, T], F32)
    yf = k.sb("l_yf", [128, T], F32)
    ob = [k.sb("l_ob%d" % i, [128, T], BF16) for i in range(2)]
    h0 = k.sb("l_h0", [128, 16], F32)
    tmpa = k.sb("l_tmpa", [128, 16], F32)
    p.op("vector", lambda e: e.memset(xpad[:, 0:3], 0.0), w=["xpad0"])
    for n in range(8):
        b = n % 2
        p.dma("sync", xbb[b][:], projT[19 + n], w=[("xbb", b)])
        p.dma("sync", ybb[b][:], projT[27 + n], w=[("ybb", b)])
        p.op("vector", lambda e, b=b: e.tensor_copy(out=xpad[:, 3:3 + NP], in_=xbb[b][:, 0:NP]),
             r=[("xbb", b), "xpad0"], w=["xpad"])
        p.op("tensor", lambda e, n=n: e.transpose(out=k.ps[4][:, 0:48], in_=scv[:, n * 128:(n + 1) * 128],
                                                  identity=k.ident_f[:48, :48]), r=["scv"], w=[("ps", 4)])
        p.op("tensor", lambda e, n=n: e.transpose(out=k.ps[5][:, 0:16], in_=slr[:, n * 128:(n + 1) * 128],
                                                  identity=k.ident_f[:16, :16]), r=["slr"], w=[("ps", 5)])
        p.op("vector", lambda e: e.tensor_copy(out=xps[:, :, 0:3], in_=k.ps[4][:, 0:48].rearrange("p (b j) -> p b j", j=3)),
             r=[("ps", 4)], w=["xps0"])
        p.op("vector", lambda e, b=b: e.tensor_copy(out=xps[:, :, 3:7],
                                                    in_=xbb[b][:, NP:T].rearrange("p (b t) -> p b t", t=4)),
             r=[("xbb", b)], w=["xps1"])
        p.op("vector", lambda e: e.tensor_copy(out=h0[:], in_=k.ps[5][:, 0:16]), r=[("ps", 5)], w=["h0"])
        p.op("scalar", lambda e, n=n: e.activation(out=xc[:, 0:NP], in_=xpad[:, 3:3 + NP], func=AF.Identity,
                                                   bias=pv[:, 0, n:n + 1], scale=cw[:, n, 3:4]),
             r=["xpad", "cw", ("pv", 0)], w=["xc_p"])
        p.op("scalar", lambda e, n=n: e.activation(out=xc[:, NP:T].rearrange("p (b t) -> p b t", t=4),
                                                   in_=xps[:, :, 3:7], func=AF.Identity,
                                                   bias=pv[:, 0, n:n + 1], scale=cw[:, n, 3:4]),
             r=["xps0", "xps1", "cw", ("pv", 0)], w=["xc_s"])
        for j in range(3):
            p.op("vector", lambda e, n=n, j=j: e.scalar_tensor_tensor(
                out=xc[:, 0:NP], in0=xpad[:, j:j + NP], scalar=cw[:, n, j:j + 1], in1=xc[:, 0:NP],
                op0=ALU.mult, op1=ALU.add), r=["xpad", "xc_p"], w=["xc_p"])
            p.op("vector", lambda e, n=n, j=j: e.scalar_tensor_tensor(
                out=xc[:, NP:T].rearrange("p (b t) -> p b t", t=4), in0=xps[:, :, j:j + 4], scalar=cw[:, n, j:j + 1],
                in1=xc[:, NP:T].rearrange("p (b t) -> p b t", t=4), op0=ALU.mult, op1=ALU.add),
                r=["xps0", "xps1", "xc_s"], w=["xc_s"])
        p.op("gpsimd", lambda e: e.tensor_copy(out=xcb[:], in_=xc[:]), r=["xc_p", "xc_s"], w=["xcb"])
        for gi, dst in ((0, rr), (1, ii)):
            for si in range(5):
                t0 = si * 512
                tn = min(512, T - t0)
                bank = (gi * 5 + si) % 4
                p.op("tensor", lambda e, gi=gi, n=n, t0=t0, tn=tn, bank=bank: e.matmul(
                    out=k.ps[bank][:, :tn], lhsT=wg[:, gi, n, :], rhs=xcb[:, t0:t0 + tn], start=True, stop=True),
                    r=["wg", "xcb"], w=[("ps", bank)])
                p.op("scalar", lambda e, gi=gi, n=n, t0=t0, tn=tn, bank=bank, dst=dst: e.activation(
                    out=dst[:, t0:t0 + tn], in_=k.ps[bank][:, :tn], func=AF.Sigmoid, bias=pv[:, 1 + gi, n:n + 1]),
                    r=[("ps", bank), ("pv", 1 + gi)], w=[("gate", gi)])
        p.op("scalar", lambda e, n=n: e.activation(out=aa[:], in_=rr[:], func=AF.Exp, scale=sc[:, 0, n:n + 1]),
             r=[("gate", 0), "sc0"], w=["aa"])
        p.op("scalar", lambda e, n=n: e.activation(out=uu[:], in_=rr[:], func=AF.Exp, scale=sc[:, 1, n:n + 1]),
             r=[("gate", 0), "sc1"], w=["uu"])
        p.op("scalar", lambda e: e.activation(out=uu[:], in_=uu[:], func=AF.Sqrt, bias=1.0, scale=-1.0),
             r=["uu"], w=["uu"])
        p.op("vector", lambda e: e.tensor_tensor(out=uu[:], in0=uu[:], in1=ii[:], op=ALU.mult),
             r=["uu", ("gate", 1)], w=["uu"])
        p.op("vector", lambda e: e.tensor_tensor(out=uu[:], in0=uu[:], in1=xc[:], op=ALU.mult),
             r=["uu", "xc_p", "xc_s"], w=["uu"])
        p.op("vector", lambda e: e.tensor_tensor_scan(out=hh[:, 0:NP], data0=aa[:, 0:NP], data1=uu[:, 0:NP],
                                                      initial=0.0, op0=ALU.mult, op1=ALU.add),
             r=["aa", "uu"], w=["hh_p"])
        hs = hh[:, NP:T].rearrange("p (b t) -> p b t", t=4)
        as_ = aa[:, NP:T].rearrange("p (b t) -> p b t", t=4)
        us = uu[:, NP:T].rearrange("p (b t) -> p b t", t=4)
        for t in range(4):
            prev = h0[:, :] if t == 0 else hs[:, :, t - 1]
            p.op("vector", lambda e, t=t, prev=prev: e.tensor_tensor(out=tmpa[:], in0=as_[:, :, t], in1=prev,
                                                                     op=ALU.mult),
                 r=["aa", "h0", "hh_s"], w=["tmpa"])
            p.op("vector", lambda e, t=t: e.tensor_tensor(out=hs[:, :, t], in0=tmpa[:], in1=us[:, :, t], op=ALU.add),
                 r=["tmpa", "uu"], w=["hh_s"])
        p.op("vector", lambda e, n=n: e.tensor_copy(out=hl_p[:, n:n + 1], in_=hh[:, NP - 1:NP]), r=["hh_p"], w=["hlp"])
        p.op("vector", lambda e, n=n: e.tensor_copy(out=hl_s[:, n, :], in_=hs[:, :, 3]), r=["hh_s"], w=["hls"])
        p.op("gpsimd", lambda e, b=b: e.tensor_copy(out=yf[:], in_=ybb[b][:]), r=[("ybb", b)], w=["yf"])
        gelu_mul(k, yf[:], hh[:], ob[b][:], T, rr[:], ii[:], ["yf", "hh_p", "hh_s", ("gate", 0), ("gate", 1), "uu"],
                 ("lob", b))
        p.dma("sync", oT[1, n], ob[b][:], r=[("lob", b)])
    hrow = k.sb("l_hrow", [16, 1024], F32)
    hrp = k.sb("l_hrp", [8, 128], F32)
    p.op("tensor", lambda e: e.transpose(out=k.ps[6][:8, 0:128], in_=hl_p[:, :], identity=k.ident_f[:, :]),
         r=["hlp"], w=[("ps", 6)])
    p.op("vector", lambda e: e.tensor_copy(out=hrp[:], in_=k.ps[6][:8, 0:128]), r=[("ps", 6)], w=["hrp"])
    p.dma("sync", k.O["h_p"].rearrange("o (n q) -> (o n) q", q=128), hrp[:], r=["hrp"])
    for n in range(8):
        bank = 6 + n % 2
        p.op("tensor", lambda e, n=n, bank=bank: e.transpose(out=k.ps[bank][:16, 0:128], in_=hl_s[:, n, :],
                                                             identity=k.ident_f[:, :]), r=["hls"], w=[("ps", bank)])
        p.op("vector", lambda e, n=n, bank=bank: e.tensor_copy(out=hrow[:, n * 128:(n + 1) * 128],
                                                               in_=k.ps[bank][:16, 0:128]),
             r=[("ps", bank)], w=["hrow"])
    p.dma("sync", k.O["h_s"][:, :], hrow[:], r=["hrow"])


PHASES.append(("lru", phase_lru))


def phase_merge(k):
    p, nc = k.p, k.nc
    oT = k.scratch("oT", [3, 8, 128, T], BF16)
    gT = k.scratch("gT", [48, 128, T], BF16)
    x1 = k.scratch("x1", [T, D], F32)
    mT = k.sb("mT", [128, 16, T], F32)
    with k.sub():
        on = k.sb("mg_on", [128, 8, T], BF16)
        wst = [k.sb("mg_wst%d" % i, [128, 8, 128], F32) for i in range(2)]
        wb = [k.sb("mg_wb%d" % i, [128, 8, 128], BF16) for i in range(2)]
        gt = [k.sb("mg_gt%d" % i, [128, T], BF16) for i in range(2)]
        tmp = [k.sb("mg_tmp%d" % i, [128, 512], F32) for i in range(2)]
        it = 0
        for n in range(3):
            p.dma("sync", on[:], oT[n].rearrange("c p t -> p c t"), w=["on"])
            for cc in range(16):
                b = it % 2
                it += 1
                p.dma("sync", wst[b][:], k.I["w_br"][n * 1024:(n + 1) * 1024, cc * 128:(cc + 1) * 128]
                      .rearrange("(c q) j -> q c j", q=128), w=[("wst", b)])
                p.dma("sync", gt[b][:], gT[n * 16 + cc], w=[("gt", b)])
                p.op("scalar", lambda e, b=b: e.copy(out=wb[b][:], in_=wst[b][:]), r=[("wst", b)], w=[("wb", b)])
                for si in range(5):
                    t0 = si * 512
                    tn = min(512, T - t0)
                    bank = si % 4
                    for c in range(8):
                        p.op("tensor", lambda e, b=b, c=c, t0=t0, tn=tn, bank=bank: e.matmul(
                            out=k.ps[bank][:, :tn], lhsT=wb[b][:, c, :], rhs=on[:, c, t0:t0 + tn],
                            start=(c == 0), stop=(c == 7)), r=[("wb", b), "on"], w=[("ps", bank)])
                    if n == 0:
                        p.op("vector", lambda e, b=b, cc=cc, t0=t0, tn=tn, bank=bank: e.tensor_tensor(
                            out=mT[:, cc, t0:t0 + tn], in0=k.ps[bank][:, :tn], in1=gt[b][:, t0:t0 + tn], op=ALU.mult),
                            r=[("ps", bank), ("gt", b)], w=[("mT", cc, si)])
                    else:
                        tb = si % 2
                        p.op("vector", lambda e, b=b, tb=tb, t0=t0, tn=tn, bank=bank: e.tensor_tensor(
                            out=tmp[tb][:, :tn], in0=k.ps[bank][:, :tn], in1=gt[b][:, t0:t0 + tn], op=ALU.mult),
                            r=[("ps", bank), ("gt", b)], w=[("tmp", tb)])
                        p.op("gpsimd", lambda e, cc=cc, tb=tb, t0=t0, tn=tn: e.tensor_tensor(
                            out=mT[:, cc, t0:t0 + tn], in0=mT[:, cc, t0:t0 + tn], in1=tmp[tb][:, :tn], op=ALU.add),
                            r=[("tmp", tb), ("mT", cc, si)], w=[("mT", cc, si)])
    with k.sub():
        stage = k.sb("wo_st", [128, 16, 512], F32)
        wo = k.sb("wo_wb", [128, 16, 512], BF16)
        mb = [k.sb("wo_mb%d" % i, [128, 16, 128], BF16) for i in range(2)]
        xr = [k.sb("wo_xr%d" % i, [128, 512], F32) for i in range(2)]
        xo = [k.sb("wo_xo%d" % i, [128, 512], F32) for i in range(2)]
        for cb in range(4):
            cs = slice(cb * 512, (cb + 1) * 512)
            p.dma("sync", stage[:], k.I["w_o"][:, cs].rearrange("(c q) n -> q c n", q=128), w=["wost"])
            p.op("scalar", lambda e: e.copy(out=wo[:], in_=stage[:]), r=["wost"], w=["wo"])
            for tt in range(NT):
                n = trows(tt)
                t0 = tt * 128
                b = tt % 2
                bank = tt % 4
                src = k.I["xp"][t0:t0 + 128, cs] if tt < 16 else k.I["xs"][:, cs]
                p.dma("sync", xr[b][:n, :], src, w=[("xr", b)])
                p.op("gpsimd", lambda e, b=b, t0=t0, n=n: e.tensor_copy(out=mb[b][:, :, :n], in_=mT[:, :, t0:t0 + n]),
                     w=[("mb", b)])
                for c in range(16):
                    p.op("tensor", lambda e, b=b, c=c, n=n, bank=bank: e.matmul(
                        out=k.ps[bank][:n, :], lhsT=mb[b][:, c, :n], rhs=wo[:, c, :], start=(c == 0), stop=(c == 15)),
                        r=[("mb", b), "wo"], w=[("ps", bank)])
                p.op("vector", lambda e, b=b, n=n, bank=bank: e.tensor_tensor(
                    out=xo[b][:n, :], in0=k.ps[bank][:n, :], in1=xr[b][:n, :], op=ALU.add),
                    r=[("ps", bank), ("xr", b)], w=[("xo", b)])
                p.dma("sync", x1[t0:t0 + n, cs], xo[b][:n, :], r=[("xo", b)])


PHASES.append(("merge", phase_merge))


ATT_SCALE = 128 ** -0.5


def slices(n_k):
    out = []
    s0 = 0
    while s0 < n_k:
        w = min(512, n_k - s0)
        out.append((s0, w))
        s0 += w
    return out


def topk_thr(k, Iv, Wv, n, n_k, m8, tag, ikeys=()):
    p = k.p
    for r in range(32):
        src = Iv if r == 0 else Wv
        p.op("vector", lambda e, src=src: e.max(out=m8[:n, :], in_=src[:n, :n_k]),
             r=[("I", tag), ("W", tag)] + list(ikeys), w=[("m8", tag)])
        if r < 31:
            p.op("vector", lambda e, src=src: e.match_replace(out=Wv[:n, :n_k], in_to_replace=m8[:n, :],
                                                              in_values=src[:n, :n_k], imm_value=NEG),
                 r=[("m8", tag), ("I", tag)] + list(ikeys), w=[("W", tag)])


def attend(k, st, n, n_k, lhsT, kT, kv, vt, mbv, mbkey, out_ps, out_bank, rkeys):
    p = k.p
    sl = slices(n_k)
    for i, (s0, w) in enumerate(sl):
        bank = i % 4
        p.op("tensor", lambda e, s0=s0, w=w, bank=bank: e.matmul(out=k.ps[bank][:n, :w], lhsT=lhsT,
                                                                 rhs=kT[:, kv, s0:s0 + w], start=True, stop=True),
             r=rkeys, w=[("ps", bank)])
        p.op("vector", lambda e, s0=s0, w=w, bank=bank: e.scalar_tensor_tensor(
            out=st.L[:n, s0:s0 + w], in0=k.ps[bank][:n, :w], scalar=ATT_SCALE, in1=mbv[:n, s0:s0 + w],
            op0=ALU.mult, op1=ALU.add), r=[("ps", bank), mbkey], w=[("L", st.tag)])
    p.op("vector", lambda e: e.tensor_reduce(out=st.mx[:n, :], in_=st.L[:n, :n_k], axis=AX.X, op=ALU.max),
         r=[("L", st.tag)], w=[("mx", st.tag)])
    p.op("vector", lambda e: e.tensor_scalar(out=st.mx[:n, :], in0=st.mx[:n, :], scalar1=-1.0, scalar2=None,
                                             op0=ALU.mult), r=[("mx", st.tag)], w=[("mx", st.tag)])
    p.op("scalar", lambda e: e.activation(out=st.P[:n, :n_k], in_=st.L[:n, :n_k], func=AF.Exp, bias=st.mx[:n, :],
                                          accum_out=st.rs[:n, :]), r=[("L", st.tag), ("mx", st.tag)], w=[("P", st.tag), ("rs", st.tag)])
    p.op("vector", lambda e: e.reciprocal(out=st.rs[:n, :], in_=st.rs[:n, :]), r=[("rs", st.tag)], w=[("rs", st.tag)])
    p.op("vector", lambda e: e.tensor_scalar(out=st.P[:n, :n_k], in0=st.P[:n, :n_k], scalar1=st.rs[:n, :],
                                             scalar2=None, op0=ALU.mult), r=[("rs", st.tag), ("P", st.tag)], w=[("P", st.tag)])
    nj = (n_k + 127) // 128
    for j in range(nj):
        w = min(128, n_k - j * 128)
        bank = (4, 5, 3)[j // 8]
        pv = k.ps[bank][:].bitcast(BF16)
        p.op("tensor", lambda e, j=j, w=w, pv=pv: e.transpose(out=pv[:w, (j % 8) * 128:(j % 8) * 128 + n],
                                                              in_=st.P[:n, j * 128:j * 128 + w],
                                                              identity=k.ident_b[:n, :n]),
             r=[("P", st.tag)], w=[("ps", bank)])
    for half in range((nj + 7) // 8):
        bank = (4, 5, 3)[half]
        pv = k.ps[bank][:].bitcast(BF16)
        cnt = min(8, nj - half * 8)
        rws = min(128, n_k - (half * 8 + cnt - 1) * 128) if cnt == 1 else 128
        srcv = pv.rearrange("p (j t) -> p j t", j=8)[:rws, :cnt, :n]
        dst = st.PT[:rws, half * 8:half * 8 + cnt, :n]
        if half == 0:
            p.op("scalar", lambda e, dst=dst, srcv=srcv: e.copy(out=dst, in_=srcv), r=[("ps", bank)], w=[("PT", half, st.tag)])
        else:
            p.op("vector", lambda e, dst=dst, srcv=srcv: e.tensor_copy(out=dst, in_=srcv), r=[("ps", bank)],
                 w=[("PT", half, st.tag)])
    for j in range(nj):
        w = min(128, n_k - j * 128)
        p.op("tensor", lambda e, j=j, w=w: e.matmul(out=out_ps, lhsT=vt[:w, j, kv * 128:(kv + 1) * 128],
                                                    rhs=st.PT[:w, j, :n], start=(j == 0), stop=(j == nj - 1)),
             r=[("PT", 0, st.tag), ("PT", 1, st.tag), ("PT", 2, st.tag)] + rkeys, w=[("ps", out_bank)])


def phase_dsa(k):
    p, nc = k.p, k.nc
    projT = k.scratch("projT", [43, 128, T], BF16)
    oT = k.scratch("oT", [3, 8, 128, T], BF16)
    vtok = k.scratch("v_tok", [T, 256], BF16)
    iwtok = k.scratch("iw_tok", [T, 16], F32)
    NK = SEQ + DEC
    qT = k.sb("d_qT", [128, 8, T], BF16)
    iqT = k.sb("d_iqT", [128, 8, T], BF16)
    kT = k.sb("d_kT", [128, 2, T], BF16)
    ikT = k.sb("d_ikT", [128, T], BF16)
    p.dma("sync", qT[:], projT[0:8].rearrange("c p t -> p c t"), w=["qT"])
    p.dma("sync", iqT[:], projT[10:18].rearrange("c p t -> p c t"), w=["iqT"])
    p.dma("sync", kT[:], projT[8:10].rearrange("c p t -> p c t"), w=["kT"])
    p.dma("sync", ikT[:], projT[18], w=["ikT"])
    st = K()
    st.tag = 0
    st.L = k.sb("d_L", [128, NK], F32)
    st.P = k.sb("d_P", [128, NK], BF16)
    st.PT = k.sb("d_PT", [128, 17, 128], BF16)
    st.mx = k.sb("d_mx", [128, 1], F32)
    st.rs = k.sb("d_rs", [128, 1], F32)
    Ib = k.sb("d_I", [128, NK], F32)
    Wb = k.sb("d_W", [128, NK], F32)
    mb = k.sb("d_mb", [128, NK], F32)
    rl = [k.sb("d_rl%d" % i, [128, 512], F32) for i in range(4)]
    m8 = k.sb("d_m8", [128, 8], F32)
    oc = [k.sb("d_oc%d" % i, [128, 8, 128], BF16) for i in range(2)]

    def indexer(n, n_k, tcol0, ikv, iw_fn, Iv, rk):
        it = 0
        for hi in range(16):
            c, half = hi // 2, hi % 2
            prt = slice(half * 64, half * 64 + 64)
            for i, (s0, w) in enumerate(slices(n_k)):
                bank = (hi % 2) * 4 + i % 4
                b = it % 4
                it += 1
                p.op("tensor", lambda e, c=c, prt=prt, s0=s0, w=w, bank=bank: e.matmul(
                    out=k.ps[bank][:n, :w], lhsT=iqT[prt, c, tcol0:tcol0 + n], rhs=ikv[prt, s0:s0 + w],
                    start=True, stop=True), r=["iqT"] + rk, w=[("ps", bank)])
                p.op("scalar", lambda e, w=w, bank=bank, b=b: e.activation(out=rl[b][:n, :w], in_=k.ps[bank][:n, :w],
                                                                            func=AF.Relu),
                     r=[("ps", bank)], w=[("rl", b)])
                if hi == 0:
                    p.op("vector", lambda e, s0=s0, w=w, b=b, hi=hi: e.tensor_scalar(
                        out=Iv[:n, s0:s0 + w], in0=rl[b][:n, :w], scalar1=iw_fn(hi), scalar2=None, op0=ALU.mult),
                        r=[("rl", b), "iw"], w=[("Isl", i)])
                else:
                    p.op("vector", lambda e, s0=s0, w=w, b=b, hi=hi: e.scalar_tensor_tensor(
                        out=Iv[:n, s0:s0 + w], in0=rl[b][:n, :w], scalar=iw_fn(hi), in1=Iv[:n, s0:s0 + w],
                        op0=ALU.mult, op1=ALU.add), r=[("rl", b), "iw", ("Isl", i)], w=[("Isl", i)])
        return [("Isl", i) for i in range(len(slices(n_k)))]

    with k.sub():
        vt = k.sb("d_vt", [128, 16, 256], BF16)
        iw = k.sb("d_iw", [128, 16, 16], F32)
        p.dma("sync", vt[:], vtok[0:SEQ, :].rearrange("(j q) n -> q j n", q=128), w=["vt"])
        p.dma("sync", iw[:], iwtok[0:SEQ, :].rearrange("(j q) n -> q j n", q=128), w=["iw"])
        st2 = K()
        st2.tag = 1
        st2.L = k.sb("d_L2", [128, SEQ], F32)
        st2.P = k.sb("d_P2", [128, SEQ], BF16)
        st2.PT = k.sb("d_PT2", [128, 16, 128], BF16)
        st2.mx = k.sb("d_mx2", [128, 1], F32)
        st2.rs = k.sb("d_rs2", [128, 1], F32)
        sts = (st, st2)
        for qi in range(16):
            n_k = 128 * (qi + 1)
            t0 = qi * 128
            ikeys = indexer(128, n_k, t0, ikT, lambda hi, qi=qi: iw[:, qi, hi:hi + 1], Ib, ["ikT"])
            p.op("gpsimd", lambda e, t0=t0: e.affine_select(out=Ib[:, t0:t0 + 128], in_=Ib[:, t0:t0 + 128],
                                                            pattern=[[-1, 128]], compare_op=ALU.is_ge, fill=NEG,
                                                            base=0, channel_multiplier=1),
                 r=ikeys, w=[("I", "p")] + ikeys)
            if qi >= 2:
                topk_thr(k, Ib, Wb, 128, n_k, m8, "p", ikeys)
                p.op("vector", lambda e, n_k=n_k: e.tensor_scalar(out=mb[:, :n_k], in0=Ib[:, :n_k], scalar1=m8[:, 7:8],
                                                                  scalar2=NEG, op0=ALU.is_lt, op1=ALU.mult),
                     r=[("I", "p"), ("m8", "p")] + ikeys, w=["mb"])
            else:
                p.op("vector", lambda e, n_k=n_k: e.tensor_scalar(out=mb[:, :n_k], in0=Ib[:, :n_k], scalar1=-1.0e29,
                                                                  scalar2=NEG, op0=ALU.is_lt, op1=ALU.mult),
                     r=[("I", "p")] + ikeys, w=["mb"])
            ob = oc[qi % 2]
            for h in range(8):
                bank = 6 + h // 4
                attend(k, sts[h % 2], 128, n_k, qT[:, h, t0:t0 + 128], kT, h // 4, vt, mb, "mb",
                       k.ps[bank][:, (h % 4) * 128:(h % 4) * 128 + 128], bank, ["qT", "kT", "vt"])
                if h % 4 == 3:
                    g = h // 4
                    srcv = k.ps[bank][:].rearrange("p (j t) -> p j t", j=4)
                    if g == 0:
                        p.op("scalar", lambda e, ob=ob, srcv=srcv: e.copy(out=ob[:, 0:4, :], in_=srcv),
                             r=[("ps", bank)], w=[("oc", qi % 2, 0)])
                    else:
                        p.op("vector", lambda e, ob=ob, srcv=srcv: e.tensor_copy(out=ob[:, 4:8, :], in_=srcv),
                             r=[("ps", bank)], w=[("oc", qi % 2, 1)])
            p.dma("sync", oT[0, :, :, t0:t0 + 128].rearrange("c q t -> q c t"), ob[:],
                  r=[("oc", qi % 2, 0), ("oc", qi % 2, 1)])

    with k.sub():
        ptb = k.sb("s_ptb", [128, 256], I32)
        ptf = k.sb("s_ptf", [128, 256], F32)
        idx = k.sb("s_idx", [128, 256], U32)
        iop = k.sb("s_iop", [128, 1], F32)
        p.dma("sync", ptb[:], k.I["ptab"].rearrange("b g -> (b g)").partition_broadcast(128), w=["ptb"])
        p.op("gpsimd", lambda e: e.iota(iop[:], pattern=[[0, 1]], base=0, channel_multiplier=1,
                                        allow_small_or_imprecise_dtypes=True), w=["iop"])
        p.op("vector", lambda e: e.tensor_copy(out=ptf[:], in_=ptb[:]), r=["ptb"], w=["ptf"])
        p.op("vector", lambda e: e.tensor_scalar(out=ptf[:], in0=ptf[:], scalar1=128.0, scalar2=iop[:, 0:1],
                                                 op0=ALU.mult, op1=ALU.add), r=["ptf", "iop"], w=["ptf"])
        p.op("vector", lambda e: e.tensor_copy(out=idx[:], in_=ptf[:]), r=["ptf"], w=["idx"])
        iws = k.sb("s_iw", [4, 16, 16], F32)
        p.dma("sync", iws[:], iwtok[SEQ:T, :].rearrange("(b t) h -> t b h", t=4), w=["iw"])
        Iall = k.sb("s_Iall", [64, NK], F32)
        Wall = Wb
        mball = mb
        sub1 = k.sub()
        sub1.__enter__()
        pg = [k.sb("s_pg%d" % i, [128, 128], F32) for i in range(4)]
        ikTb = [k.sb("s_ikTb%d" % i, [128, NK], BF16) for i in range(2)]
        for b in range(NS_SEQ):
            ib = ikTb[b % 2]
            for g in range(16):
                col = b * 16 + g
                t = pg[g % 4]
                for hf in range(2):
                    p.op("gpsimd", lambda e, t=t, hf=hf, col=col: e.indirect_dma_start(
                        out=t[:, hf * 64:(hf + 1) * 64], out_offset=None, in_=k.I["cache_ik"][:, :],
                        in_offset=bass.IndirectOffsetOnAxis(ap=idx[:, col:col + 1], axis=0)),
                        r=["idx"], w=[("pg", g % 4, hf)], dma=True)
                bank = 4 + (g // 4) % 2
                p.op("tensor", lambda e, t=t, g=g, bank=bank: e.transpose(
                    out=k.ps[bank][:, (g % 4) * 128:(g % 4) * 128 + 128], in_=t[:, :], identity=k.ident_f[:, :]),
                    r=[("pg", g % 4, 0), ("pg", g % 4, 1)], w=[("ps", bank)])
                if g % 4 == 3:
                    g0 = g - 3
                    p.op("scalar", lambda e, ib=ib, g0=g0, bank=bank: e.copy(out=ib[:, g0 * 128:g0 * 128 + 512],
                                                                            in_=k.ps[bank][:, :]),
                         r=[("ps", bank)], w=[("ikTb", b % 2)])
            p.op("vector", lambda e, ib=ib, b=b: e.tensor_copy(out=ib[:, SEQ:NK], in_=ikT[:, SEQ + 4 * b:SEQ + 4 * b + 4]),
                 r=["ikT"], w=[("ikTb", b % 2)])
            ikeys = indexer(4, NK, SEQ + 4 * b, ib, lambda hi, b=b: iws[:, b, hi:hi + 1], Ib, [("ikTb", b % 2)])
            p.op("gpsimd", lambda e: e.affine_select(out=Ib[:4, SEQ:NK], in_=Ib[:4, SEQ:NK], pattern=[[-1, 4]],
                                                     compare_op=ALU.is_ge, fill=NEG, base=0, channel_multiplier=1),
                 r=ikeys, w=[("I", "s")] + ikeys)
            p.dma("sync", Iall[4 * b:4 * b + 4, :], Ib[:4, :], r=[("I", "s")] + ikeys, w=[("I", "all")])
        sub1.__exit__(None, None, None)
        topk_thr(k, Iall, Wall, 64, NK, m8, "all")
        p.op("vector", lambda e: e.tensor_scalar(out=mball[:64, :], in0=Iall[:, :], scalar1=m8[:64, 7:8], scalar2=NEG,
                                                 op0=ALU.is_lt, op1=ALU.mult), r=[("I", "all"), ("m8", "all")],
             w=["mball"])
        kTb = [k.sb("s_kTb%d" % i, [128, 2, NK], BF16) for i in range(2)]
        vtb = [k.sb("s_vtb%d" % i, [128, 17, 256], BF16) for i in range(2)]
        kpg = [k.sb("s_kpg%d" % i, [128, 256], F32) for i in range(4)]
        vpg = [k.sb("s_vpg%d" % i, [128, 256], F32) for i in range(4)]
        mb16 = [k.sb("s_mb16%d" % i, [16, NK], F32) for i in range(2)]
        ocs = [k.sb("s_ocs%d" % i, [128, 8, 4], BF16) for i in range(2)]
        qs16 = [k.sb("s_qs16%d" % i, [128, 32], BF16) for i in range(2)]
        for b in range(NS_SEQ):
            kb, vb = kTb[b % 2], vtb[b % 2]
            for g in range(16):
                col = b * 16 + g
                tk, tv = kpg[g % 4], vpg[g % 4]
                p.op("gpsimd", lambda e, tk=tk, col=col: e.indirect_dma_start(
                    out=tk[:, :], out_offset=None, in_=k.I["cache_k"][:, :],
                    in_offset=bass.IndirectOffsetOnAxis(ap=idx[:, col:col + 1], axis=0)),
                    r=["idx"], w=[("kpg", g % 4)], dma=True)
                p.op("gpsimd", lambda e, tv=tv, col=col: e.indirect_dma_start(
                    out=tv[:, :], out_offset=None, in_=k.I["cache_v"][:, :],
                    in_offset=bass.IndirectOffsetOnAxis(ap=idx[:, col:col + 1], axis=0)),
                    r=["idx"], w=[("vpg", g % 4)], dma=True)
                p.op("vector", lambda e, vb=vb, g=g, tv=tv: e.tensor_copy(out=vb[:, g, :], in_=tv[:, :]),
                     r=[("vpg", g % 4)], w=[("vtb", b % 2)])
                bank = 4 + g % 2
                for kv in range(2):
                    p.op("tensor", lambda e, tk=tk, kv=kv, bank=bank: e.transpose(
                        out=k.ps[bank][:, kv * 128:(kv + 1) * 128], in_=tk[:, kv * 128:(kv + 1) * 128],
                        identity=k.ident_f[:, :]), r=[("kpg", g % 4)], w=[("ps", bank)])
                p.op("scalar", lambda e, kb=kb, g=g, bank=bank: e.copy(
                    out=kb[:, :, g * 128:(g + 1) * 128], in_=k.ps[bank][:, 0:256].rearrange("p (v s) -> p v s", v=2)),
                    r=[("ps", bank)], w=[("kTb", b % 2)])
            tc0 = SEQ + 4 * b
            p.op("vector", lambda e, kb=kb, tc0=tc0: e.tensor_copy(out=kb[:, :, SEQ:NK], in_=kT[:, :, tc0:tc0 + 4]),
                 r=["kT"], w=[("kTb", b % 2)])
            p.dma("sync", vb[:4, 16, :], vtok[tc0:tc0 + 4, :], w=[("vtb", b % 2)])
            m16 = mb16[b % 2]
            for i in range(4):
                p.dma("sync", m16[4 * i:4 * i + 4, :], mball[4 * b:4 * b + 4, :], r=["mball"], w=[("mb16", b % 2)])
            ob = ocs[b % 2]
            qs = qs16[b % 2]
            p.op("vector", lambda e, qs=qs, tc0=tc0: e.tensor_copy(
                out=qs[:, :].rearrange("p (h t) -> p h t", t=4), in_=qT[:, :, tc0:tc0 + 4]),
                r=["qT"], w=[("qs16", b % 2)])
            for kv in range(2):
                bank = 6 + kv
                attend(k, st, 16, NK, qs[:, kv * 16:kv * 16 + 16], kb, kv, vb, m16, ("mb16", b % 2),
                       k.ps[bank][:, 0:16], bank, [("qs16", b % 2), ("kTb", b % 2), ("vtb", b % 2)])
                p.op("vector", lambda e, ob=ob, kv=kv, bank=bank: e.tensor_copy(
                    out=ob[:, kv * 4:kv * 4 + 4, :], in_=k.ps[bank][:, 0:16].rearrange("p (h t) -> p h t", h=4)),
                    r=[("ps", bank)], w=[("ocs", b % 2, kv)])
            p.dma("sync", oT[0, :, :, tc0:tc0 + 4].rearrange("c q t -> q c t"), ob[:],
                  r=[("ocs", b % 2, 0), ("ocs", b % 2, 1)])


PHASES.append(("dsa", phase_dsa))


def bc(ap, shape):
    return ap.to_broadcast(shape)


def phase_peer(k):
    p, nc = k.p, k.nc
    x1 = k.scratch("x1", [T, D], F32)
    xn2f = k.scratch("xn2f", [T, D], F32)
    norm_stats(k)
    qpT = k.sb("pe_qpT", [128, 16, T], BF16)
    skT = k.sb("pe_skT", [128, 16, 128], BF16)
    with k.sub():
        xn2T = k.sb("pe_xn2T", [128, 16, T], BF16)
        with k.sub():
            g = load_bcast(k, "gf_bc", k.I["g_ffn"], D)
            xt = [k.sb("pxt%d" % i, [128, D], F32) for i in range(2)]
            xn = [k.sb("pxn%d" % i, [128, D], BF16) for i in range(2)]
            xf = [k.sb("pxf%d" % i, [128, D], F32) for i in range(2)]
            for tt in range(NT):
                b = tt % 2
                n = trows(tt)
                t0 = tt * 128
                p.dma("sync", xt[b][:n, :], x1[t0:t0 + n, :], w=[("xt", b)])
                rms_tile(k, xt[b], n, g, "gf_bc", xn[b], b, tt)
                p.op("gpsimd", lambda e, b=b, n=n: e.tensor_copy(out=xf[b][:n, :], in_=xn[b][:n, :]),
                     r=[("xn", b)], w=[("xf", b)])
                p.dma("sync", xn2f[t0:t0 + n, :], xf[b][:n, :], r=[("xf", b)])
                transpose_to_T(k, xn[b], n, b, xn2T, t0, b * 2)
            skt = [k.sb("pskt%d" % i, [128, 128], F32) for i in range(2)]
            for j in range(16):
                h, pp = j // 2, j % 2
                r0 = (pp * 8 + h) * 128
                p.dma("sync", skt[j % 2][:], k.I["sub_keys"][r0:r0 + 128, :], w=[("skt", j % 2)])
                bank = 4 + j % 2
                p.op("tensor", lambda e, j=j, bank=bank: e.transpose(out=k.ps[bank][:, 0:128], in_=skt[j % 2][:],
                                                                     identity=k.ident_f[:]),
                     r=[("skt", j % 2)], w=[("ps", bank)])
                p.op("vector", lambda e, j=j, bank=bank: e.tensor_copy(out=skT[:, j, :], in_=k.ps[bank][:, 0:128]),
                     r=[("ps", bank)], w=["skT"])

        def evac_q(ci, si, ps, bank, rows, t0, tn):
            if si % 2 == 0:
                p.op("scalar", lambda e: e.copy(out=qpT[:, ci, t0:t0 + tn], in_=ps[:, :tn]), r=[("ps", bank)],
                     w=[("qpT", ci, si)])
            else:
                p.op("vector", lambda e: e.tensor_copy(out=qpT[:, ci, t0:t0 + tn], in_=ps[:, :tn]), r=[("ps", bank)],
                     w=[("qpT", ci, si)])
        linear_fm(k, xn2T, [], "w_pq", [[(c0, 128)] for c0 in range(0, D, 128)], evac_q, "pq")

    with k.sub():
        sc = k.sb("pe_sc", [128, 16, 128], F32)
        scw = k.sb("pe_scw", [128, 16, 128], F32)
        vv = k.sb("pe_v", [128, 16, 16], F32)
        ix = k.sb("pe_ix", [128, 16, 16], U32)
        ixf = k.sb("pe_ixf", [128, 16, 16], F32)
        cand = k.sb("pe_cand", [128, 8, 256], F32)
        candw = k.sb("pe_candw", [128, 8, 256], F32)
        tv = k.sb("pe_tv", [128, 8, 16], F32)
        pos = k.sb("pe_pos", [128, 8, 16], U32)
        pa = k.sb("pe_pa", [128, 8, 16], U32)
        pb = k.sb("pe_pb", [128, 8, 16], U32)
        paf = k.sb("pe_paf", [128, 8, 16], F32)
        pbf = k.sb("pe_pbf", [128, 8, 16], F32)
        eq = k.sb("pe_eq", [128, 8, 16, 16], F32)
        sel = k.sb("pe_sel", [128, 2, 8, 16], F32)
        ef = k.sb("pe_ef", [128, 128], F32)
        eidx = k.sb("pe_eidx", [128, 128], U32)
        gw = k.sb("pe_gw", [128, 8, 16], F32)
        gs = k.sb("pe_gs", [128, 8], F32)
        act = k.sb("pe_act", [128, 128], F32)
        ga = k.sb("pe_ga", [128, 128], F32)
        t1 = k.sb("pe_t1", [128, 128], F32)
        t2 = k.sb("pe_t2", [128, 128], F32)
        io16 = k.sb("pe_io16", [128, 16], F32)
        gb = [k.sb("pe_gb%d" % i, [128, D], BF16) for i in range(8)]
        ubf = k.scratch("peer_u_bf", [16384, D], BF16)
        vbf = k.scratch("peer_v_bf", [16384, D], BF16)
        xq = k.sb("pe_xq", [128, D], F32)
        x1t = k.sb("pe_x1t", [128, D], F32)
        acc = k.sb("pe_acc", [128, D], F32)
        junkf = k.sb("pe_junkf", [128, D], F32)
        yo = k.sb("pe_yo", [128, D], F32)
        gfin = load_bcast(k, "gfin_bc", k.I["g_final"], D)
        p.op("gpsimd", lambda e: e.iota(io16[:], pattern=[[1, 16]], base=0, channel_multiplier=0,
                                        allow_small_or_imprecise_dtypes=True), w=["io16"])
        S4 = [128, 8, 16, 16]
        def tile_body(tt, n, t0):
            p.dma("sync", xq[:n, :], xn2f[t0:t0 + n, :], w=["xq"])
            p.dma("sync", x1t[:n, :], x1[t0:t0 + n, :], w=[("xt", "f")])
            for j in range(16):
                bank = j // 4
                p.op("tensor", lambda e, j=j, bank=bank: e.matmul(
                    out=k.ps[bank][:n, (j % 4) * 128:(j % 4) * 128 + 128], lhsT=qpT[:, j, t0:t0 + n], rhs=skT[:, j, :],
                    start=True, stop=True), r=["skT"], w=[("ps", bank)])
            for bank in range(4):
                dst = sc[:n, bank * 4:bank * 4 + 4, :]
                srcv = k.ps[bank][:n, :].rearrange("p (j q) -> p j q", j=4)
                if bank % 2 == 0:
                    p.op("scalar", lambda e, dst=dst, srcv=srcv: e.copy(out=dst, in_=srcv), r=[("ps", bank)],
                         w=[("sc", bank)])
                else:
                    p.op("vector", lambda e, dst=dst, srcv=srcv: e.tensor_copy(out=dst, in_=srcv), r=[("ps", bank)],
                         w=[("sc", bank)])
            for j in range(16):
                kj = [("sc", j // 4)]
                p.op("vector", lambda e, j=j: e.max(out=vv[:n, j, 0:8], in_=sc[:n, j, :]), r=kj, w=[("vv", j)])
                p.op("vector", lambda e, j=j: e.max_index(out=ix[:n, j, 0:8], in_max=vv[:n, j, 0:8], in_values=sc[:n, j, :]),
                     r=kj + [("vv", j)], w=[("ix", j)])
                p.op("vector", lambda e, j=j: e.match_replace(out=scw[:n, j, :], in_to_replace=vv[:n, j, 0:8],
                                                              in_values=sc[:n, j, :], imm_value=NEG),
                     r=kj + [("vv", j)], w=[("scw", j)])
                p.op("vector", lambda e, j=j: e.max(out=vv[:n, j, 8:16], in_=scw[:n, j, :]), r=[("scw", j)],
                     w=[("vv", j)])
                p.op("vector", lambda e, j=j: e.max_index(out=ix[:n, j, 8:16], in_max=vv[:n, j, 8:16],
                                                          in_values=scw[:n, j, :]),
                     r=[("scw", j), ("vv", j)], w=[("ix", j)])
            allv = [("vv", j) for j in range(16)]
            alli = [("ix", j) for j in range(16)]
            p.op("vector", lambda e: e.tensor_copy(out=ixf[:n], in_=ix[:n]), r=alli, w=["ixf"])
            v4 = vv[:n].rearrange("p (h two) a -> p h two a", two=2)
            i4 = ixf[:n].rearrange("p (h two) a -> p h two a", two=2)
            S4n = [n, 8, 16, 16]
            p.op("vector", lambda e: e.tensor_tensor(
                out=cand[:n].rearrange("p h (a b) -> p h a b", a=16), in0=bc(v4[:, :, 0, :].unsqueeze(3), S4n),
                in1=bc(v4[:, :, 1, :].unsqueeze(2), S4n), op=ALU.add), r=allv, w=["cand"])
            for h in range(8):
                p.op("vector", lambda e, h=h: e.max(out=tv[:n, h, 0:8], in_=cand[:n, h, :]), r=["cand"], w=[("tv", h)])
                p.op("vector", lambda e, h=h: e.max_index(out=pos[:n, h, 0:8], in_max=tv[:n, h, 0:8],
                                                          in_values=cand[:n, h, :]), r=["cand", ("tv", h)],
                     w=[("pos", h)])
                p.op("vector", lambda e, h=h: e.match_replace(out=candw[:n, h, :], in_to_replace=tv[:n, h, 0:8],
                                                              in_values=cand[:n, h, :], imm_value=NEG),
                     r=["cand", ("tv", h)], w=[("candw", h)])
                p.op("vector", lambda e, h=h: e.max(out=tv[:n, h, 8:16], in_=candw[:n, h, :]), r=[("candw", h)],
                     w=[("tv", h)])
                p.op("vector", lambda e, h=h: e.max_index(out=pos[:n, h, 8:16], in_max=tv[:n, h, 8:16],
                                                          in_values=candw[:n, h, :]), r=[("candw", h), ("tv", h)],
                     w=[("pos", h)])
            allt = [("tv", h) for h in range(8)]
            allp = [("pos", h) for h in range(8)]
            p.op("vector", lambda e: e.tensor_tensor(out=gw[:n], in0=tv[:n], in1=bc(tv[:n, :, 0:1], [n, 8, 16]),
                                                     op=ALU.subtract), r=allt, w=["gw"])
            p.op("scalar", lambda e: e.activation(out=gw[:n], in_=gw[:n], func=AF.Exp), r=["gw"], w=["gw"])
            p.op("vector", lambda e: e.tensor_reduce(out=gs[:n, :], in_=gw[:n], axis=AX.X, op=ALU.add), r=["gw"],
                 w=["gs"])
            p.op("vector", lambda e: e.reciprocal(out=gs[:n, :], in_=gs[:n, :]), r=["gs"], w=["gs"])
            p.op("vector", lambda e: e.tensor_tensor(out=gw[:n], in0=gw[:n], in1=bc(gs[:n, :].unsqueeze(2), [n, 8, 16]),
                                                     op=ALU.mult), r=["gs", "gw"], w=["gw"])
            p.op("vector", lambda e: e.tensor_single_scalar(out=pa[:n], in_=pos[:n], scalar=4,
                                                            op=ALU.logical_shift_right), r=allp, w=["pa"])
            p.op("vector", lambda e: e.tensor_single_scalar(out=pb[:n], in_=pos[:n], scalar=15, op=ALU.bitwise_and),
                 r=allp, w=["pb"])
            p.op("vector", lambda e: e.tensor_copy(out=paf[:n], in_=pa[:n]), r=["pa"], w=["paf"])
            p.op("vector", lambda e: e.tensor_copy(out=pbf[:n], in_=pb[:n]), r=["pb"], w=["pbf"])
            for w_, pf in ((0, paf), (1, pbf)):
                p.op("vector", lambda e, pf=pf: e.tensor_tensor(
                    out=eq[:n], in0=bc(pf[:n].unsqueeze(3), S4n), in1=bc(io16[:n, :].unsqueeze(1).unsqueeze(1), S4n),
                    op=ALU.is_equal), r=["paf", "pbf", "io16", "sel"], w=["eq"])
                p.op("vector", lambda e, w_=w_: e.tensor_tensor(
                    out=eq[:n], in0=eq[:n], in1=bc(i4[:, :, w_, :].unsqueeze(2), S4n), op=ALU.mult),
                    r=["eq", "ixf"], w=["eq"])
                p.op("vector", lambda e, w_=w_: e.tensor_reduce(out=sel[:n, w_], in_=eq[:n], axis=AX.X, op=ALU.add),
                     r=["eq"], w=["sel"])
            p.op("vector", lambda e: e.scalar_tensor_tensor(
                out=ef[:n, :], in0=sel[:n, 0].rearrange("p h k -> p (h k)"), scalar=128.0,
                in1=sel[:n, 1].rearrange("p h k -> p (h k)"), op0=ALU.mult, op1=ALU.add), r=["sel"], w=["ef"])
            p.op("vector", lambda e: e.tensor_copy(out=eidx[:n, :], in_=ef[:n, :]), r=["ef"], w=["eidx"])
            p.op("vector", lambda e: e.memset(act[:], 0.0), r=["ga"], w=[("act", s_) for s_ in range(128)])
            for s_ in range(128):
                b = s_ % 8
                p.op("gpsimd", lambda e, s_=s_, b=b: e.indirect_dma_start(
                    out=gb[b][:n, :], out_offset=None, in_=ubf[:, :],
                    in_offset=bass.IndirectOffsetOnAxis(ap=eidx[:n, s_:s_ + 1], axis=0)),
                    r=["eidx"], w=[("gb", b)], dma=True)
                p.op("vector", lambda e, s_=s_, b=b: e.scalar_tensor_tensor(
                    out=junkf[:n, :], in0=gb[b][:n, :], scalar=1.0, in1=xq[:n, :], op0=ALU.mult, op1=ALU.mult,
                    accum_out=act[:n, s_:s_ + 1]), r=[("gb", b), "xq"], w=[("act", s_), "junkf"])
            gelu_mul(k, act[:n, :], gw[:n].rearrange("p h k -> p (h k)"), ga[:n, :], n, t1[:n, :], t2[:n, :],
                     [("act", s_) for s_ in range(128)] + ["gw"], "ga")
            for s_ in range(128):
                b = s_ % 8
                p.op("gpsimd", lambda e, s_=s_, b=b: e.indirect_dma_start(
                    out=gb[b][:n, :], out_offset=None, in_=vbf[:, :],
                    in_offset=bass.IndirectOffsetOnAxis(ap=eidx[:n, s_:s_ + 1], axis=0)),
                    r=["eidx"], w=[("gb", b)], dma=True)
                if s_ == 0:
                    p.op("vector", lambda e, b=b: e.scalar_tensor_tensor(
                        out=acc[:n, :], in0=gb[b][:n, :], scalar=ga[:n, 0:1], in1=x1t[:n, :], op0=ALU.mult,
                        op1=ALU.add), r=[("gb", b), "ga", ("xt", "f")], w=["acc"])
                else:
                    p.op("vector", lambda e, s_=s_, b=b: e.scalar_tensor_tensor(
                        out=acc[:n, :], in0=gb[b][:n, :], scalar=ga[:n, s_:s_ + 1], in1=acc[:n, :], op0=ALU.mult,
                        op1=ALU.add), r=[("gb", b), "ga", "acc"], w=["acc"])
            if tt == 0 and "dbg" in DEBUG_IO:
                dbg = k.scratch("dbg", [6, 128, 128], F32)
                p.dma("sync", dbg[0], ef[:, :], r=["ef", "eidx"])
                p.dma("sync", dbg[1], gw[:].rearrange("p h k -> p (h k)"), r=["gw", "ga"])
                p.dma("sync", dbg[2], act[:, :], r=["ga"])
                p.dma("sync", dbg[3], ga[:, :], r=["ga"])
                p.dma("sync", dbg[4], tv[:].rearrange("p h k -> p (h k)"), r=allt + ["gw"])
                p.dma("sync", dbg[5], paf[:].rearrange("p h k -> p (h k)"), r=["paf", "eq"])
            p.op("vector", lambda e: e.tensor_copy(out=x1t[:n, :], in_=acc[:n, :]), r=["acc"], w=[("xt", "f")])
            rms_tile(k, x1t, n, gfin, "gfin_bc", yo, "f", 32 + tt)
            dst = k.O["y_p"][t0:t0 + 128, :] if tt < 16 else k.O["y_s"][:, :]
            p.dma("sync", dst, yo[:n, :], r=[("xn", "f")])

        for tt in range(NT):
            tile_body(tt, trows(tt), tt * 128)


PHASES.append(("peer", phase_peer))


def phase_cast(k):
    p = k.p
    stg = [k.sb("ct_st%d" % i, [128, 8192], F32) for i in range(2)]
    obf = [k.sb("ct_ob%d" % i, [128, 8192], BF16) for i in range(2)]
    it = 0
    for name in ("peer_u", "peer_v"):
        dst = k.scratch(name + "_bf", [16384, D], BF16)
        src = k.I[name].rearrange("(c q r) d -> c q (r d)", q=128, r=4)
        dstv = dst.rearrange("(c q r) d -> c q (r d)", q=128, r=4)
        for c in range(32):
            b = it % 2
            eng = ("scalar", "vector", "gpsimd")[it % 3]
            it += 1
            p.dma("sync", stg[b][:], src[c], w=[("cst", b)])
            if eng == "scalar":
                p.op("scalar", lambda e, b=b: e.copy(out=obf[b][:], in_=stg[b][:]), r=[("cst", b)], w=[("cob", b)])
            else:
                p.op(eng, lambda e, b=b: e.tensor_copy(out=obf[b][:], in_=stg[b][:]), r=[("cst", b)], w=[("cob", b)])
            p.dma("sync", dstv[c], obf[b][:], r=[("cob", b)])


PHASES.append(("cast", phase_cast))
```

```python
import numpy as np
from contextlib import ExitStack
import concourse.bass as bass
import concourse.mybir as mybir
from concourse.bass_utils import run_bass_kernel_spmd

F32 = mybir.dt.float32
BF16 = mybir.dt.bfloat16
I32 = mybir.dt.int32
U32 = mybir.dt.uint32
AF = mybir.ActivationFunctionType
ALU = mybir.AluOpType
AX = mybir.AxisListType

NCORES = 8
D = 2048
SEQ = 2048
NS_SEQ = 16
DEC = 4
TS = NS_SEQ * DEC
T = SEQ + TS
NT = 17
N_IN = 5712
EPS = 1e-6
NEG = -1.0e30
O_Q, O_K, O_V, O_IQ, O_IK, O_IW, O_XB, O_YB, O_QM = 0, 1024, 1280, 1536, 2560, 2624, 2640, 3664, 4688


def trows(tt):
    return 128 if tt < 16 else 64


class _Op:
    __slots__ = ("eng", "idx", "fn", "dma", "deps", "signaled", "cum", "sem", "tgt", "prev_tgt", "k")

    def __init__(self, eng, idx, fn, dma):
        self.eng = eng
        self.idx = idx
        self.fn = fn
        self.dma = dma
        self.deps = []
        self.signaled = False
        self.cum = 0
        self.sem = None
        self.tgt = 0
        self.prev_tgt = 0
        self.k = 0


class _Res:
    __slots__ = ("lw", "rd")

    def __init__(self):
        self.lw = None
        self.rd = []


class Prog:
    ENG = ("tensor", "vector", "scalar", "gpsimd", "sync")
    NS = 12

    def __init__(self, nc):
        self.nc = nc
        self.ops = {e: [] for e in self.ENG}
        self.res = {}
        self.ndma = {e: 0 for e in self.ENG}

    def _r(self, k):
        r = self.res.get(k)
        if r is None:
            r = self.res[k] = _Res()
        return r

    def op(self, eng, fn, r=(), w=(), dma=False):
        o = _Op(eng, len(self.ops[eng]), fn, dma)
        deps = {}
        for k in r:
            rr = self._r(k)
            if rr.lw is not None:
                deps[id(rr.lw)] = (rr.lw, True)
        for k in w:
            rr = self._r(k)
            if rr.lw is not None:
                deps[id(rr.lw)] = (rr.lw, True)
            for x in rr.rd:
                if id(x) not in deps:
                    deps[id(x)] = (x, False)
        for d, hard in deps.values():
            if d is o:
                continue
            if d.dma or o.dma or d.eng != o.eng:
                o.deps.append(d)
            elif hard and o.eng != "tensor":
                o.deps.append(d)
        for k in r:
            self._r(k).rd.append(o)
        for k in w:
            rr = self._r(k)
            rr.lw = o
            rr.rd = []
        if dma:
            o.k = self.ndma[eng]
            self.ndma[eng] += 1
        self.ops[eng].append(o)
        return o

    def dma(self, eng, out, in_, r=(), w=(), **kw):
        return self.op(eng, lambda e: e.dma_start(out=out, in_=in_, **kw), r=r, w=w, dma=True)

    def barrier(self):
        last = []
        for e in self.ENG:
            for o in reversed(self.ops[e]):
                if o.fn is not None and not o.dma:
                    last.append(o)
                    break
        alld = []
        for e in self.ENG:
            seen = 0
            for o in reversed(self.ops[e]):
                if o.dma:
                    alld.append(o)
                    seen += 1
                    if seen >= self.NS:
                        break
        for e in self.ENG:
            o = _Op(e, len(self.ops[e]), None, False)
            o.deps = [d for d in last + alld if d.eng != e or d.dma or e != "tensor"]
            self.ops[e].append(o)
        self.res = {}

    def emit(self, es):
        nc = self.nc
        sem_e = {e: es.enter_context(nc.semaphore("se_" + e)) for e in self.ENG}
        sem_d = {e: [es.enter_context(nc.semaphore("sd_%s%d" % (e, i))) for i in range(self.NS)]
                 for e in self.ENG if self.ndma[e]}
        for e in self.ENG:
            for o in self.ops[e]:
                for d in o.deps:
                    d.signaled = True
        for e in self.ENG:
            c = 0
            for o in self.ops[e]:
                if o.dma:
                    o.sem = sem_d[e][o.k % self.NS]
                    o.tgt = 16 * (o.k // self.NS + 1)
                    o.prev_tgt = o.tgt - 16
                elif o.signaled:
                    c += 1
                    o.cum = c
        finals = {}
        for e in self.ENG:
            f = {}
            for o in self.ops[e]:
                if o.dma:
                    f[o.k % self.NS] = (o.sem, o.tgt)
            finals[e] = list(f.values())
        block = es.enter_context(nc.Block())

        def mk(e):
            def body(eng):
                seen = {}
                for o in self.ops[e]:
                    waits = {}
                    for d in o.deps:
                        if d.dma:
                            key, sem, v = ("d", d.eng, d.k % self.NS), d.sem, d.tgt
                        else:
                            key, sem, v = ("e", d.eng), sem_e[d.eng], d.cum
                        if seen.get(key, 0) >= v:
                            continue
                        if key not in waits or waits[key][1] < v:
                            waits[key] = (sem, v)
                    if o.dma and o.prev_tgt > 0:
                        key = ("d", e, o.k % self.NS)
                        if seen.get(key, 0) < o.prev_tgt:
                            waits[key] = (o.sem, max(o.prev_tgt, waits.get(key, (None, 0))[1]))
                    for key, (sem, v) in waits.items():
                        eng.wait_ge(sem, v)
                        seen[key] = v
                    if o.fn is None:
                        continue
                    ins = o.fn(eng)
                    if o.dma:
                        ins.then_inc(o.sem, 16)
                    elif o.signaled:
                        ins.then_inc(sem_e[e], 1)
                for sem, v in finals[e]:
                    eng.wait_ge(sem, v)
            return body

        for e in self.ENG:
            getattr(block, e)(mk(e))


IN_SPECS = [
    ("xp", [SEQ, D], F32), ("xs", [TS, D], F32),
    ("cache_k", [2560 * 128, 256], F32), ("cache_v", [2560 * 128, 256], F32),
    ("cache_ik", [2560 * 128, 64], F32), ("ptab", [NS_SEQ, 16], I32),
    ("st_conv", [NS_SEQ * 3, 1024], F32), ("st_lru", [NS_SEQ, 1024], F32),
    ("cmk", [NS_SEQ * 256, 1024], F32), ("cmv", [NS_SEQ * 256, 1024], F32),
    ("mem", [256, D], F32),
    ("g_mix", [1, D], F32), ("w_in", [D, N_IN], F32), ("conv_w", [4, 1024], F32), ("conv_b", [1, 1024], F32),
    ("w_rg", [1024, 128], F32), ("b_rg", [1, 1024], F32), ("w_ig", [1024, 128], F32), ("b_ig", [1, 1024], F32),
    ("lam", [1, 1024], F32), ("g_mem", [1, D], F32), ("w_mem_kv", [D, 2048], F32),
    ("w_gate", [D, 3 * D], F32), ("w_br", [3 * 1024, D], F32), ("w_o", [D, D], F32), ("g_ffn", [1, D], F32),
    ("w_pq", [D, 2048], F32), ("sub_keys", [2 * 8 * 128, 128], F32), ("peer_u", [16384, D], F32),
    ("peer_v", [16384, D], F32), ("g_final", [1, D], F32),
]
OUT_SPECS = [
    ("y_p", [SEQ, D]), ("y_s", [TS, D]), ("k_p", [SEQ, 256]), ("v_p", [SEQ, 256]), ("ik_p", [SEQ, 64]),
    ("conv_p", [3, 1024]), ("h_p", [1, 1024]), ("mk_p", [256, 1024]), ("mv_p", [256, 1024]),
    ("k_s", [TS, 256]), ("v_s", [TS, 256]), ("ik_s", [TS, 64]), ("conv_s", [NS_SEQ * 3, 1024]),
    ("h_s", [NS_SEQ, 1024]),
]


_INS = {n: (s, d) for n, s, d in IN_SPECS}
_OUTS = {n: s for n, s in OUT_SPECS}


class _Lazy(dict):
    def __init__(self, mk):
        super().__init__()
        self.mk = mk

    def __missing__(self, n):
        v = self[n] = self.mk(n)
        return v


DEBUG_IO = {}


class K:
    def sub(self):
        return _Sub(self)


class _Sub:
    def __init__(self, k):
        self.k = k

    def __enter__(self):
        self.prev = self.k.scope
        self.st = ExitStack()
        self.k.scope = self.st
        return self

    def __exit__(self, *a):
        self.k.p.barrier()
        self.st.close()
        self.k.scope = self.prev
        return False


def build(phases=("all",)):
    nc = bass.Bass("TRN2", target_bir_lowering=False)
    es = ExitStack()
    k = K()
    k.nc = nc
    k.es = es
    k.I = _Lazy(lambda n: nc.dram_tensor(n, _INS[n][0], _INS[n][1], kind="ExternalInput").ap())
    k.O = _Lazy(lambda n: nc.dram_tensor(n, _OUTS[n], F32, kind="ExternalOutput").ap())
    k.S = {}
    p = k.p = Prog(nc)

    def scratch(name, shape, dt):
        if name not in k.S:
            kind = DEBUG_IO.get(name)
            if kind:
                k.S[name] = nc.dram_tensor(name, shape, dt, kind=kind).ap()
            else:
                k.S[name] = nc.dram_tensor(name, shape, dt).ap()
        return k.S[name]
    k.scratch = scratch
    k.scope = es

    k.nsb = 0

    def sb(name, shape, dt):
        k.nsb += 1
        return k.scope.enter_context(nc.sbuf_tensor("%s_%d" % (name, k.nsb), shape, dt))
    k.sb = sb
    k.ps = [es.enter_context(nc.psum_tensor("ps%d" % i, [128, 512], F32)) for i in range(8)]

    k.ident_f = sb("ident_f", [128, 128], F32)
    k.ident_b = sb("ident_b", [128, 128], BF16)
    k.ones_f = sb("ones_f", [128, 128], F32)
    p.op("gpsimd", lambda e: e.memset(k.ones_f[:], 1.0), w=["ones_f"])
    p.op("gpsimd", lambda e: e.affine_select(out=k.ident_f[:], in_=k.ones_f[:], pattern=[[-1, 128]],
                                             compare_op=ALU.is_equal, fill=0.0, base=0, channel_multiplier=1),
         r=["ones_f"], w=["ident_f"])
    p.op("vector", lambda e: e.tensor_copy(out=k.ident_b[:], in_=k.ident_f[:]), r=["ident_f"], w=["ident_b"])
    p.barrier()

    def run_phase(fn, *a):
        with ExitStack() as sc:
            k.scope = sc
            fn(k, *a)
            p.barrier()
        k.scope = es

    if "proj" in phases or "all" in phases:
        run_phase(phase_proj)
    pd = dict(PHASES)
    for name in ("cast", "mem", "lru", "dsa", "merge", "peer"):
        if name in phases or "all" in phases:
            run_phase(pd[name])
    p.emit(es)
    es.close()
    nc._used_in = list(k.I.keys())
    nc._used_out = list(k.O.keys())
    return nc


def load_bcast(k, name, dram_row, n):
    t = k.sb(name, [128, n], F32)
    k.p.dma("sync", t[:], dram_row[0, :].partition_broadcast(128), w=[name])
    return t


def rms_tile(k, xt, n, g, gkey, xn_out, tag, col):
    p = k.p
    ss, rs, junk = k.ss, k.rs, k.junk
    xk = ("xt", tag)
    p.op("scalar", lambda e: e.activation(out=junk[:n, :], in_=xt[:n, :], func=AF.Square,
                                          accum_out=ss[:n, col:col + 1]), r=[xk], w=[("ss", col), "junk"])
    p.op("vector", lambda e: e.tensor_scalar(out=rs[:n, col:col + 1], in0=ss[:n, col:col + 1],
                                             scalar1=1.0 / D, scalar2=EPS, op0=ALU.mult, op1=ALU.add),
         r=[("ss", col)], w=[("rs", col)])
    p.op("scalar", lambda e: e.sqrt(out=rs[:n, col:col + 1], in_=rs[:n, col:col + 1]),
         r=[("rs", col)], w=[("rs", col)])
    p.op("vector", lambda e: e.reciprocal(out=rs[:n, col:col + 1], in_=rs[:n, col:col + 1]),
         r=[("rs", col)], w=[("rs", col)])
    p.op("vector", lambda e: e.scalar_tensor_tensor(out=xn_out[:n, :], in0=xt[:n, :], scalar=rs[:n, col:col + 1],
                                                    in1=g[:n, :], op0=ALU.mult, op1=ALU.mult),
         r=[xk, ("rs", col), gkey], w=[("xn", tag)])


def transpose_to_T(k, xn, n, tag, dstT, t0, pbase):
    p = k.p
    for half in range(2):
        bank = pbase + half
        pv = k.ps[bank][:].bitcast(BF16)
        for j in range(8):
            c = half * 8 + j
            p.op("tensor", lambda e, c=c, j=j, pv=pv: e.transpose(
                out=pv[:, j * 128:j * 128 + n], in_=xn[:n, c * 128:(c + 1) * 128], identity=k.ident_b[:n, :n]),
                r=[("xn", tag)], w=[("ps", bank)])
        dst = dstT[:, half * 8:half * 8 + 8, t0:t0 + n]
        srcv = pv.rearrange("p (c t) -> p c t", c=8)[:, :, :n]
        if half == 0:
            p.op("scalar", lambda e, dst=dst, srcv=srcv: e.copy(out=dst, in_=srcv),
                 r=[("ps", bank)], w=[("T", id(dstT), t0, half)])
        else:
            p.op("vector", lambda e, dst=dst, srcv=srcv: e.tensor_copy(out=dst, in_=srcv),
                 r=[("ps", bank)], w=[("T", id(dstT), t0, half)])


def Tkeys(dstT, t0s):
    return [("T", id(dstT), t0, h) for t0 in t0s for h in range(2)]


def norm_stats(k):
    k.ss = k.sb("ss", [128, 64], F32)
    k.rs = k.sb("rs", [128, 64], F32)
    k.junk = k.sb("junk", [128, D], BF16)


def linear_fm(k, xT, xkeys, wname, chunks, evac, tag):
    p = k.p
    stage = [k.sb("st_%s%d" % (tag, i), [128, 16, 128], F32) for i in range(2)]
    wb = [k.sb("wb_%s%d" % (tag, i), [128, 16, 128], BF16) for i in range(2)]
    W = k.I[wname]
    for ci, pieces in enumerate(chunks):
        b = ci % 2
        r0 = 0
        for (c0, ncl) in pieces:
            src = W[:, c0:c0 + ncl].rearrange("(c p) n -> p c n", p=128)
            p.dma("sync", stage[b][:, :, r0:r0 + ncl], src, w=[("st", tag, b)])
            r0 += ncl
        rows = r0
        if ci % 2 == 0:
            p.op("scalar", lambda e, b=b, rows=rows: e.copy(out=wb[b][:, :, :rows], in_=stage[b][:, :, :rows]),
                 r=[("st", tag, b)], w=[("wb", tag, b)])
        else:
            p.op("gpsimd", lambda e, b=b, rows=rows: e.tensor_copy(out=wb[b][:, :, :rows], in_=stage[b][:, :, :rows]),
                 r=[("st", tag, b)], w=[("wb", tag, b)])
        for si in range(5):
            t0 = si * 512
            tn = min(512, T - t0)
            bank = (ci * 5 + si) % 4
            for c in range(16):
                p.op("tensor", lambda e, b=b, c=c, rows=rows, t0=t0, tn=tn, bank=bank: e.matmul(
                    out=k.ps[bank][:rows, :tn], lhsT=wb[b][:, c, :rows], rhs=xT[:, c, t0:t0 + tn],
                    start=(c == 0), stop=(c == 15)), r=[("wb", tag, b)] + xkeys, w=[("ps", bank)])
            evac(ci, si, k.ps[bank], bank, rows, t0, tn)


def phase_proj(k):
    p, nc = k.p, k.nc
    norm_stats(k)
    xnT = k.sb("xnT", [128, 16, T], BF16)
    with k.sub():
        g = load_bcast(k, "g_bc", k.I["g_mix"], D)
        xt = [k.sb("xt%d" % i, [128, D], F32) for i in range(2)]
        xn = [k.sb("xnb%d" % i, [128, D], BF16) for i in range(2)]
        for tt in range(NT):
            b = tt % 2
            n = trows(tt)
            src = k.I["xp"][tt * 128:(tt + 1) * 128, :] if tt < 16 else k.I["xs"][:, :]
            p.dma("sync", xt[b][:n, :], src, w=[("xt", b)])
            rms_tile(k, xt[b], n, g, "g_bc", xn[b], b, tt)
            transpose_to_T(k, xn[b], n, b, xnT, tt * 128, b * 2)
    allx = []
    with k.sub():
        phase_proj_tok(k, xnT)
    phase_proj_fm(k, xnT, allx)


def phase_proj_tok(k, xnT):
    p, nc = k.p, k.nc

    stage = k.sb("kv_stage", [128, 16, 592], F32)
    wbk = k.sb("kv_wb", [128, 16, 592], BF16)
    p.dma("sync", stage[:, :, 0:512], k.I["w_in"][:, O_K:O_K + 512].rearrange("(c p) n -> p c n", p=128),
          w=["kvst0"])
    p.dma("sync", stage[:, :, 512:592], k.I["w_in"][:, O_IK:O_IK + 80].rearrange("(c p) n -> p c n", p=128),
          w=["kvst1"])
    p.op("scalar", lambda e: e.copy(out=wbk[:, :, 0:512], in_=stage[:, :, 0:512]), r=["kvst0"], w=["kvwb0"])
    p.op("vector", lambda e: e.tensor_copy(out=wbk[:, :, 512:592], in_=stage[:, :, 512:592]), r=["kvst1"], w=["kvwb1"])
    vtok = k.scratch("v_tok", [T, 256], BF16)
    iwtok = k.scratch("iw_tok", [T, 16], F32)
    ob = [k.sb("kv_ob%d" % i, [128, 592], F32) for i in range(2)]
    vb = [k.sb("kv_vb%d" % i, [128, 256], BF16) for i in range(2)]
    for tt in range(NT):
        n = trows(tt)
        b = tt % 2
        t0 = tt * 128
        xk = Tkeys(xnT, [t0])
        ba, bb = 4 + b * 2, 5 + b * 2
        for c in range(16):
            p.op("tensor", lambda e, c=c, n=n, t0=t0, ba=ba: e.matmul(
                out=k.ps[ba][:n, :512], lhsT=xnT[:, c, t0:t0 + n], rhs=wbk[:, c, 0:512],
                start=(c == 0), stop=(c == 15)), r=["kvwb0"], w=[("ps", ba)])
        for c in range(16):
            p.op("tensor", lambda e, c=c, n=n, t0=t0, bb=bb: e.matmul(
                out=k.ps[bb][:n, :80], lhsT=xnT[:, c, t0:t0 + n], rhs=wbk[:, c, 512:592],
                start=(c == 0), stop=(c == 15)), r=["kvwb1"], w=[("ps", bb)])
        p.op("scalar", lambda e, n=n, b=b, ba=ba: e.copy(out=ob[b][:n, 0:512], in_=k.ps[ba][:n, :512]),
             r=[("ps", ba)], w=[("ob", b, 0)])
        p.op("vector", lambda e, n=n, b=b, bb=bb: e.tensor_copy(out=ob[b][:n, 512:592], in_=k.ps[bb][:n, :80]),
             r=[("ps", bb)], w=[("ob", b, 1)])
        p.op("vector", lambda e, n=n, b=b: e.tensor_copy(out=vb[b][:n, :], in_=ob[b][:n, 256:512]),
             r=[("ob", b, 0)], w=[("vb", b)])
        p.dma("sync", vtok[t0:t0 + n, :], vb[b][:n, :], r=[("vb", b)])
        p.dma("sync", iwtok[t0:t0 + n, :], ob[b][:n, 576:592], r=[("ob", b, 1)])
        if tt < 16:
            rows = slice(t0, t0 + 128)
            p.dma("sync", k.O["k_p"][rows, :], ob[b][:, 0:256], r=[("ob", b, 0)])
            p.dma("sync", k.O["v_p"][rows, :], ob[b][:, 256:512], r=[("ob", b, 0)])
            p.dma("sync", k.O["ik_p"][rows, :], ob[b][:, 512:576], r=[("ob", b, 1)])
        else:
            p.dma("sync", k.O["k_s"][:, :], ob[b][:64, 0:256], r=[("ob", b, 0)])
            p.dma("sync", k.O["v_s"][:, :], ob[b][:64, 256:512], r=[("ob", b, 0)])
            p.dma("sync", k.O["ik_s"][:, :], ob[b][:64, 512:576], r=[("ob", b, 1)])
    for half in range(2):
        c0 = O_XB + half * 512
        p.dma("sync", stage[:, :, 0:512], k.I["w_in"][:, c0:c0 + 512].rearrange("(c p) n -> p c n", p=128),
              w=["kvst0"])
        p.op("scalar", lambda e: e.copy(out=wbk[:, :, 0:512], in_=stage[:, :, 0:512]), r=["kvst0"], w=["kvwb0"])
        for tt in (15, 16):
            n = trows(tt)
            b = tt % 2
            t0 = tt * 128
            ba = 4 + b * 2
            for c in range(16):
                p.op("tensor", lambda e, c=c, n=n, t0=t0, ba=ba: e.matmul(
                    out=k.ps[ba][:n, :512], lhsT=xnT[:, c, t0:t0 + n], rhs=wbk[:, c, 0:512],
                    start=(c == 0), stop=(c == 15)), r=["kvwb0"], w=[("ps", ba)])
            p.op("scalar", lambda e, n=n, b=b, ba=ba: e.copy(out=ob[b][:n, 0:512], in_=k.ps[ba][:n, :512]),
                 r=[("ps", ba)], w=[("ob", b, 0)])
            cs = slice(half * 512, half * 512 + 512)
            if tt == 15:
                p.dma("sync", k.O["conv_p"][:, cs], ob[b][125:128, 0:512], r=[("ob", b, 0)])
            else:
                srcv = ob[b][:64, 0:512]
                xbs = k.scratch("xb_s", [TS, 1024], F32)
                p.dma("sync", xbs[:, cs], srcv, r=[("ob", b, 0)], w=[("xbs", half)])
                p.dma("sync", k.O["conv_s"].rearrange("(b j) n -> b j n", j=3)[:, :, cs],
                      xbs.rearrange("(b t) n -> b t n", t=4)[:, 1:4, cs], r=[("xbs", half)])


def phase_proj_fm(k, xnT, allx):
    p, nc = k.p, k.nc
    projT = k.scratch("projT", [43, 128, T], BF16)
    chunks = []
    for c0 in list(range(O_Q, O_Q + 1024, 128)) + list(range(O_K, O_K + 256, 128)) + list(range(O_IQ, O_IQ + 1024, 128)):
        chunks.append([(c0, 128)])
    chunks.append([(O_IK, 64), (O_IK, 64)])
    for base in (O_XB, O_YB, O_QM):
        for c0 in range(base, base + 1024, 128):
            chunks.append([(c0, 128)])
    obf = [k.sb("pj_ob%d" % i, [128, T], BF16) for i in range(2)]

    def evac_proj(dst):
        def evac(ci, si, ps, bank, rows, t0, tn):
            b = ci % 2
            if si % 2 == 0:
                p.op("scalar", lambda e: e.copy(out=obf[b][:rows, t0:t0 + tn], in_=ps[:rows, :tn]),
                     r=[("ps", bank)], w=[("obf", b, si)])
            else:
                p.op("vector", lambda e: e.tensor_copy(out=obf[b][:rows, t0:t0 + tn], in_=ps[:rows, :tn]),
                     r=[("ps", bank)], w=[("obf", b, si)])
            if si == 4:
                p.dma("sync", dst[ci, :rows, :], obf[b][:rows, :], r=[("obf", b, s_) for s_ in range(5)])
        return evac
    linear_fm(k, xnT, allx, "w_in", chunks, evac_proj(projT), "pj")

    gT = k.scratch("gT", [48, 128, T], BF16)

    def evac_gate(ci, si, ps, bank, rows, t0, tn):
        b = ci % 2
        p.op("scalar", lambda e: e.activation(out=obf[b][:rows, t0:t0 + tn], in_=ps[:rows, :tn], func=AF.Sigmoid),
             r=[("ps", bank)], w=[("obf", b, si)])
        if si == 4:
            p.dma("sync", gT[ci, :rows, :], obf[b][:rows, :], r=[("obf", b, s_) for s_ in range(5)])
    linear_fm(k, xnT, allx, "w_gate", [[(c0, 128)] for c0 in range(0, 3 * D, 128)], evac_gate, "gt")


PHASES = []


_NC_CACHE = {}


def make_in_maps(inp):
    f = lambda a: np.ascontiguousarray(a, dtype=np.float32)
    shared = {
        "cache_k": f(inp["cache_k"]).reshape(2560 * 128, 256), "cache_v": f(inp["cache_v"]).reshape(2560 * 128, 256),
        "cache_ik": f(inp["cache_idx_k"]).reshape(2560 * 128, 64),
        "g_mix": f(inp["g_mix"]).reshape(1, D), "w_in": f(inp["w_in"]).reshape(D, N_IN),
        "conv_w": f(inp["conv_w"]).reshape(4, 1024), "conv_b": f(inp["conv_b"]).reshape(1, 1024),
        "w_rg": f(inp["w_rg"]).reshape(1024, 128), "b_rg": f(inp["b_rg"]).reshape(1, 1024),
        "w_ig": f(inp["w_ig"]).reshape(1024, 128), "b_ig": f(inp["b_ig"]).reshape(1, 1024),
        "lam": f(inp["lru_lambda"]).reshape(1, 1024), "g_mem": f(inp["g_mem"]).reshape(1, D),
        "w_mem_kv": f(inp["w_mem_kv"]).reshape(D, 2048), "w_gate": f(inp["w_gate"]).reshape(D, 3 * D),
        "w_br": f(inp["w_br"]).reshape(3 * 1024, D), "w_o": f(inp["w_o"]).reshape(D, D),
        "g_ffn": f(inp["g_ffn"]).reshape(1, D), "w_pq": f(inp["w_peer_q"]).reshape(D, 2048),
        "sub_keys": f(inp["peer_sub_keys"]).reshape(2 * 8 * 128, 128), "peer_u": f(inp["peer_u"]).reshape(16384, D),
        "peer_v": f(inp["peer_v"]).reshape(16384, D), "g_final": f(inp["g_final"]).reshape(1, D),
    }
    maps = []
    for c in range(NCORES):
        sl = slice(c * NS_SEQ, (c + 1) * NS_SEQ)
        m = dict(shared)
        m["xp"] = f(inp["x_prompt"][c])
        m["xs"] = f(inp["x_sample"][sl]).reshape(TS, D)
        m["ptab"] = np.ascontiguousarray(inp["page_table"][sl], dtype=np.int32)
        m["st_conv"] = f(inp["state_conv"][0, sl]).reshape(NS_SEQ * 3, 1024)
        m["st_lru"] = f(inp["state_lru"][0, sl]).reshape(NS_SEQ, 1024)
        m["cmk"] = f(inp["cache_mem_k"][0, sl]).reshape(NS_SEQ * 256, 1024)
        m["cmv"] = f(inp["cache_mem_v"][0, sl]).reshape(NS_SEQ * 256, 1024)
        m["mem"] = f(inp["mem_prompt"][c])
        maps.append(m)
    return maps


def assemble(res):
    g = lambda n: [np.asarray(r[n], dtype=np.float32) for r in res]
    y_p = np.stack(g("y_p"))
    y_s = np.concatenate(g("y_s")).reshape(128, DEC, D)
    k_p = np.stack(g("k_p")).reshape(1, 8, SEQ, 2, 128)
    v_p = np.stack(g("v_p")).reshape(1, 8, SEQ, 2, 128)
    ik_p = np.stack(g("ik_p")).reshape(1, 8, SEQ, 64)
    conv_p = np.stack(g("conv_p")).reshape(1, 8, 3, 1024)
    h_p = np.stack(g("h_p")).reshape(1, 8, 1024)
    mk_p = np.stack(g("mk_p")).reshape(1, 8, 256, 4, 256)
    mv_p = np.stack(g("mv_p")).reshape(1, 8, 256, 4, 256)
    k_s = np.concatenate(g("k_s")).reshape(1, 128, DEC, 2, 128)
    v_s = np.concatenate(g("v_s")).reshape(1, 128, DEC, 2, 128)
    ik_s = np.concatenate(g("ik_s")).reshape(1, 128, DEC, 64)
    conv_s = np.concatenate(g("conv_s")).reshape(1, 128, 3, 1024)
    h_s = np.concatenate(g("h_s")).reshape(1, 128, 1024)
    return (y_p, y_s, k_p, v_p, ik_p, conv_p, h_p, mk_p, mv_p, k_s, v_s, ik_s, conv_s, h_s)


def kernel(**inputs):
    if "nc" not in _NC_CACHE:
        _NC_CACHE["nc"] = build()
    nc = _NC_CACHE["nc"]
    in_maps = [{n: m[n] for n in nc._used_in} for m in make_in_maps(inputs)]
    res = run_bass_kernel_spmd(nc, in_maps, core_ids=list(range(NCORES)))
    outs = []
    for r in res.results:
        d = dict(r)
        for n, s in OUT_SPECS:
            if n not in d:
                d[n] = np.zeros(s, np.float32)
        outs.append(d)
    return assemble(outs)


def phase_mem(k):
    p, nc = k.p, k.nc
    projT = k.scratch("projT", [43, 128, T], BF16)
    oT = k.scratch("oT", [3, 8, 128, T], BF16)
    MS = 256 ** -0.5
    norm_stats(k)
    memT = k.sb("memT", [128, 16, 256], BF16)
    mkT = k.sb("mkT", [128, 8, 256], BF16)
    mvb = k.sb("mvb", [128, 2, 1024], BF16)
    qmT = k.sb("qmT", [128, 8, T], BF16)
    p.dma("sync", qmT[:], projT[35:43].rearrange("c p t -> p c t"), w=["qmT"])
    with k.sub():
        g = load_bcast(k, "gm_bc", k.I["g_mem"], D)
        xt = [k.sb("mxt%d" % i, [128, D], F32) for i in range(2)]
        xn = [k.sb("mxn%d" % i, [128, D], BF16) for i in range(2)]
        for mt in range(2):
            p.dma("sync", xt[mt][:, :], k.I["mem"][mt * 128:(mt + 1) * 128, :], w=[("xt", mt)])
            rms_tile(k, xt[mt], 128, g, "gm_bc", xn[mt], mt, mt)
            transpose_to_T(k, xn[mt], 128, mt, memT, mt * 128, mt * 2)
    with k.sub():
        stage = k.sb("mst", [128, 16, 512], F32)
        wb = k.sb("mwb", [128, 16, 512], BF16)
        ob = [k.sb("mob%d" % i, [128, 512], F32) for i in range(2)]
        for cb in range(4):
            p.dma("sync", stage[:], k.I["w_mem_kv"][:, cb * 512:(cb + 1) * 512].rearrange("(c p) n -> p c n", p=128),
                  w=["mst"])
            p.op("scalar", lambda e: e.copy(out=wb[:], in_=stage[:]), r=["mst"], w=["mwb"])
            for mt in range(2):
                bank = mt
                for c in range(16):
                    p.op("tensor", lambda e, c=c, mt=mt, bank=bank: e.matmul(
                        out=k.ps[bank][:, :], lhsT=memT[:, c, mt * 128:(mt + 1) * 128], rhs=wb[:, c, :],
                        start=(c == 0), stop=(c == 15)), r=["mwb"], w=[("ps", bank)])
                p.op("scalar", lambda e, mt=mt, bank=bank: e.copy(out=ob[mt][:, :], in_=k.ps[bank][:, :]),
                     r=[("ps", bank)], w=[("mob", mt)])
                dst = k.O["mk_p"] if cb < 2 else k.O["mv_p"]
                cs = slice((cb % 2) * 512, (cb % 2) * 512 + 512)
                p.dma("sync", dst[mt * 128:(mt + 1) * 128, cs], ob[mt][:, :], r=[("mob", mt)])
                if cb >= 2:
                    p.op("vector", lambda e, mt=mt, cs=cs: e.tensor_copy(out=mvb[:, mt, cs], in_=ob[mt][:, :]),
                         r=[("mob", mt)], w=["mvb"])
            if cb < 2:
                for j in range(4):
                    bank = 2 + j % 2
                    for c in range(16):
                        p.op("tensor", lambda e, c=c, j=j, bank=bank: e.matmul(
                            out=k.ps[bank][:, :256], lhsT=wb[:, c, j * 128:(j + 1) * 128], rhs=memT[:, c, :],
                            start=(c == 0), stop=(c == 15)), r=["mwb"], w=[("ps", bank)])
                    p.op("vector", lambda e, j=j, bank=bank, cb=cb: e.tensor_copy(
                        out=mkT[:, cb * 4 + j, :], in_=k.ps[bank][:, :256]), r=[("ps", bank)], w=["mkT"])
    with k.sub():
        mem_attend_loop(k, qmT, mkT, mvb, oT, MS, [(tt * 128, 128) for tt in range(16)], "p")
    with k.sub():
        cm = [k.sb("cmk%d" % i, [128, 2, 1024], F32) for i in range(2)]
        cv = [k.sb("cmv%d" % i, [128, 2, 1024], F32) for i in range(2)]
        mkTs = [k.sb("mkTs%d" % i, [128, 8, 256], BF16) for i in range(2)]
        mvbs = [k.sb("mvbs%d" % i, [128, 2, 1024], BF16) for i in range(2)]
        st = mem_attend_state(k, "s")
        for b in range(NS_SEQ):
            i = b % 2
            p.dma("sync", cm[i][:], k.I["cmk"][b * 256:(b + 1) * 256, :].rearrange("(m p) n -> p m n", p=128),
                  w=[("cm", i)])
            p.dma("sync", cv[i][:], k.I["cmv"][b * 256:(b + 1) * 256, :].rearrange("(m p) n -> p m n", p=128),
                  w=[("cv", i)])
            p.op("gpsimd", lambda e, i=i: e.tensor_copy(out=mvbs[i][:], in_=cv[i][:]), r=[("cv", i)], w=[("mvbs", i)])
            for c in range(8):
                bank = 6 + c % 2
                for mt in range(2):
                    p.op("tensor", lambda e, c=c, mt=mt, bank=bank, i=i: e.transpose(
                        out=k.ps[bank][:, mt * 128:(mt + 1) * 128], in_=cm[i][:, mt, c * 128:(c + 1) * 128],
                        identity=k.ident_f[:]), r=[("cm", i)], w=[("ps", bank)])
                p.op("scalar", lambda e, c=c, bank=bank, i=i: e.copy(out=mkTs[i][:, c, :], in_=k.ps[bank][:, :256]),
                     r=[("ps", bank)], w=[("mkTs", i)])
            mem_attend_tile(k, st, qmT, mkTs[i], mvbs[i], oT, MS, SEQ + 4 * b, 4, [("mkTs", i), ("mvbs", i)])


def mem_attend_state(k, tag):
    st = K()
    st.mx = k.sb("ma_mx" + tag, [128, 4], F32)
    st.rsum = k.sb("ma_rs" + tag, [128, 4], F32)
    st.P = k.sb("ma_P" + tag, [128, 4, 256], BF16)
    st.PT = k.sb("ma_PT" + tag, [128, 8, 128], BF16)
    st.oc = k.sb("ma_oc" + tag, [128, 8, 128], BF16)
    return st


def mem_attend_loop(k, qmT, mkT, mvb, oT, MS, tiles, tag):
    st = mem_attend_state(k, tag)
    for (t0, n) in tiles:
        mem_attend_tile(k, st, qmT, mkT, mvb, oT, MS, t0, n, ["mkT", "mvb"])


def mem_attend_tile(k, st, qmT, mkT, mvb, oT, MS, t0, n, kvkeys):
    p = k.p
    for h in range(4):
        bank = h // 2
        for kc in range(2):
            p.op("tensor", lambda e, h=h, kc=kc, bank=bank: e.matmul(
                out=k.ps[bank][:n, (h % 2) * 256:(h % 2) * 256 + 256], lhsT=qmT[:, 2 * h + kc, t0:t0 + n],
                rhs=mkT[:, 2 * h + kc, :], start=(kc == 0), stop=(kc == 1)),
                r=["qmT"] + kvkeys, w=[("ps", bank)])
    for bank in range(2):
        p.op("vector", lambda e, bank=bank: e.tensor_reduce(
            out=st.mx[:n, bank * 2:bank * 2 + 2], in_=k.ps[bank][:n, :].rearrange("p (h m) -> p h m", h=2),
            axis=AX.X, op=ALU.max), r=[("ps", bank)], w=[("mx", bank)])
        p.op("vector", lambda e, bank=bank: e.tensor_scalar(
            out=st.mx[:n, bank * 2:bank * 2 + 2], in0=st.mx[:n, bank * 2:bank * 2 + 2], scalar1=-MS, scalar2=None,
            op0=ALU.mult), r=[("mx", bank)], w=[("mx", bank)])
    for h in range(4):
        bank = h // 2
        p.op("scalar", lambda e, h=h, bank=bank: e.activation(
            out=st.P[:n, h, :], in_=k.ps[bank][:n, (h % 2) * 256:(h % 2) * 256 + 256], func=AF.Exp,
            bias=st.mx[:n, h:h + 1], scale=MS, accum_out=st.rsum[:n, h:h + 1]),
            r=[("ps", bank), ("mx", bank)], w=[("P", h), ("rsum", h)])
    p.op("vector", lambda e: e.reciprocal(out=st.rsum[:n, :], in_=st.rsum[:n, :]),
         r=[("rsum", h) for h in range(4)], w=["rinv"])
    for h in range(4):
        p.op("vector", lambda e, h=h: e.tensor_scalar(out=st.P[:n, h, :], in0=st.P[:n, h, :],
                                                      scalar1=st.rsum[:n, h:h + 1], scalar2=None, op0=ALU.mult),
             r=["rinv", ("P", h)], w=[("P", h)])
    pv = k.ps[2][:].bitcast(BF16)
    for h in range(4):
        for mt in range(2):
            j = h * 2 + mt
            p.op("tensor", lambda e, h=h, mt=mt, j=j: e.transpose(
                out=pv[:, j * 128:j * 128 + n], in_=st.P[:n, h, mt * 128:(mt + 1) * 128],
                identity=k.ident_b[:n, :n]), r=[("P", h)], w=[("ps", 2)])
    p.op("scalar", lambda e: e.copy(out=st.PT[:, :, :n], in_=pv.rearrange("p (j t) -> p j t", j=8)[:, :, :n]),
         r=[("ps", 2)], w=["PT"])
    for h in range(4):
        for c2 in range(2):
            j = h * 2 + c2
            bank = 3 + j // 4
            for mt in range(2):
                p.op("tensor", lambda e, h=h, c2=c2, mt=mt, j=j, bank=bank: e.matmul(
                    out=k.ps[bank][:, (j % 4) * 128:(j % 4) * 128 + n],
                    lhsT=mvb[:, mt, h * 256 + c2 * 128:h * 256 + c2 * 128 + 128], rhs=st.PT[:, h * 2 + mt, :n],
                    start=(mt == 0), stop=(mt == 1)), r=["PT"] + kvkeys, w=[("ps", bank)])
    for half in range(2):
        bank = 3 + half
        eng = "scalar" if half == 0 else "vector"
        srcv = k.ps[bank][:].rearrange("p (j t) -> p j t", j=4)[:, :, :n]
        dst = st.oc[:, half * 4:half * 4 + 4, :n]
        if half == 0:
            p.op("scalar", lambda e, dst=dst, srcv=srcv: e.copy(out=dst, in_=srcv), r=[("ps", bank)], w=[("oc", half)])
        else:
            p.op("vector", lambda e, dst=dst, srcv=srcv: e.tensor_copy(out=dst, in_=srcv), r=[("ps", bank)],
                 w=[("oc", half)])
    p.dma("sync", oT[2, :, :, t0:t0 + n].rearrange("c p t -> p c t"), st.oc[:, :, :n], r=[("oc", 0), ("oc", 1)])


PHASES.append(("mem", phase_mem))


def gelu_mul(k, y, h, out, n, tmp1, tmp2, keys_r, key_w):
    p = k.p
    p.op("vector", lambda e: e.tensor_tensor(out=tmp1, in0=y, in1=y, op=ALU.mult), r=keys_r, w=[("g1", key_w)])
    p.op("vector", lambda e: e.tensor_scalar(out=tmp1, in0=tmp1, scalar1=0.044715, scalar2=1.0, op0=ALU.mult,
                                             op1=ALU.add), r=[("g1", key_w)], w=[("g1", key_w)])
    p.op("vector", lambda e: e.tensor_tensor(out=tmp1, in0=tmp1, in1=y, op=ALU.mult), r=[("g1", key_w)] + keys_r,
         w=[("g1", key_w)])
    p.op("scalar", lambda e: e.activation(out=tmp2, in_=tmp1, func=AF.Sigmoid, scale=1.5957691216057308),
         r=[("g1", key_w)], w=[("g2", key_w)])
    p.op("vector", lambda e: e.tensor_tensor(out=tmp2, in0=tmp2, in1=y, op=ALU.mult), r=[("g2", key_w)] + keys_r,
         w=[("g2", key_w)])
    p.op("vector", lambda e: e.tensor_tensor(out=out, in0=tmp2, in1=h, op=ALU.mult), r=[("g2", key_w)] + keys_r,
         w=[key_w])


def phase_lru(k):
    p, nc = k.p, k.nc
    projT = k.scratch("projT", [43, 128, T], BF16)
    oT = k.scratch("oT", [3, 8, 128, T], BF16)
    cw = k.sb("l_cw", [128, 8, 4], F32)
    pv = k.sb("l_pv", [128, 5, 8], F32)
    sc = k.sb("l_sc", [128, 2, 8], F32)
    for j in range(4):
        p.dma("sync", cw[:, :, j], k.I["conv_w"][j:j + 1, :].rearrange("o (n q) -> q (o n)", q=128), w=["cw"],
              allow_slow_non_contiguous=True)
    for i, nm in enumerate(("conv_b", "b_rg", "b_ig", "lam")):
        p.dma("sync", pv[:, i, :], k.I[nm].rearrange("o (n q) -> q (o n)", q=128), w=[("pv", i)],
              allow_slow_non_contiguous=True)
    p.op("scalar", lambda e: e.activation(out=pv[:, 4, :], in_=pv[:, 3, :], func=AF.Exp, scale=-1.0),
         r=[("pv", 3)], w=[("pv", 4)])
    p.op("scalar", lambda e: e.activation(out=pv[:, 4, :], in_=pv[:, 4, :], func=AF.Ln, bias=1.0),
         r=[("pv", 4)], w=[("pv", 4)])
    p.op("vector", lambda e: e.tensor_scalar(out=sc[:, 0, :], in0=pv[:, 4, :], scalar1=-8.0, scalar2=None,
                                             op0=ALU.mult), r=[("pv", 4)], w=["sc0"])
    p.op("vector", lambda e: e.tensor_scalar(out=sc[:, 1, :], in0=pv[:, 4, :], scalar1=-16.0, scalar2=None,
                                             op0=ALU.mult), r=[("pv", 4)], w=["sc1"])
    wst = k.sb("l_wst", [128, 2, 8, 128], F32)
    wg = k.sb("l_wg", [128, 2, 8, 128], BF16)
    p.dma("sync", wst[:, 0], k.I["w_rg"].rearrange("(n i) j -> i n j", i=128), w=["wst0"])
    p.dma("sync", wst[:, 1], k.I["w_ig"].rearrange("(n i) j -> i n j", i=128), w=["wst1"])
    p.op("vector", lambda e: e.tensor_copy(out=wg[:], in_=wst[:]), r=["wst0", "wst1"], w=["wg"])
    scv = k.sb("l_scv", [48, 1024], F32)
    slr = k.sb("l_slr", [16, 1024], F32)
    p.dma("sync", scv[:], k.I["st_conv"][:, :], w=["scv"])
    p.dma("sync", slr[:], k.I["st_lru"][:, :], w=["slr"])
    hl_p = k.sb("l_hlp", [128, 8], F32)
    hl_s = k.sb("l_hls", [128, 8, 16], F32)
    NP = SEQ
    xbb = [k.sb("l_xbb%d" % i, [128, T], BF16) for i in range(2)]
    ybb = [k.sb("l_ybb%d" % i, [128, T], BF16) for i in range(2)]
    xpad = k.sb("l_xpad", [128, 3 + NP], F32)
    xps = k.sb("l_xps", [128, 16, 7], F32)
    xc = k.sb("l_xc", [128, T], F32)
    xcb = k.sb("l_xcb", [128, T], BF16)
    rr = k.sb("l_r", [128, T], F32)
    ii = k.sb("l_i", [128, T], F32)
    aa = k.sb("l_a", [128, T], F32)
    uu = k.sb("l_u", [128, T], F32)
    hh = k.sb("l_h", [128, T], F32)
    yf = k.sb("l_yf", [128, T], F32)
    ob = [k.sb("l_ob%d" % i, [128, T], BF16) for i in range(2)]
    h0 = k.sb("l_h0", [128, 16], F32)
    tmpa = k.sb("l_tmpa", [128, 16], F32)
    p.op("vector", lambda e: e.memset(xpad[:, 0:3], 0.0), w=["xpad0"])
    for n in range(8):
        b = n % 2
        p.dma("sync", xbb[b][:], projT[19 + n], w=[("xbb", b)])
        p.dma("sync", ybb[b][:], projT[27 + n], w=[("ybb", b)])
        p.op("vector", lambda e, b=b: e.tensor_copy(out=xpad[:, 3:3 + NP], in_=xbb[b][:, 0:NP]),
             r=[("xbb", b), "xpad0"], w=["xpad"])
        p.op("tensor", lambda e, n=n: e.transpose(out=k.ps[4][:, 0:48], in_=scv[:, n * 128:(n + 1) * 128],
                                                  identity=k.ident_f[:48, :48]), r=["scv"], w=[("ps", 4)])
        p.op("tensor", lambda e, n=n: e.transpose(out=k.ps[5][:, 0:16], in_=slr[:, n * 128:(n + 1) * 128],
                                                  identity=k.ident_f[:16, :16]), r=["slr"], w=[("ps", 5)])
        p.op("vector", lambda e: e.tensor_copy(out=xps[:, :, 0:3], in_=k.ps[4][:, 0:48].rearrange("p (b j) -> p b j", j=3)),
             r=[("ps", 4)], w=["xps0"])
        p.op("vector", lambda e, b=b: e.tensor_copy(out=xps[:, :, 3:7],
                                                    in_=xbb[b][:, NP:T].rearrange("p (b t) -> p b t", t=4)),
             r=[("xbb", b)], w=["xps1"])
        p.op("vector", lambda e: e.tensor_copy(out=h0[:], in_=k.ps[5][:, 0:16]), r=[("ps", 5)], w=["h0"])
        p.op("scalar", lambda e, n=n: e.activation(out=xc[:, 0:NP], in_=xpad[:, 3:3 + NP], func=AF.Identity,
                                                   bias=pv[:, 0, n:n + 1], scale=cw[:, n, 3:4]),
             r=["xpad", "cw", ("pv", 0)], w=["xc_p"])
        p.op("scalar", lambda e, n=n: e.activation(out=xc[:, NP:T].rearrange("p (b t) -> p b t", t=4),
                                                   in_=xps[:, :, 3:7], func=AF.Identity,
                                                   bias=pv[:, 0, n:n + 1], scale=cw[:, n, 3:4]),
             r=["xps0", "xps1", "cw", ("pv", 0)], w=["xc_s"])
        for j in range(3):
            p.op("vector", lambda e, n=n, j=j: e.scalar_tensor_tensor(
                out=xc[:, 0:NP], in0=xpad[:, j:j + NP], scalar=cw[:, n, j:j + 1], in1=xc[:, 0:NP],
                op0=ALU.mult, op1=ALU.add), r=["xpad", "xc_p"], w=["xc_p"])
            p.op("vector", lambda e, n=n, j=j: e.scalar_tensor_tensor(
                out=xc[:, NP:T].rearrange("p (b t) -> p b t", t=4), in0=xps[:, :, j:j + 4], scalar=cw[:, n, j:j + 1],
                in1=xc[:, NP:T].rearrange("p (b t) -> p b t", t=4), op0=ALU.mult, op1=ALU.add),
                r=["xps0", "xps1", "xc_s"], w=["xc_s"])
        p.op("gpsimd", lambda e: e.tensor_copy(out=xcb[:], in_=xc[:]), r=["xc_p", "xc_s"], w=["xcb"])
        for gi, dst in ((0, rr), (1, ii)):
            for si in range(5):
                t0 = si * 512
                tn = min(512, T - t0)
                bank = (gi * 5 + si) % 4
                p.op("tensor", lambda e, gi=gi, n=n, t0=t0, tn=tn, bank=bank: e.matmul(
                    out=k.ps[bank][:, :tn], lhsT=wg[:, gi, n, :], rhs=xcb[:, t0:t0 + tn], start=True, stop=True),
                    r=["wg", "xcb"], w=[("ps", bank)])
                p.op("scalar", lambda e, gi=gi, n=n, t0=t0, tn=tn, bank=bank, dst=dst: e.activation(
                    out=dst[:, t0:t0 + tn], in_=k.ps[bank][:, :tn], func=AF.Sigmoid, bias=pv[:, 1 + gi, n:n + 1]),
                    r=[("ps", bank), ("pv", 1 + gi)], w=[("gate", gi)])
        p.op("scalar", lambda e, n=n: e.activation(out=aa[:], in_=rr[:], func=AF.Exp, scale=sc[:, 0, n:n + 1]),
             r=[("gate", 0), "sc0"], w=["aa"])
        p.op("scalar", lambda e, n=n: e.activation(out=uu[:], in_=rr[:], func=AF.Exp, scale=sc[:, 1, n:n + 1]),
             r=[("gate", 0), "sc1"], w=["uu"])
        p.op("scalar", lambda e: e.activation(out=uu[:], in_=uu[:], func=AF.Sqrt, bias=1.0, scale=-1.0),
             r=["uu"], w=["uu"])
        p.op("vector", lambda e: e.tensor_tensor(out=uu[:], in0=uu[:], in1=ii[:], op=ALU.mult),
             r=["uu", ("gate", 1)], w=["uu"])
        p.op("vector", lambda e: e.tensor_tensor(out=uu[:], in0=uu[:], in1=xc[:], op=ALU.mult),
             r=["uu", "xc_p", "xc_s"], w=["uu"])
        p.op("vector", lambda e: e.tensor_tensor_scan(out=hh[:, 0:NP], data0=aa[:, 0:NP], data1=uu[:, 0:NP],
                                                      initial=0.0, op0=ALU.mult, op1=ALU.add),
             r=["aa", "uu"], w=["hh_p"])
        hs = hh[:, NP:T].rearrange("p (b t) -> p b t", t=4)
        as_ = aa[:, NP:T].rearrange("p (b t) -> p b t", t=4)
        us = uu[:, NP:T].rearrange("p (b t) -> p b t", t=4)
        for t in range(4):
            prev = h0[:, :] if t == 0 else hs[:, :, t - 1]
            p.op("vector", lambda e, t=t, prev=prev: e.tensor_tensor(out=tmpa[:], in0=as_[:, :, t], in1=prev,
                                                                     op=ALU.mult),
                 r=["aa", "h0", "hh_s"], w=["tmpa"])
            p.op("vector", lambda e, t=t: e.tensor_tensor(out=hs[:, :, t], in0=tmpa[:], in1=us[:, :, t], op=ALU.add),
                 r=["tmpa", "uu"], w=["hh_s"])
        p.op("vector", lambda e, n=n: e.tensor_copy(out=hl_p[:, n:n + 1], in_=hh[:, NP - 1:NP]), r=["hh_p"], w=["hlp"])
        p.op("vector", lambda e, n=n: e.tensor_copy(out=hl_s[:, n, :], in_=hs[:, :, 3]), r=["hh_s"], w=["hls"])
        p.op("gpsimd", lambda e, b=b: e.tensor_copy(out=yf[:], in_=ybb[b][:]), r=[("ybb", b)], w=["yf"])
        gelu_mul(k, yf[:], hh[:], ob[b][:], T, rr[:], ii[:], ["yf", "hh_p", "hh_s", ("gate", 0), ("gate", 1), "uu"],
                 ("lob", b))
        p.dma("sync", oT[1, n], ob[b][:], r=[("lob", b)])
    hrow = k.sb("l_hrow", [16, 1024], F32)
    hrp = k.sb("l_hrp", [8, 128], F32)
    p.op("tensor", lambda e: e.transpose(out=k.ps[6][:8, 0:128], in_=hl_p[:, :], identity=k.ident_f[:, :]),
         r=["hlp"], w=[("ps", 6)])
    p.op("vector", lambda e: e.tensor_copy(out=hrp[:], in_=k.ps[6][:8, 0:128]), r=[("ps", 6)], w=["hrp"])
    p.dma("sync", k.O["h_p"].rearrange("o (n q) -> (o n) q", q=128), hrp[:], r=["hrp"])
    for n in range(8):
        bank = 6 + n % 2
        p.op("tensor", lambda e, n=n, bank=bank: e.transpose(out=k.ps[bank][:16, 0:128], in_=hl_s[:, n, :],
                                                             identity=k.ident_f[:, :]), r=["hls"], w=[("ps", bank)])
        p.op("vector", lambda e, n=n, bank=bank: e.tensor_copy(out=hrow[:, n * 128:(n + 1) * 128],
                                                               in_=k.ps[bank][:16, 0:128]),
             r=[("ps", bank)], w=["hrow"])
    p.dma("sync", k.O["h_s"][:, :], hrow[:], r=["hrow"])


PHASES.append(("lru", phase_lru))


def phase_merge(k):
    p, nc = k.p, k.nc
    oT = k.scratch("oT", [3, 8, 128, T], BF16)
    gT = k.scratch("gT", [48, 128, T], BF16)
    x1 = k.scratch("x1", [T, D], F32)
    mT = k.sb("mT", [128, 16, T], F32)
    with k.sub():
        on = k.sb("mg_on", [128, 8, T], BF16)
        wst = [k.sb("mg_wst%d" % i, [128, 8, 128], F32) for i in range(2)]
        wb = [k.sb("mg_wb%d" % i, [128, 8, 128], BF16) for i in range(2)]
        gt = [k.sb("mg_gt%d" % i, [128, T], BF16) for i in range(2)]
        tmp = [k.sb("mg_tmp%d" % i, [128, 512], F32) for i in range(2)]
        it = 0
        for n in range(3):
            p.dma("sync", on[:], oT[n].rearrange("c p t -> p c t"), w=["on"])
            for cc in range(16):
                b = it % 2
                it += 1
                p.dma("sync", wst[b][:], k.I["w_br"][n * 1024:(n + 1) * 1024, cc * 128:(cc + 1) * 128]
                      .rearrange("(c q) j -> q c j", q=128), w=[("wst", b)])
                p.dma("sync", gt[b][:], gT[n * 16 + cc], w=[("gt", b)])
                p.op("scalar", lambda e, b=b: e.copy(out=wb[b][:], in_=wst[b][:]), r=[("wst", b)], w=[("wb", b)])
                for si in range(5):
                    t0 = si * 512
                    tn = min(512, T - t0)
                    bank = si % 4
                    for c in range(8):
                        p.op("tensor", lambda e, b=b, c=c, t0=t0, tn=tn, bank=bank: e.matmul(
                            out=k.ps[bank][:, :tn], lhsT=wb[b][:, c, :], rhs=on[:, c, t0:t0 + tn],
                            start=(c == 0), stop=(c == 7)), r=[("wb", b), "on"], w=[("ps", bank)])
                    if n == 0:
                        p.op("vector", lambda e, b=b, cc=cc, t0=t0, tn=tn, bank=bank: e.tensor_tensor(
                            out=mT[:, cc, t0:t0 + tn], in0=k.ps[bank][:, :tn], in1=gt[b][:, t0:t0 + tn], op=ALU.mult),
                            r=[("ps", bank), ("gt", b)], w=[("mT", cc, si)])
                    else:
                        tb = si % 2
                        p.op("vector", lambda e, b=b, tb=tb, t0=t0, tn=tn, bank=bank: e.tensor_tensor(
                            out=tmp[tb][:, :tn], in0=k.ps[bank][:, :tn], in1=gt[b][:, t0:t0 + tn], op=ALU.mult),
                            r=[("ps", bank), ("gt", b)], w=[("tmp", tb)])
                        p.op("gpsimd", lambda e, cc=cc, tb=tb, t0=t0, tn=tn: e.tensor_tensor(
                            out=mT[:, cc, t0:t0 + tn], in0=mT[:, cc, t0:t0 + tn], in1=tmp[tb][:, :tn], op=ALU.add),
                            r=[("tmp", tb), ("mT", cc, si)], w=[("mT", cc, si)])
    with k.sub():
        stage = k.sb("wo_st", [128, 16, 512], F32)
        wo = k.sb("wo_wb", [128, 16, 512], BF16)
        mb = [k.sb("wo_mb%d" % i, [128, 16, 128], BF16) for i in range(2)]
        xr = [k.sb("wo_xr%d" % i, [128, 512], F32) for i in range(2)]
        xo = [k.sb("wo_xo%d" % i, [128, 512], F32) for i in range(2)]
        for cb in range(4):
            cs = slice(cb * 512, (cb + 1) * 512)
            p.dma("sync", stage[:], k.I["w_o"][:, cs].rearrange("(c q) n -> q c n", q=128), w=["wost"])
            p.op("scalar", lambda e: e.copy(out=wo[:], in_=stage[:]), r=["wost"], w=["wo"])
            for tt in range(NT):
                n = trows(tt)
                t0 = tt * 128
                b = tt % 2
                bank = tt % 4
                src = k.I["xp"][t0:t0 + 128, cs] if tt < 16 else k.I["xs"][:, cs]
                p.dma("sync", xr[b][:n, :], src, w=[("xr", b)])
                p.op("gpsimd", lambda e, b=b, t0=t0, n=n: e.tensor_copy(out=mb[b][:, :, :n], in_=mT[:, :, t0:t0 + n]),
                     w=[("mb", b)])
                for c in range(16):
                    p.op("tensor", lambda e, b=b, c=c, n=n, bank=bank: e.matmul(
                        out=k.ps[bank][:n, :], lhsT=mb[b][:, c, :n], rhs=wo[:, c, :], start=(c == 0), stop=(c == 15)),
                        r=[("mb", b), "wo"], w=[("ps", bank)])
                p.op("vector", lambda e, b=b, n=n, bank=bank: e.tensor_tensor(
                    out=xo[b][:n, :], in0=k.ps[bank][:n, :], in1=xr[b][:n, :], op=ALU.add),
                    r=[("ps", bank), ("xr", b)], w=[("xo", b)])
                p.dma("sync", x1[t0:t0 + n, cs], xo[b][:n, :], r=[("xo", b)])


PHASES.append(("merge", phase_merge))


ATT_SCALE = 128 ** -0.5


def slices(n_k):
    out = []
    s0 = 0
    while s0 < n_k:
        w = min(512, n_k - s0)
        out.append((s0, w))
        s0 += w
    return out


def topk_thr(k, Iv, Wv, n, n_k, m8, tag, ikeys=()):
    p = k.p
    for r in range(32):
        src = Iv if r == 0 else Wv
        p.op("vector", lambda e, src=src: e.max(out=m8[:n, :], in_=src[:n, :n_k]),
             r=[("I", tag), ("W", tag)] + list(ikeys), w=[("m8", tag)])
        if r < 31:
            p.op("vector", lambda e, src=src: e.match_replace(out=Wv[:n, :n_k], in_to_replace=m8[:n, :],
                                                              in_values=src[:n, :n_k], imm_value=NEG),
                 r=[("m8", tag), ("I", tag)] + list(ikeys), w=[("W", tag)])


def attend(k, st, n, n_k, lhsT, kT, kv, vt, mbv, mbkey, out_ps, out_bank, rkeys):
    p = k.p
    sl = slices(n_k)
    for i, (s0, w) in enumerate(sl):
        bank = i % 4
        p.op("tensor", lambda e, s0=s0, w=w, bank=bank: e.matmul(out=k.ps[bank][:n, :w], lhsT=lhsT,
                                                                 rhs=kT[:, kv, s0:s0 + w], start=True, stop=True),
             r=rkeys, w=[("ps", bank)])
        p.op("vector", lambda e, s0=s0, w=w, bank=bank: e.scalar_tensor_tensor(
            out=st.L[:n, s0:s0 + w], in0=k.ps[bank][:n, :w], scalar=ATT_SCALE, in1=mbv[:n, s0:s0 + w],
            op0=ALU.mult, op1=ALU.add), r=[("ps", bank), mbkey], w=[("L", st.tag)])
    p.op("vector", lambda e: e.tensor_reduce(out=st.mx[:n, :], in_=st.L[:n, :n_k], axis=AX.X, op=ALU.max),
         r=[("L", st.tag)], w=[("mx", st.tag)])
    p.op("vector", lambda e: e.tensor_scalar(out=st.mx[:n, :], in0=st.mx[:n, :], scalar1=-1.0, scalar2=None,
                                             op0=ALU.mult), r=[("mx", st.tag)], w=[("mx", st.tag)])
    p.op("scalar", lambda e: e.activation(out=st.P[:n, :n_k], in_=st.L[:n, :n_k], func=AF.Exp, bias=st.mx[:n, :],
                                          accum_out=st.rs[:n, :]), r=[("L", st.tag), ("mx", st.tag)], w=[("P", st.tag), ("rs", st.tag)])
    p.op("vector", lambda e: e.reciprocal(out=st.rs[:n, :], in_=st.rs[:n, :]), r=[("rs", st.tag)], w=[("rs", st.tag)])
    p.op("vector", lambda e: e.tensor_scalar(out=st.P[:n, :n_k], in0=st.P[:n, :n_k], scalar1=st.rs[:n, :],
                                             scalar2=None, op0=ALU.mult), r=[("rs", st.tag), ("P", st.tag)], w=[("P", st.tag)])
    nj = (n_k + 127) // 128
    for j in range(nj):
        w = min(128, n_k - j * 128)
        bank = (4, 5, 3)[j // 8]
        pv = k.ps[bank][:].bitcast(BF16)
        p.op("tensor", lambda e, j=j, w=w, pv=pv: e.transpose(out=pv[:w, (j % 8) * 128:(j % 8) * 128 + n],
                                                              in_=st.P[:n, j * 128:j * 128 + w],
                                                              identity=k.ident_b[:n, :n]),
             r=[("P", st.tag)], w=[("ps", bank)])
    for half in range((nj + 7) // 8):
        bank = (4, 5, 3)[half]
        pv = k.ps[bank][:].bitcast(BF16)
        cnt = min(8, nj - half * 8)
        rws = min(128, n_k - (half * 8 + cnt - 1) * 128) if cnt == 1 else 128
        srcv = pv.rearrange("p (j t) -> p j t", j=8)[:rws, :cnt, :n]
        dst = st.PT[:rws, half * 8:half * 8 + cnt, :n]
        if half == 0:
            p.op("scalar", lambda e, dst=dst, srcv=srcv: e.copy(out=dst, in_=srcv), r=[("ps", bank)], w=[("PT", half, st.tag)])
        else:
            p.op("vector", lambda e, dst=dst, srcv=srcv: e.tensor_copy(out=dst, in_=srcv), r=[("ps", bank)],
                 w=[("PT", half, st.tag)])
    for j in range(nj):
        w = min(128, n_k - j * 128)
        p.op("tensor", lambda e, j=j, w=w: e.matmul(out=out_ps, lhsT=vt[:w, j, kv * 128:(kv + 1) * 128],
                                                    rhs=st.PT[:w, j, :n], start=(j == 0), stop=(j == nj - 1)),
             r=[("PT", 0, st.tag), ("PT", 1, st.tag), ("PT", 2, st.tag)] + rkeys, w=[("ps", out_bank)])


def phase_dsa(k):
    p, nc = k.p, k.nc
    projT = k.scratch("projT", [43, 128, T], BF16)
    oT = k.scratch("oT", [3, 8, 128, T], BF16)
    vtok = k.scratch("v_tok", [T, 256], BF16)
    iwtok = k.scratch("iw_tok", [T, 16], F32)
    NK = SEQ + DEC
    qT = k.sb("d_qT", [128, 8, T], BF16)
    iqT = k.sb("d_iqT", [128, 8, T], BF16)
    kT = k.sb("d_kT", [128, 2, T], BF16)
    ikT = k.sb("d_ikT", [128, T], BF16)
    p.dma("sync", qT[:], projT[0:8].rearrange("c p t -> p c t"), w=["qT"])
    p.dma("sync", iqT[:], projT[10:18].rearrange("c p t -> p c t"), w=["iqT"])
    p.dma("sync", kT[:], projT[8:10].rearrange("c p t -> p c t"), w=["kT"])
    p.dma("sync", ikT[:], projT[18], w=["ikT"])
    st = K()
    st.tag = 0
    st.L = k.sb("d_L", [128, NK], F32)
    st.P = k.sb("d_P", [128, NK], BF16)
    st.PT = k.sb("d_PT", [128, 17, 128], BF16)
    st.mx = k.sb("d_mx", [128, 1], F32)
    st.rs = k.sb("d_rs", [128, 1], F32)
    Ib = k.sb("d_I", [128, NK], F32)
    Wb = k.sb("d_W", [128, NK], F32)
    mb = k.sb("d_mb", [128, NK], F32)
    rl = [k.sb("d_rl%d" % i, [128, 512], F32) for i in range(4)]
    m8 = k.sb("d_m8", [128, 8], F32)
    oc = [k.sb("d_oc%d" % i, [128, 8, 128], BF16) for i in range(2)]

    def indexer(n, n_k, tcol0, ikv, iw_fn, Iv, rk):
        it = 0
        for hi in range(16):
            c, half = hi // 2, hi % 2
            prt = slice(half * 64, half * 64 + 64)
            for i, (s0, w) in enumerate(slices(n_k)):
                bank = (hi % 2) * 4 + i % 4
                b = it % 4
                it += 1
                p.op("tensor", lambda e, c=c, prt=prt, s0=s0, w=w, bank=bank: e.matmul(
                    out=k.ps[bank][:n, :w], lhsT=iqT[prt, c, tcol0:tcol0 + n], rhs=ikv[prt, s0:s0 + w],
                    start=True, stop=True), r=["iqT"] + rk, w=[("ps", bank)])
                p.op("scalar", lambda e, w=w, bank=bank, b=b: e.activation(out=rl[b][:n, :w], in_=k.ps[bank][:n, :w],
                                                                            func=AF.Relu),
                     r=[("ps", bank)], w=[("rl", b)])
                if hi == 0:
                    p.op("vector", lambda e, s0=s0, w=w, b=b, hi=hi: e.tensor_scalar(
                        out=Iv[:n, s0:s0 + w], in0=rl[b][:n, :w], scalar1=iw_fn(hi), scalar2=None, op0=ALU.mult),
                        r=[("rl", b), "iw"], w=[("Isl", i)])
                else:
                    p.op("vector", lambda e, s0=s0, w=w, b=b, hi=hi: e.scalar_tensor_tensor(
                        out=Iv[:n, s0:s0 + w], in0=rl[b][:n, :w], scalar=iw_fn(hi), in1=Iv[:n, s0:s0 + w],
                        op0=ALU.mult, op1=ALU.add), r=[("rl", b), "iw", ("Isl", i)], w=[("Isl", i)])
        return [("Isl", i) for i in range(len(slices(n_k)))]

    with k.sub():
        vt = k.sb("d_vt", [128, 16, 256], BF16)
        iw = k.sb("d_iw", [128, 16, 16], F32)
        p.dma("sync", vt[:], vtok[0:SEQ, :].rearrange("(j q) n -> q j n", q=128), w=["vt"])
        p.dma("sync", iw[:], iwtok[0:SEQ, :].rearrange("(j q) n -> q j n", q=128), w=["iw"])
        st2 = K()
        st2.tag = 1
        st2.L = k.sb("d_L2", [128, SEQ], F32)
        st2.P = k.sb("d_P2", [128, SEQ], BF16)
        st2.PT = k.sb("d_PT2", [128, 16, 128], BF16)
        st2.mx = k.sb("d_mx2", [128, 1], F32)
        st2.rs = k.sb("d_rs2", [128, 1], F32)
        sts = (st, st2)
        for qi in range(16):
            n_k = 128 * (qi + 1)
            t0 = qi * 128
            ikeys = indexer(128, n_k, t0, ikT, lambda hi, qi=qi: iw[:, qi, hi:hi + 1], Ib, ["ikT"])
            p.op("gpsimd", lambda e, t0=t0: e.affine_select(out=Ib[:, t0:t0 + 128], in_=Ib[:, t0:t0 + 128],
                                                            pattern=[[-1, 128]], compare_op=ALU.is_ge, fill=NEG,
                                                            base=0, channel_multiplier=1),
                 r=ikeys, w=[("I", "p")] + ikeys)
            if qi >= 2:
                topk_thr(k, Ib, Wb, 128, n_k, m8, "p", ikeys)
                p.op("vector", lambda e, n_k=n_k: e.tensor_scalar(out=mb[:, :n_k], in0=Ib[:, :n_k], scalar1=m8[:, 7:8],
                                                                  scalar2=NEG, op0=ALU.is_lt, op1=ALU.mult),
                     r=[("I", "p"), ("m8", "p")] + ikeys, w=["mb"])
            else:
                p.op("vector", lambda e, n_k=n_k: e.tensor_scalar(out=mb[:, :n_k], in0=Ib[:, :n_k], scalar1=-1.0e29,
                                                                  scalar2=NEG, op0=ALU.is_lt, op1=ALU.mult),
                     r=[("I", "p")] + ikeys, w=["mb"])
            ob = oc[qi % 2]
            for h in range(8):
                bank = 6 + h // 4
                attend(k, sts[h % 2], 128, n_k, qT[:, h, t0:t0 + 128], kT, h // 4, vt, mb, "mb",
                       k.ps[bank][:, (h % 4) * 128:(h % 4) * 128 + 128], bank, ["qT", "kT", "vt"])
                if h % 4 == 3:
                    g = h // 4
                    srcv = k.ps[bank][:].rearrange("p (j t) -> p j t", j=4)
                    if g == 0:
                        p.op("scalar", lambda e, ob=ob, srcv=srcv: e.copy(out=ob[:, 0:4, :], in_=srcv),
                             r=[("ps", bank)], w=[("oc", qi % 2, 0)])
                    else:
                        p.op("vector", lambda e, ob=ob, srcv=srcv: e.tensor_copy(out=ob[:, 4:8, :], in_=srcv),
                             r=[("ps", bank)], w=[("oc", qi % 2, 1)])
            p.dma("sync", oT[0, :, :, t0:t0 + 128].rearrange("c q t -> q c t"), ob[:],
                  r=[("oc", qi % 2, 0), ("oc", qi % 2, 1)])

    with k.sub():
        ptb = k.sb("s_ptb", [128, 256], I32)
        ptf = k.sb("s_ptf", [128, 256], F32)
        idx = k.sb("s_idx", [128, 256], U32)
        iop = k.sb("s_iop", [128, 1], F32)
        p.dma("sync", ptb[:], k.I["ptab"].rearrange("b g -> (b g)").partition_broadcast(128), w=["ptb"])
        p.op("gpsimd", lambda e: e.iota(iop[:], pattern=[[0, 1]], base=0, channel_multiplier=1,
                                        allow_small_or_imprecise_dtypes=True), w=["iop"])
        p.op("vector", lambda e: e.tensor_copy(out=ptf[:], in_=ptb[:]), r=["ptb"], w=["ptf"])
        p.op("vector", lambda e: e.tensor_scalar(out=ptf[:], in0=ptf[:], scalar1=128.0, scalar2=iop[:, 0:1],
                                                 op0=ALU.mult, op1=ALU.add), r=["ptf", "iop"], w=["ptf"])
        p.op("vector", lambda e: e.tensor_copy(out=idx[:], in_=ptf[:]), r=["ptf"], w=["idx"])
        iws = k.sb("s_iw", [4, 16, 16], F32)
        p.dma("sync", iws[:], iwtok[SEQ:T, :].rearrange("(b t) h -> t b h", t=4), w=["iw"])
        Iall = k.sb("s_Iall", [64, NK], F32)
        Wall = Wb
        mball = mb
        sub1 = k.sub()
        sub1.__enter__()
        pg = [k.sb("s_pg%d" % i, [128, 128], F32) for i in range(4)]
        ikTb = [k.sb("s_ikTb%d" % i, [128, NK], BF16) for i in range(2)]
        for b in range(NS_SEQ):
            ib = ikTb[b % 2]
            for g in range(16):
                col = b * 16 + g
                t = pg[g % 4]
                for hf in range(2):
                    p.op("gpsimd", lambda e, t=t, hf=hf, col=col: e.indirect_dma_start(
                        out=t[:, hf * 64:(hf + 1) * 64], out_offset=None, in_=k.I["cache_ik"][:, :],
                        in_offset=bass.IndirectOffsetOnAxis(ap=idx[:, col:col + 1], axis=0)),
                        r=["idx"], w=[("pg", g % 4, hf)], dma=True)
                bank = 4 + (g // 4) % 2
                p.op("tensor", lambda e, t=t, g=g, bank=bank: e.transpose(
                    out=k.ps[bank][:, (g % 4) * 128:(g % 4) * 128 + 128], in_=t[:, :], identity=k.ident_f[:, :]),
                    r=[("pg", g % 4, 0), ("pg", g % 4, 1)], w=[("ps", bank)])
                if g % 4 == 3:
                    g0 = g - 3
                    p.op("scalar", lambda e, ib=ib, g0=g0, bank=bank: e.copy(out=ib[:, g0 * 128:g0 * 128 + 512],
                                                                            in_=k.ps[bank][:, :]),
                         r=[("ps", bank)], w=[("ikTb", b % 2)])
            p.op("vector", lambda e, ib=ib, b=b: e.tensor_copy(out=ib[:, SEQ:NK], in_=ikT[:, SEQ + 4 * b:SEQ + 4 * b + 4]),
                 r=["ikT"], w=[("ikTb", b % 2)])
            ikeys = indexer(4, NK, SEQ + 4 * b, ib, lambda hi, b=b: iws[:, b, hi:hi + 1], Ib, [("ikTb", b % 2)])
            p.op("gpsimd", lambda e: e.affine_select(out=Ib[:4, SEQ:NK], in_=Ib[:4, SEQ:NK], pattern=[[-1, 4]],
                                                     compare_op=ALU.is_ge, fill=NEG, base=0, channel_multiplier=1),
                 r=ikeys, w=[("I", "s")] + ikeys)
            p.dma("sync", Iall[4 * b:4 * b + 4, :], Ib[:4, :], r=[("I", "s")] + ikeys, w=[("I", "all")])
        sub1.__exit__(None, None, None)
        topk_thr(k, Iall, Wall, 64, NK, m8, "all")
        p.op("vector", lambda e: e.tensor_scalar(out=mball[:64, :], in0=Iall[:, :], scalar1=m8[:64, 7:8], scalar2=NEG,
                                                 op0=ALU.is_lt, op1=ALU.mult), r=[("I", "all"), ("m8", "all")],
             w=["mball"])
        kTb = [k.sb("s_kTb%d" % i, [128, 2, NK], BF16) for i in range(2)]
        vtb = [k.sb("s_vtb%d" % i, [128, 17, 256], BF16) for i in range(2)]
        kpg = [k.sb("s_kpg%d" % i, [128, 256], F32) for i in range(4)]
        vpg = [k.sb("s_vpg%d" % i, [128, 256], F32) for i in range(4)]
        mb16 = [k.sb("s_mb16%d" % i, [16, NK], F32) for i in range(2)]
        ocs = [k.sb("s_ocs%d" % i, [128, 8, 4], BF16) for i in range(2)]
        qs16 = [k.sb("s_qs16%d" % i, [128, 32], BF16) for i in range(2)]
        for b in range(NS_SEQ):
            kb, vb = kTb[b % 2], vtb[b % 2]
            for g in range(16):
                col = b * 16 + g
                tk, tv = kpg[g % 4], vpg[g % 4]
                p.op("gpsimd", lambda e, tk=tk, col=col: e.indirect_dma_start(
                    out=tk[:, :], out_offset=None, in_=k.I["cache_k"][:, :],
                    in_offset=bass.IndirectOffsetOnAxis(ap=idx[:, col:col + 1], axis=0)),
                    r=["idx"], w=[("kpg", g % 4)], dma=True)
                p.op("gpsimd", lambda e, tv=tv, col=col: e.indirect_dma_start(
                    out=tv[:, :], out_offset=None, in_=k.I["cache_v"][:, :],
                    in_offset=bass.IndirectOffsetOnAxis(ap=idx[:, col:col + 1], axis=0)),
                    r=["idx"], w=[("vpg", g % 4)], dma=True)
                p.op("vector", lambda e, vb=vb, g=g, tv=tv: e.tensor_copy(out=vb[:, g, :], in_=tv[:, :]),
                     r=[("vpg", g % 4)], w=[("vtb", b % 2)])
                bank = 4 + g % 2
                for kv in range(2):
                    p.op("tensor", lambda e, tk=tk, kv=kv, bank=bank: e.transpose(
                        out=k.ps[bank][:, kv * 128:(kv + 1) * 128], in_=tk[:, kv * 128:(kv + 1) * 128],
                        identity=k.ident_f[:, :]), r=[("kpg", g % 4)], w=[("ps", bank)])
                p.op("scalar", lambda e, kb=kb, g=g, bank=bank: e.copy(
                    out=kb[:, :, g * 128:(g + 1) * 128], in_=k.ps[bank][:, 0:256].rearrange("p (v s) -> p v s", v=2)),
                    r=[("ps", bank)], w=[("kTb", b % 2)])
            tc0 = SEQ + 4 * b
            p.op("vector", lambda e, kb=kb, tc0=tc0: e.tensor_copy(out=kb[:, :, SEQ:NK], in_=kT[:, :, tc0:tc0 + 4]),
                 r=["kT"], w=[("kTb", b % 2)])
            p.dma("sync", vb[:4, 16, :], vtok[tc0:tc0 + 4, :], w=[("vtb", b % 2)])
            m16 = mb16[b % 2]
            for i in range(4):
                p.dma("sync", m16[4 * i:4 * i + 4, :], mball[4 * b:4 * b + 4, :], r=["mball"], w=[("mb16", b % 2)])
            ob = ocs[b % 2]
            qs = qs16[b % 2]
            p.op("vector", lambda e, qs=qs, tc0=tc0: e.tensor_copy(
                out=qs[:, :].rearrange("p (h t) -> p h t", t=4), in_=qT[:, :, tc0:tc0 + 4]),
                r=["qT"], w=[("qs16", b % 2)])
            for kv in range(2):
                bank = 6 + kv
                attend(k, st, 16, NK, qs[:, kv * 16:kv * 16 + 16], kb, kv, vb, m16, ("mb16", b % 2),
                       k.ps[bank][:, 0:16], bank, [("qs16", b % 2), ("kTb", b % 2), ("vtb", b % 2)])
                p.op("vector", lambda e, ob=ob, kv=kv, bank=bank: e.tensor_copy(
                    out=ob[:, kv * 4:kv * 4 + 4, :], in_=k.ps[bank][:, 0:16].rearrange("p (h t) -> p h t", h=4)),
                    r=[("ps", bank)], w=[("ocs", b % 2, kv)])
            p.dma("sync", oT[0, :, :, tc0:tc0 + 4].rearrange("c q t -> q c t"), ob[:],
                  r=[("ocs", b % 2, 0), ("ocs", b % 2, 1)])


PHASES.append(("dsa", phase_dsa))


def bc(ap, shape):
    return ap.to_broadcast(shape)


def phase_peer(k):
    p, nc = k.p, k.nc
    x1 = k.scratch("x1", [T, D], F32)
    xn2f = k.scratch("xn2f", [T, D], F32)
    norm_stats(k)
    qpT = k.sb("pe_qpT", [128, 16, T], BF16)
    skT = k.sb("pe_skT", [128, 16, 128], BF16)
    with k.sub():
        xn2T = k.sb("pe_xn2T", [128, 16, T], BF16)
        with k.sub():
            g = load_bcast(k, "gf_bc", k.I["g_ffn"], D)
            xt = [k.sb("pxt%d" % i, [128, D], F32) for i in range(2)]
            xn = [k.sb("pxn%d" % i, [128, D], BF16) for i in range(2)]
            xf = [k.sb("pxf%d" % i, [128, D], F32) for i in range(2)]
            for tt in range(NT):
                b = tt % 2
                n = trows(tt)
                t0 = tt * 128
                p.dma("sync", xt[b][:n, :], x1[t0:t0 + n, :], w=[("xt", b)])
                rms_tile(k, xt[b], n, g, "gf_bc", xn[b], b, tt)
                p.op("gpsimd", lambda e, b=b, n=n: e.tensor_copy(out=xf[b][:n, :], in_=xn[b][:n, :]),
                     r=[("xn", b)], w=[("xf", b)])
                p.dma("sync", xn2f[t0:t0 + n, :], xf[b][:n, :], r=[("xf", b)])
                transpose_to_T(k, xn[b], n, b, xn2T, t0, b * 2)
            skt = [k.sb("pskt%d" % i, [128, 128], F32) for i in range(2)]
            for j in range(16):
                h, pp = j // 2, j % 2
                r0 = (pp * 8 + h) * 128
                p.dma("sync", skt[j % 2][:], k.I["sub_keys"][r0:r0 + 128, :], w=[("skt", j % 2)])
                bank = 4 + j % 2
                p.op("tensor", lambda e, j=j, bank=bank: e.transpose(out=k.ps[bank][:, 0:128], in_=skt[j % 2][:],
                                                                     identity=k.ident_f[:]),
                     r=[("skt", j % 2)], w=[("ps", bank)])
                p.op("vector", lambda e, j=j, bank=bank: e.tensor_copy(out=skT[:, j, :], in_=k.ps[bank][:, 0:128]),
                     r=[("ps", bank)], w=["skT"])

        def evac_q(ci, si, ps, bank, rows, t0, tn):
            if si % 2 == 0:
                p.op("scalar", lambda e: e.copy(out=qpT[:, ci, t0:t0 + tn], in_=ps[:, :tn]), r=[("ps", bank)],
                     w=[("qpT", ci, si)])
            else:
                p.op("vector", lambda e: e.tensor_copy(out=qpT[:, ci, t0:t0 + tn], in_=ps[:, :tn]), r=[("ps", bank)],
                     w=[("qpT", ci, si)])
        linear_fm(k, xn2T, [], "w_pq", [[(c0, 128)] for c0 in range(0, D, 128)], evac_q, "pq")

    with k.sub():
        sc = k.sb("pe_sc", [128, 16, 128], F32)
        scw = k.sb("pe_scw", [128, 16, 128], F32)
        vv = k.sb("pe_v", [128, 16, 16], F32)
        ix = k.sb("pe_ix", [128, 16, 16], U32)
        ixf = k.sb("pe_ixf", [128, 16, 16], F32)
        cand = k.sb("pe_cand", [128, 8, 256], F32)
        candw = k.sb("pe_candw", [128, 8, 256], F32)
        tv = k.sb("pe_tv", [128, 8, 16], F32)
        pos = k.sb("pe_pos", [128, 8, 16], U32)
        pa = k.sb("pe_pa", [128, 8, 16], U32)
        pb = k.sb("pe_pb", [128, 8, 16], U32)
        paf = k.sb("pe_paf", [128, 8, 16], F32)
        pbf = k.sb("pe_pbf", [128, 8, 16], F32)
        eq = k.sb("pe_eq", [128, 8, 16, 16], F32)
        sel = k.sb("pe_sel", [128, 2, 8, 16], F32)
        ef = k.sb("pe_ef", [128, 128], F32)
        eidx = k.sb("pe_eidx", [128, 128], U32)
        gw = k.sb("pe_gw", [128, 8, 16], F32)
        gs = k.sb("pe_gs", [128, 8], F32)
        act = k.sb("pe_act", [128, 128], F32)
        ga = k.sb("pe_ga", [128, 128], F32)
        t1 = k.sb("pe_t1", [128, 128], F32)
        t2 = k.sb("pe_t2", [128, 128], F32)
        io16 = k.sb("pe_io16", [128, 16], F32)
        gb = [k.sb("pe_gb%d" % i, [128, D], BF16) for i in range(8)]
        ubf = k.scratch("peer_u_bf", [16384, D], BF16)
        vbf = k.scratch("peer_v_bf", [16384, D], BF16)
        xq = k.sb("pe_xq", [128, D], F32)
        x1t = k.sb("pe_x1t", [128, D], F32)
        acc = k.sb("pe_acc", [128, D], F32)
        junkf = k.sb("pe_junkf", [128, D], F32)
        yo = k.sb("pe_yo", [128, D], F32)
        gfin = load_bcast(k, "gfin_bc", k.I["g_final"], D)
        diag = [k.sb("pe_dg%d" % i, [128, 128], BF16) for i in range(4)]
        p.op("gpsimd", lambda e: e.iota(io16[:], pattern=[[1, 16]], base=0, channel_multiplier=0,
                                        allow_small_or_imprecise_dtypes=True), w=["io16"])
        S4 = [128, 8, 16, 16]
        def tile_body(tt, n, t0):
            p.dma("sync", xq[:n, :], xn2f[t0:t0 + n, :], w=["xq"])
            p.dma("sync", x1t[:n, :], x1[t0:t0 + n, :], w=[("xt", "f")])
            for j in range(16):
                bank = j // 4
                p.op("tensor", lambda e, j=j, bank=bank: e.matmul(
                    out=k.ps[bank][:n, (j % 4) * 128:(j % 4) * 128 + 128], lhsT=qpT[:, j, t0:t0 + n], rhs=skT[:, j, :],
                    start=True, stop=True), r=["skT"], w=[("ps", bank)])
            for bank in range(4):
                dst = sc[:n, bank * 4:bank * 4 + 4, :]
                srcv = k.ps[bank][:n, :].rearrange("p (j q) -> p j q", j=4)
                if bank % 2 == 0:
                    p.op("scalar", lambda e, dst=dst, srcv=srcv: e.copy(out=dst, in_=srcv), r=[("ps", bank)],
                         w=[("sc", bank)])
                else:
                    p.op("vector", lambda e, dst=dst, srcv=srcv: e.tensor_copy(out=dst, in_=srcv), r=[("ps", bank)],
                         w=[("sc", bank)])
            for j in range(16):
                kj = [("sc", j // 4)]
                p.op("vector", lambda e, j=j: e.max(out=vv[:n, j, 0:8], in_=sc[:n, j, :]), r=kj, w=[("vv", j)])
                p.op("vector", lambda e, j=j: e.max_index(out=ix[:n, j, 0:8], in_max=vv[:n, j, 0:8], in_values=sc[:n, j, :]),
                     r=kj + [("vv", j)], w=[("ix", j)])
                p.op("vector", lambda e, j=j: e.match_replace(out=scw[:n, j, :], in_to_replace=vv[:n, j, 0:8],
                                                              in_values=sc[:n, j, :], imm_value=NEG),
                     r=kj + [("vv", j)], w=[("scw", j)])
                p.op("vector", lambda e, j=j: e.max(out=vv[:n, j, 8:16], in_=scw[:n, j, :]), r=[("scw", j)],
                     w=[("vv", j)])
                p.op("vector", lambda e, j=j: e.max_index(out=ix[:n, j, 8:16], in_max=vv[:n, j, 8:16],
                                                          in_values=scw[:n, j, :]),
                     r=[("scw", j), ("vv", j)], w=[("ix", j)])
            allv = [("vv", j) for j in range(16)]
            alli = [("ix", j) for j in range(16)]
            p.op("vector", lambda e: e.tensor_copy(out=ixf[:n], in_=ix[:n]), r=alli, w=["ixf"])
            v4 = vv[:n].rearrange("p (h two) a -> p h two a", two=2)
            i4 = ixf[:n].rearrange("p (h two) a -> p h two a", two=2)
            S4n = [n, 8, 16, 16]
            p.op("vector", lambda e: e.tensor_tensor(
                out=cand[:n].rearrange("p h (a b) -> p h a b", a=16), in0=bc(v4[:, :, 0, :].unsqueeze(3), S4n),
                in1=bc(v4[:, :, 1, :].unsqueeze(2), S4n), op=ALU.add), r=allv, w=["cand"])
            for h in range(8):
                p.op("vector", lambda e, h=h: e.max(out=tv[:n, h, 0:8], in_=cand[:n, h, :]), r=["cand"], w=[("tv", h)])
                p.op("vector", lambda e, h=h: e.max_index(out=pos[:n, h, 0:8], in_max=tv[:n, h, 0:8],
                                                          in_values=cand[:n, h, :]), r=["cand", ("tv", h)],
                     w=[("pos", h)])
                p.op("vector", lambda e, h=h: e.match_replace(out=candw[:n, h, :], in_to_replace=tv[:n, h, 0:8],
                                                              in_values=cand[:n, h, :], imm_value=NEG),
                     r=["cand", ("tv", h)], w=[("candw", h)])
                p.op("vector", lambda e, h=h: e.max(out=tv[:n, h, 8:16], in_=candw[:n, h, :]), r=[("candw", h)],
                     w=[("tv", h)])
                p.op("vector", lambda e, h=h: e.max_index(out=pos[:n, h, 8:16], in_max=tv[:n, h, 8:16],
                                                          in_values=candw[:n, h, :]), r=[("candw", h), ("tv", h)],
                     w=[("pos", h)])
            allt = [("tv", h) for h in range(8)]
            allp = [("pos", h) for h in range(8)]
            p.op("vector", lambda e: e.tensor_tensor(out=gw[:n], in0=tv[:n], in1=bc(tv[:n, :, 0:1], [n, 8, 16]),
                                                     op=ALU.subtract), r=allt, w=["gw"])
            p.op("scalar", lambda e: e.activation(out=gw[:n], in_=gw[:n], func=AF.Exp), r=["gw"], w=["gw"])
            p.op("vector", lambda e: e.tensor_reduce(out=gs[:n, :], in_=gw[:n], axis=AX.X, op=ALU.add), r=["gw"],
                 w=["gs"])
            p.op("vector", lambda e: e.reciprocal(out=gs[:n, :], in_=gs[:n, :]), r=["gs"], w=["gs"])
            p.op("vector", lambda e: e.tensor_tensor(out=gw[:n], in0=gw[:n], in1=bc(gs[:n, :].unsqueeze(2), [n, 8, 16]),
                                                     op=ALU.mult), r=["gs", "gw"], w=["gw"])
            p.op("vector", lambda e: e.tensor_single_scalar(out=pa[:n], in_=pos[:n], scalar=4,
                                                            op=ALU.logical_shift_right), r=allp, w=["pa"])
            p.op("vector", lambda e: e.tensor_single_scalar(out=pb[:n], in_=pos[:n], scalar=15, op=ALU.bitwise_and),
                 r=allp, w=["pb"])
            p.op("vector", lambda e: e.tensor_copy(out=paf[:n], in_=pa[:n]), r=["pa"], w=["paf"])
            p.op("vector", lambda e: e.tensor_copy(out=pbf[:n], in_=pb[:n]), r=["pb"], w=["pbf"])
            for w_, pf in ((0, paf), (1, pbf)):
                p.op("vector", lambda e, pf=pf: e.tensor_tensor(
                    out=eq[:n], in0=bc(pf[:n].unsqueeze(3), S4n), in1=bc(io16[:n, :].unsqueeze(1).unsqueeze(1), S4n),
                    op=ALU.is_equal), r=["paf", "pbf", "io16", "sel"], w=["eq"])
                p.op("vector", lambda e, w_=w_: e.tensor_tensor(
                    out=eq[:n], in0=eq[:n], in1=bc(i4[:, :, w_, :].unsqueeze(2), S4n), op=ALU.mult),
                    r=["eq", "ixf"], w=["eq"])
                p.op("vector", lambda e, w_=w_: e.tensor_reduce(out=sel[:n, w_], in_=eq[:n], axis=AX.X, op=ALU.add),
                     r=["eq"], w=["sel"])
            p.op("vector", lambda e: e.scalar_tensor_tensor(
                out=ef[:n, :], in0=sel[:n, 0].rearrange("p h k -> p (h k)"), scalar=128.0,
                in1=sel[:n, 1].rearrange("p h k -> p (h k)"), op0=ALU.mult, op1=ALU.add), r=["sel"], w=["ef"])
            p.op("vector", lambda e: e.tensor_copy(out=eidx[:n, :], in_=ef[:n, :]), r=["ef"], w=["eidx"])
            p.op("vector", lambda e: e.memset(act[:], 0.0), r=["ga"], w=[("act", s_) for s_ in range(128)])
            for s_ in range(128):
                b = s_ % 8
                p.op("gpsimd", lambda e, s_=s_, b=b: e.indirect_dma_start(
                    out=gb[b][:n, :], out_offset=None, in_=ubf[:, :],
                    in_offset=bass.IndirectOffsetOnAxis(ap=eidx[:n, s_:s_ + 1], axis=0)),
                    r=["eidx"], w=[("gb", b)], dma=True)
                p.op("vector", lambda e, s_=s_, b=b: e.scalar_tensor_tensor(
                    out=junkf[:n, :], in0=gb[b][:n, :], scalar=1.0, in1=xq[:n, :], op0=ALU.mult, op1=ALU.mult,
                    accum_out=act[:n, s_:s_ + 1]), r=[("gb", b), "xq"], w=[("act", s_), "junkf"])
            gelu_mul(k, act[:n, :], gw[:n].rearrange("p h k -> p (h k)"), ga[:n, :], n, t1[:n, :], t2[:n, :],
                     [("act", s_) for s_ in range(128)] + ["gw"], "ga")
            for s_ in range(128):
                b = s_ % 8
                p.op("gpsimd", lambda e, s_=s_, b=b: e.indirect_dma_start(
                    out=gb[b][:n, :], out_offset=None, in_=vbf[:, :],
                    in_offset=bass.IndirectOffsetOnAxis(ap=eidx[:n, s_:s_ + 1], axis=0)),
                    r=["eidx"], w=[("gb", b)], dma=True)
                d = s_ % 4
                p.op("scalar", lambda e, s_=s_, d=d: e.activation(out=diag[d][:n, :n], in_=k.ident_f[:n, :n],
                                                               func=AF.Identity, scale=ga[:n, s_:s_ + 1]),
                     r=["ga"], w=[("dg", d)])
                for cb in range(4):
                    p.op("tensor", lambda e, s_=s_, d=d, b=b, cb=cb: e.matmul(
                        out=k.ps[4 + cb][:n, :], lhsT=diag[d][:n, :n], rhs=gb[b][:n, cb * 512:(cb + 1) * 512],
                        start=(s_ == 0), stop=(s_ == 127)), r=[("dg", d), ("gb", b)], w=[("ps", 4 + cb)])
            for cb in range(4):
                p.op("vector", lambda e, cb=cb: e.tensor_tensor(
                    out=acc[:n, cb * 512:(cb + 1) * 512], in0=k.ps[4 + cb][:n, :],
                    in1=x1t[:n, cb * 512:(cb + 1) * 512], op=ALU.add), r=[("ps", 4 + cb), ("xt", "f")], w=["acc"])
            if tt == 0 and "dbg" in DEBUG_IO:
                dbg = k.scratch("dbg", [6, 128, 128], F32)
                p.dma("sync", dbg[0], ef[:, :], r=["ef", "eidx"])
                p.dma("sync", dbg[1], gw[:].rearrange("p h k -> p (h k)"), r=["gw", "ga"])
                p.dma("sync", dbg[2], act[:, :], r=["ga"])
                p.dma("sync", dbg[3], ga[:, :], r=["ga"])
                p.dma("sync", dbg[4], tv[:].rearrange("p h k -> p (h k)"), r=allt + ["gw"])
                p.dma("sync", dbg[5], paf[:].rearrange("p h k -> p (h k)"), r=["paf", "eq"])
            p.op("vector", lambda e: e.tensor_copy(out=x1t[:n, :], in_=acc[:n, :]), r=["acc"], w=[("xt", "f")])
            rms_tile(k, x1t, n, gfin, "gfin_bc", yo, "f", 32 + tt)
            dst = k.O["y_p"][t0:t0 + 128, :] if tt < 16 else k.O["y_s"][:, :]
            p.dma("sync", dst, yo[:n, :], r=[("xn", "f")])

        for tt in range(NT):
            tile_body(tt, trows(tt), tt * 128)


PHASES.append(("peer", phase_peer))


def phase_cast(k):
    p = k.p
    stg = [k.sb("ct_st%d" % i, [128, 8192], F32) for i in range(2)]
    obf = [k.sb("ct_ob%d" % i, [128, 8192], BF16) for i in range(2)]
    it = 0
    for name in ("peer_u", "peer_v"):
        dst = k.scratch(name + "_bf", [16384, D], BF16)
        src = k.I[name].rearrange("(c q r) d -> c q (r d)", q=128, r=4)
        dstv = dst.rearrange("(c q r) d -> c q (r d)", q=128, r=4)
        for c in range(32):
            b = it % 2
            eng = ("scalar", "vector", "gpsimd")[it % 3]
            it += 1
            p.dma("sync", stg[b][:], src[c], w=[("cst", b)])
            if eng == "scalar":
                p.op("scalar", lambda e, b=b: e.copy(out=obf[b][:], in_=stg[b][:]), r=[("cst", b)], w=[("cob", b)])
            else:
                p.op(eng, lambda e, b=b: e.tensor_copy(out=obf[b][:], in_=stg[b][:]), r=[("cst", b)], w=[("cob", b)])
            p.dma("sync", dstv[c], obf[b][:], r=[("cob", b)])


PHASES.append(("cast", phase_cast))
```
